# Optimizing a Trainium2 kernel written in Bass

```python
import math
import jax, jax.numpy as jnp
from jax import lax
import numpy as np

D_MODEL = 1024
BATCH = 8
SEQ = 2048
DEPTH = 2

GRID_W = 64
CTX_LEN = 256
HEAD_DIM = 64
A_HEADS = 4
A_VDIM = 2 * HEAD_DIM
B_HEADS = 8
B_KV_HEADS = 2
C_HEADS = 8
D_GROUPS = 4
D_GROUP_DIM = 128
POOL_WINDOWS = (2, 4, 8, 16)
NA_ROWS = 8
NA_COLS = 16
N_EXPERTS = 16
EXPERT_FF = 2048
EC_FACTOR = 2
Q_BLOCK = 128
ROPE_THETA = 10000.0
ROPE_AXIS_DIM = HEAD_DIM // 2
LN_EPS = 1e-5
RMS_EPS = 1e-6
DEEPNORM_ALPHA = (2 * DEPTH) ** 0.25
DEEPNORM_BETA = (8 * DEPTH) ** -0.25

A_Q = A_HEADS * 2 * HEAD_DIM
A_K = A_HEADS * 2 * HEAD_DIM
A_V = A_HEADS * A_VDIM
B_Q = B_HEADS * HEAD_DIM
B_K = B_KV_HEADS * HEAD_DIM
B_V = B_KV_HEADS * HEAD_DIM
EVEN_IN = A_Q + A_K + A_V + B_Q + B_K + B_V
EVEN_SPLITS = (A_Q, A_Q + A_K, A_Q + A_K + A_V, A_Q + A_K + A_V + B_Q, A_Q + A_K + A_V + B_Q + B_K)
EVEN_MIX = A_V + B_Q
C_W = C_HEADS * HEAD_DIM
D_W = D_GROUPS * D_GROUP_DIM
ODD_IN = 3 * C_W + D_W
ODD_MIX = C_W + D_W

kernel_name = "hybrid_diffattn_gqa_natten_pool_ecmoe_dit"


def layer_norm(x, g, b):
    xf = x.astype(jnp.float32)
    mu = jnp.mean(xf, axis=-1, keepdims=True)
    var = jnp.mean(jnp.square(xf - mu), axis=-1, keepdims=True)
    return ((xf - mu) * lax.rsqrt(var + LN_EPS) * g + b).astype(x.dtype)


def rms_norm(x, g):
    xf = x.astype(jnp.float32)
    return (xf * lax.rsqrt(jnp.mean(jnp.square(xf), axis=-1, keepdims=True) + RMS_EPS) * g).astype(x.dtype)


def softmax_f32(s, dtype):
    return jax.nn.softmax(s.astype(jnp.float32), axis=-1).astype(dtype)


def rope_angles(n_tokens):
    t = jnp.arange(n_tokens, dtype=jnp.int32)
    n_freq = ROPE_AXIS_DIM // 2
    inv = ROPE_THETA ** (-jnp.arange(n_freq, dtype=jnp.float32) / n_freq)
    ang_r = (t // GRID_W).astype(jnp.float32)[:, None] * inv
    ang_c = (t % GRID_W).astype(jnp.float32)[:, None] * inv
    return (jnp.cos(ang_r), jnp.sin(ang_r), jnp.cos(ang_c), jnp.sin(ang_c))


def _rotate(x, cos, sin):
    x1, x2 = jnp.split(x, 2, axis=-1)
    return jnp.concatenate([x1 * cos - x2 * sin, x2 * cos + x1 * sin], axis=-1)


def rope2d(x, rope):
    cos_r, sin_r, cos_c, sin_c = (a[None, :, None, :].astype(x.dtype) for a in rope)
    xr, xc = jnp.split(x, 2, axis=-1)
    return jnp.concatenate([_rotate(xr, cos_r, sin_r), _rotate(xc, cos_c, sin_c)], axis=-1)


def sweep_query_blocks(fn, *qs):
    b, s = qs[0].shape[:2]
    nb = s // Q_BLOCK
    blocks = tuple(jnp.moveaxis(q.reshape(b, nb, Q_BLOCK, *q.shape[2:]), 1, 0) for q in qs)
    out = lax.map(lambda qb: fn(*qb), blocks)
    return jnp.moveaxis(out, 0, 1).reshape(b, s, *out.shape[3:])


def diff_attention(q1, q2, k1, k2, v, lam):
    scale = HEAD_DIM ** -0.5
    s1 = jnp.einsum('bqhd,bkhd->bhqk', q1, k1) * scale
    s2 = jnp.einsum('bqhd,bkhd->bhqk', q2, k2) * scale
    p = jax.nn.softmax(s1.astype(jnp.float32), axis=-1) - lam * jax.nn.softmax(s2.astype(jnp.float32), axis=-1)
    return jnp.einsum('bhqk,bkhd->bqhd', p.astype(v.dtype), v)


def gqa_attention(q, k, v):
    b, nq = q.shape[:2]
    qg = q.reshape(b, nq, B_KV_HEADS, B_HEADS // B_KV_HEADS, HEAD_DIM)
    s = jnp.einsum('bqngd,bknd->bngqk', qg, k) * (HEAD_DIM ** -0.5)
    p = softmax_f32(s, v.dtype)
    o = jnp.einsum('bngqk,bknd->bqngd', p, v)
    return o.reshape(b, nq, B_HEADS, HEAD_DIM)


def even_heads(p):
    lead = p.shape[:2]
    aq, ak, av, bq, bk, bv = jnp.split(p, list(EVEN_SPLITS), axis=-1)
    aq = aq.reshape(*lead, A_HEADS, 2, HEAD_DIM)
    ak = ak.reshape(*lead, A_HEADS, 2, HEAD_DIM)
    return (aq[..., 0, :], aq[..., 1, :], ak[..., 0, :], ak[..., 1, :],
            av.reshape(*lead, A_HEADS, A_VDIM),
            bq.reshape(*lead, B_HEADS, HEAD_DIM),
            bk.reshape(*lead, B_KV_HEADS, HEAD_DIM),
            bv.reshape(*lead, B_KV_HEADS, HEAD_DIM))


def even_mixer(h, h_ctx, rope, lam_init, w_in, w_out, lam_q1, lam_k1, lam_q2, lam_k2, subln_g, qn_g, kn_g):
    b, s, _ = h.shape
    nc = h_ctx.shape[1]
    a_q1, a_q2, a_k1, a_k2, a_v, b_q, b_k, b_v = even_heads(h @ w_in)
    c_q1, c_q2, c_k1, c_k2, c_v, cb_q, cb_k, cb_v = even_heads(h_ctx @ w_in)
    lam = (jnp.exp(jnp.sum(lam_q1 * lam_k1).astype(jnp.float32))
           - jnp.exp(jnp.sum(lam_q2 * lam_k2).astype(jnp.float32)) + lam_init)
    a_q1, a_q2, a_k1, a_k2 = (rope2d(t, rope) for t in (a_q1, a_q2, a_k1, a_k2))
    b_q = rope2d(rms_norm(b_q, qn_g), rope)
    b_k = rope2d(rms_norm(b_k, kn_g), rope)
    cb_q = rms_norm(cb_q, qn_g)
    cb_k = rms_norm(cb_k, kn_g)
    cat = lambda u, w: jnp.concatenate([u, w], axis=1)
    ka1, ka2, va = cat(a_k1, c_k1), cat(a_k2, c_k2), cat(a_v, c_v)
    kb, vb = cat(b_k, cb_k), cat(b_v, cb_v)
    o_a = sweep_query_blocks(lambda q1, q2: diff_attention(q1, q2, ka1, ka2, va, lam), a_q1, a_q2)
    o_b = sweep_query_blocks(lambda q: gqa_attention(q, kb, vb), b_q)
    oc_a = diff_attention(c_q1, c_q2, c_k1, c_k2, c_v, lam)
    oc_b = gqa_attention(cb_q, cb_k, cb_v)
    sub = lambda o: rms_norm(o, subln_g) * (1.0 - lam_init)
    y = jnp.concatenate([sub(o_a).reshape(b, s, A_V), o_b.reshape(b, s, B_Q)], axis=-1) @ w_out
    y_ctx = jnp.concatenate([sub(oc_a).reshape(b, nc, A_V), oc_b.reshape(b, nc, B_Q)], axis=-1) @ w_out
    return y, y_ctx


def neighbourhood_attention(q, k, v, k_ctx, v_ctx, rpb):
    b, s, h, d = q.shape
    rows = s // GRID_W
    kh = min(NA_ROWS, rows)
    kw = NA_COLS
    grid = lambda t: t.reshape(b, rows, GRID_W, h, d)
    qg, kg, vg = grid(q), grid(k), grid(v)
    cols = jnp.arange(GRID_W, dtype=jnp.int32)
    col_start = jnp.clip(cols - kw // 2, 0, GRID_W - kw)
    col_idx = col_start[:, None] + jnp.arange(kw, dtype=jnp.int32)[None, :]
    dc = col_idx - cols[:, None] + (NA_COLS - 1)
    scale = d ** -0.5
    n_loc = kh * kw

    def row_block(r):
        rs = jnp.clip(r - kh // 2, 0, rows - kh)
        k_rows = lax.dynamic_slice_in_dim(kg, rs, kh, axis=1)
        v_rows = lax.dynamic_slice_in_dim(vg, rs, kh, axis=1)
        k_nb = jnp.moveaxis(k_rows[:, :, col_idx], 1, 2).reshape(b, GRID_W, n_loc, h, d)
        v_nb = jnp.moveaxis(v_rows[:, :, col_idx], 1, 2).reshape(b, GRID_W, n_loc, h, d)
        q_r = lax.dynamic_index_in_dim(qg, r, axis=1, keepdims=False)
        dr = rs + jnp.arange(kh, dtype=jnp.int32) - r + (NA_ROWS - 1)
        bias = rpb[:, dr[None, :, None], dc[:, None, :]].reshape(h, GRID_W, n_loc)
        s_loc = jnp.einsum('bwhd,bwnhd->bhwn', q_r, k_nb) * scale + bias[None]
        s_ctx = jnp.einsum('bwhd,bkhd->bhwk', q_r, k_ctx) * scale
        p = softmax_f32(jnp.concatenate([s_loc, s_ctx], axis=-1), v.dtype)
        return (jnp.einsum('bhwn,bwnhd->bwhd', p[..., :n_loc], v_nb)
                + jnp.einsum('bhwk,bkhd->bwhd', p[..., n_loc:], v_ctx))

    out = lax.map(row_block, jnp.arange(rows, dtype=jnp.int32))
    return jnp.moveaxis(out, 0, 1).reshape(b, s, h * d)


def multi_scale_pool(u):
    s = u.shape[1]
    uf = u.astype(jnp.float32)
    csum = jnp.concatenate([jnp.zeros_like(uf[:, :1]), jnp.cumsum(uf, axis=1)], axis=1)
    t = jnp.arange(s, dtype=jnp.int32)[:, None]
    half = jnp.array(POOL_WINDOWS, dtype=jnp.int32)[None, :] // 2
    lo = jnp.clip(t - half, 0, s)
    hi = jnp.clip(t + half, 0, s)
    g = jnp.arange(D_GROUPS, dtype=jnp.int32)[None, :]
    mean = (csum[:, hi, g] - csum[:, lo, g]) / (hi - lo).astype(jnp.float32)[None, :, :, None]
    return (mean - uf).astype(u.dtype)


def odd_mixer(h, h_ctx, w_in, w_out, rpb, pool_w, pool_scale):
    b, s, _ = h.shape
    p = h @ w_in
    cq, ck, cv, du = jnp.split(p, [C_W, 2 * C_W, 3 * C_W], axis=-1)
    ctx_k, ctx_v = jnp.split(h_ctx @ w_in[:, C_W:3 * C_W], 2, axis=-1)
    heads = lambda t: t.reshape(*t.shape[:2], C_HEADS, HEAD_DIM)
    o_c = neighbourhood_attention(heads(cq), heads(ck), heads(cv), heads(ctx_k), heads(ctx_v), rpb)
    pooled = multi_scale_pool(du.reshape(b, s, D_GROUPS, D_GROUP_DIM))
    o_d = jnp.einsum('bsgc,gce->bsge', pooled, pool_w).reshape(b, s, D_W) * pool_scale
    return jnp.concatenate([o_c, o_d], axis=-1) @ w_out


def expert_choice_ffn(h, router, w_gate, w_up, w_down):
    b, n, _ = h.shape
    cap = EC_FACTOR * n // N_EXPERTS
    aff = jax.nn.softmax((h @ router).astype(jnp.float32), axis=-1)
    gates, idx = lax.top_k(jnp.swapaxes(aff, 1, 2), cap)
    bidx = jnp.arange(b, dtype=jnp.int32)[:, None, None]
    xg = h[bidx, idx]
    hid = jax.nn.silu(jnp.einsum('becd,edf->becf', xg, w_gate)) * jnp.einsum('becd,edf->becf', xg, w_up)
    y = jnp.einsum('becf,efd->becd', hid, w_down) * gates[..., None].astype(h.dtype)
    return jnp.zeros_like(h).at[bidx, idx].add(y)


def ffn_sublayer(x, shift, scale, gate, g, b, router, w_gate, w_up, w_down):
    hm = x * (1 + scale) + shift
    return layer_norm(DEEPNORM_ALPHA * x + gate * expert_choice_ffn(hm, router, w_gate, w_up, w_down), g, b)


def setup_inputs(seed: int = 0) -> dict:
    key = jax.random.key(seed)
    ks = iter(jax.random.split(key, 32))
    nrm = lambda shape, s: jax.random.normal(next(ks), shape, jnp.float32) * s
    D = D_MODEL
    return {
        "x": nrm((BATCH, SEQ, D), 1.0),
        "c": nrm((BATCH, D), 1.0),
        "ctx": nrm((BATCH, CTX_LEN, D), 1.0),
        "c_ctx": nrm((D,), 1.0),
        "ada_w": nrm((DEPTH, D, 6 * D), D ** -0.5),
        "ada_b": nrm((DEPTH, 6 * D), 0.02),
        "ln_g": 1.0 + nrm((DEPTH, 2, D), 0.02),
        "ln_b": nrm((DEPTH, 2, D), 0.02),
        "l0_w_in": nrm((D, EVEN_IN), D ** -0.5),
        "l0_w_out": nrm((EVEN_MIX, D), EVEN_MIX ** -0.5 * DEEPNORM_BETA),
        "l0_lam_q1": nrm((HEAD_DIM,), 0.1),
        "l0_lam_k1": nrm((HEAD_DIM,), 0.1),
        "l0_lam_q2": nrm((HEAD_DIM,), 0.1),
        "l0_lam_k2": nrm((HEAD_DIM,), 0.1),
        "l0_subln_g": 1.0 + nrm((A_VDIM,), 0.02),
        "l0_qnorm_g": 1.0 + nrm((HEAD_DIM,), 0.02),
        "l0_knorm_g": 1.0 + nrm((HEAD_DIM,), 0.02),
        "l1_w_in": nrm((D, ODD_IN), D ** -0.5),
        "l1_w_out": nrm((ODD_MIX, D), ODD_MIX ** -0.5 * DEEPNORM_BETA),
        "l1_rpb": nrm((C_HEADS, 2 * NA_ROWS - 1, 2 * NA_COLS - 1), 0.1),
        "l1_pool_w": nrm((D_GROUPS, D_GROUP_DIM, D_GROUP_DIM), D_GROUP_DIM ** -0.5),
        "l1_pool_scale": 1.0 + nrm((D_W,), 0.02),
        "moe_router": nrm((DEPTH, D, N_EXPERTS), D ** -0.5),
        "moe_w_gate": nrm((DEPTH, N_EXPERTS, D, EXPERT_FF), D ** -0.5),
        "moe_w_up": nrm((DEPTH, N_EXPERTS, D, EXPERT_FF), D ** -0.5),
        "moe_w_down": nrm((DEPTH, N_EXPERTS, EXPERT_FF, D), EXPERT_FF ** -0.5 * DEEPNORM_BETA),
    }


def reference(x, c, ctx, c_ctx, ada_w, ada_b, ln_g, ln_b,
              l0_w_in, l0_w_out, l0_lam_q1, l0_lam_k1, l0_lam_q2, l0_lam_k2, l0_subln_g, l0_qnorm_g, l0_knorm_g,
              l1_w_in, l1_w_out, l1_rpb, l1_pool_w, l1_pool_scale,
              moe_router, moe_w_gate, moe_w_up, moe_w_down):
    rope = rope_angles(x.shape[1])
    for i in range(DEPTH):
        mod = jax.nn.silu(c) @ ada_w[i] + ada_b[i]
        mod_c = jax.nn.silu(c_ctx) @ ada_w[i] + ada_b[i]
        sh1, sc1, g1, sh2, sc2, g2 = (m[:, None, :] for m in jnp.split(mod, 6, axis=-1))
        csh1, csc1, cg1, csh2, csc2, cg2 = (m[None, None, :] for m in jnp.split(mod_c, 6, axis=-1))
        h = x * (1 + sc1) + sh1
        h_ctx = ctx * (1 + csc1) + csh1
        if i % 2 == 0:
            y, y_ctx = even_mixer(h, h_ctx, rope, 0.8 - 0.6 * math.exp(-0.3 * i),
                                  l0_w_in, l0_w_out, l0_lam_q1, l0_lam_k1, l0_lam_q2, l0_lam_k2,
                                  l0_subln_g, l0_qnorm_g, l0_knorm_g)
        else:
            y = odd_mixer(h, h_ctx, l1_w_in, l1_w_out, l1_rpb, l1_pool_w, l1_pool_scale)
        x = layer_norm(DEEPNORM_ALPHA * x + g1 * y, ln_g[i, 0], ln_b[i, 0])
        x = ffn_sublayer(x, sh2, sc2, g2, ln_g[i, 1], ln_b[i, 1],
                         moe_router[i], moe_w_gate[i], moe_w_up[i], moe_w_down[i])
        if i % 2 == 0:
            ctx = layer_norm(DEEPNORM_ALPHA * ctx + cg1 * y_ctx, ln_g[i, 0], ln_b[i, 0])
            ctx = ffn_sublayer(ctx, csh2, csc2, cg2, ln_g[i, 1], ln_b[i, 1],
                               moe_router[i], moe_w_gate[i], moe_w_up[i], moe_w_down[i])
    return x
```

```python
import math
import numpy as np
from contextlib import ExitStack
import concourse.bass as bass
import concourse.mybir as mybir
from concourse.bass_utils import run_bass_kernel_spmd

F32 = mybir.dt.float32
BF16 = mybir.dt.bfloat16
AF = mybir.ActivationFunctionType
ALU = mybir.AluOpType
AX = mybir.AxisListType

D = 1024
SEQ = 2048
CTX = 256
NT = SEQ + CTX
NCH = NT // 128
NLAT = SEQ // 128
NE = 16
FF = 2048
ALPHA = 4.0 ** 0.25
LN_EPS = 1e-5
RMS_EPS = 1e-6
LAM_INIT0 = 0.8 - 0.6 * math.exp(0.0)
NEG = -30000.0


class Sched:
    def __init__(self):
        self.ops = []
        self.lw = {}
        self.rd = {}

    def add(self, eng, fn, reads=(), writes=(), dma=None):
        i = len(self.ops)
        stream = ("dma", dma) if dma is not None else eng
        pk = [k for k in reads if k.startswith("ps") and k[2:].isdigit()]
        if pk:
            writes = list(writes) + [k for k in pk if k not in writes]
        raw = set()
        war = set()
        for k in reads:
            w = self.lw.get(k)
            if w is not None:
                raw.add(w)
        for k in writes:
            w = self.lw.get(k)
            if w is not None:
                raw.add(w)
            for r in self.rd.get(k, {}).values():
                war.add(r)
        if fn is not None:
            for k in writes:
                self.lw[k] = i
                self.rd[k] = {}
            for k in reads:
                self.rd.setdefault(k, {})[stream] = i
        self.ops.append(dict(eng=eng, fn=fn, raw=raw, war=war, dma=dma, stream=stream, cons=False, ms=0))
        return i

    def emit(self, nc, es):
        ops = self.ops
        for o in ops:
            for d in o["raw"] | o["war"]:
                if d == len(ops):
                    continue
                p = ops[d]
                same = (p["stream"] == o["stream"])
                if same and (o["stream"] == "pe"):
                    continue
                if same and d in o["war"] and d not in o["raw"]:
                    continue
                p["cons"] = True
        sems = {}
        cnt = {}

        def get_sem(stream):
            if stream not in sems:
                nm = "s_" + (stream if isinstance(stream, str) else "d_" + str(stream[1]))
                sems[stream] = es.enter_context(nc.semaphore(nm))
                cnt[stream] = 0
            return sems[stream]

        for o in ops:
            st = o["stream"]
            get_sem(st)
            if o["dma"] is not None:
                cnt[st] += 16
                o["ms"] = cnt[st]
            elif o["cons"]:
                cnt[st] += 1
                o["ms"] = cnt[st]
        self.nsem = len(sems)
        block = es.enter_context(nc.Block())
        engs = ["pe", "act", "dve", "pool", "sp"]
        per = {e: [o for o in ops if o["eng"] == e] for e in engs}

        def run(e, eh):
            known = {}
            for o in per[e]:
                need = {}
                for d in o["raw"] | o["war"]:
                    p = ops[d]
                    same = (p["stream"] == o["stream"])
                    if same and o["stream"] == "pe":
                        continue
                    if same and d in o["war"] and d not in o["raw"]:
                        continue
                    if p["ms"] <= 0:
                        continue
                    s = p["stream"]
                    if need.get(s, 0) < p["ms"]:
                        need[s] = p["ms"]
                for s, v in need.items():
                    if known.get(s, 0) < v:
                        eh.wait_ge(sems[s], v)
                        known[s] = v
                if o["fn"] is None:
                    continue
                ins = o["fn"](eh)
                if o["dma"] is not None:
                    ins.then_inc(sems[o["stream"]], 16)
                elif o["cons"]:
                    ins.then_inc(sems[o["stream"]], 1)

        @block.tensor
        def _(eh):
            run("pe", eh)

        @block.scalar
        def _(eh):
            run("act", eh)

        @block.vector
        def _(eh):
            run("dve", eh)

        @block.gpsimd
        def _(eh):
            run("pool", eh)

        @block.sync
        def _(eh):
            run("sp", eh)


def _l1_patterns():
    W, rows, kh, kw = 64, 32, 8, 16
    cols = np.arange(W)
    cs = np.clip(cols - kw // 2, 0, W - kw)
    colok = (cols[None, :] >= cs[:, None]) & (cols[None, :] < cs[:, None] + kw)
    dcol = cols[None, :] - cols[:, None] + 15
    pats, sigs, local = [], {}, []
    for c in range(16):
        lst = []
        for kc in range(16):
            valid = np.zeros((128, 128), bool)
            dr = np.zeros((128, 128), np.int64)
            dc = np.zeros((128, 128), np.int64)
            for ql in range(2):
                r = 2 * c + ql
                rs = min(max(r - kh // 2, 0), rows - kh)
                for kl in range(2):
                    rk = 2 * kc + kl
                    rowok = rs <= rk < rs + kh
                    qs = slice(ql * 64, ql * 64 + 64)
                    ks = slice(kl * 64, kl * 64 + 64)
                    valid[qs, ks] = colok & rowok
                    dr[qs, ks] = rk - r + 7
                    dc[qs, ks] = dcol
            if not valid.any():
                continue
            dr = np.where(valid, dr, 0)
            dc = np.where(valid, dc, 0)
            sig = (valid.tobytes(), dr.tobytes())
            if sig not in sigs:
                sigs[sig] = len(pats)
                pats.append((valid, dr, dc))
            lst.append((kc, sigs[sig]))
        local.append(lst)
    return pats, local


_L1_PATS, L1_LOCAL = _l1_patterns()
NPAT = len(_L1_PATS)
INT_PATS = [p for (_, p) in L1_LOCAL[2]]
for _c in range(2, 14):
    assert [p for (_, p) in L1_LOCAL[_c]] == INT_PATS and len(INT_PATS) == 5
for _c in (0, 1, 14, 15):
    assert len(L1_LOCAL[_c]) <= 4


def _l1_table(rpb):
    rpb = np.asarray(rpb, np.float32)
    tab = np.full((NPAT, 8, 128, 128), NEG, np.float32)
    for i, (valid, dr, dc) in enumerate(_L1_PATS):
        g = rpb[:, dr, dc]
        tab[i] = np.where(valid[None], g, np.float32(NEG))
    return tab


def _band_mats():
    out = np.zeros((20, 128, 128), np.float32)
    tp = np.arange(128)[:, None]
    t = np.arange(128)[None, :]
    for g, w in enumerate((2, 4, 8, 16)):
        half = w // 2
        cnt = float(2 * half)
        out[g * 5 + 0] = (tp >= t + 128 - half) / cnt
        out[g * 5 + 1] = ((tp >= t - half) & (tp < t + half)) / cnt - (tp == t)
        out[g * 5 + 2] = (tp < t + half - 128) / cnt
        lo = np.maximum(t - half, 0)
        hi = t + half
        out[g * 5 + 3] = ((tp >= lo) & (tp < hi)) / (hi - lo).astype(np.float32) - (tp == t)
        lo = t - half
        hi = np.minimum(t + half, 128)
        out[g * 5 + 4] = ((tp >= lo) & (tp < hi)) / (hi - lo).astype(np.float32) - (tp == t)
    return out


GSTOP = 0


def build(stage=99, dbg_cols=1024):
    nc = bass.Bass("TRN2", target_bir_lowering=False)
    S = Sched()
    es = ExitStack()

    def dram(name, shape, dt=F32, kind="ExternalInput"):
        return nc.dram_tensor(name, list(shape), dt, kind=kind).ap()

    x_d = dram("x", [NT, D])
    cc_d = dram("cc", [128, 8, 2])
    ada_w_d = dram("ada_w", [2, D, 6 * D])
    ada_b_d = dram("ada_b", [2, 6 * D])
    ln_g_d = dram("ln_g", [2, 2, D])
    ln_b_d = dram("ln_b", [2, 2, D])
    w_in0_d = dram("l0_w_in", [D, 2304])
    w_out0_d = dram("l0_w_out", [D, D])
    lam_d = dram("lamv", [4, 64])
    subg_d = dram("l0_subln_g", [128])
    qkg_d = dram("qkg", [5 * 64])
    rope_d = dram("rope", [NT, 128])
    ident_d = dram("ident", [128, 128])
    out_d = dram("out", [SEQ, D], kind="ExternalOutput")
    dbg_d = dram("dbg", [128 if stage == 1 else NT, dbg_cols], kind="ExternalOutput") if stage < 99 else None
    modd = dram("modd", [2, 2, 6 * D], kind="Internal")

    def sb(name, shape, dt=F32):
        return es.enter_context(nc.sbuf_tensor(name, list(shape), dt))

    Xraw = sb("X", [128, NCH * D])
    X = Xraw[:].rearrange("p (c f) -> p c f", c=NCH)
    hT = sb("hT", [128, 8, NT], BF16)
    AOT = sb("AOT", [128, 8, NT], BF16)
    WO = sb("WO", [128, 8, D], BF16)
    identb = sb("identb", [128, 128], BF16)
    identf = sb("identf", [128, 128])
    ropet = sb("ropet", [128, NCH, 128])
    big = sb("big", [128, 6 * 1024])
    bc = [big[:, i * 1024:(i + 1) * 1024] for i in range(6)]
    st = sb("st", [128, 64])
    PS = [es.enter_context(nc.psum_tensor(f"ps{i}", [128, 512], F32)) for i in range(8)]
    PSB = [p[:].bitcast(BF16) for p in PS]
    stn = [0]

    def stat():
        i = stn[0] % 64
        stn[0] += 1
        return st[:, i:i + 1], f"st{i}"

    arena_off = [0]

    def xview(nelem_f32, shape, dt=F32, pattern=None, **kw):
        a = arena_off[0]
        arena_off[0] += nelem_f32
        assert arena_off[0] <= NCH * D
        v = Xraw[:, a:a + nelem_f32]
        if dt != F32:
            v = v.bitcast(dt)
        if pattern is not None:
            v = v.rearrange(pattern, **kw)
        return v

    dkn = [0]

    def dk(prefix="d"):
        dkn[0] += 1
        return f"{prefix}{dkn[0]}"

    def DMA(q, out, in_, reads, writes, key):
        return S.add(q, lambda e: e.dma_start(out=out, in_=in_), reads=reads, writes=writes, dma=key)

    def MM(out, lhsT, rhs, start, stop, reads, writes):
        return S.add("pe", lambda e: e.matmul(out, lhsT, rhs, start=start, stop=stop), reads=reads, writes=writes)

    def TR(out, in_, ident, reads, writes):
        return S.add("pe", lambda e: e.transpose(out, in_, ident), reads=reads, writes=writes)

    def ACT(out, in_, func, reads, writes, bias=None, scale=None, accum_out=None):
        kw = {}
        if bias is not None:
            kw["bias"] = bias
        if scale is not None:
            kw["scale"] = scale
        if accum_out is not None:
            kw["accum_out"] = accum_out
        return S.add("act", lambda e: e.activation(out, in_, func, **kw), reads=reads, writes=writes)

    def TT(eng, out, in0, in1, op, reads, writes):
        return S.add(eng, lambda e: e.tensor_tensor(out, in0, in1, op), reads=reads, writes=writes)

    def TS(eng, out, in0, s1, s2, op0, op1, reads, writes):
        if op1 is None:
            return S.add(eng, lambda e: e.tensor_scalar(out, in0, s1, None, op0), reads=reads, writes=writes)
        return S.add(eng, lambda e: e.tensor_scalar(out, in0, s1, s2, op0, op1), reads=reads, writes=writes)

    def STT(eng, out, in0, scalar, in1, op0, op1, reads, writes):
        return S.add(eng, lambda e: e.scalar_tensor_tensor(out, in0, scalar, in1, op0, op1), reads=reads, writes=writes)

    def CP(eng, out, in_, reads, writes):
        if eng == "act":
            return S.add(eng, lambda e: e.copy(out, in_), reads=reads, writes=writes)
        return S.add(eng, lambda e: e.tensor_copy(out, in_), reads=reads, writes=writes)

    def MEMSET(eng, ap, val, writes):
        return S.add(eng, lambda e: e.memset(ap, val), reads=(), writes=writes)

    def fence(keys, engs=("pe", "act", "dve", "pool", "sp")):
        for e in engs:
            S.add(e, None, reads=keys, writes=())

    def barrier():
        allk = list(S.lw.keys())
        for e in ("pe", "act", "dve", "pool", "sp"):
            S.add(e, None, reads=allk, writes=allk)

    nc_lp = es.enter_context(nc.allow_low_precision("bf16 matmul operands, fp32 accumulation"))
    es.enter_context(nc.allow_non_contiguous_dma("small strided constant loads"))

    for c in range(NCH):
        DMA("sp", X[:, c, :], x_d[c * 128:(c + 1) * 128, :], (), [f"X{c}"], "ldx")
    ccs = sb("ccs", [128, 8, 2])
    DMA("sp", ccs[:], cc_d, (), ["ccs"], "const")
    DMA("sp", identf[:], ident_d, (), ["identf"], "const")
    DMA("sp", ropet[:], rope_d.rearrange("(c p) f -> p c f", p=128), (), ["ropet"], "const")
    fence(["ccs", "identf", "ropet"])
    CP("dve", identb[:], identf[:], ["identf"], ["identb"])

    scs = sb("scs", [128, 8, 2])
    S.add("act", lambda e: e.activation(scs[:], ccs[:], AF.Silu), reads=["ccs"], writes=["scs"])
    adab = sb("adab", [2, 512])
    modrow = sb("modrow", [2, 512])
    NST = 3
    aotf = AOT[:].rearrange("p k t -> p (k t)").bitcast(F32)
    hTf = hT[:].rearrange("p k t -> p (k t)").bitcast(F32)
    stg = [aotf[:, 0:4096], aotf[:, 4096:8192], hTf[:, 0:4096]]
    gi = 0
    for l in range(2):
        for j in range(12):
            s = gi % NST
            st3 = stg[s].rearrange("p (k c) -> p k c", k=8)
            DMA("sp", st3, ada_w_d[l, :, j * 512:(j + 1) * 512].rearrange("(k p) c -> p k c", p=128),
                (), [f"stg{s}"], f"adaw{s}")
            for r in range(2):
                DMA("sp", adab[r:r + 1, :], ada_b_d[l:l + 1, j * 512:(j + 1) * 512], (), ["adab"], "adab")
            pb = gi % 2
            for k in range(8):
                MM(PS[pb][0:2, :], scs[:, k, :], st3[:, k, :], k == 0, k == 7,
                   ["scs", f"stg{s}"], [f"ps{pb}"])
            TT("dve", modrow[:], PS[pb][0:2, :], adab[:], ALU.add, [f"ps{pb}", "adab"], ["modrow"])
            DMA("sp", modd[l, :, j * 512:(j + 1) * 512], modrow[:], ["modrow"], ["modd"], "modw")
            gi += 1
    barrier()

    def dbg_out(ap, cols):
        tmpd = sb("tmpd", [128, cols])
        CP("dve", tmpd[:], ap, list(S.lw.keys()), ["tmpd"])
        DMA("sp", dbg_d[:, 0:cols], tmpd[:], ["tmpd"], ["dbgd"], "dbg")

    if stage == 1:
        t1 = sb("t1", [128, 192])
        DMA("sp", t1[:], modd.rearrange("l r (a b) -> (l r a) b", b=192), ["modd"], ["t1"], "t1")
        dbg_out(t1[:], 192)
        S.add("sp", None, reads=list(S.lw.keys()), writes=())
        S.emit(nc, es)
        es.close()
        return nc

    xscr = dram("xscr", [NT, D], kind="Internal")

    def bcast_mod(i, l, r, idx, plus1=False):
        src = modd[l, r:r + 1, idx * 1024:(idx + 1) * 1024].partition_broadcast(128)
        DMA("sp", bc[i].rearrange("p (o f) -> p o f", o=1), src, ["modd"], [f"bc{i}"], f"bcl{i}")
        if plus1:
            TS("dve", bc[i], bc[i], 1.0, None, ALU.add, None, [f"bc{i}"], [f"bc{i}"])

    def bcast_row(i, row_ap):
        DMA("sp", bc[i].rearrange("p (o f) -> p o f", o=1), row_ap.partition_broadcast(128), (), [f"bc{i}"], f"bcl{i}")

    hb = [sb(f"hb{i}", [128, D], BF16) for i in range(2)]

    def modulate_T(l, isc, ish, nch):
        bcast_mod(0, l, 0, isc, True)
        bcast_mod(1, l, 0, ish)
        if nch > NLAT:
            bcast_mod(2, l, 1, isc, True)
            bcast_mod(3, l, 1, ish)
        for c in range(nch):
            a, b_ = (0, 1) if c < NLAT else (2, 3)
            t = 4 + (c % 2)
            TT("dve", bc[t], X[:, c, :], bc[a], ALU.mult, [f"X{c}", f"bc{a}"], [f"bc{t}"])
            TT("dve", hb[c % 2][:], bc[t], bc[b_], ALU.add, [f"bc{t}", f"bc{b_}"], [f"hb{c % 2}"])
            pb = 4 + (c % 2)
            for k in range(8):
                TR(PSB[pb][:, k * 128:(k + 1) * 128], hb[c % 2][:, k * 128:(k + 1) * 128], identb[:],
                   [f"hb{c % 2}", "identb"], [f"ps{pb}"])
            CP("act", hT[:, :, c * 128:(c + 1) * 128], PSB[pb].rearrange("p (k t) -> p k t", k=8),
               [f"ps{pb}"], [f"hT{c}"])

    def spill_X(nch):
        for c in range(nch):
            DMA("sp", xscr[c * 128:(c + 1) * 128, :], X[:, c, :], [f"X{c}"], ["xscr"], "xsp")

    def reload_X(nch):
        for c in range(nch):
            DMA("sp", X[:, c, :], xscr[c * 128:(c + 1) * 128, :], ["xscr"], [f"X{c}"], "xrl")

    def rstd_from(out_ap, okey, in_ap, ikey, scale, eps):
        tmp, tk = stat()
        ACT(tmp, in_ap, AF.Ln, [ikey], [tk], bias=epst[eps], scale=scale)
        ACT(out_ap, tmp, AF.Exp, [tk], [okey], scale=-0.5)

    epsv = sb("epsv", [128, 2])
    MEMSET("dve", epsv[:, 0:1], LN_EPS, ["epsv"])
    MEMSET("dve", epsv[:, 1:2], RMS_EPS, ["epsv"])
    fence(["epsv"], ("act",))
    epst = {LN_EPS: epsv[:, 0:1], RMS_EPS: epsv[:, 1:2]}

    def layer_norm_chunk(c, ig, ib):
        stt = sb(f"lnst{c}", [128, 2, 6]) if False else lnstat
        S.add("dve", lambda e: e.bn_stats(stt[:, 0, :], X[:, c, 0:512]), reads=[f"X{c}"], writes=["lnst0"])
        S.add("dve", lambda e: e.bn_stats(stt[:, 1, :], X[:, c, 512:1024]), reads=[f"X{c}"], writes=["lnst1"])
        mv, mk = lnmv, "lnmv"
        S.add("dve", lambda e: e.bn_aggr(mv[:], stt[:]), reads=["lnst0", "lnst1"], writes=[mk])
        rs, rk = stat()
        rstd_from(rs, rk, mv[:, 1:2], mk, 1.0, LN_EPS)
        nm, nk = stat()
        TS("dve", nm, mv[:, 0:1], rs, -1.0, ALU.mult, ALU.mult, [mk, rk], [nk])
        t = 4 + (c % 2)
        ACT(bc[t], X[:, c, :], AF.Identity, [f"X{c}", rk, nk], [f"bc{t}"], bias=nm, scale=rs)
        TT("dve", bc[t], bc[t], bc[ig], ALU.mult, [f"bc{t}", f"bc{ig}"], [f"bc{t}"])
        TT("dve", X[:, c, :], bc[t], bc[ib], ALU.add, [f"bc{t}", f"bc{ib}"], [f"X{c}"])

    lnstat = sb("lnstat", [128, 2, 6])
    lnmv = sb("lnmv", [128, 2])


    def outproj_ln(l, nch):
        bcast_mod(0, l, 0, 2)
        if nch > NLAT:
            bcast_mod(1, l, 1, 2)
        bcast_row(2, ln_g_d[l, 0:1, :])
        bcast_row(3, ln_b_d[l, 0:1, :])
        for c in range(nch):
            gtile = 0 if c < NLAT else 1
            t = 4 + (c % 2)
            for hf in range(2):
                pb = 2 * (c % 2) + hf
                for m in range(8):
                    MM(PS[pb][:, :], AOT[:, m, c * 128:(c + 1) * 128], WO[:, m, hf * 512:(hf + 1) * 512], m == 0, m == 7,
                       [f"AOT{c}", "WO"], [f"ps{pb}"])
                TT("dve", bc[t][:, hf * 512:(hf + 1) * 512], PS[pb][:, :], bc[gtile][:, hf * 512:(hf + 1) * 512], ALU.mult,
                   [f"ps{pb}", f"bc{gtile}"], [f"bc{t}"])
            STT("dve", X[:, c, :], X[:, c, :], ALPHA, bc[t], ALU.mult, ALU.add, [f"X{c}", f"bc{t}"], [f"X{c}"])
            layer_norm_chunk(c, 2, 3)
        barrier()

    if True:
        l = 0
        DMA("pool", WO[:], w_out0_d.rearrange("(k p) c -> p k c", p=128), (), ["WO"], "wo")
        modulate_T(0, 1, 0, NCH)
        spill_X(NCH)
        barrier()
        if stage == 15:
            dbgb = dram("dbgb", [128, 8 * NT], BF16, kind="ExternalOutput")
            DMA("sp", dbgb, hT[:].rearrange("p k t -> p (k t)"), [f"hT{c}" for c in range(NCH)], ["dbgd"], "dbg")
            S.add("sp", None, reads=list(S.lw.keys()) + ["dbgd"], writes=())
            S.emit(nc, es)
            es.close()
            return nc
        arena_off[0] = 0
        WG = [xview(1536, [128, 8, 384], BF16, "p (k c) -> p k c", k=8) for _ in range(2)]
        QKT = xview(3 * NT // 2, None, BF16, "p (s t) -> p s t", s=3)
        VX = xview(NCH * 130 // 2, None, BF16, "p (c f) -> p c f", c=NCH)
        QKR = [xview(192, None, BF16) for _ in range(2)]
        T1 = [xview(384, None) for _ in range(2)]
        T2 = [xview(384, None) for _ in range(2)]
        SQ = xview(384, None)
        PT = [xview(256, None, BF16) for _ in range(4)]
        O1 = xview(512, None, F32, "p (j f) -> p j f", j=4)
        OO = [xview(128, None) for _ in range(2)]
        AOK = xview(512, None, BF16, "p (j f) -> p j f", j=4)
        SUBG = xview(128, None)
        G5 = xview(320, None)
        LAMT = xview(256, None, F32, "p (a f) -> p a f", a=4)
        lamt = sb("lamt", [128, 4])
        RSS = sb("RSS", [128, 24])
        DMA("sp", SUBG.rearrange("p (o f) -> p o f", o=1), subg_d.rearrange("(o f) -> o f", o=1).partition_broadcast(128),
            (), ["SUBG"], "c2")
        DMA("sp", G5.rearrange("p (o f) -> p o f", o=1), qkg_d.rearrange("(o f) -> o f", o=1).partition_broadcast(128),
            (), ["G5"], "c2")
        for a in range(4):
            DMA("sp", LAMT[:, a:a + 1, :], lam_d[a:a + 1, :].partition_broadcast(128), (), ["LAMT"], "c2")
        fence(["SUBG", "G5", "LAMT"])
        TS("dve", SUBG, SUBG, 1.0 - LAM_INIT0, None, ALU.mult, None, ["SUBG"], ["SUBG"])
        TT("dve", LAMT[:, 0, :], LAMT[:, 0, :], LAMT[:, 1, :], ALU.mult, ["LAMT"], ["LAMT"])
        TT("dve", LAMT[:, 2, :], LAMT[:, 2, :], LAMT[:, 3, :], ALU.mult, ["LAMT"], ["LAMT"])
        S.add("dve", lambda e: e.tensor_reduce(lamt[:, 0:1], LAMT[:, 0, :], AX.X, ALU.add), reads=["LAMT"], writes=["lamt"])
        S.add("dve", lambda e: e.tensor_reduce(lamt[:, 1:2], LAMT[:, 2, :], AX.X, ALU.add), reads=["LAMT"], writes=["lamt"])
        ACT(lamt[:, 0:2], lamt[:, 0:2], AF.Exp, ["lamt"], ["lamt"])
        TT("dve", lamt[:, 2:3], lamt[:, 1:2], lamt[:, 0:1], ALU.subtract, ["lamt"], ["lamt"])
        TS("dve", lamt[:, 3:4], lamt[:, 2:3], -LAM_INIT0, None, ALU.add, None, ["lamt"], ["lamt"])
        nlam = lamt[:, 3:4]

        groups = [("A", h) for h in range(4)] + [("B", n) for n in range(2)]

        def load_wg(gi):
            kind, idx = groups[gi]
            w = WG[gi % 2]
            if kind == "A":
                cols = [(idx * 128, 128), (512 + idx * 128, 128), (1024 + idx * 128, 128)]
            else:
                cols = [(1536 + idx * 256, 256), (2048 + idx * 64, 64), (2176 + idx * 64, 64)]
            o = 0
            for (c0, n) in cols:
                DMA("pool", w[:, :, o:o + n], w_in0_d[:, c0:c0 + n].rearrange("(k p) c -> p k c", p=128),
                    (), [f"WG{gi % 2}"], f"wg{gi % 2}")
                o += n

        load_wg(0)
        ptn = [0]
        for gi, (kind, idx) in enumerate(groups):
            if gi + 1 < len(groups):
                load_wg(gi + 1)
            w = WG[gi % 2]
            nv = 4 if kind == "A" else 5
            nr = nv * 64
            vcol = 256 if kind == "A" else 320
            vw = 128 if kind == "A" else 64
            MEMSET("dve", VX[:, :, vw:vw + 1], 1.0, [f"VX{c}" for c in range(NCH)])
            for c in range(NCH):
                pb = 6 + (c % 2)
                for k in range(8):
                    MM(PS[pb][:, 0:384], hT[:, k, c * 128:(c + 1) * 128], w[:, k, :], k == 0, k == 7,
                       [f"hT{c}", f"WG{gi % 2}"], [f"ps{pb}"])
                CP("act", VX[:, c, 0:vw], PS[pb][:, vcol:vcol + vw], [f"ps{pb}"], [f"VX{c}"])
                src = PS[pb][:, 0:nr]
                skey = f"ps{pb}"
                i2 = c % 2
                if kind == "B":
                    ACT(SQ[:, 0:nr], src, AF.Square, [skey], ["SQ"])
                    S.add("dve", lambda e, c=c, nr=nr: e.tensor_reduce(RSS[:, 0:5], SQ[:, 0:nr].rearrange("p (v f) -> p v f", v=5), AX.X, ALU.add),
                          reads=["SQ"], writes=["RSS"])
                    ACT(RSS[:, 8:13], RSS[:, 0:5], AF.Ln, ["RSS"], ["RSS2"], bias=epst[RMS_EPS], scale=1.0 / 64)
                    ACT(RSS[:, 16:21], RSS[:, 8:13], AF.Exp, ["RSS2"], ["RSS3"], scale=-0.5)
                    TT("dve", SQ[:, 0:nr].rearrange("p (v f) -> p v f", v=5), src.rearrange("p (v f) -> p v f", v=5),
                       RSS[:, 16:21].unsqueeze(2).to_broadcast([128, 5, 64]), ALU.mult, [skey, "RSS3"], ["SQ"])
                    TT("dve", SQ[:, 0:nr], SQ[:, 0:nr], G5, ALU.mult, ["SQ", "G5"], ["SQ"])
                    src = SQ[:, 0:nr]
                    skey = "SQ"
                cosb = ropet[:, c, 0:64].unsqueeze(1).to_broadcast([128, nv, 64])
                TT("dve", T1[i2][:, 0:nr].rearrange("p (v f) -> p v f", v=nv), src.rearrange("p (v f) -> p v f", v=nv),
                   cosb, ALU.mult, [skey, "ropet"], [f"T1{i2}"])
                s5 = src.rearrange("p (v a j f) -> p v a j f", v=nv, a=2, j=2)
                t5 = T2[i2][:, 0:nr].rearrange("p (v a j f) -> p v a j f", v=nv, a=2, j=2)
                sn5 = ropet[:, c, 64:128].rearrange("p (a j f) -> p a j f", a=2, j=2)
                for j in range(2):
                    for a in range(2):
                        sinb = sn5[:, a, j, :].unsqueeze(1).to_broadcast([128, nv, 16])
                        TT("dve", t5[:, :, a, j, :], s5[:, :, a, 1 - j, :], sinb, ALU.mult,
                           [skey, "ropet"], [f"T2{i2}"])
                TT("dve", QKR[i2][:, 0:nr], T1[i2][:, 0:nr], T2[i2][:, 0:nr], ALU.add, [f"T1{i2}", f"T2{i2}"], [f"QKR{i2}"])
                if kind == "B":
                    TT("dve", QKR[i2][:, 320:384], T1[i2][:, 256:320], T2[i2][:, 256:320], ALU.add,
                       [f"T1{i2}", f"T2{i2}"], [f"QKR{i2}"])
                ntr = 2 if kind == "A" else 3
                tb = 4 + (c % 2)
                for t in range(ntr):
                    TR(PSB[tb][:, t * 128:(t + 1) * 128], QKR[i2][:, t * 128:(t + 1) * 128], identb[:],
                       [f"QKR{i2}", "identb"], [f"ps{tb}"])
                CP("act", QKT[:, 0:ntr, c * 128:(c + 1) * 128], PSB[tb][:, 0:ntr * 128].rearrange("p (s t) -> p s t", s=ntr),
                   [f"ps{tb}"], [f"QKT{c}"])
            if stage == 16 and gi == GSTOP:
                dbgb = dram("dbgb", [128, 3 * NT], BF16, kind="ExternalOutput")
                DMA("sp", dbgb, QKT.rearrange("p s t -> p (s t)"), [f"QKT{c}" for c in range(NCH)], ["dbgd"], "dbg")
                dbgv = dram("dbgv", [128, NCH * 130], BF16, kind="ExternalOutput")
                DMA("sp", dbgv, VX.rearrange("p c f -> p (c f)"), [f"VX{c}" for c in range(NCH)], ["dbgd"], "dbg")
                S.add("sp", None, reads=list(S.lw.keys()) + ["dbgd"], writes=())
                S.emit(nc, es)
                es.close()
                return nc
            blocks = [(qb * 4, 4, list(range(NCH))) for qb in range(4)] + [(16, 2, [16, 17])]
            if kind == "A":
                heads = [(0, 0, 1, s * 64) for s in range(2)]
            else:
                heads = [(j4 // 2, 2, (j4 % 2) * 64) for j4 in range(4)]
            for (qc0, nqc, kcs) in blocks:
                ncols = nqc * 128
                q0 = qc0 * 128
                qkeys = [f"QKT{qc0 + j}" for j in range(nqc)]
                for hi, hd in enumerate(heads):
                    if kind == "A":
                        qs, ks, pbase = 0, 1, hi * 64
                    else:
                        qs, ks, pbase = hd
                    for kn, kc in enumerate(kcs):
                        sp_ = 4 + (ptn[0] % 2)
                        MM(PS[sp_][:, 0:ncols], QKT[pbase:pbase + 64, ks, kc * 128:(kc + 1) * 128],
                           QKT[pbase:pbase + 64, qs, q0:q0 + ncols], True, True,
                           [f"QKT{kc}"] + qkeys, [f"ps{sp_}"])
                        pi = ptn[0] % 4
                        ptn[0] += 1
                        ACT(PT[pi][:, 0:ncols], PS[sp_][:, 0:ncols], AF.Exp, [f"ps{sp_}"], [f"PT{pi}"], scale=0.125)
                        for j in range(nqc):
                            MM(PS[j][:, 0:vw + 1], PT[pi][:, j * 128:(j + 1) * 128], VX[:, kc, 0:vw + 1],
                               kn == 0, kn == len(kcs) - 1, [f"PT{pi}", f"VX{kc}"], [f"ps{j}"])
                    for j in range(nqc):
                        rz, rk = stat()
                        S.add("dve", lambda e, rz=rz, j=j, vw=vw: e.reciprocal(rz, PS[j][:, vw:vw + 1]), reads=[f"ps{j}"], writes=[rk])
                        if kind == "B":
                            TS("dve", AOK[:, j, hi * 64:(hi + 1) * 64], PS[j][:, 0:64], rz, None, ALU.mult, None,
                               [f"ps{j}", rk], [f"AOK{j}"])
                        elif hi == 0:
                            TS("dve", O1[:, j, :], PS[j][:, 0:128], rz, None, ALU.mult, None, [f"ps{j}", rk], [f"O1{j}"])
                        else:
                            r2, r2k = stat()
                            TT("dve", r2, rz, nlam, ALU.mult, [rk, "lamt"], [r2k])
                            oo = OO[j % 2]
                            STT("dve", oo, PS[j][:, 0:128], r2, O1[:, j, :], ALU.mult, ALU.add,
                                [f"ps{j}", r2k, f"O1{j}"], [f"OO{j % 2}"])
                            ssq, ssk = stat()
                            TT("dve", T1[j % 2][:, 0:128], oo, oo, ALU.mult, [f"OO{j % 2}"], [f"T1{j % 2}"])
                            S.add("dve", lambda e, ssq=ssq, j=j: e.tensor_reduce(ssq, T1[j % 2][:, 0:128], AX.X, ALU.add),
                                  reads=[f"T1{j % 2}"], writes=[ssk])
                            rs, rsk = stat()
                            rstd_from(rs, rsk, ssq, ssk, 1.0 / 128, RMS_EPS)
                            STT("dve", AOK[:, j, 0:128], oo, rs, SUBG, ALU.mult, ALU.mult,
                                [f"OO{j % 2}", rsk, "SUBG"], [f"AOK{j}"])
                    last = (kind == "A" and hi == 1) or (kind == "B" and hi == 3)
                    if last:
                        for j in range(nqc):
                            tb = 6 + (j % 2)
                            ntr = 1 if kind == "A" else 2
                            for t in range(ntr):
                                TR(PSB[tb][:, t * 128:(t + 1) * 128], AOK[:, j, t * 128:(t + 1) * 128], identb[:],
                                   [f"AOK{j}", "identb"], [f"ps{tb}"])
                            m0 = idx if kind == "A" else 4 + idx * 2
                            tc = qc0 + j
                            CP("act", AOT[:, m0:m0 + ntr, tc * 128:(tc + 1) * 128],
                               PSB[tb][:, 0:ntr * 128].rearrange("p (s t) -> p s t", s=ntr), [f"ps{tb}"], [f"AOT{tc}"])
        barrier()
        reload_X(NCH)
        outproj_ln(0, NCH)

    if stage == 2:
        for c in range(NCH):
            DMA("sp", dbg_d[c * 128:(c + 1) * 128, :], X[:, c, :], [f"X{c}"], ["dbgd"], "dbg")
        S.add("sp", None, reads=list(S.lw.keys()) + ["dbgd"], writes=())
        S.emit(nc, es)
        es.close()
        return nc
    router_d = dram("moe_router", [2, D, NE])
    NEX = 1 if stage == 31 else NE
    wg_d = dram("moe_w_gate", [2, NEX, D, FF])
    wu_d = dram("moe_w_up", [2, NEX, D, FF])
    wd_d = dram("moe_w_down", [2, NEX, FF, D])
    iota_d = dram("iota288", [128, 288])
    utri_d = dram("utri", [128, 128])
    aotb = AOT[:].rearrange("p k t -> p (k t)")
    wob = WO[:].rearrange("p k c -> p (k c)")
    hmb = hT[:].rearrange("p k t -> p (k t)").rearrange("p (c f) -> p c f", c=NCH)
    NRING = 5
    RING = [aotb[:, i * 2048:(i + 1) * 2048] for i in range(NRING)]
    XGT = aotb[:, 10240:12544].rearrange("p (k s) -> p k s", k=8)
    HIDT = aotb[:, 12544:17152].rearrange("p (j s) -> p j s", j=16)
    SELR = [aotb[:, 17152 + i * 288:17152 + (i + 1) * 288] for i in range(2)]
    SELGT = [aotb[:, 17728 + i * 384:17728 + (i + 1) * 384].rearrange("p (s t) -> p s t", s=3) for i in range(1)]
    SELR += [wob[:, 5280 + i * 288:5280 + (i + 1) * 288] for i in range(3)]
    SELGT += [wob[:, 6144 + i * 384:6144 + (i + 1) * 384].rearrange("p (s t) -> p s t", s=3) for i in range(2)]
    NSEL = len(SELR)
    NSGT = len(SELGT)
    YG = wob[:, 0:3072].rearrange("p (s f) -> p s f", s=3)
    IOTA = wob[:, 3072:3648].bitcast(F32)
    RW = wob[:, 3648:3904].bitcast(F32).rearrange("p (k e) -> p k e", k=8)
    UTRI = wob[:, 3904:4032]
    ONESB = wob[:, 4032:4160]
    MASKB = wob[:, 4160:4448].rearrange("p (c e) -> p c e", c=NCH)
    SGR = [wob[:, 4448:5024].bitcast(F32), wob[:, 6912:7488].bitcast(F32)]
    UTF = wob[:, 5024:5280].bitcast(F32)
    WEX = ropet[:].rearrange("p c f -> p (c f)")
    AFF = hb[0][:].bitcast(F32)[:, 0:288].rearrange("p (c e) -> p c e", c=NCH)
    MASK = hb[1][:].bitcast(F32)[:, 0:288].rearrange("p (c e) -> p c e", c=NCH)
    mo2 = sb("mo2", [128, 2, NCH * NE])
    SLOT = mo2[:, 0, :].rearrange("p (c e) -> p c e", c=NCH)
    GS = mo2[:, 1, :].rearrange("p (c e) -> p c e", c=NCH)
    m8 = sb("m8", [16, 8])

    def moe_layer(l, nch, final_out=False):
        ns = 256 + (32 if nch > NLAT else 0)
        nsc = (ns + 127) // 128
        scsz = [min(128, ns - s * 128) for s in range(nsc)]
        barrier()
        DMA("sp", IOTA, iota_d, (), ["IOTA"], "c3")
        DMA("sp", UTF, utri_d, (), ["UTF"], "c3")
        DMA("sp", RW, router_d[l].rearrange("(k p) e -> p k e", p=128), (), ["RW"], "c3")
        fence(["IOTA", "UTF", "RW"])
        CP("dve", UTRI, UTF, ["UTF"], ["UTRI"])
        MEMSET("dve", ONESB, 1.0, ["ONESB"])
        bcast_mod(0, l, 0, 4, True)
        bcast_mod(1, l, 0, 3)
        if nch > NLAT:
            bcast_mod(2, l, 1, 4, True)
            bcast_mod(3, l, 1, 3)
        for c in range(nch):
            a, b_ = (0, 1) if c < NLAT else (2, 3)
            TT("dve", bc[4], X[:, c, :], bc[a], ALU.mult, [f"X{c}", f"bc{a}"], ["bc4"])
            TT("dve", bc[4], bc[4], bc[b_], ALU.add, ["bc4", f"bc{b_}"], ["bc4"])
            CP("act", hmb[:, c, :], bc[4], ["bc4"], [f"hmb{c}"])
            TS("dve", X[:, c, :], X[:, c, :], ALPHA, None, ALU.mult, None, [f"X{c}"], [f"X{c}"])
            for k in range(8):
                pb = k // 4
                TR(PS[pb][:, (k % 4) * 128:(k % 4 + 1) * 128], bc[4][:, k * 128:(k + 1) * 128], identf[:],
                   ["bc4", "identf"], [f"ps{pb}"])
            hmT = bc[5].rearrange("p (k t) -> p k t", k=8)
            CP("act", hmT[:, 0:4, :], PS[0][:, :].rearrange("p (k t) -> p k t", k=4), ["ps0"], ["bc5"])
            CP("dve", hmT[:, 4:8, :], PS[1][:, :].rearrange("p (k t) -> p k t", k=4), ["ps1"], ["bc5"])
            for k in range(8):
                MM(PS[2][:, 0:NE], hmT[:, k, :], RW[:, k, :], k == 0, k == 7, ["bc5", "RW"], ["ps2"])
            mx, mxk = stat()
            S.add("dve", lambda e, mx=mx: e.tensor_reduce(mx, PS[2][:, 0:NE], AX.X, ALU.max), reads=["ps2"], writes=[mxk])
            nmx, nmk = stat()
            TS("dve", nmx, mx, -1.0, None, ALU.mult, None, [mxk], [nmk])
            ACT(AFF[:, c, :], PS[2][:, 0:NE], AF.Exp, ["ps2", nmk], [f"AFF{c}"], bias=nmx)
            sm, smk = stat()
            S.add("dve", lambda e, sm=sm, c=c: e.tensor_reduce(sm, AFF[:, c, :], AX.X, ALU.add), reads=[f"AFF{c}"], writes=[smk])
            rc, rck = stat()
            S.add("dve", lambda e, rc=rc, sm=sm: e.reciprocal(rc, sm), reads=[smk], writes=[rck])
            TS("dve", AFF[:, c, :], AFF[:, c, :], rc, None, ALU.mult, None, [f"AFF{c}", rck], [f"AFF{c}"])
            TR(PS[3][0:NE, 0:128], AFF[:, c, :], identf[:], [f"AFF{c}", "identf"], ["ps3"])
            CP("dve", WEX[0:NE, c * 128:(c + 1) * 128], PS[3][0:NE, 0:128], ["ps3"], ["WEX"])
        sets = [(0, SEQ, 256)] + ([(SEQ, NT, 32)] if nch > NLAT else [])
        for (t0, t1, cap) in sets:
            wv = WEX[0:NE, t0:t1]
            for r in range(cap // 8):
                S.add("dve", lambda e, wv=wv: e.max(m8[:], wv), reads=["WEX"], writes=["m8"])
                S.add("dve", lambda e, wv=wv: e.match_replace(wv, m8[:], wv, -1.0), reads=["WEX", "m8"], writes=["WEX"])
        TS("dve", WEX[0:NE, 0:nch * 128], WEX[0:NE, 0:nch * 128], 0.0, None, ALU.is_lt, None, ["WEX"], ["WEX"])
        for c in range(nch):
            TR(PS[4][:, c * NE:(c + 1) * NE], WEX[0:NE, c * 128:(c + 1) * 128], identf[0:NE, 0:NE], ["WEX", "identf"], ["ps4"])
        mflat = mo2[:, 0, 0:nch * NE]
        CP("dve", MASK[:, 0:nch, :], PS[4][:, 0:nch * NE].rearrange("p (c e) -> p c e", c=nch), ["ps4"], ["MASK"])
        CP("dve", MASKB[:, 0:nch, :], MASK[:, 0:nch, :], ["MASK"], ["MASKB"])
        TT("dve", GS[:, 0:nch, :], AFF[:, 0:nch, :], MASK[:, 0:nch, :], ALU.mult, [f"AFF{c}" for c in range(nch)] + ["MASK"], ["GS"])
        for c in range(nch):
            c0 = 0 if c < NLAT else NLAT
            prev = list(range(c0, c))
            for i, cp in enumerate(prev):
                MM(PS[5][:, c * NE:(c + 1) * NE], ONESB, MASKB[:, cp, :], i == 0, False, ["ONESB", "MASKB"], ["ps5"])
            MM(PS[5][:, c * NE:(c + 1) * NE], UTRI, MASKB[:, c, :], len(prev) == 0, True, ["UTRI", "MASKB"], ["ps5"])
        TS("dve", SLOT[:, 0:NLAT, :], PS[5][:, 0:NLAT * NE].rearrange("p (c e) -> p c e", c=NLAT), 1.0, None, ALU.add, None, ["ps5"], ["SLOT"])
        if nch > NLAT:
            TS("dve", SLOT[:, NLAT:nch, :], PS[5][:, NLAT * NE:nch * NE].rearrange("p (c e) -> p c e", c=nch - NLAT), 257.0, None,
               ALU.add, None, ["ps5"], ["SLOT"])
        TT("dve", SLOT[:, 0:nch, :], SLOT[:, 0:nch, :], MASK[:, 0:nch, :], ALU.mult, ["SLOT", "MASK"], ["SLOT"])
        TS("dve", SLOT[:, 0:nch, :], SLOT[:, 0:nch, :], -1.0, None, ALU.add, None, ["SLOT"], ["SLOT"])
        bcast_mod(0, l, 0, 5)
        if nch > NLAT:
            bcast_mod(1, l, 1, 5)
        bcast_row(2, ln_g_d[l, 1:2, :])
        bcast_row(3, ln_b_d[l, 1:2, :])
        units = []
        for e_ in range(NEX):
            for fu in range(8):
                units.append(("g", e_, fu))
                units.append(("u", e_, fu))
            for du in range(8):
                units.append(("d", e_, du))
        issued = [0]

        def issue_until(n):
            while issued[0] < min(n, len(units)):
                kind, e_, i = units[issued[0]]
                slot = issued[0] % NRING
                if kind == "d":
                    src = wd_d[l, e_, i * 256:(i + 1) * 256, :].rearrange("(j p) c -> p j c", p=128)
                    dst = RING[slot].rearrange("p (j c) -> p j c", j=2)
                else:
                    wsrc = wg_d if kind == "g" else wu_d
                    src = wsrc[l, e_, :, i * 256:(i + 1) * 256].rearrange("(k p) c -> p k c", p=128)
                    dst = RING[slot].rearrange("p (k c) -> p k c", k=8)
                DMA("pool", dst, src, (), [f"wr{slot}"], f"wr{slot}")
                issued[0] += 1

        ui = [0]

        def next_unit():
            i = ui[0]
            ui[0] += 1
            return i % NRING

        def refill():
            issue_until(ui[0] + NRING)

        issue_until(NRING)
        seln = [0]
        sgn = [0]
        for e_ in range(NEX):
            for half in range(2):
                for c in range(nch):
                    si = seln[0] % NSEL
                    seln[0] += 1
                    TS("dve", SELR[si][:, 0:ns], IOTA[:, 0:ns], SLOT[:, c, e_:e_ + 1], None, ALU.is_equal, None,
                       ["IOTA", "SLOT"], [f"SEL{si}"])
                    for f4 in range(4):
                        f = half * 4 + f4
                        MM(PS[f4][:, 0:ns], hmb[:, c, f * 128:(f + 1) * 128], SELR[si][:, 0:ns], c == 0, c == nch - 1,
                           [f"hmb{c}", f"SEL{si}"], [f"ps{f4}"])
                for f4 in range(4):
                    f = half * 4 + f4
                    CP("act" if f4 % 2 == 0 else "dve", XGT[:, f, 0:ns], PS[f4][:, 0:ns], [f"ps{f4}"], ["XGT"])
            for fu in range(8):
                sg_ = next_unit()
                su_ = next_unit()
                wgv = RING[sg_].rearrange("p (k c) -> p k c", k=8)
                wuv = RING[su_].rearrange("p (k c) -> p k c", k=8)
                for j2 in range(2):
                    jg = fu * 2 + j2
                    bg, bu = (4, 5) if jg % 2 == 0 else (6, 7)
                    for k in range(8):
                        MM(PS[bg][:, 0:ns], wgv[:, k, j2 * 128:(j2 + 1) * 128], XGT[:, k, 0:ns], k == 0, k == 7,
                           [f"wr{sg_}", "XGT"], [f"ps{bg}"])
                    for k in range(8):
                        MM(PS[bu][:, 0:ns], wuv[:, k, j2 * 128:(j2 + 1) * 128], XGT[:, k, 0:ns], k == 0, k == 7,
                           [f"wr{su_}", "XGT"], [f"ps{bu}"])
                    SG = SGR[jg % 2]
                    ACT(SG[:, 0:ns], PS[bg][:, 0:ns], AF.Silu, [f"ps{bg}"], [f"SG{jg % 2}"])
                    TT("dve", HIDT[:, jg, 0:ns], PS[bu][:, 0:ns], SG[:, 0:ns], ALU.mult, [f"ps{bu}", f"SG{jg % 2}"], [f"HID{jg}"])
                refill()
            for du in range(8):
                sd_ = next_unit()
                wdv = RING[sd_].rearrange("p (j c) -> p j c", j=2)
                for sc in range(nsc):
                    sz = scsz[sc]
                    for hf in range(2):
                        pb = sc * 2 + hf
                        for jj in range(2):
                            jg = du * 2 + jj
                            MM(PS[pb][0:sz, :], HIDT[:, jg, sc * 128:sc * 128 + sz], wdv[:, jj, hf * 512:(hf + 1) * 512],
                               du == 0 and jj == 0, du == 7 and jj == 1, [f"HID{jg}", f"wr{sd_}"], [f"ps{pb}"])
                refill()
            for sc in range(nsc):
                sz = scsz[sc]
                gt = 0 if sc < 2 else 1
                for hf in range(2):
                    pb = sc * 2 + hf
                    TT("dve", YG[0:sz, sc, hf * 512:(hf + 1) * 512], PS[pb][0:sz, :], bc[gt][0:sz, hf * 512:(hf + 1) * 512], ALU.mult,
                       [f"ps{pb}", f"bc{gt}"], ["YG"])
            if stage == 31:
                d1 = dram("d_slot", [128, 2 * NCH * NE], kind="ExternalOutput")
                DMA("sp", d1, mo2[:].rearrange("p a f -> p (a f)"), ["SLOT", "GS"], ["dbgd"], "dbg")
                d2 = dram("d_xgt", [128, 8 * 288], BF16, kind="ExternalOutput")
                DMA("sp", d2, XGT.rearrange("p k s -> p (k s)"), ["XGT"], ["dbgd"], "dbg")
                d3 = dram("d_yg", [128, 3 * 1024], BF16, kind="ExternalOutput")
                DMA("sp", d3, YG.rearrange("p s f -> p (s f)"), ["YG"], ["dbgd"], "dbg")
                d4 = dram("d_hid", [128, 16 * 288], BF16, kind="ExternalOutput")
                DMA("sp", d4, HIDT.rearrange("p j s -> p (j s)"), [f"HID{j}" for j in range(16)], ["dbgd"], "dbg")
                S.add("sp", None, reads=list(S.lw.keys()) + ["dbgd"], writes=())
                S.emit(nc, es)
                es.close()
                return "STOP"
            for c in range(nch):
                si = seln[0] % NSEL
                seln[0] += 1
                TS("dve", SELR[si][:, 0:ns], IOTA[:, 0:ns], SLOT[:, c, e_:e_ + 1], GS[:, c, e_:e_ + 1], ALU.is_equal, ALU.mult,
                   ["IOTA", "SLOT", "GS"], [f"SEL{si}"])
                tb = 6 + (c % 2)
                for sc in range(nsc):
                    sz = scsz[sc]
                    TR(PSB[tb][0:sz, sc * 128:(sc + 1) * 128], SELR[si][:, sc * 128:sc * 128 + sz], identb[:],
                       [f"SEL{si}", "identb"], [f"ps{tb}"])
                gi_ = sgn[0] % NSGT
                sgn[0] += 1
                sgt = SELGT[gi_]
                CP("act", sgt[:, 0:2, :], PSB[tb][:, 0:256].rearrange("p (s t) -> p s t", s=2), [f"ps{tb}"], [f"SELGT{gi_}"])
                if nsc > 2:
                    CP("act", sgt[0:scsz[2], 2, :], PSB[tb][0:scsz[2], 256:384], [f"ps{tb}"], [f"SELGT{gi_}"])
                for hf in range(2):
                    pb = 2 * (c % 2) + hf
                    for sc in range(nsc):
                        sz = scsz[sc]
                        MM(PS[pb][:, :], sgt[0:sz, sc, :], YG[0:sz, sc, hf * 512:(hf + 1) * 512], sc == 0, sc == nsc - 1,
                           [f"SELGT{gi_}", "YG"], [f"ps{pb}"])
                    TT("dve", X[:, c, hf * 512:(hf + 1) * 512], PS[pb][:, :], X[:, c, hf * 512:(hf + 1) * 512], ALU.add,
                       [f"ps{pb}", f"X{c}"], [f"X{c}"])
        for c in range(nch):
            layer_norm_chunk(c, 2, 3)
            if final_out:
                DMA("sp", out_d[c * 128:(c + 1) * 128, :], X[:, c, :], [f"X{c}"], ["outd"], "outw")
        barrier()

    if moe_layer(0, NCH) == "STOP":
        return nc

    if stage == 3:
        for c in range(NCH):
            DMA("sp", dbg_d[c * 128:(c + 1) * 128, :], X[:, c, :], [f"X{c}"], ["dbgd"], "dbg")
        S.add("sp", None, reads=list(S.lw.keys()) + ["dbgd"], writes=())
        S.emit(nc, es)
        es.close()
        return nc

    w_in1_d = dram("l1_w_in", [D, 2048])
    w_out1_d = dram("l1_w_out", [D, D])
    poolw_d = dram("l1_pool_w", [4, 128, 128])
    psc_d = dram("psc", [128, 4])
    band_d = dram("band", [20, 128, 128])
    l1tab_d = dram("l1tab", [NPAT, 8, 128, 128])

    def layer1_mixer():
        l = 1
        barrier()
        DMA("pool", WO[:], w_out1_d.rearrange("(k p) c -> p k c", p=128), (), ["WO"], "wo")
        modulate_T(1, 1, 0, NCH)
        spill_X(NLAT)
        barrier()
        arena_off[0] = 0
        W1R = [xview(2048, None, BF16, "p (k c) -> p k c", k=8) for _ in range(2)]
        QT1 = xview(4 * SEQ // 2, None, BF16, "p (g t) -> p g t", g=4)
        KT1 = xview(4 * NT // 2, None, BF16, "p (g t) -> p g t", g=4)
        VXU = xview(NCH * 8 * 65 // 2, None, BF16)
        VX1 = VXU.rearrange("p (c h f) -> p c h f", c=NCH, h=8)
        U1 = VXU[:, 0:NLAT * 512].rearrange("p (c f) -> p c f", c=NLAT)
        PT1 = [xview(256, None, BF16) for _ in range(3)]
        rp = ropet[:].rearrange("p c f -> p (c f)")
        TBI = rp[:, 0:1280].bitcast(BF16).rearrange("p (a h k) -> p a h k", a=5, h=4)
        TBB = rp[:, 1280:2304].bitcast(BF16).rearrange("p (a h k) -> p a h k", a=4, h=4)
        BAND = hb[0][:].rearrange("p (m t) -> p m t", m=8)
        PW = hb[1][:, 0:512].rearrange("p (g e) -> p g e", g=4)
        AOK1 = hb[1][:, 512:768]
        POOLT = hb[1][:, 768:1024].bitcast(F32) if False else None
        psc = sb("pscs", [128, 4])
        qflat = QT1.rearrange("p g t -> p (g t)")
        bandb = qflat[:, 0:2560].rearrange("p (m t) -> p m t", m=20)
        poolt = [qflat[:, 2560 + i * 512:2560 + (i + 1) * 512] for i in range(2)]
        DMA("sp", psc[:], psc_d, (), ["psc"], "c4")
        DMA("pool", bandb, band_d.rearrange("m a b -> a m b"), (), ["bandb"], "c4p")
        DMA("pool", PW, poolw_d.rearrange("g c e -> c g e"), (), ["PW"], "c4p")
        fence(["psc", "bandb", "PW"])

        def load_w1(sec, slot):
            DMA("pool", W1R[slot][:], w_in1_d[:, sec * 512:(sec + 1) * 512].rearrange("(k p) c -> p k c", p=128),
                (), [f"W1R{slot}"], f"w1r{slot}")

        load_w1(3, 0)
        load_w1(0, 1)
        for c in range(NLAT):
            pb = 4 + (c % 2)
            for k in range(8):
                MM(PS[pb][:, :], hT[:, k, c * 128:(c + 1) * 128], W1R[0][:, k, :], k == 0, k == 7, [f"hT{c}", "W1R0"], [f"ps{pb}"])
            CP("act" if c % 2 == 0 else "dve", U1[:, c, :], PS[pb][:, :], [f"ps{pb}"], [f"U1{c}"])
        pn = [0]
        for g in range(4):
            for cb in range(4):
                pb = 6 + (pn[0] % 2)
                for j in range(4):
                    c = cb * 4 + j
                    srcs = []
                    if c > 0:
                        srcs.append((c - 1, g * 5 + 0))
                    srcs.append((c, g * 5 + (3 if c == 0 else 4 if c == NLAT - 1 else 1)))
                    if c < NLAT - 1:
                        srcs.append((c + 1, g * 5 + 2))
                    for i, (cs, m) in enumerate(srcs):
                        MM(PS[pb][:, j * 128:(j + 1) * 128], U1[:, cs, g * 128:(g + 1) * 128], bandb[:, m, :], i == 0, i == len(srcs) - 1,
                           [f"U1{cs}", "bandb"], [f"ps{pb}"])
                pt_ = poolt[pn[0] % 2]
                CP("act", pt_, PS[pb][:, :], [f"ps{pb}"], [f"poolt{pn[0] % 2}"])
                pb2 = 4 + (pn[0] % 2)
                MM(PS[pb2][:, :], PW[:, g, :], pt_, True, True, ["PW", f"poolt{pn[0] % 2}"], [f"ps{pb2}"])
                TS("dve", AOT[:, 4 + g, cb * 512:(cb + 1) * 512], PS[pb2][:, :], psc[:, g:g + 1], None, ALU.mult, None,
                   [f"ps{pb2}", "psc"], [f"AOT{cb * 4 + j}" for j in range(4)])
                pn[0] += 1
        barrier()
        MEMSET("dve", VX1[:, :, :, 64:65], 1.0, [f"VX{c}" for c in range(NCH)])
        for g in range(4):
            for tb_ in range(4):
                pb = 4 + ((g * 4 + tb_) % 2)
                for k in range(8):
                    MM(PS[pb][:, :], W1R[1][:, k, g * 128:(g + 1) * 128], hT[:, k, tb_ * 512:(tb_ + 1) * 512], k == 0, k == 7,
                       ["W1R1"] + [f"hT{tb_ * 4 + j}" for j in range(4)], [f"ps{pb}"])
                ACT(QT1[:, g, tb_ * 512:(tb_ + 1) * 512], PS[pb][:, :], AF.Identity, [f"ps{pb}"], [f"QT{tb_ * 4 + j}" for j in range(4)], scale=0.125)
        load_w1(1, 0)
        load_w1(2, 1)
        blocks_k = [(i * 512, 512) for i in range(4)] + [(2048, 256)]
        for g in range(4):
            for bi, (t0, tn) in enumerate(blocks_k):
                pb = 4 + ((g * 5 + bi) % 2)
                chs = list(range(t0 // 128, (t0 + tn) // 128))
                for k in range(8):
                    MM(PS[pb][:, 0:tn], W1R[0][:, k, g * 128:(g + 1) * 128], hT[:, k, t0:t0 + tn], k == 0, k == 7,
                       ["W1R0"] + [f"hT{c}" for c in chs], [f"ps{pb}"])
                CP("act" if bi % 2 == 0 else "dve", KT1[:, g, t0:t0 + tn], PS[pb][:, 0:tn], [f"ps{pb}"], [f"KT{c}" for c in chs])
        for c in range(NCH):
            pb = 6 + (c % 2)
            for k in range(8):
                MM(PS[pb][:, :], hT[:, k, c * 128:(c + 1) * 128], W1R[1][:, k, :], k == 0, k == 7, [f"hT{c}", "W1R1"], [f"ps{pb}"])
            CP("act" if c % 2 == 0 else "dve", VX1[:, c, :, 0:64], PS[pb][:, :].rearrange("p (h f) -> p h f", h=8), [f"ps{pb}"], [f"VX{c}"])
        ptn = [0]
        for hh in range(2):
            for a in range(5):
                DMA("pool", TBI[:, a, :, :], l1tab_d[INT_PATS[a], hh * 4:(hh + 1) * 4].rearrange("h q k -> q h k"), (), ["TBI"], "tbi")
            for c in range(NLAT):
                loc = L1_LOCAL[c]
                interior = (2 <= c <= 13)
                if not interior:
                    for a, (kc, pat) in enumerate(loc):
                        DMA("pool", TBB[:, a, :, :], l1tab_d[pat, hh * 4:(hh + 1) * 4].rearrange("h q k -> q h k"), (), ["TBB"], "tbb")
                kcs = [(kc, a) for a, (kc, pat) in enumerate(loc)] + [(16, None), (17, None)]
                for kn, (kc, a) in enumerate(kcs):
                    bpair = (4, 5) if ptn[0] % 2 == 0 else (6, 7)
                    pi = ptn[0] % 3
                    ptn[0] += 1
                    for par in range(2):
                        bk = bpair[par]
                        for i2, h4 in enumerate((par, par + 2)):
                            h = hh * 4 + h4
                            g = h // 2
                            pbase = (h % 2) * 64
                            MM(PS[bk][:, i2 * 128:(i2 + 1) * 128], KT1[pbase:pbase + 64, g, kc * 128:(kc + 1) * 128],
                               QT1[pbase:pbase + 64, g, c * 128:(c + 1) * 128], True, a is None, [f"KT{kc}", f"QT{c}"], [f"ps{bk}"])
                            if a is not None:
                                tb_ap = TBI[:, a, h4, :] if interior else TBB[:, a, h4, :]
                                MM(PS[bk][:, i2 * 128:(i2 + 1) * 128], tb_ap, identb[:], False, True,
                                   ["TBI" if interior else "TBB", "identb"], [f"ps{bk}"])
                        ACT(PT1[pi][:, par * 256:(par + 1) * 256], PS[bk][:, 0:256], AF.Exp, [f"ps{bk}"], [f"PT{pi}_{par}"])
                    for h4 in range(4):
                        h = hh * 4 + h4
                        par, i2 = h4 % 2, h4 // 2
                        o_ = par * 256 + i2 * 128
                        MM(PS[h4][:, 0:65], PT1[pi][:, o_:o_ + 128], VX1[:, kc, h, :], kn == 0, kn == len(kcs) - 1,
                           [f"PT{pi}_{par}", f"VX{kc}"], [f"ps{h4}"])
                for h4 in range(4):
                    rz, rk = stat()
                    S.add("dve", lambda e, rz=rz, h4=h4: e.reciprocal(rz, PS[h4][:, 64:65]), reads=[f"ps{h4}"], writes=[rk])
                    TS("dve", AOK1[:, h4 * 64:(h4 + 1) * 64], PS[h4][:, 0:64], rz, None, ALU.mult, None, [f"ps{h4}", rk], ["AOK1"])
                tb = 6 + (c % 2)
                for t in range(2):
                    TR(PSB[tb][:, t * 128:(t + 1) * 128], AOK1[:, t * 128:(t + 1) * 128], identb[:], ["AOK1", "identb"], [f"ps{tb}"])
                CP("act", AOT[:, 2 * hh:2 * hh + 2, c * 128:(c + 1) * 128], PSB[tb][:, 0:256].rearrange("p (s t) -> p s t", s=2),
                   [f"ps{tb}"], [f"AOT{c}"])
        barrier()
        reload_X(NLAT)
        outproj_ln(1, NLAT)

    layer1_mixer()
    if stage == 4:
        for c in range(NLAT):
            DMA("sp", dbg_d[c * 128:(c + 1) * 128, :], X[:, c, :], [f"X{c}"], ["dbgd"], "dbg")
        S.add("sp", None, reads=list(S.lw.keys()) + ["dbgd"], writes=())
        S.emit(nc, es)
        es.close()
        return nc
    moe_layer(1, NLAT, final_out=True)
    S.add("sp", None, reads=list(S.lw.keys()) + ["outd"], writes=())
    S.emit(nc, es)
    es.close()
    return nc


def _rope_table():
    t = np.arange(SEQ)
    inv = (10000.0 ** (-np.arange(16, dtype=np.float32) / 16)).astype(np.float32)
    ar = (t // 64).astype(np.float32)[:, None] * inv
    ac = (t % 64).astype(np.float32)[:, None] * inv
    cr, sr, cc_, sc_ = np.cos(ar), np.sin(ar), np.cos(ac), np.sin(ac)
    cosf = np.concatenate([cr, cr, cc_, cc_], axis=1)
    sins = np.concatenate([-sr, sr, -sc_, sc_], axis=1)
    tab = np.concatenate([cosf, sins], axis=1).astype(np.float32)
    ctxr = np.tile(np.array([1.0] * 64 + [0.0] * 64, np.float32), (CTX, 1))
    return np.concatenate([tab, ctxr], axis=0)


def make_in_maps(inp):
    f = lambda a: np.ascontiguousarray(np.asarray(a, dtype=np.float32))
    shared = {
        "ada_w": f(inp["ada_w"]), "ada_b": f(inp["ada_b"]), "ln_g": f(inp["ln_g"]), "ln_b": f(inp["ln_b"]),
        "l0_w_in": f(inp["l0_w_in"]), "l0_w_out": f(inp["l0_w_out"]),
        "lamv": f(np.stack([inp["l0_lam_q1"], inp["l0_lam_k1"], inp["l0_lam_q2"], inp["l0_lam_k2"]])),
        "l0_subln_g": f(inp["l0_subln_g"]),
        "qkg": f(np.concatenate([np.tile(np.asarray(inp["l0_qnorm_g"]), 4), np.asarray(inp["l0_knorm_g"])])),
        "rope": _rope_table(), "ident": np.eye(128, dtype=np.float32),
        "moe_router": f(inp["moe_router"]), "moe_w_gate": f(inp["moe_w_gate"]), "moe_w_up": f(inp["moe_w_up"]),
        "moe_w_down": f(inp["moe_w_down"]),
        "l1_w_in": f(inp["l1_w_in"]), "l1_w_out": f(inp["l1_w_out"]), "l1_pool_w": f(inp["l1_pool_w"]),
        "psc": f(np.asarray(inp["l1_pool_scale"]).reshape(4, 128).T), "band": _band_mats(),
        "l1tab": _l1_table(inp["l1_rpb"]),
        "iota288": np.tile(np.arange(288, dtype=np.float32), (128, 1)),
        "utri": np.triu(np.ones((128, 128), np.float32), 1),
    }
    maps = []
    x = np.asarray(inp["x"]); ctx = np.asarray(inp["ctx"]); c = np.asarray(inp["c"]); cctx = np.asarray(inp["c_ctx"])
    for b in range(8):
        m = dict(shared)
        m["x"] = f(np.concatenate([x[b], ctx[b]], axis=0))
        cc = np.stack([c[b].reshape(8, 128).T, cctx.reshape(8, 128).T], axis=-1)
        m["cc"] = f(cc)
        maps.append(m)
    return maps


def kernel(**inputs):
    nc = build()
    maps = make_in_maps(inputs)
    res = run_bass_kernel_spmd(nc, maps, core_ids=list(range(8)))
    return np.stack([r["out"] for r in res.results], axis=0)
```

```python
import math
import numpy as np
from contextlib import ExitStack
import concourse.bass as bass
import concourse.mybir as mybir
from concourse.bass_utils import run_bass_kernel_spmd

F32 = mybir.dt.float32
BF16 = mybir.dt.bfloat16
AF = mybir.ActivationFunctionType
ALU = mybir.AluOpType
AX = mybir.AxisListType

D = 1024
SEQ = 2048
CTX = 256
NT = SEQ + CTX
NCH = NT // 128
NLAT = SEQ // 128
NE = 16
FF = 2048
ALPHA = 4.0 ** 0.25
LN_EPS = 1e-5
RMS_EPS = 1e-6
LAM_INIT0 = 0.8 - 0.6 * math.exp(0.0)
NEG = -30000.0


class Sched:
    def __init__(self):
        self.ops = []
        self.lw = {}
        self.rd = {}

    def add(self, eng, fn, reads=(), writes=(), dma=None):
        i = len(self.ops)
        stream = ("dma", dma) if dma is not None else eng
        pk = [k for k in reads if k.startswith("ps") and k[2:].isdigit()]
        if pk:
            writes = list(writes) + [k for k in pk if k not in writes]
        raw = set()
        war = set()
        for k in reads:
            w = self.lw.get(k)
            if w is not None:
                raw.add(w)
        for k in writes:
            w = self.lw.get(k)
            if w is not None:
                raw.add(w)
            for r in self.rd.get(k, {}).values():
                war.add(r)
        if fn is not None:
            for k in writes:
                self.lw[k] = i
                self.rd[k] = {}
            for k in reads:
                self.rd.setdefault(k, {})[stream] = i
        self.ops.append(dict(eng=eng, fn=fn, raw=raw, war=war, dma=dma, stream=stream, cons=False, ms=0))
        return i

    def emit(self, nc, es):
        ops = self.ops
        for o in ops:
            for d in o["raw"] | o["war"]:
                if d == len(ops):
                    continue
                p = ops[d]
                same = (p["stream"] == o["stream"])
                if same and (o["stream"] == "pe"):
                    continue
                if same and d in o["war"] and d not in o["raw"]:
                    continue
                p["cons"] = True
        sems = {}
        cnt = {}

        def get_sem(stream):
            if stream not in sems:
                nm = "s_" + (stream if isinstance(stream, str) else "d_" + str(stream[1]))
                sems[stream] = es.enter_context(nc.semaphore(nm))
                cnt[stream] = 0
            return sems[stream]

        for o in ops:
            st = o["stream"]
            get_sem(st)
            if o["dma"] is not None:
                cnt[st] += 16
                o["ms"] = cnt[st]
            elif o["cons"]:
                cnt[st] += 1
                o["ms"] = cnt[st]
        self.nsem = len(sems)
        block = es.enter_context(nc.Block())
        engs = ["pe", "act", "dve", "pool", "sp"]
        per = {e: [o for o in ops if o["eng"] == e] for e in engs}

        def run(e, eh):
            known = {}
            for o in per[e]:
                need = {}
                for d in o["raw"] | o["war"]:
                    p = ops[d]
                    same = (p["stream"] == o["stream"])
                    if same and o["stream"] == "pe":
                        continue
                    if same and d in o["war"] and d not in o["raw"]:
                        continue
                    if p["ms"] <= 0:
                        continue
                    s = p["stream"]
                    if need.get(s, 0) < p["ms"]:
                        need[s] = p["ms"]
                for s, v in need.items():
                    if known.get(s, 0) < v:
                        eh.wait_ge(sems[s], v)
                        known[s] = v
                if o["fn"] is None:
                    continue
                ins = o["fn"](eh)
                if o["dma"] is not None:
                    ins.then_inc(sems[o["stream"]], 16)
                elif o["cons"]:
                    ins.then_inc(sems[o["stream"]], 1)

        @block.tensor
        def _(eh):
            run("pe", eh)

        @block.scalar
        def _(eh):
            run("act", eh)

        @block.vector
        def _(eh):
            run("dve", eh)

        @block.gpsimd
        def _(eh):
            run("pool", eh)

        @block.sync
        def _(eh):
            run("sp", eh)


def _l1_patterns():
    W, rows, kh, kw = 64, 32, 8, 16
    cols = np.arange(W)
    cs = np.clip(cols - kw // 2, 0, W - kw)
    colok = (cols[None, :] >= cs[:, None]) & (cols[None, :] < cs[:, None] + kw)
    dcol = cols[None, :] - cols[:, None] + 15
    pats, sigs, local = [], {}, []
    for c in range(16):
        lst = []
        for kc in range(16):
            valid = np.zeros((128, 128), bool)
            dr = np.zeros((128, 128), np.int64)
            dc = np.zeros((128, 128), np.int64)
            for ql in range(2):
                r = 2 * c + ql
                rs = min(max(r - kh // 2, 0), rows - kh)
                for kl in range(2):
                    rk = 2 * kc + kl
                    rowok = rs <= rk < rs + kh
                    qs = slice(ql * 64, ql * 64 + 64)
                    ks = slice(kl * 64, kl * 64 + 64)
                    valid[qs, ks] = colok & rowok
                    dr[qs, ks] = rk - r + 7
                    dc[qs, ks] = dcol
            if not valid.any():
                continue
            dr = np.where(valid, dr, 0)
            dc = np.where(valid, dc, 0)
            sig = (valid.tobytes(), dr.tobytes())
            if sig not in sigs:
                sigs[sig] = len(pats)
                pats.append((valid, dr, dc))
            lst.append((kc, sigs[sig]))
        local.append(lst)
    return pats, local


_L1_PATS, L1_LOCAL = _l1_patterns()
NPAT = len(_L1_PATS)
INT_PATS = [p for (_, p) in L1_LOCAL[2]]
for _c in range(2, 14):
    assert [p for (_, p) in L1_LOCAL[_c]] == INT_PATS and len(INT_PATS) == 5
for _c in (0, 1, 14, 15):
    assert len(L1_LOCAL[_c]) <= 4


def _l1_table(rpb):
    rpb = np.asarray(rpb, np.float32)
    tab = np.full((NPAT, 8, 128, 128), NEG, np.float32)
    for i, (valid, dr, dc) in enumerate(_L1_PATS):
        g = rpb[:, dr, dc]
        tab[i] = np.where(valid[None], g, np.float32(NEG))
    return tab


def _band_mats():
    out = np.zeros((20, 128, 128), np.float32)
    tp = np.arange(128)[:, None]
    t = np.arange(128)[None, :]
    for g, w in enumerate((2, 4, 8, 16)):
        half = w // 2
        cnt = float(2 * half)
        out[g * 5 + 0] = (tp >= t + 128 - half) / cnt
        out[g * 5 + 1] = ((tp >= t - half) & (tp < t + half)) / cnt - (tp == t)
        out[g * 5 + 2] = (tp < t + half - 128) / cnt
        lo = np.maximum(t - half, 0)
        hi = t + half
        out[g * 5 + 3] = ((tp >= lo) & (tp < hi)) / (hi - lo).astype(np.float32) - (tp == t)
        lo = t - half
        hi = np.minimum(t + half, 128)
        out[g * 5 + 4] = ((tp >= lo) & (tp < hi)) / (hi - lo).astype(np.float32) - (tp == t)
    return out


GSTOP = 0


def build(stage=99, dbg_cols=1024):
    nc = bass.Bass("TRN2", target_bir_lowering=False)
    S = Sched()
    es = ExitStack()

    def dram(name, shape, dt=F32, kind="ExternalInput"):
        return nc.dram_tensor(name, list(shape), dt, kind=kind).ap()

    x_d = dram("x", [NT, D])
    cc_d = dram("cc", [128, 8, 2])
    ada_w_d = dram("ada_w", [2, D, 6 * D])
    ada_b_d = dram("ada_b", [2, 6 * D])
    ln_g_d = dram("ln_g", [2, 2, D])
    ln_b_d = dram("ln_b", [2, 2, D])
    w_in0_d = dram("l0_w_in", [D, 2304])
    w_out0_d = dram("l0_w_out", [D, D])
    lam_d = dram("lamv", [4, 64])
    subg_d = dram("l0_subln_g", [128])
    qkg_d = dram("qkg", [5 * 64])
    rope_d = dram("rope", [NT, 128])
    ident_d = dram("ident", [128, 128])
    out_d = dram("out", [SEQ, D], kind="ExternalOutput")
    dbg_d = dram("dbg", [128 if stage == 1 else NT, dbg_cols], kind="ExternalOutput") if stage < 99 else None
    modd = dram("modd", [2, 2, 6 * D], kind="Internal")

    def sb(name, shape, dt=F32):
        return es.enter_context(nc.sbuf_tensor(name, list(shape), dt))

    Xraw = sb("X", [128, NCH * D])
    X = Xraw[:].rearrange("p (c f) -> p c f", c=NCH)
    hT = sb("hT", [128, 8, NT], BF16)
    AOT = sb("AOT", [128, 8, NT], BF16)
    WO = sb("WO", [128, 8, D], BF16)
    identb = sb("identb", [128, 128], BF16)
    identf = sb("identf", [128, 128])
    ropet = sb("ropet", [128, NCH, 128])
    big = sb("big", [128, 6 * 1024])
    bc = [big[:, i * 1024:(i + 1) * 1024] for i in range(6)]
    st = sb("st", [128, 64])
    PS = [es.enter_context(nc.psum_tensor(f"ps{i}", [128, 512], F32)) for i in range(8)]
    PSB = [p[:].bitcast(BF16) for p in PS]
    stn = [0]

    def stat():
        i = stn[0] % 64
        stn[0] += 1
        return st[:, i:i + 1], f"st{i}"

    arena_off = [0]

    def xview(nelem_f32, shape, dt=F32, pattern=None, **kw):
        a = arena_off[0]
        arena_off[0] += nelem_f32
        assert arena_off[0] <= NCH * D
        v = Xraw[:, a:a + nelem_f32]
        if dt != F32:
            v = v.bitcast(dt)
        if pattern is not None:
            v = v.rearrange(pattern, **kw)
        return v

    dkn = [0]

    def dk(prefix="d"):
        dkn[0] += 1
        return f"{prefix}{dkn[0]}"

    def DMA(q, out, in_, reads, writes, key):
        return S.add(q, lambda e: e.dma_start(out=out, in_=in_), reads=reads, writes=writes, dma=key)

    def MM(out, lhsT, rhs, start, stop, reads, writes):
        return S.add("pe", lambda e: e.matmul(out, lhsT, rhs, start=start, stop=stop), reads=reads, writes=writes)

    def TR(out, in_, ident, reads, writes):
        return S.add("pe", lambda e: e.transpose(out, in_, ident), reads=reads, writes=writes)

    def ACT(out, in_, func, reads, writes, bias=None, scale=None, accum_out=None):
        kw = {}
        if bias is not None:
            kw["bias"] = bias
        if scale is not None:
            kw["scale"] = scale
        if accum_out is not None:
            kw["accum_out"] = accum_out
        return S.add("act", lambda e: e.activation(out, in_, func, **kw), reads=reads, writes=writes)

    def TT(eng, out, in0, in1, op, reads, writes):
        return S.add(eng, lambda e: e.tensor_tensor(out, in0, in1, op), reads=reads, writes=writes)

    def TS(eng, out, in0, s1, s2, op0, op1, reads, writes):
        if op1 is None:
            return S.add(eng, lambda e: e.tensor_scalar(out, in0, s1, None, op0), reads=reads, writes=writes)
        return S.add(eng, lambda e: e.tensor_scalar(out, in0, s1, s2, op0, op1), reads=reads, writes=writes)

    def STT(eng, out, in0, scalar, in1, op0, op1, reads, writes):
        return S.add(eng, lambda e: e.scalar_tensor_tensor(out, in0, scalar, in1, op0, op1), reads=reads, writes=writes)

    def CP(eng, out, in_, reads, writes):
        if eng == "act":
            return S.add(eng, lambda e: e.copy(out, in_), reads=reads, writes=writes)
        return S.add(eng, lambda e: e.tensor_copy(out, in_), reads=reads, writes=writes)

    def MEMSET(eng, ap, val, writes):
        return S.add(eng, lambda e: e.memset(ap, val), reads=(), writes=writes)

    def fence(keys, engs=("pe", "act", "dve", "pool", "sp")):
        for e in engs:
            S.add(e, None, reads=keys, writes=())

    def barrier():
        allk = list(S.lw.keys())
        for e in ("pe", "act", "dve", "pool", "sp"):
            S.add(e, None, reads=allk, writes=allk)

    nc_lp = es.enter_context(nc.allow_low_precision("bf16 matmul operands, fp32 accumulation"))
    es.enter_context(nc.allow_non_contiguous_dma("small strided constant loads"))

    for c in range(NCH):
        DMA("sp", X[:, c, :], x_d[c * 128:(c + 1) * 128, :], (), [f"X{c}"], "ldx")
    ccs = sb("ccs", [128, 8, 2])
    DMA("sp", ccs[:], cc_d, (), ["ccs"], "const")
    DMA("sp", identf[:], ident_d, (), ["identf"], "const")
    DMA("sp", ropet[:], rope_d.rearrange("(c p) f -> p c f", p=128), (), ["ropet"], "const")
    fence(["ccs", "identf", "ropet"])
    CP("dve", identb[:], identf[:], ["identf"], ["identb"])

    scs = sb("scs", [128, 8, 2])
    S.add("act", lambda e: e.activation(scs[:], ccs[:], AF.Silu), reads=["ccs"], writes=["scs"])
    adab = sb("adab", [2, 512])
    modrow = sb("modrow", [2, 512])
    NST = 3
    aotf = AOT[:].rearrange("p k t -> p (k t)").bitcast(F32)
    hTf = hT[:].rearrange("p k t -> p (k t)").bitcast(F32)
    stg = [aotf[:, 0:4096], aotf[:, 4096:8192], hTf[:, 0:4096]]
    gi = 0
    for l in range(2):
        for j in range(12):
            s = gi % NST
            st3 = stg[s].rearrange("p (k c) -> p k c", k=8)
            DMA("sp", st3, ada_w_d[l, :, j * 512:(j + 1) * 512].rearrange("(k p) c -> p k c", p=128),
                (), [f"stg{s}"], f"adaw{s}")
            for r in range(2):
                DMA("sp", adab[r:r + 1, :], ada_b_d[l:l + 1, j * 512:(j + 1) * 512], (), ["adab"], "adab")
            pb = gi % 2
            for k in range(8):
                MM(PS[pb][0:2, :], scs[:, k, :], st3[:, k, :], k == 0, k == 7,
                   ["scs", f"stg{s}"], [f"ps{pb}"])
            TT("dve", modrow[:], PS[pb][0:2, :], adab[:], ALU.add, [f"ps{pb}", "adab"], ["modrow"])
            DMA("sp", modd[l, :, j * 512:(j + 1) * 512], modrow[:], ["modrow"], ["modd"], "modw")
            gi += 1
    barrier()

    def dbg_out(ap, cols):
        tmpd = sb("tmpd", [128, cols])
        CP("dve", tmpd[:], ap, list(S.lw.keys()), ["tmpd"])
        DMA("sp", dbg_d[:, 0:cols], tmpd[:], ["tmpd"], ["dbgd"], "dbg")

    if stage == 1:
        t1 = sb("t1", [128, 192])
        DMA("sp", t1[:], modd.rearrange("l r (a b) -> (l r a) b", b=192), ["modd"], ["t1"], "t1")
        dbg_out(t1[:], 192)
        S.add("sp", None, reads=list(S.lw.keys()), writes=())
        S.emit(nc, es)
        es.close()
        return nc

    xscr = dram("xscr", [NT, D], kind="Internal")

    def bcast_mod(i, l, r, idx, plus1=False):
        src = modd[l, r:r + 1, idx * 1024:(idx + 1) * 1024].partition_broadcast(128)
        DMA("sp", bc[i].rearrange("p (o f) -> p o f", o=1), src, ["modd"], [f"bc{i}"], f"bcl{i}")
        if plus1:
            TS("dve", bc[i], bc[i], 1.0, None, ALU.add, None, [f"bc{i}"], [f"bc{i}"])

    def bcast_row(i, row_ap):
        DMA("sp", bc[i].rearrange("p (o f) -> p o f", o=1), row_ap.partition_broadcast(128), (), [f"bc{i}"], f"bcl{i}")

    hb = [sb(f"hb{i}", [128, D], BF16) for i in range(2)]

    def modulate_T(l, isc, ish, nch):
        bcast_mod(0, l, 0, isc, True)
        bcast_mod(1, l, 0, ish)
        if nch > NLAT:
            bcast_mod(2, l, 1, isc, True)
            bcast_mod(3, l, 1, ish)
        for c in range(nch):
            a, b_ = (0, 1) if c < NLAT else (2, 3)
            t = 4 + (c % 2)
            TT("dve", bc[t], X[:, c, :], bc[a], ALU.mult, [f"X{c}", f"bc{a}"], [f"bc{t}"])
            TT("dve", hb[c % 2][:], bc[t], bc[b_], ALU.add, [f"bc{t}", f"bc{b_}"], [f"hb{c % 2}"])
            pb = 4 + (c % 2)
            for k in range(8):
                TR(PSB[pb][:, k * 128:(k + 1) * 128], hb[c % 2][:, k * 128:(k + 1) * 128], identb[:],
                   [f"hb{c % 2}", "identb"], [f"ps{pb}"])
            CP("act", hT[:, :, c * 128:(c + 1) * 128], PSB[pb].rearrange("p (k t) -> p k t", k=8),
               [f"ps{pb}"], [f"hT{c}"])

    def spill_X(nch):
        for c in range(nch):
            DMA("sp", xscr[c * 128:(c + 1) * 128, :], X[:, c, :], [f"X{c}"], ["xscr"], "xsp")

    def reload_X(nch):
        for c in range(nch):
            DMA("sp", X[:, c, :], xscr[c * 128:(c + 1) * 128, :], ["xscr"], [f"X{c}"], "xrl")

    def rstd_from(out_ap, okey, in_ap, ikey, scale, eps):
        tmp, tk = stat()
        ACT(tmp, in_ap, AF.Ln, [ikey], [tk], bias=epst[eps], scale=scale)
        ACT(out_ap, tmp, AF.Exp, [tk], [okey], scale=-0.5)

    epsv = sb("epsv", [128, 2])
    MEMSET("dve", epsv[:, 0:1], LN_EPS, ["epsv"])
    MEMSET("dve", epsv[:, 1:2], RMS_EPS, ["epsv"])
    fence(["epsv"], ("act",))
    epst = {LN_EPS: epsv[:, 0:1], RMS_EPS: epsv[:, 1:2]}

    def layer_norm_chunk(c, ig, ib):
        stt = sb(f"lnst{c}", [128, 2, 6]) if False else lnstat
        S.add("dve", lambda e: e.bn_stats(stt[:, 0, :], X[:, c, 0:512]), reads=[f"X{c}"], writes=["lnst0"])
        S.add("dve", lambda e: e.bn_stats(stt[:, 1, :], X[:, c, 512:1024]), reads=[f"X{c}"], writes=["lnst1"])
        mv, mk = lnmv, "lnmv"
        S.add("dve", lambda e: e.bn_aggr(mv[:], stt[:]), reads=["lnst0", "lnst1"], writes=[mk])
        rs, rk = stat()
        rstd_from(rs, rk, mv[:, 1:2], mk, 1.0, LN_EPS)
        nm, nk = stat()
        TS("dve", nm, mv[:, 0:1], rs, -1.0, ALU.mult, ALU.mult, [mk, rk], [nk])
        t = 4 + (c % 2)
        ACT(bc[t], X[:, c, :], AF.Identity, [f"X{c}", rk, nk], [f"bc{t}"], bias=nm, scale=rs)
        TT("dve", bc[t], bc[t], bc[ig], ALU.mult, [f"bc{t}", f"bc{ig}"], [f"bc{t}"])
        TT("dve", X[:, c, :], bc[t], bc[ib], ALU.add, [f"bc{t}", f"bc{ib}"], [f"X{c}"])

    lnstat = sb("lnstat", [128, 2, 6])
    lnmv = sb("lnmv", [128, 2])


    def outproj_ln(l, nch):
        bcast_mod(0, l, 0, 2)
        if nch > NLAT:
            bcast_mod(1, l, 1, 2)
        bcast_row(2, ln_g_d[l, 0:1, :])
        bcast_row(3, ln_b_d[l, 0:1, :])
        for c in range(nch):
            gtile = 0 if c < NLAT else 1
            t = 4 + (c % 2)
            for hf in range(2):
                pb = 2 * (c % 2) + hf
                for m in range(8):
                    MM(PS[pb][:, :], AOT[:, m, c * 128:(c + 1) * 128], WO[:, m, hf * 512:(hf + 1) * 512], m == 0, m == 7,
                       [f"AOT{c}", "WO"], [f"ps{pb}"])
                TT("dve", bc[t][:, hf * 512:(hf + 1) * 512], PS[pb][:, :], bc[gtile][:, hf * 512:(hf + 1) * 512], ALU.mult,
                   [f"ps{pb}", f"bc{gtile}"], [f"bc{t}"])
            STT("dve", X[:, c, :], X[:, c, :], ALPHA, bc[t], ALU.mult, ALU.add, [f"X{c}", f"bc{t}"], [f"X{c}"])
            layer_norm_chunk(c, 2, 3)
        barrier()

    if True:
        l = 0
        DMA("pool", WO[:], w_out0_d.rearrange("(k p) c -> p k c", p=128), (), ["WO"], "wo")
        modulate_T(0, 1, 0, NCH)
        spill_X(NCH)
        barrier()
        if stage == 15:
            dbgb = dram("dbgb", [128, 8 * NT], BF16, kind="ExternalOutput")
            DMA("sp", dbgb, hT[:].rearrange("p k t -> p (k t)"), [f"hT{c}" for c in range(NCH)], ["dbgd"], "dbg")
            S.add("sp", None, reads=list(S.lw.keys()) + ["dbgd"], writes=())
            S.emit(nc, es)
            es.close()
            return nc
        arena_off[0] = 0
        WG = [xview(1536, [128, 8, 384], BF16, "p (k c) -> p k c", k=8) for _ in range(2)]
        QKT = xview(3 * NT // 2, None, BF16, "p (s t) -> p s t", s=3)
        VX = xview(NCH * 130 // 2, None, BF16, "p (c f) -> p c f", c=NCH)
        QKR = [xview(192, None, BF16) for _ in range(2)]
        T1 = [xview(384, None) for _ in range(2)]
        T2 = [xview(384, None) for _ in range(2)]
        SQ = xview(384, None)
        PT = [xview(256, None, BF16) for _ in range(4)]
        O1 = xview(512, None, F32, "p (j f) -> p j f", j=4)
        OO = [xview(128, None) for _ in range(2)]
        AOK = xview(512, None, BF16, "p (j f) -> p j f", j=4)
        SUBG = xview(128, None)
        G5 = xview(320, None)
        LAMT = xview(256, None, F32, "p (a f) -> p a f", a=4)
        lamt = sb("lamt", [128, 4])
        RSS = sb("RSS", [128, 24])
        DMA("sp", SUBG.rearrange("p (o f) -> p o f", o=1), subg_d.rearrange("(o f) -> o f", o=1).partition_broadcast(128),
            (), ["SUBG"], "c2")
        DMA("sp", G5.rearrange("p (o f) -> p o f", o=1), qkg_d.rearrange("(o f) -> o f", o=1).partition_broadcast(128),
            (), ["G5"], "c2")
        for a in range(4):
            DMA("sp", LAMT[:, a:a + 1, :], lam_d[a:a + 1, :].partition_broadcast(128), (), ["LAMT"], "c2")
        fence(["SUBG", "G5", "LAMT"])
        TS("dve", SUBG, SUBG, 1.0 - LAM_INIT0, None, ALU.mult, None, ["SUBG"], ["SUBG"])
        TT("dve", LAMT[:, 0, :], LAMT[:, 0, :], LAMT[:, 1, :], ALU.mult, ["LAMT"], ["LAMT"])
        TT("dve", LAMT[:, 2, :], LAMT[:, 2, :], LAMT[:, 3, :], ALU.mult, ["LAMT"], ["LAMT"])
        S.add("dve", lambda e: e.tensor_reduce(lamt[:, 0:1], LAMT[:, 0, :], AX.X, ALU.add), reads=["LAMT"], writes=["lamt"])
        S.add("dve", lambda e: e.tensor_reduce(lamt[:, 1:2], LAMT[:, 2, :], AX.X, ALU.add), reads=["LAMT"], writes=["lamt"])
        ACT(lamt[:, 0:2], lamt[:, 0:2], AF.Exp, ["lamt"], ["lamt"])
        TT("dve", lamt[:, 2:3], lamt[:, 1:2], lamt[:, 0:1], ALU.subtract, ["lamt"], ["lamt"])
        TS("dve", lamt[:, 3:4], lamt[:, 2:3], -LAM_INIT0, None, ALU.add, None, ["lamt"], ["lamt"])
        nlam = lamt[:, 3:4]

        groups = [("A", h) for h in range(4)] + [("B", n) for n in range(2)]

        def load_wg(gi):
            kind, idx = groups[gi]
            w = WG[gi % 2]
            if kind == "A":
                cols = [(idx * 128, 128), (512 + idx * 128, 128), (1024 + idx * 128, 128)]
            else:
                cols = [(1536 + idx * 256, 256), (2048 + idx * 64, 64), (2176 + idx * 64, 64)]
            o = 0
            for (c0, n) in cols:
                DMA("pool", w[:, :, o:o + n], w_in0_d[:, c0:c0 + n].rearrange("(k p) c -> p k c", p=128),
                    (), [f"WG{gi % 2}"], f"wg{gi % 2}")
                o += n

        load_wg(0)
        ptn = [0]
        for gi, (kind, idx) in enumerate(groups):
            if gi + 1 < len(groups):
                load_wg(gi + 1)
            w = WG[gi % 2]
            nv = 4 if kind == "A" else 5
            nr = nv * 64
            vcol = 256 if kind == "A" else 320
            vw = 128 if kind == "A" else 64
            MEMSET("dve", VX[:, :, vw:vw + 1], 1.0, [f"VX{c}" for c in range(NCH)])
            for c in range(NCH):
                pb = 6 + (c % 2)
                for k in range(8):
                    MM(PS[pb][:, 0:384], hT[:, k, c * 128:(c + 1) * 128], w[:, k, :], k == 0, k == 7,
                       [f"hT{c}", f"WG{gi % 2}"], [f"ps{pb}"])
                CP("act", VX[:, c, 0:vw], PS[pb][:, vcol:vcol + vw], [f"ps{pb}"], [f"VX{c}"])
                src = PS[pb][:, 0:nr]
                skey = f"ps{pb}"
                i2 = c % 2
                if kind == "B":
                    ACT(SQ[:, 0:nr], src, AF.Square, [skey], ["SQ"])
                    S.add("dve", lambda e, c=c, nr=nr: e.tensor_reduce(RSS[:, 0:5], SQ[:, 0:nr].rearrange("p (v f) -> p v f", v=5), AX.X, ALU.add),
                          reads=["SQ"], writes=["RSS"])
                    ACT(RSS[:, 8:13], RSS[:, 0:5], AF.Ln, ["RSS"], ["RSS2"], bias=epst[RMS_EPS], scale=1.0 / 64)
                    ACT(RSS[:, 16:21], RSS[:, 8:13], AF.Exp, ["RSS2"], ["RSS3"], scale=-0.5)
                    TT("dve", SQ[:, 0:nr].rearrange("p (v f) -> p v f", v=5), src.rearrange("p (v f) -> p v f", v=5),
                       RSS[:, 16:21].unsqueeze(2).to_broadcast([128, 5, 64]), ALU.mult, [skey, "RSS3"], ["SQ"])
                    TT("dve", SQ[:, 0:nr], SQ[:, 0:nr], G5, ALU.mult, ["SQ", "G5"], ["SQ"])
                    src = SQ[:, 0:nr]
                    skey = "SQ"
                cosb = ropet[:, c, 0:64].unsqueeze(1).to_broadcast([128, nv, 64])
                TT("dve", T1[i2][:, 0:nr].rearrange("p (v f) -> p v f", v=nv), src.rearrange("p (v f) -> p v f", v=nv),
                   cosb, ALU.mult, [skey, "ropet"], [f"T1{i2}"])
                s5 = src.rearrange("p (v a j f) -> p v a j f", v=nv, a=2, j=2)
                t5 = T2[i2][:, 0:nr].rearrange("p (v a j f) -> p v a j f", v=nv, a=2, j=2)
                sn5 = ropet[:, c, 64:128].rearrange("p (a j f) -> p a j f", a=2, j=2)
                for j in range(2):
                    for a in range(2):
                        sinb = sn5[:, a, j, :].unsqueeze(1).to_broadcast([128, nv, 16])
                        TT("dve", t5[:, :, a, j, :], s5[:, :, a, 1 - j, :], sinb, ALU.mult,
                           [skey, "ropet"], [f"T2{i2}"])
                TT("dve", QKR[i2][:, 0:nr], T1[i2][:, 0:nr], T2[i2][:, 0:nr], ALU.add, [f"T1{i2}", f"T2{i2}"], [f"QKR{i2}"])
                if kind == "B":
                    TT("dve", QKR[i2][:, 320:384], T1[i2][:, 256:320], T2[i2][:, 256:320], ALU.add,
                       [f"T1{i2}", f"T2{i2}"], [f"QKR{i2}"])
                ntr = 2 if kind == "A" else 3
                tb = 4 + (c % 2)
                for t in range(ntr):
                    TR(PSB[tb][:, t * 128:(t + 1) * 128], QKR[i2][:, t * 128:(t + 1) * 128], identb[:],
                       [f"QKR{i2}", "identb"], [f"ps{tb}"])
                CP("act", QKT[:, 0:ntr, c * 128:(c + 1) * 128], PSB[tb][:, 0:ntr * 128].rearrange("p (s t) -> p s t", s=ntr),
                   [f"ps{tb}"], [f"QKT{c}"])
            if stage == 16 and gi == GSTOP:
                dbgb = dram("dbgb", [128, 3 * NT], BF16, kind="ExternalOutput")
                DMA("sp", dbgb, QKT.rearrange("p s t -> p (s t)"), [f"QKT{c}" for c in range(NCH)], ["dbgd"], "dbg")
                dbgv = dram("dbgv", [128, NCH * 130], BF16, kind="ExternalOutput")
                DMA("sp", dbgv, VX.rearrange("p c f -> p (c f)"), [f"VX{c}" for c in range(NCH)], ["dbgd"], "dbg")
                S.add("sp", None, reads=list(S.lw.keys()) + ["dbgd"], writes=())
                S.emit(nc, es)
                es.close()
                return nc
            blocks = [(qb * 4, 4, list(range(NCH))) for qb in range(4)] + [(16, 2, [16, 17])]
            if kind == "A":
                heads = [(0, 0, 1, s * 64) for s in range(2)]
            else:
                heads = [(j4 // 2, 2, (j4 % 2) * 64) for j4 in range(4)]
            for (qc0, nqc, kcs) in blocks:
                ncols = nqc * 128
                q0 = qc0 * 128
                qkeys = [f"QKT{qc0 + j}" for j in range(nqc)]
                for hi, hd in enumerate(heads):
                    if kind == "A":
                        qs, ks, pbase = 0, 1, hi * 64
                    else:
                        qs, ks, pbase = hd
                    def score(kn):
                        kc = kcs[kn]
                        sp_ = 4 + (ptn[0] % 2)
                        pi = ptn[0] % 4
                        ptn[0] += 1
                        MM(PS[sp_][:, 0:ncols], QKT[pbase:pbase + 64, ks, kc * 128:(kc + 1) * 128],
                           QKT[pbase:pbase + 64, qs, q0:q0 + ncols], True, True,
                           [f"QKT{kc}"] + qkeys, [f"ps{sp_}"])
                        return sp_, pi

                    pend = score(0)
                    for kn, kc in enumerate(kcs):
                        nxt = score(kn + 1) if kn + 1 < len(kcs) else None
                        sp_, pi = pend
                        ACT(PT[pi][:, 0:ncols], PS[sp_][:, 0:ncols], AF.Exp, [f"ps{sp_}"], [f"PT{pi}"], scale=0.125)
                        for j in range(nqc):
                            MM(PS[j][:, 0:vw + 1], PT[pi][:, j * 128:(j + 1) * 128], VX[:, kc, 0:vw + 1],
                               kn == 0, kn == len(kcs) - 1, [f"PT{pi}", f"VX{kc}"], [f"ps{j}"])
                        pend = nxt
                    for j in range(nqc):
                        rz, rk = stat()
                        S.add("dve", lambda e, rz=rz, j=j, vw=vw: e.reciprocal(rz, PS[j][:, vw:vw + 1]), reads=[f"ps{j}"], writes=[rk])
                        if kind == "B":
                            TS("dve", AOK[:, j, hi * 64:(hi + 1) * 64], PS[j][:, 0:64], rz, None, ALU.mult, None,
                               [f"ps{j}", rk], [f"AOK{j}"])
                        elif hi == 0:
                            TS("dve", O1[:, j, :], PS[j][:, 0:128], rz, None, ALU.mult, None, [f"ps{j}", rk], [f"O1{j}"])
                        else:
                            r2, r2k = stat()
                            TT("dve", r2, rz, nlam, ALU.mult, [rk, "lamt"], [r2k])
                            oo = OO[j % 2]
                            STT("dve", oo, PS[j][:, 0:128], r2, O1[:, j, :], ALU.mult, ALU.add,
                                [f"ps{j}", r2k, f"O1{j}"], [f"OO{j % 2}"])
                            ssq, ssk = stat()
                            TT("dve", T1[j % 2][:, 0:128], oo, oo, ALU.mult, [f"OO{j % 2}"], [f"T1{j % 2}"])
                            S.add("dve", lambda e, ssq=ssq, j=j: e.tensor_reduce(ssq, T1[j % 2][:, 0:128], AX.X, ALU.add),
                                  reads=[f"T1{j % 2}"], writes=[ssk])
                            rs, rsk = stat()
                            rstd_from(rs, rsk, ssq, ssk, 1.0 / 128, RMS_EPS)
                            STT("dve", AOK[:, j, 0:128], oo, rs, SUBG, ALU.mult, ALU.mult,
                                [f"OO{j % 2}", rsk, "SUBG"], [f"AOK{j}"])
                    last = (kind == "A" and hi == 1) or (kind == "B" and hi == 3)
                    if last:
                        for j in range(nqc):
                            tb = 6 + (j % 2)
                            ntr = 1 if kind == "A" else 2
                            for t in range(ntr):
                                TR(PSB[tb][:, t * 128:(t + 1) * 128], AOK[:, j, t * 128:(t + 1) * 128], identb[:],
                                   [f"AOK{j}", "identb"], [f"ps{tb}"])
                            m0 = idx if kind == "A" else 4 + idx * 2
                            tc = qc0 + j
                            CP("act", AOT[:, m0:m0 + ntr, tc * 128:(tc + 1) * 128],
                               PSB[tb][:, 0:ntr * 128].rearrange("p (s t) -> p s t", s=ntr), [f"ps{tb}"], [f"AOT{tc}"])
        barrier()
        reload_X(NCH)
        outproj_ln(0, NCH)

    if stage == 2:
        for c in range(NCH):
            DMA("sp", dbg_d[c * 128:(c + 1) * 128, :], X[:, c, :], [f"X{c}"], ["dbgd"], "dbg")
        S.add("sp", None, reads=list(S.lw.keys()) + ["dbgd"], writes=())
        S.emit(nc, es)
        es.close()
        return nc
    router_d = dram("moe_router", [2, D, NE])
    NEX = 1 if stage == 31 else NE
    wg_d = dram("moe_w_gate", [2, NEX, D, FF])
    wu_d = dram("moe_w_up", [2, NEX, D, FF])
    wd_d = dram("moe_w_down", [2, NEX, FF, D])
    iota_d = dram("iota288", [128, 288])
    utri_d = dram("utri", [128, 128])
    aotb = AOT[:].rearrange("p k t -> p (k t)")
    wob = WO[:].rearrange("p k c -> p (k c)")
    hmb = hT[:].rearrange("p k t -> p (k t)").rearrange("p (c f) -> p c f", c=NCH)
    NRING = 5
    RING = [aotb[:, i * 2048:(i + 1) * 2048] for i in range(NRING)]
    XGT = aotb[:, 10240:12544].rearrange("p (k s) -> p k s", k=8)
    HIDT = aotb[:, 12544:17152].rearrange("p (j s) -> p j s", j=16)
    SELR = [aotb[:, 17152 + i * 288:17152 + (i + 1) * 288] for i in range(2)]
    SELGT = [aotb[:, 17728 + i * 384:17728 + (i + 1) * 384].rearrange("p (s t) -> p s t", s=3) for i in range(1)]
    SELR += [wob[:, 5280 + i * 288:5280 + (i + 1) * 288] for i in range(3)]
    SELGT += [wob[:, 6144 + i * 384:6144 + (i + 1) * 384].rearrange("p (s t) -> p s t", s=3) for i in range(2)]
    NSEL = len(SELR)
    NSGT = len(SELGT)
    YG = wob[:, 0:3072].rearrange("p (s f) -> p s f", s=3)
    IOTA = wob[:, 3072:3648].bitcast(F32)
    RW = wob[:, 3648:3904].bitcast(F32).rearrange("p (k e) -> p k e", k=8)
    UTRI = wob[:, 3904:4032]
    ONESB = wob[:, 4032:4160]
    MASKB = wob[:, 4160:4448].rearrange("p (c e) -> p c e", c=NCH)
    SGR = [wob[:, 4448:5024].bitcast(F32), wob[:, 6912:7488].bitcast(F32)]
    UTF = wob[:, 5024:5280].bitcast(F32)
    WEX = ropet[:].rearrange("p c f -> p (c f)")
    AFF = hb[0][:].bitcast(F32)[:, 0:288].rearrange("p (c e) -> p c e", c=NCH)
    MASK = hb[1][:].bitcast(F32)[:, 0:288].rearrange("p (c e) -> p c e", c=NCH)
    mo2 = sb("mo2", [128, 2, NCH * NE])
    SLOT = mo2[:, 0, :].rearrange("p (c e) -> p c e", c=NCH)
    GS = mo2[:, 1, :].rearrange("p (c e) -> p c e", c=NCH)
    m8 = sb("m8", [16, 8])

    def moe_layer(l, nch, final_out=False):
        ns = 256 + (32 if nch > NLAT else 0)
        nsc = (ns + 127) // 128
        scsz = [min(128, ns - s * 128) for s in range(nsc)]
        barrier()
        DMA("sp", IOTA, iota_d, (), ["IOTA"], "c3")
        DMA("sp", UTF, utri_d, (), ["UTF"], "c3")
        DMA("sp", RW, router_d[l].rearrange("(k p) e -> p k e", p=128), (), ["RW"], "c3")
        fence(["IOTA", "UTF", "RW"])
        CP("dve", UTRI, UTF, ["UTF"], ["UTRI"])
        MEMSET("dve", ONESB, 1.0, ["ONESB"])
        bcast_mod(0, l, 0, 4, True)
        bcast_mod(1, l, 0, 3)
        if nch > NLAT:
            bcast_mod(2, l, 1, 4, True)
            bcast_mod(3, l, 1, 3)
        for c in range(nch):
            a, b_ = (0, 1) if c < NLAT else (2, 3)
            TT("dve", bc[4], X[:, c, :], bc[a], ALU.mult, [f"X{c}", f"bc{a}"], ["bc4"])
            TT("dve", bc[4], bc[4], bc[b_], ALU.add, ["bc4", f"bc{b_}"], ["bc4"])
            CP("act", hmb[:, c, :], bc[4], ["bc4"], [f"hmb{c}"])
            TS("dve", X[:, c, :], X[:, c, :], ALPHA, None, ALU.mult, None, [f"X{c}"], [f"X{c}"])
            for k in range(8):
                pb = k // 4
                TR(PS[pb][:, (k % 4) * 128:(k % 4 + 1) * 128], bc[4][:, k * 128:(k + 1) * 128], identf[:],
                   ["bc4", "identf"], [f"ps{pb}"])
            hmT = bc[5].rearrange("p (k t) -> p k t", k=8)
            CP("act", hmT[:, 0:4, :], PS[0][:, :].rearrange("p (k t) -> p k t", k=4), ["ps0"], ["bc5"])
            CP("dve", hmT[:, 4:8, :], PS[1][:, :].rearrange("p (k t) -> p k t", k=4), ["ps1"], ["bc5"])
            for k in range(8):
                MM(PS[2][:, 0:NE], hmT[:, k, :], RW[:, k, :], k == 0, k == 7, ["bc5", "RW"], ["ps2"])
            mx, mxk = stat()
            S.add("dve", lambda e, mx=mx: e.tensor_reduce(mx, PS[2][:, 0:NE], AX.X, ALU.max), reads=["ps2"], writes=[mxk])
            nmx, nmk = stat()
            TS("dve", nmx, mx, -1.0, None, ALU.mult, None, [mxk], [nmk])
            ACT(AFF[:, c, :], PS[2][:, 0:NE], AF.Exp, ["ps2", nmk], [f"AFF{c}"], bias=nmx)
            sm, smk = stat()
            S.add("dve", lambda e, sm=sm, c=c: e.tensor_reduce(sm, AFF[:, c, :], AX.X, ALU.add), reads=[f"AFF{c}"], writes=[smk])
            rc, rck = stat()
            S.add("dve", lambda e, rc=rc, sm=sm: e.reciprocal(rc, sm), reads=[smk], writes=[rck])
            TS("dve", AFF[:, c, :], AFF[:, c, :], rc, None, ALU.mult, None, [f"AFF{c}", rck], [f"AFF{c}"])
            TR(PS[3][0:NE, 0:128], AFF[:, c, :], identf[:], [f"AFF{c}", "identf"], ["ps3"])
            CP("dve", WEX[0:NE, c * 128:(c + 1) * 128], PS[3][0:NE, 0:128], ["ps3"], ["WEX"])
        sets = [(0, SEQ, 256)] + ([(SEQ, NT, 32)] if nch > NLAT else [])
        for (t0, t1, cap) in sets:
            wv = WEX[0:NE, t0:t1]
            for r in range(cap // 8):
                S.add("dve", lambda e, wv=wv: e.max(m8[:], wv), reads=["WEX"], writes=["m8"])
                S.add("dve", lambda e, wv=wv: e.match_replace(wv, m8[:], wv, -1.0), reads=["WEX", "m8"], writes=["WEX"])
        TS("dve", WEX[0:NE, 0:nch * 128], WEX[0:NE, 0:nch * 128], 0.0, None, ALU.is_lt, None, ["WEX"], ["WEX"])
        for c in range(nch):
            TR(PS[4][:, c * NE:(c + 1) * NE], WEX[0:NE, c * 128:(c + 1) * 128], identf[0:NE, 0:NE], ["WEX", "identf"], ["ps4"])
        mflat = mo2[:, 0, 0:nch * NE]
        CP("dve", MASK[:, 0:nch, :], PS[4][:, 0:nch * NE].rearrange("p (c e) -> p c e", c=nch), ["ps4"], ["MASK"])
        CP("dve", MASKB[:, 0:nch, :], MASK[:, 0:nch, :], ["MASK"], ["MASKB"])
        TT("dve", GS[:, 0:nch, :], AFF[:, 0:nch, :], MASK[:, 0:nch, :], ALU.mult, [f"AFF{c}" for c in range(nch)] + ["MASK"], ["GS"])
        for c in range(nch):
            c0 = 0 if c < NLAT else NLAT
            prev = list(range(c0, c))
            for i, cp in enumerate(prev):
                MM(PS[5][:, c * NE:(c + 1) * NE], ONESB, MASKB[:, cp, :], i == 0, False, ["ONESB", "MASKB"], ["ps5"])
            MM(PS[5][:, c * NE:(c + 1) * NE], UTRI, MASKB[:, c, :], len(prev) == 0, True, ["UTRI", "MASKB"], ["ps5"])
        TS("dve", SLOT[:, 0:NLAT, :], PS[5][:, 0:NLAT * NE].rearrange("p (c e) -> p c e", c=NLAT), 1.0, None, ALU.add, None, ["ps5"], ["SLOT"])
        if nch > NLAT:
            TS("dve", SLOT[:, NLAT:nch, :], PS[5][:, NLAT * NE:nch * NE].rearrange("p (c e) -> p c e", c=nch - NLAT), 257.0, None,
               ALU.add, None, ["ps5"], ["SLOT"])
        TT("dve", SLOT[:, 0:nch, :], SLOT[:, 0:nch, :], MASK[:, 0:nch, :], ALU.mult, ["SLOT", "MASK"], ["SLOT"])
        TS("dve", SLOT[:, 0:nch, :], SLOT[:, 0:nch, :], -1.0, None, ALU.add, None, ["SLOT"], ["SLOT"])
        bcast_mod(0, l, 0, 5)
        if nch > NLAT:
            bcast_mod(1, l, 1, 5)
        bcast_row(2, ln_g_d[l, 1:2, :])
        bcast_row(3, ln_b_d[l, 1:2, :])
        units = []
        for e_ in range(NEX):
            for fu in range(8):
                units.append(("g", e_, fu))
                units.append(("u", e_, fu))
            for du in range(8):
                units.append(("d", e_, du))
        issued = [0]

        def issue_until(n):
            while issued[0] < min(n, len(units)):
                kind, e_, i = units[issued[0]]
                slot = issued[0] % NRING
                if kind == "d":
                    src = wd_d[l, e_, i * 256:(i + 1) * 256, :].rearrange("(j p) c -> p j c", p=128)
                    dst = RING[slot].rearrange("p (j c) -> p j c", j=2)
                else:
                    wsrc = wg_d if kind == "g" else wu_d
                    src = wsrc[l, e_, :, i * 256:(i + 1) * 256].rearrange("(k p) c -> p k c", p=128)
                    dst = RING[slot].rearrange("p (k c) -> p k c", k=8)
                DMA("pool", dst, src, (), [f"wr{slot}"], f"wr{slot}")
                issued[0] += 1

        ui = [0]

        def next_unit():
            i = ui[0]
            ui[0] += 1
            return i % NRING

        def refill():
            issue_until(ui[0] + NRING)

        issue_until(NRING)
        seln = [0]
        sgn = [0]
        for e_ in range(NEX):
            for half in range(2):
                for c in range(nch):
                    si = seln[0] % NSEL
                    seln[0] += 1
                    TS("dve", SELR[si][:, 0:ns], IOTA[:, 0:ns], SLOT[:, c, e_:e_ + 1], None, ALU.is_equal, None,
                       ["IOTA", "SLOT"], [f"SEL{si}"])
                    for f4 in range(4):
                        f = half * 4 + f4
                        MM(PS[f4][:, 0:ns], hmb[:, c, f * 128:(f + 1) * 128], SELR[si][:, 0:ns], c == 0, c == nch - 1,
                           [f"hmb{c}", f"SEL{si}"], [f"ps{f4}"])
                for f4 in range(4):
                    f = half * 4 + f4
                    CP("act" if f4 % 2 == 0 else "dve", XGT[:, f, 0:ns], PS[f4][:, 0:ns], [f"ps{f4}"], ["XGT"])
            for fu in range(8):
                sg_ = next_unit()
                su_ = next_unit()
                wgv = RING[sg_].rearrange("p (k c) -> p k c", k=8)
                wuv = RING[su_].rearrange("p (k c) -> p k c", k=8)
                for j2 in range(2):
                    jg = fu * 2 + j2
                    bg, bu = (4, 5) if jg % 2 == 0 else (6, 7)
                    for k in range(8):
                        MM(PS[bg][:, 0:ns], wgv[:, k, j2 * 128:(j2 + 1) * 128], XGT[:, k, 0:ns], k == 0, k == 7,
                           [f"wr{sg_}", "XGT"], [f"ps{bg}"])
                    for k in range(8):
                        MM(PS[bu][:, 0:ns], wuv[:, k, j2 * 128:(j2 + 1) * 128], XGT[:, k, 0:ns], k == 0, k == 7,
                           [f"wr{su_}", "XGT"], [f"ps{bu}"])
                    SG = SGR[jg % 2]
                    ACT(SG[:, 0:ns], PS[bg][:, 0:ns], AF.Silu, [f"ps{bg}"], [f"SG{jg % 2}"])
                    TT("dve", HIDT[:, jg, 0:ns], PS[bu][:, 0:ns], SG[:, 0:ns], ALU.mult, [f"ps{bu}", f"SG{jg % 2}"], [f"HID{jg}"])
                refill()
            for du in range(8):
                sd_ = next_unit()
                wdv = RING[sd_].rearrange("p (j c) -> p j c", j=2)
                for sc in range(nsc):
                    sz = scsz[sc]
                    for hf in range(2):
                        pb = sc * 2 + hf
                        for jj in range(2):
                            jg = du * 2 + jj
                            MM(PS[pb][0:sz, :], HIDT[:, jg, sc * 128:sc * 128 + sz], wdv[:, jj, hf * 512:(hf + 1) * 512],
                               du == 0 and jj == 0, du == 7 and jj == 1, [f"HID{jg}", f"wr{sd_}"], [f"ps{pb}"])
                refill()
            for sc in range(nsc):
                sz = scsz[sc]
                gt = 0 if sc < 2 else 1
                for hf in range(2):
                    pb = sc * 2 + hf
                    TT("dve", YG[0:sz, sc, hf * 512:(hf + 1) * 512], PS[pb][0:sz, :], bc[gt][0:sz, hf * 512:(hf + 1) * 512], ALU.mult,
                       [f"ps{pb}", f"bc{gt}"], ["YG"])
            if stage == 31:
                d1 = dram("d_slot", [128, 2 * NCH * NE], kind="ExternalOutput")
                DMA("sp", d1, mo2[:].rearrange("p a f -> p (a f)"), ["SLOT", "GS"], ["dbgd"], "dbg")
                d2 = dram("d_xgt", [128, 8 * 288], BF16, kind="ExternalOutput")
                DMA("sp", d2, XGT.rearrange("p k s -> p (k s)"), ["XGT"], ["dbgd"], "dbg")
                d3 = dram("d_yg", [128, 3 * 1024], BF16, kind="ExternalOutput")
                DMA("sp", d3, YG.rearrange("p s f -> p (s f)"), ["YG"], ["dbgd"], "dbg")
                d4 = dram("d_hid", [128, 16 * 288], BF16, kind="ExternalOutput")
                DMA("sp", d4, HIDT.rearrange("p j s -> p (j s)"), [f"HID{j}" for j in range(16)], ["dbgd"], "dbg")
                S.add("sp", None, reads=list(S.lw.keys()) + ["dbgd"], writes=())
                S.emit(nc, es)
                es.close()
                return "STOP"
            def sc_stage_a(c):
                si = seln[0] % NSEL
                seln[0] += 1
                TS("dve", SELR[si][:, 0:ns], IOTA[:, 0:ns], SLOT[:, c, e_:e_ + 1], GS[:, c, e_:e_ + 1], ALU.is_equal, ALU.mult,
                   ["IOTA", "SLOT", "GS"], [f"SEL{si}"])
                tb = 6 + (c % 2)
                for sc in range(nsc):
                    sz = scsz[sc]
                    TR(PSB[tb][0:sz, sc * 128:(sc + 1) * 128], SELR[si][:, sc * 128:sc * 128 + sz], identb[:],
                       [f"SEL{si}", "identb"], [f"ps{tb}"])
                gi_ = sgn[0] % NSGT
                sgn[0] += 1
                sgt = SELGT[gi_]
                CP("act", sgt[:, 0:2, :], PSB[tb][:, 0:256].rearrange("p (s t) -> p s t", s=2), [f"ps{tb}"], [f"SELGT{gi_}"])
                if nsc > 2:
                    CP("act", sgt[0:scsz[2], 2, :], PSB[tb][0:scsz[2], 256:384], [f"ps{tb}"], [f"SELGT{gi_}"])
                return gi_

            def sc_stage_b(c, gi_):
                sgt = SELGT[gi_]
                for hf in range(2):
                    pb = 2 * (c % 2) + hf
                    for sc in range(nsc):
                        sz = scsz[sc]
                        MM(PS[pb][:, :], sgt[0:sz, sc, :], YG[0:sz, sc, hf * 512:(hf + 1) * 512], sc == 0, sc == nsc - 1,
                           [f"SELGT{gi_}", "YG"], [f"ps{pb}"])
                    TT("dve", X[:, c, hf * 512:(hf + 1) * 512], PS[pb][:, :], X[:, c, hf * 512:(hf + 1) * 512], ALU.add,
                       [f"ps{pb}", f"X{c}"], [f"X{c}"])

            prev = None
            for c in range(nch):
                g_ = sc_stage_a(c)
                if prev is not None:
                    sc_stage_b(*prev)
                prev = (c, g_)
            sc_stage_b(*prev)
        for c in range(nch):
            layer_norm_chunk(c, 2, 3)
            if final_out:
                DMA("sp", out_d[c * 128:(c + 1) * 128, :], X[:, c, :], [f"X{c}"], ["outd"], "outw")
        barrier()

    if moe_layer(0, NCH) == "STOP":
        return nc

    if stage == 3:
        for c in range(NCH):
            DMA("sp", dbg_d[c * 128:(c + 1) * 128, :], X[:, c, :], [f"X{c}"], ["dbgd"], "dbg")
        S.add("sp", None, reads=list(S.lw.keys()) + ["dbgd"], writes=())
        S.emit(nc, es)
        es.close()
        return nc

    w_in1_d = dram("l1_w_in", [D, 2048])
    w_out1_d = dram("l1_w_out", [D, D])
    poolw_d = dram("l1_pool_w", [4, 128, 128])
    psc_d = dram("psc", [128, 4])
    band_d = dram("band", [20, 128, 128])
    l1tab_d = dram("l1tab", [NPAT, 8, 128, 128])

    def layer1_mixer():
        l = 1
        barrier()
        DMA("pool", WO[:], w_out1_d.rearrange("(k p) c -> p k c", p=128), (), ["WO"], "wo")
        modulate_T(1, 1, 0, NCH)
        spill_X(NLAT)
        barrier()
        arena_off[0] = 0
        W1R = [xview(2048, None, BF16, "p (k c) -> p k c", k=8) for _ in range(2)]
        QT1 = xview(4 * SEQ // 2, None, BF16, "p (g t) -> p g t", g=4)
        KT1 = xview(4 * NT // 2, None, BF16, "p (g t) -> p g t", g=4)
        VXU = xview(NCH * 8 * 65 // 2, None, BF16)
        VX1 = VXU.rearrange("p (c h f) -> p c h f", c=NCH, h=8)
        U1 = VXU[:, 0:NLAT * 512].rearrange("p (c f) -> p c f", c=NLAT)
        PT1 = [xview(256, None, BF16) for _ in range(3)]
        rp = ropet[:].rearrange("p c f -> p (c f)")
        TBI = rp[:, 0:1280].bitcast(BF16).rearrange("p (a h k) -> p a h k", a=5, h=4)
        TBB = rp[:, 1280:2304].bitcast(BF16).rearrange("p (a h k) -> p a h k", a=4, h=4)
        BAND = hb[0][:].rearrange("p (m t) -> p m t", m=8)
        PW = hb[1][:, 0:512].rearrange("p (g e) -> p g e", g=4)
        AOK1 = hb[1][:, 512:768]
        POOLT = hb[1][:, 768:1024].bitcast(F32) if False else None
        psc = sb("pscs", [128, 4])
        qflat = QT1.rearrange("p g t -> p (g t)")
        bandb = qflat[:, 0:2560].rearrange("p (m t) -> p m t", m=20)
        poolt = [qflat[:, 2560 + i * 512:2560 + (i + 1) * 512] for i in range(2)]
        DMA("sp", psc[:], psc_d, (), ["psc"], "c4")
        DMA("pool", bandb, band_d.rearrange("m a b -> a m b"), (), ["bandb"], "c4p")
        DMA("pool", PW, poolw_d.rearrange("g c e -> c g e"), (), ["PW"], "c4p")
        fence(["psc", "bandb", "PW"])

        def load_w1(sec, slot):
            DMA("pool", W1R[slot][:], w_in1_d[:, sec * 512:(sec + 1) * 512].rearrange("(k p) c -> p k c", p=128),
                (), [f"W1R{slot}"], f"w1r{slot}")

        load_w1(3, 0)
        load_w1(0, 1)
        for c in range(NLAT):
            pb = 4 + (c % 2)
            for k in range(8):
                MM(PS[pb][:, :], hT[:, k, c * 128:(c + 1) * 128], W1R[0][:, k, :], k == 0, k == 7, [f"hT{c}", "W1R0"], [f"ps{pb}"])
            CP("act" if c % 2 == 0 else "dve", U1[:, c, :], PS[pb][:, :], [f"ps{pb}"], [f"U1{c}"])
        pn = [0]
        for g in range(4):
            for cb in range(4):
                pb = 6 + (pn[0] % 2)
                for j in range(4):
                    c = cb * 4 + j
                    srcs = []
                    if c > 0:
                        srcs.append((c - 1, g * 5 + 0))
                    srcs.append((c, g * 5 + (3 if c == 0 else 4 if c == NLAT - 1 else 1)))
                    if c < NLAT - 1:
                        srcs.append((c + 1, g * 5 + 2))
                    for i, (cs, m) in enumerate(srcs):
                        MM(PS[pb][:, j * 128:(j + 1) * 128], U1[:, cs, g * 128:(g + 1) * 128], bandb[:, m, :], i == 0, i == len(srcs) - 1,
                           [f"U1{cs}", "bandb"], [f"ps{pb}"])
                pt_ = poolt[pn[0] % 2]
                CP("act", pt_, PS[pb][:, :], [f"ps{pb}"], [f"poolt{pn[0] % 2}"])
                pb2 = 4 + (pn[0] % 2)
                MM(PS[pb2][:, :], PW[:, g, :], pt_, True, True, ["PW", f"poolt{pn[0] % 2}"], [f"ps{pb2}"])
                TS("dve", AOT[:, 4 + g, cb * 512:(cb + 1) * 512], PS[pb2][:, :], psc[:, g:g + 1], None, ALU.mult, None,
                   [f"ps{pb2}", "psc"], [f"AOT{cb * 4 + j}" for j in range(4)])
                pn[0] += 1
        barrier()
        MEMSET("dve", VX1[:, :, :, 64:65], 1.0, [f"VX{c}" for c in range(NCH)])
        for g in range(4):
            for tb_ in range(4):
                pb = 4 + ((g * 4 + tb_) % 2)
                for k in range(8):
                    MM(PS[pb][:, :], W1R[1][:, k, g * 128:(g + 1) * 128], hT[:, k, tb_ * 512:(tb_ + 1) * 512], k == 0, k == 7,
                       ["W1R1"] + [f"hT{tb_ * 4 + j}" for j in range(4)], [f"ps{pb}"])
                ACT(QT1[:, g, tb_ * 512:(tb_ + 1) * 512], PS[pb][:, :], AF.Identity, [f"ps{pb}"], [f"QT{tb_ * 4 + j}" for j in range(4)], scale=0.125)
        load_w1(1, 0)
        load_w1(2, 1)
        blocks_k = [(i * 512, 512) for i in range(4)] + [(2048, 256)]
        for g in range(4):
            for bi, (t0, tn) in enumerate(blocks_k):
                pb = 4 + ((g * 5 + bi) % 2)
                chs = list(range(t0 // 128, (t0 + tn) // 128))
                for k in range(8):
                    MM(PS[pb][:, 0:tn], W1R[0][:, k, g * 128:(g + 1) * 128], hT[:, k, t0:t0 + tn], k == 0, k == 7,
                       ["W1R0"] + [f"hT{c}" for c in chs], [f"ps{pb}"])
                CP("act" if bi % 2 == 0 else "dve", KT1[:, g, t0:t0 + tn], PS[pb][:, 0:tn], [f"ps{pb}"], [f"KT{c}" for c in chs])
        for c in range(NCH):
            pb = 6 + (c % 2)
            for k in range(8):
                MM(PS[pb][:, :], hT[:, k, c * 128:(c + 1) * 128], W1R[1][:, k, :], k == 0, k == 7, [f"hT{c}", "W1R1"], [f"ps{pb}"])
            CP("act" if c % 2 == 0 else "dve", VX1[:, c, :, 0:64], PS[pb][:, :].rearrange("p (h f) -> p h f", h=8), [f"ps{pb}"], [f"VX{c}"])
        ptn = [0]
        for hh in range(2):
            for a in range(5):
                DMA("pool", TBI[:, a, :, :], l1tab_d[INT_PATS[a], hh * 4:(hh + 1) * 4].rearrange("h q k -> q h k"), (), ["TBI"], "tbi")
            for c in range(NLAT):
                loc = L1_LOCAL[c]
                interior = (2 <= c <= 13)
                if not interior:
                    for a, (kc, pat) in enumerate(loc):
                        DMA("pool", TBB[:, a, :, :], l1tab_d[pat, hh * 4:(hh + 1) * 4].rearrange("h q k -> q h k"), (), ["TBB"], "tbb")
                kcs = [(kc, a) for a, (kc, pat) in enumerate(loc)] + [(16, None), (17, None)]
                def score1(kn):
                    kc, a = kcs[kn]
                    bpair = (4, 5) if ptn[0] % 2 == 0 else (6, 7)
                    pi = ptn[0] % 3
                    ptn[0] += 1
                    for par in range(2):
                        bk = bpair[par]
                        for i2, h4 in enumerate((par, par + 2)):
                            h = hh * 4 + h4
                            g = h // 2
                            pbase = (h % 2) * 64
                            MM(PS[bk][:, i2 * 128:(i2 + 1) * 128], KT1[pbase:pbase + 64, g, kc * 128:(kc + 1) * 128],
                               QT1[pbase:pbase + 64, g, c * 128:(c + 1) * 128], True, a is None, [f"KT{kc}", f"QT{c}"], [f"ps{bk}"])
                            if a is not None:
                                tb_ap = TBI[:, a, h4, :] if interior else TBB[:, a, h4, :]
                                MM(PS[bk][:, i2 * 128:(i2 + 1) * 128], tb_ap, identb[:], False, True,
                                   ["TBI" if interior else "TBB", "identb"], [f"ps{bk}"])
                    return bpair, pi

                pend = score1(0)
                for kn, (kc, a) in enumerate(kcs):
                    nxt = score1(kn + 1) if kn + 1 < len(kcs) else None
                    bpair, pi = pend
                    for par in range(2):
                        bk = bpair[par]
                        ACT(PT1[pi][:, par * 256:(par + 1) * 256], PS[bk][:, 0:256], AF.Exp, [f"ps{bk}"], [f"PT{pi}_{par}"])
                    for h4 in range(4):
                        h = hh * 4 + h4
                        par, i2 = h4 % 2, h4 // 2
                        o_ = par * 256 + i2 * 128
                        MM(PS[h4][:, 0:65], PT1[pi][:, o_:o_ + 128], VX1[:, kc, h, :], kn == 0, kn == len(kcs) - 1,
                           [f"PT{pi}_{par}", f"VX{kc}"], [f"ps{h4}"])
                    pend = nxt
                for h4 in range(4):
                    rz, rk = stat()
                    S.add("dve", lambda e, rz=rz, h4=h4: e.reciprocal(rz, PS[h4][:, 64:65]), reads=[f"ps{h4}"], writes=[rk])
                    TS("dve", AOK1[:, h4 * 64:(h4 + 1) * 64], PS[h4][:, 0:64], rz, None, ALU.mult, None, [f"ps{h4}", rk], ["AOK1"])
                tb = 6 + (c % 2)
                for t in range(2):
                    TR(PSB[tb][:, t * 128:(t + 1) * 128], AOK1[:, t * 128:(t + 1) * 128], identb[:], ["AOK1", "identb"], [f"ps{tb}"])
                CP("act", AOT[:, 2 * hh:2 * hh + 2, c * 128:(c + 1) * 128], PSB[tb][:, 0:256].rearrange("p (s t) -> p s t", s=2),
                   [f"ps{tb}"], [f"AOT{c}"])
        barrier()
        reload_X(NLAT)
        outproj_ln(1, NLAT)

    layer1_mixer()
    if stage == 4:
        for c in range(NLAT):
            DMA("sp", dbg_d[c * 128:(c + 1) * 128, :], X[:, c, :], [f"X{c}"], ["dbgd"], "dbg")
        S.add("sp", None, reads=list(S.lw.keys()) + ["dbgd"], writes=())
        S.emit(nc, es)
        es.close()
        return nc
    moe_layer(1, NLAT, final_out=True)
    S.add("sp", None, reads=list(S.lw.keys()) + ["outd"], writes=())
    S.emit(nc, es)
    es.close()
    return nc


def _rope_table():
    t = np.arange(SEQ)
    inv = (10000.0 ** (-np.arange(16, dtype=np.float32) / 16)).astype(np.float32)
    ar = (t // 64).astype(np.float32)[:, None] * inv
    ac = (t % 64).astype(np.float32)[:, None] * inv
    cr, sr, cc_, sc_ = np.cos(ar), np.sin(ar), np.cos(ac), np.sin(ac)
    cosf = np.concatenate([cr, cr, cc_, cc_], axis=1)
    sins = np.concatenate([-sr, sr, -sc_, sc_], axis=1)
    tab = np.concatenate([cosf, sins], axis=1).astype(np.float32)
    ctxr = np.tile(np.array([1.0] * 64 + [0.0] * 64, np.float32), (CTX, 1))
    return np.concatenate([tab, ctxr], axis=0)


def make_in_maps(inp):
    f = lambda a: np.ascontiguousarray(np.asarray(a, dtype=np.float32))
    shared = {
        "ada_w": f(inp["ada_w"]), "ada_b": f(inp["ada_b"]), "ln_g": f(inp["ln_g"]), "ln_b": f(inp["ln_b"]),
        "l0_w_in": f(inp["l0_w_in"]), "l0_w_out": f(inp["l0_w_out"]),
        "lamv": f(np.stack([inp["l0_lam_q1"], inp["l0_lam_k1"], inp["l0_lam_q2"], inp["l0_lam_k2"]])),
        "l0_subln_g": f(inp["l0_subln_g"]),
        "qkg": f(np.concatenate([np.tile(np.asarray(inp["l0_qnorm_g"]), 4), np.asarray(inp["l0_knorm_g"])])),
        "rope": _rope_table(), "ident": np.eye(128, dtype=np.float32),
        "moe_router": f(inp["moe_router"]), "moe_w_gate": f(inp["moe_w_gate"]), "moe_w_up": f(inp["moe_w_up"]),
        "moe_w_down": f(inp["moe_w_down"]),
        "l1_w_in": f(inp["l1_w_in"]), "l1_w_out": f(inp["l1_w_out"]), "l1_pool_w": f(inp["l1_pool_w"]),
        "psc": f(np.asarray(inp["l1_pool_scale"]).reshape(4, 128).T), "band": _band_mats(),
        "l1tab": _l1_table(inp["l1_rpb"]),
        "iota288": np.tile(np.arange(288, dtype=np.float32), (128, 1)),
        "utri": np.triu(np.ones((128, 128), np.float32), 1),
    }
    maps = []
    x = np.asarray(inp["x"]); ctx = np.asarray(inp["ctx"]); c = np.asarray(inp["c"]); cctx = np.asarray(inp["c_ctx"])
    for b in range(8):
        m = dict(shared)
        m["x"] = f(np.concatenate([x[b], ctx[b]], axis=0))
        cc = np.stack([c[b].reshape(8, 128).T, cctx.reshape(8, 128).T], axis=-1)
        m["cc"] = f(cc)
        maps.append(m)
    return maps


def kernel(**inputs):
    nc = build()
    maps = make_in_maps(inputs)
    res = run_bass_kernel_spmd(nc, maps, core_ids=list(range(8)))
    return np.stack([r["out"] for r in res.results], axis=0)
```

```python
import math
import numpy as np
from contextlib import ExitStack
import concourse.bass as bass
import concourse.mybir as mybir
from concourse.bass_utils import run_bass_kernel_spmd

F32 = mybir.dt.float32
BF16 = mybir.dt.bfloat16
AF = mybir.ActivationFunctionType
ALU = mybir.AluOpType
AX = mybir.AxisListType

D = 1024
SEQ = 2048
CTX = 256
NT = SEQ + CTX
NCH = NT // 128
NLAT = SEQ // 128
NE = 16
FF = 2048
ALPHA = 4.0 ** 0.25
LN_EPS = 1e-5
RMS_EPS = 1e-6
LAM_INIT0 = 0.8 - 0.6 * math.exp(0.0)
NEG = -30000.0


class Sched:
    def __init__(self):
        self.ops = []
        self.lw = {}
        self.rd = {}

    def add(self, eng, fn, reads=(), writes=(), dma=None):
        i = len(self.ops)
        stream = ("dma", dma) if dma is not None else eng
        pk = [k for k in reads if k.startswith("ps") and k[2:].isdigit()]
        if pk:
            writes = list(writes) + [k for k in pk if k not in writes]
        raw = set()
        war = set()
        for k in reads:
            w = self.lw.get(k)
            if w is not None:
                raw.add(w)
        for k in writes:
            w = self.lw.get(k)
            if w is not None:
                raw.add(w)
            for r in self.rd.get(k, {}).values():
                war.add(r)
        if fn is not None:
            for k in writes:
                self.lw[k] = i
                self.rd[k] = {}
            for k in reads:
                self.rd.setdefault(k, {})[stream] = i
        self.ops.append(dict(eng=eng, fn=fn, raw=raw, war=war, dma=dma, stream=stream, cons=False, ms=0))
        return i

    def emit(self, nc, es):
        ops = self.ops
        for o in ops:
            for d in o["raw"] | o["war"]:
                if d == len(ops):
                    continue
                p = ops[d]
                same = (p["stream"] == o["stream"])
                if same and (o["stream"] == "pe"):
                    continue
                if same and d in o["war"] and d not in o["raw"]:
                    continue
                p["cons"] = True
        sems = {}
        cnt = {}

        def get_sem(stream):
            if stream not in sems:
                nm = "s_" + (stream if isinstance(stream, str) else "d_" + str(stream[1]))
                sems[stream] = es.enter_context(nc.semaphore(nm))
                cnt[stream] = 0
            return sems[stream]

        for o in ops:
            st = o["stream"]
            get_sem(st)
            if o["dma"] is not None:
                cnt[st] += 16
                o["ms"] = cnt[st]
            elif o["cons"]:
                cnt[st] += 1
                o["ms"] = cnt[st]
        self.nsem = len(sems)
        block = es.enter_context(nc.Block())
        engs = ["pe", "act", "dve", "pool", "sp"]
        per = {e: [o for o in ops if o["eng"] == e] for e in engs}

        def run(e, eh):
            known = {}
            for o in per[e]:
                need = {}
                for d in o["raw"] | o["war"]:
                    p = ops[d]
                    same = (p["stream"] == o["stream"])
                    if same and o["stream"] == "pe":
                        continue
                    if same and d in o["war"] and d not in o["raw"]:
                        continue
                    if p["ms"] <= 0:
                        continue
                    s = p["stream"]
                    if need.get(s, 0) < p["ms"]:
                        need[s] = p["ms"]
                for s, v in need.items():
                    if known.get(s, 0) < v:
                        eh.wait_ge(sems[s], v)
                        known[s] = v
                if o["fn"] is None:
                    continue
                ins = o["fn"](eh)
                if o["dma"] is not None:
                    ins.then_inc(sems[o["stream"]], 16)
                elif o["cons"]:
                    ins.then_inc(sems[o["stream"]], 1)

        @block.tensor
        def _(eh):
            run("pe", eh)

        @block.scalar
        def _(eh):
            run("act", eh)

        @block.vector
        def _(eh):
            run("dve", eh)

        @block.gpsimd
        def _(eh):
            run("pool", eh)

        @block.sync
        def _(eh):
            run("sp", eh)


def _l1_patterns():
    W, rows, kh, kw = 64, 32, 8, 16
    cols = np.arange(W)
    cs = np.clip(cols - kw // 2, 0, W - kw)
    colok = (cols[None, :] >= cs[:, None]) & (cols[None, :] < cs[:, None] + kw)
    dcol = cols[None, :] - cols[:, None] + 15
    pats, sigs, local = [], {}, []
    for c in range(16):
        lst = []
        for kc in range(16):
            valid = np.zeros((128, 128), bool)
            dr = np.zeros((128, 128), np.int64)
            dc = np.zeros((128, 128), np.int64)
            for ql in range(2):
                r = 2 * c + ql
                rs = min(max(r - kh // 2, 0), rows - kh)
                for kl in range(2):
                    rk = 2 * kc + kl
                    rowok = rs <= rk < rs + kh
                    qs = slice(ql * 64, ql * 64 + 64)
                    ks = slice(kl * 64, kl * 64 + 64)
                    valid[qs, ks] = colok & rowok
                    dr[qs, ks] = rk - r + 7
                    dc[qs, ks] = dcol
            if not valid.any():
                continue
            dr = np.where(valid, dr, 0)
            dc = np.where(valid, dc, 0)
            sig = (valid.tobytes(), dr.tobytes())
            if sig not in sigs:
                sigs[sig] = len(pats)
                pats.append((valid, dr, dc))
            lst.append((kc, sigs[sig]))
        local.append(lst)
    return pats, local


_L1_PATS, L1_LOCAL = _l1_patterns()
NPAT = len(_L1_PATS)
INT_PATS = [p for (_, p) in L1_LOCAL[2]]
for _c in range(2, 14):
    assert [p for (_, p) in L1_LOCAL[_c]] == INT_PATS and len(INT_PATS) == 5
for _c in (0, 1, 14, 15):
    assert len(L1_LOCAL[_c]) <= 4


def _l1_table(rpb):
    rpb = np.asarray(rpb, np.float32)
    tab = np.full((NPAT, 8, 128, 128), NEG, np.float32)
    for i, (valid, dr, dc) in enumerate(_L1_PATS):
        g = rpb[:, dr, dc]
        tab[i] = np.where(valid[None], g, np.float32(NEG))
    return tab


def _band_mats():
    out = np.zeros((20, 128, 128), np.float32)
    tp = np.arange(128)[:, None]
    t = np.arange(128)[None, :]
    for g, w in enumerate((2, 4, 8, 16)):
        half = w // 2
        cnt = float(2 * half)
        out[g * 5 + 0] = (tp >= t + 128 - half) / cnt
        out[g * 5 + 1] = ((tp >= t - half) & (tp < t + half)) / cnt - (tp == t)
        out[g * 5 + 2] = (tp < t + half - 128) / cnt
        lo = np.maximum(t - half, 0)
        hi = t + half
        out[g * 5 + 3] = ((tp >= lo) & (tp < hi)) / (hi - lo).astype(np.float32) - (tp == t)
        lo = t - half
        hi = np.minimum(t + half, 128)
        out[g * 5 + 4] = ((tp >= lo) & (tp < hi)) / (hi - lo).astype(np.float32) - (tp == t)
    return out


GSTOP = 0


def build(stage=99, dbg_cols=1024):
    nc = bass.Bass("TRN2", target_bir_lowering=False)
    S = Sched()
    es = ExitStack()

    def dram(name, shape, dt=F32, kind="ExternalInput"):
        return nc.dram_tensor(name, list(shape), dt, kind=kind).ap()

    x_d = dram("x", [NT, D])
    cc_d = dram("cc", [128, 8, 2])
    ada_w_d = dram("ada_w", [2, D, 6 * D])
    ada_b_d = dram("ada_b", [2, 6 * D])
    ln_g_d = dram("ln_g", [2, 2, D])
    ln_b_d = dram("ln_b", [2, 2, D])
    w_in0_d = dram("l0_w_in", [D, 2304])
    w_out0_d = dram("l0_w_out", [D, D])
    lam_d = dram("lamv", [4, 64])
    subg_d = dram("l0_subln_g", [128])
    qkg_d = dram("qkg", [5 * 64])
    rope_d = dram("rope", [NT, 128])
    ident_d = dram("ident", [128, 128])
    out_d = dram("out", [SEQ, D], kind="ExternalOutput")
    dbg_d = dram("dbg", [128 if stage == 1 else NT, dbg_cols], kind="ExternalOutput") if stage < 99 else None
    modd = dram("modd", [2, 2, 6 * D], kind="Internal")

    def sb(name, shape, dt=F32):
        return es.enter_context(nc.sbuf_tensor(name, list(shape), dt))

    Xraw = sb("X", [128, NCH * D])
    X = Xraw[:].rearrange("p (c f) -> p c f", c=NCH)
    hT = sb("hT", [128, 8, NT], BF16)
    AOT = sb("AOT", [128, 8, NT], BF16)
    WO = sb("WO", [128, 8, D], BF16)
    identb = sb("identb", [128, 128], BF16)
    identf = sb("identf", [128, 128])
    ropet = sb("ropet", [128, NCH, 128])
    big = sb("big", [128, 6 * 1024])
    bc = [big[:, i * 1024:(i + 1) * 1024] for i in range(6)]
    st = sb("st", [128, 64])
    PS = [es.enter_context(nc.psum_tensor(f"ps{i}", [128, 512], F32)) for i in range(8)]
    PSB = [p[:].bitcast(BF16) for p in PS]
    stn = [0]

    def stat():
        i = stn[0] % 64
        stn[0] += 1
        return st[:, i:i + 1], f"st{i}"

    arena_off = [0]

    def xview(nelem_f32, shape, dt=F32, pattern=None, **kw):
        a = arena_off[0]
        arena_off[0] += nelem_f32
        assert arena_off[0] <= NCH * D
        v = Xraw[:, a:a + nelem_f32]
        if dt != F32:
            v = v.bitcast(dt)
        if pattern is not None:
            v = v.rearrange(pattern, **kw)
        return v

    dkn = [0]

    def dk(prefix="d"):
        dkn[0] += 1
        return f"{prefix}{dkn[0]}"

    def DMA(q, out, in_, reads, writes, key):
        return S.add(q, lambda e: e.dma_start(out=out, in_=in_), reads=reads, writes=writes, dma=key)

    def MM(out, lhsT, rhs, start, stop, reads, writes):
        return S.add("pe", lambda e: e.matmul(out, lhsT, rhs, start=start, stop=stop), reads=reads, writes=writes)

    def TR(out, in_, ident, reads, writes):
        return S.add("pe", lambda e: e.transpose(out, in_, ident), reads=reads, writes=writes)

    def ACT(out, in_, func, reads, writes, bias=None, scale=None, accum_out=None):
        kw = {}
        if bias is not None:
            kw["bias"] = bias
        if scale is not None:
            kw["scale"] = scale
        if accum_out is not None:
            kw["accum_out"] = accum_out
        return S.add("act", lambda e: e.activation(out, in_, func, **kw), reads=reads, writes=writes)

    def TT(eng, out, in0, in1, op, reads, writes):
        return S.add(eng, lambda e: e.tensor_tensor(out, in0, in1, op), reads=reads, writes=writes)

    def TS(eng, out, in0, s1, s2, op0, op1, reads, writes):
        if op1 is None:
            return S.add(eng, lambda e: e.tensor_scalar(out, in0, s1, None, op0), reads=reads, writes=writes)
        return S.add(eng, lambda e: e.tensor_scalar(out, in0, s1, s2, op0, op1), reads=reads, writes=writes)

    def STT(eng, out, in0, scalar, in1, op0, op1, reads, writes):
        return S.add(eng, lambda e: e.scalar_tensor_tensor(out, in0, scalar, in1, op0, op1), reads=reads, writes=writes)

    def CP(eng, out, in_, reads, writes):
        if eng == "act":
            return S.add(eng, lambda e: e.copy(out, in_), reads=reads, writes=writes)
        return S.add(eng, lambda e: e.tensor_copy(out, in_), reads=reads, writes=writes)

    def MEMSET(eng, ap, val, writes):
        return S.add(eng, lambda e: e.memset(ap, val), reads=(), writes=writes)

    def fence(keys, engs=("pe", "act", "dve", "pool", "sp")):
        for e in engs:
            S.add(e, None, reads=keys, writes=())

    def barrier():
        allk = list(S.lw.keys())
        for e in ("pe", "act", "dve", "pool", "sp"):
            S.add(e, None, reads=allk, writes=allk)

    nc_lp = es.enter_context(nc.allow_low_precision("bf16 matmul operands, fp32 accumulation"))
    es.enter_context(nc.allow_non_contiguous_dma("small strided constant loads"))

    for c in range(NCH):
        DMA("sp", X[:, c, :], x_d[c * 128:(c + 1) * 128, :], (), [f"X{c}"], "ldx")
    ccs = sb("ccs", [128, 8, 2])
    DMA("sp", ccs[:], cc_d, (), ["ccs"], "const")
    DMA("sp", identf[:], ident_d, (), ["identf"], "const")
    DMA("sp", ropet[:], rope_d.rearrange("(c p) f -> p c f", p=128), (), ["ropet"], "const")
    fence(["ccs", "identf", "ropet"])
    CP("dve", identb[:], identf[:], ["identf"], ["identb"])

    scs = sb("scs", [128, 8, 2])
    S.add("act", lambda e: e.activation(scs[:], ccs[:], AF.Silu), reads=["ccs"], writes=["scs"])
    adab = sb("adab", [2, 512])
    modrow = sb("modrow", [2, 512])
    NST = 3
    aotf = AOT[:].rearrange("p k t -> p (k t)").bitcast(F32)
    hTf = hT[:].rearrange("p k t -> p (k t)").bitcast(F32)
    stg = [aotf[:, 0:4096], aotf[:, 4096:8192], hTf[:, 0:4096]]
    gi = 0
    for l in range(2):
        for j in range(12):
            s = gi % NST
            st3 = stg[s].rearrange("p (k c) -> p k c", k=8)
            DMA("sp", st3, ada_w_d[l, :, j * 512:(j + 1) * 512].rearrange("(k p) c -> p k c", p=128),
                (), [f"stg{s}"], f"adaw{s}")
            for r in range(2):
                DMA("sp", adab[r:r + 1, :], ada_b_d[l:l + 1, j * 512:(j + 1) * 512], (), ["adab"], "adab")
            pb = gi % 2
            for k in range(8):
                MM(PS[pb][0:2, :], scs[:, k, :], st3[:, k, :], k == 0, k == 7,
                   ["scs", f"stg{s}"], [f"ps{pb}"])
            TT("dve", modrow[:], PS[pb][0:2, :], adab[:], ALU.add, [f"ps{pb}", "adab"], ["modrow"])
            DMA("sp", modd[l, :, j * 512:(j + 1) * 512], modrow[:], ["modrow"], ["modd"], "modw")
            gi += 1
    barrier()

    def dbg_out(ap, cols):
        tmpd = sb("tmpd", [128, cols])
        CP("dve", tmpd[:], ap, list(S.lw.keys()), ["tmpd"])
        DMA("sp", dbg_d[:, 0:cols], tmpd[:], ["tmpd"], ["dbgd"], "dbg")

    if stage == 1:
        t1 = sb("t1", [128, 192])
        DMA("sp", t1[:], modd.rearrange("l r (a b) -> (l r a) b", b=192), ["modd"], ["t1"], "t1")
        dbg_out(t1[:], 192)
        S.add("sp", None, reads=list(S.lw.keys()), writes=())
        S.emit(nc, es)
        es.close()
        return nc

    xscr = dram("xscr", [NT, D], kind="Internal")

    def bcast_mod(i, l, r, idx, plus1=False):
        src = modd[l, r:r + 1, idx * 1024:(idx + 1) * 1024].partition_broadcast(128)
        DMA("sp", bc[i].rearrange("p (o f) -> p o f", o=1), src, ["modd"], [f"bc{i}"], f"bcl{i}")
        if plus1:
            TS("dve", bc[i], bc[i], 1.0, None, ALU.add, None, [f"bc{i}"], [f"bc{i}"])

    def bcast_row(i, row_ap):
        DMA("sp", bc[i].rearrange("p (o f) -> p o f", o=1), row_ap.partition_broadcast(128), (), [f"bc{i}"], f"bcl{i}")

    hb = [sb(f"hb{i}", [128, D], BF16) for i in range(2)]

    def modulate_T(l, isc, ish, nch):
        bcast_mod(0, l, 0, isc, True)
        bcast_mod(1, l, 0, ish)
        if nch > NLAT:
            bcast_mod(2, l, 1, isc, True)
            bcast_mod(3, l, 1, ish)
        for c in range(nch):
            a, b_ = (0, 1) if c < NLAT else (2, 3)
            t = 4 + (c % 2)
            TT("dve", bc[t], X[:, c, :], bc[a], ALU.mult, [f"X{c}", f"bc{a}"], [f"bc{t}"])
            TT("dve", hb[c % 2][:], bc[t], bc[b_], ALU.add, [f"bc{t}", f"bc{b_}"], [f"hb{c % 2}"])
            pb = 4 + (c % 2)
            for k in range(8):
                TR(PSB[pb][:, k * 128:(k + 1) * 128], hb[c % 2][:, k * 128:(k + 1) * 128], identb[:],
                   [f"hb{c % 2}", "identb"], [f"ps{pb}"])
            CP("act", hT[:, :, c * 128:(c + 1) * 128], PSB[pb].rearrange("p (k t) -> p k t", k=8),
               [f"ps{pb}"], [f"hT{c}"])

    def spill_X(nch):
        for c in range(nch):
            DMA("sp", xscr[c * 128:(c + 1) * 128, :], X[:, c, :], [f"X{c}"], ["xscr"], "xsp")

    def reload_X(nch):
        for c in range(nch):
            DMA("sp", X[:, c, :], xscr[c * 128:(c + 1) * 128, :], ["xscr"], [f"X{c}"], "xrl")

    def rstd_from(out_ap, okey, in_ap, ikey, scale, eps):
        tmp, tk = stat()
        ACT(tmp, in_ap, AF.Ln, [ikey], [tk], bias=epst[eps], scale=scale)
        ACT(out_ap, tmp, AF.Exp, [tk], [okey], scale=-0.5)

    epsv = sb("epsv", [128, 2])
    MEMSET("dve", epsv[:, 0:1], LN_EPS, ["epsv"])
    MEMSET("dve", epsv[:, 1:2], RMS_EPS, ["epsv"])
    fence(["epsv"], ("act",))
    epst = {LN_EPS: epsv[:, 0:1], RMS_EPS: epsv[:, 1:2]}

    def layer_norm_chunk(c, ig, ib):
        stt = sb(f"lnst{c}", [128, 2, 6]) if False else lnstat
        S.add("dve", lambda e: e.bn_stats(stt[:, 0, :], X[:, c, 0:512]), reads=[f"X{c}"], writes=["lnst0"])
        S.add("dve", lambda e: e.bn_stats(stt[:, 1, :], X[:, c, 512:1024]), reads=[f"X{c}"], writes=["lnst1"])
        mv, mk = lnmv, "lnmv"
        S.add("dve", lambda e: e.bn_aggr(mv[:], stt[:]), reads=["lnst0", "lnst1"], writes=[mk])
        rs, rk = stat()
        rstd_from(rs, rk, mv[:, 1:2], mk, 1.0, LN_EPS)
        nm, nk = stat()
        TS("dve", nm, mv[:, 0:1], rs, -1.0, ALU.mult, ALU.mult, [mk, rk], [nk])
        t = 4 + (c % 2)
        ACT(bc[t], X[:, c, :], AF.Identity, [f"X{c}", rk, nk], [f"bc{t}"], bias=nm, scale=rs)
        TT("dve", bc[t], bc[t], bc[ig], ALU.mult, [f"bc{t}", f"bc{ig}"], [f"bc{t}"])
        TT("dve", X[:, c, :], bc[t], bc[ib], ALU.add, [f"bc{t}", f"bc{ib}"], [f"X{c}"])

    lnstat = sb("lnstat", [128, 2, 6])
    lnmv = sb("lnmv", [128, 2])


    def outproj_ln(l, nch):
        bcast_mod(0, l, 0, 2)
        if nch > NLAT:
            bcast_mod(1, l, 1, 2)
        bcast_row(2, ln_g_d[l, 0:1, :])
        bcast_row(3, ln_b_d[l, 0:1, :])
        for c in range(nch):
            gtile = 0 if c < NLAT else 1
            t = 4 + (c % 2)
            for hf in range(2):
                pb = 2 * (c % 2) + hf
                for m in range(8):
                    MM(PS[pb][:, :], AOT[:, m, c * 128:(c + 1) * 128], WO[:, m, hf * 512:(hf + 1) * 512], m == 0, m == 7,
                       [f"AOT{c}", "WO"], [f"ps{pb}"])
                TT("dve", bc[t][:, hf * 512:(hf + 1) * 512], PS[pb][:, :], bc[gtile][:, hf * 512:(hf + 1) * 512], ALU.mult,
                   [f"ps{pb}", f"bc{gtile}"], [f"bc{t}"])
            STT("dve", X[:, c, :], X[:, c, :], ALPHA, bc[t], ALU.mult, ALU.add, [f"X{c}", f"bc{t}"], [f"X{c}"])
            layer_norm_chunk(c, 2, 3)
        barrier()

    if True:
        l = 0
        DMA("pool", WO[:], w_out0_d.rearrange("(k p) c -> p k c", p=128), (), ["WO"], "wo")
        modulate_T(0, 1, 0, NCH)
        spill_X(NCH)
        barrier()
        if stage == 15:
            dbgb = dram("dbgb", [128, 8 * NT], BF16, kind="ExternalOutput")
            DMA("sp", dbgb, hT[:].rearrange("p k t -> p (k t)"), [f"hT{c}" for c in range(NCH)], ["dbgd"], "dbg")
            S.add("sp", None, reads=list(S.lw.keys()) + ["dbgd"], writes=())
            S.emit(nc, es)
            es.close()
            return nc
        arena_off[0] = 0
        WG = [xview(1536, [128, 8, 384], BF16, "p (k c) -> p k c", k=8) for _ in range(2)]
        QKT = xview(3 * NT // 2, None, BF16, "p (s t) -> p s t", s=3)
        VX = xview(NCH * 130 // 2, None, BF16, "p (c f) -> p c f", c=NCH)
        QKR = [xview(192, None, BF16) for _ in range(2)]
        T1 = [xview(384, None) for _ in range(2)]
        T2 = [xview(384, None) for _ in range(2)]
        SQ = xview(384, None)
        PT = [xview(256, None, BF16) for _ in range(4)]
        O1 = xview(512, None, F32, "p (j f) -> p j f", j=4)
        OO = [xview(128, None) for _ in range(2)]
        AOK = xview(512, None, BF16, "p (j f) -> p j f", j=4)
        SUBG = xview(128, None)
        G5 = xview(320, None)
        LAMT = xview(256, None, F32, "p (a f) -> p a f", a=4)
        lamt = sb("lamt", [128, 4])
        RSS = sb("RSS", [128, 24])
        DMA("sp", SUBG.rearrange("p (o f) -> p o f", o=1), subg_d.rearrange("(o f) -> o f", o=1).partition_broadcast(128),
            (), ["SUBG"], "c2")
        DMA("sp", G5.rearrange("p (o f) -> p o f", o=1), qkg_d.rearrange("(o f) -> o f", o=1).partition_broadcast(128),
            (), ["G5"], "c2")
        for a in range(4):
            DMA("sp", LAMT[:, a:a + 1, :], lam_d[a:a + 1, :].partition_broadcast(128), (), ["LAMT"], "c2")
        fence(["SUBG", "G5", "LAMT"])
        TS("dve", SUBG, SUBG, 1.0 - LAM_INIT0, None, ALU.mult, None, ["SUBG"], ["SUBG"])
        TT("dve", LAMT[:, 0, :], LAMT[:, 0, :], LAMT[:, 1, :], ALU.mult, ["LAMT"], ["LAMT"])
        TT("dve", LAMT[:, 2, :], LAMT[:, 2, :], LAMT[:, 3, :], ALU.mult, ["LAMT"], ["LAMT"])
        S.add("dve", lambda e: e.tensor_reduce(lamt[:, 0:1], LAMT[:, 0, :], AX.X, ALU.add), reads=["LAMT"], writes=["lamt"])
        S.add("dve", lambda e: e.tensor_reduce(lamt[:, 1:2], LAMT[:, 2, :], AX.X, ALU.add), reads=["LAMT"], writes=["lamt"])
        ACT(lamt[:, 0:2], lamt[:, 0:2], AF.Exp, ["lamt"], ["lamt"])
        TT("dve", lamt[:, 2:3], lamt[:, 1:2], lamt[:, 0:1], ALU.subtract, ["lamt"], ["lamt"])
        TS("dve", lamt[:, 3:4], lamt[:, 2:3], -LAM_INIT0, None, ALU.add, None, ["lamt"], ["lamt"])
        nlam = lamt[:, 3:4]

        groups = [("A", h) for h in range(4)] + [("B", n) for n in range(2)]

        def load_wg(gi):
            kind, idx = groups[gi]
            w = WG[gi % 2]
            if kind == "A":
                cols = [(idx * 128, 128), (512 + idx * 128, 128), (1024 + idx * 128, 128)]
            else:
                cols = [(1536 + idx * 256, 256), (2048 + idx * 64, 64), (2176 + idx * 64, 64)]
            o = 0
            for (c0, n) in cols:
                DMA("pool", w[:, :, o:o + n], w_in0_d[:, c0:c0 + n].rearrange("(k p) c -> p k c", p=128),
                    (), [f"WG{gi % 2}"], f"wg{gi % 2}")
                o += n

        load_wg(0)
        ptn = [0]
        for gi, (kind, idx) in enumerate(groups):
            if gi + 1 < len(groups):
                load_wg(gi + 1)
            w = WG[gi % 2]
            nv = 4 if kind == "A" else 5
            nr = nv * 64
            vcol = 256 if kind == "A" else 320
            vw = 128 if kind == "A" else 64
            MEMSET("dve", VX[:, :, vw:vw + 1], 1.0, [f"VX{c}" for c in range(NCH)])
            for c in range(NCH):
                pb = 6 + (c % 2)
                for k in range(8):
                    MM(PS[pb][:, 0:384], hT[:, k, c * 128:(c + 1) * 128], w[:, k, :], k == 0, k == 7,
                       [f"hT{c}", f"WG{gi % 2}"], [f"ps{pb}"])
                CP("act", VX[:, c, 0:vw], PS[pb][:, vcol:vcol + vw], [f"ps{pb}"], [f"VX{c}"])
                src = PS[pb][:, 0:nr]
                skey = f"ps{pb}"
                i2 = c % 2
                if kind == "B":
                    ACT(SQ[:, 0:nr], src, AF.Square, [skey], ["SQ"])
                    S.add("dve", lambda e, c=c, nr=nr: e.tensor_reduce(RSS[:, 0:5], SQ[:, 0:nr].rearrange("p (v f) -> p v f", v=5), AX.X, ALU.add),
                          reads=["SQ"], writes=["RSS"])
                    ACT(RSS[:, 8:13], RSS[:, 0:5], AF.Ln, ["RSS"], ["RSS2"], bias=epst[RMS_EPS], scale=1.0 / 64)
                    ACT(RSS[:, 16:21], RSS[:, 8:13], AF.Exp, ["RSS2"], ["RSS3"], scale=-0.5)
                    TT("dve", SQ[:, 0:nr].rearrange("p (v f) -> p v f", v=5), src.rearrange("p (v f) -> p v f", v=5),
                       RSS[:, 16:21].unsqueeze(2).to_broadcast([128, 5, 64]), ALU.mult, [skey, "RSS3"], ["SQ"])
                    TT("dve", SQ[:, 0:nr], SQ[:, 0:nr], G5, ALU.mult, ["SQ", "G5"], ["SQ"])
                    src = SQ[:, 0:nr]
                    skey = "SQ"
                cosb = ropet[:, c, 0:64].unsqueeze(1).to_broadcast([128, nv, 64])
                TT("dve", T1[i2][:, 0:nr].rearrange("p (v f) -> p v f", v=nv), src.rearrange("p (v f) -> p v f", v=nv),
                   cosb, ALU.mult, [skey, "ropet"], [f"T1{i2}"])
                s5 = src.rearrange("p (v a j f) -> p v a j f", v=nv, a=2, j=2)
                t5 = T2[i2][:, 0:nr].rearrange("p (v a j f) -> p v a j f", v=nv, a=2, j=2)
                sn5 = ropet[:, c, 64:128].rearrange("p (a j f) -> p a j f", a=2, j=2)
                for j in range(2):
                    for a in range(2):
                        sinb = sn5[:, a, j, :].unsqueeze(1).to_broadcast([128, nv, 16])
                        TT("dve", t5[:, :, a, j, :], s5[:, :, a, 1 - j, :], sinb, ALU.mult,
                           [skey, "ropet"], [f"T2{i2}"])
                TT("dve", QKR[i2][:, 0:nr], T1[i2][:, 0:nr], T2[i2][:, 0:nr], ALU.add, [f"T1{i2}", f"T2{i2}"], [f"QKR{i2}"])
                if kind == "B":
                    TT("dve", QKR[i2][:, 320:384], T1[i2][:, 256:320], T2[i2][:, 256:320], ALU.add,
                       [f"T1{i2}", f"T2{i2}"], [f"QKR{i2}"])
                ntr = 2 if kind == "A" else 3
                tb = 4 + (c % 2)
                for t in range(ntr):
                    TR(PSB[tb][:, t * 128:(t + 1) * 128], QKR[i2][:, t * 128:(t + 1) * 128], identb[:],
                       [f"QKR{i2}", "identb"], [f"ps{tb}"])
                CP("act", QKT[:, 0:ntr, c * 128:(c + 1) * 128], PSB[tb][:, 0:ntr * 128].rearrange("p (s t) -> p s t", s=ntr),
                   [f"ps{tb}"], [f"QKT{c}"])
            if stage == 16 and gi == GSTOP:
                dbgb = dram("dbgb", [128, 3 * NT], BF16, kind="ExternalOutput")
                DMA("sp", dbgb, QKT.rearrange("p s t -> p (s t)"), [f"QKT{c}" for c in range(NCH)], ["dbgd"], "dbg")
                dbgv = dram("dbgv", [128, NCH * 130], BF16, kind="ExternalOutput")
                DMA("sp", dbgv, VX.rearrange("p c f -> p (c f)"), [f"VX{c}" for c in range(NCH)], ["dbgd"], "dbg")
                S.add("sp", None, reads=list(S.lw.keys()) + ["dbgd"], writes=())
                S.emit(nc, es)
                es.close()
                return nc
            blocks = [(qb * 4, 4, list(range(NCH))) for qb in range(4)] + [(16, 2, [16, 17])]
            if kind == "A":
                heads = [(0, 0, 1, s * 64) for s in range(2)]
            else:
                heads = [(j4 // 2, 2, (j4 % 2) * 64) for j4 in range(4)]
            for (qc0, nqc, kcs) in blocks:
                ncols = nqc * 128
                q0 = qc0 * 128
                qkeys = [f"QKT{qc0 + j}" for j in range(nqc)]
                for hi, hd in enumerate(heads):
                    if kind == "A":
                        qs, ks, pbase = 0, 1, hi * 64
                    else:
                        qs, ks, pbase = hd
                    def score(kn):
                        kc = kcs[kn]
                        sp_ = 4 + (ptn[0] % 3)
                        pi = ptn[0] % 4
                        ptn[0] += 1
                        MM(PS[sp_][:, 0:ncols], QKT[pbase:pbase + 64, ks, kc * 128:(kc + 1) * 128],
                           QKT[pbase:pbase + 64, qs, q0:q0 + ncols], True, True,
                           [f"QKT{kc}"] + qkeys, [f"ps{sp_}"])
                        return sp_, pi

                    LOOK = 2
                    pend = [score(i) for i in range(min(LOOK, len(kcs)))]
                    for kn, kc in enumerate(kcs):
                        if kn + LOOK < len(kcs):
                            pend.append(score(kn + LOOK))
                        sp_, pi = pend.pop(0)
                        ACT(PT[pi][:, 0:ncols], PS[sp_][:, 0:ncols], AF.Exp, [f"ps{sp_}"], [f"PT{pi}"], scale=0.125)
                        for j in range(nqc):
                            MM(PS[j][:, 0:vw + 1], PT[pi][:, j * 128:(j + 1) * 128], VX[:, kc, 0:vw + 1],
                               kn == 0, kn == len(kcs) - 1, [f"PT{pi}", f"VX{kc}"], [f"ps{j}"])
                    for j in range(nqc):
                        rz, rk = stat()
                        S.add("dve", lambda e, rz=rz, j=j, vw=vw: e.reciprocal(rz, PS[j][:, vw:vw + 1]), reads=[f"ps{j}"], writes=[rk])
                        if kind == "B":
                            TS("dve", AOK[:, j, hi * 64:(hi + 1) * 64], PS[j][:, 0:64], rz, None, ALU.mult, None,
                               [f"ps{j}", rk], [f"AOK{j}"])
                        elif hi == 0:
                            TS("dve", O1[:, j, :], PS[j][:, 0:128], rz, None, ALU.mult, None, [f"ps{j}", rk], [f"O1{j}"])
                        else:
                            r2, r2k = stat()
                            TT("dve", r2, rz, nlam, ALU.mult, [rk, "lamt"], [r2k])
                            oo = OO[j % 2]
                            STT("dve", oo, PS[j][:, 0:128], r2, O1[:, j, :], ALU.mult, ALU.add,
                                [f"ps{j}", r2k, f"O1{j}"], [f"OO{j % 2}"])
                            ssq, ssk = stat()
                            TT("dve", T1[j % 2][:, 0:128], oo, oo, ALU.mult, [f"OO{j % 2}"], [f"T1{j % 2}"])
                            S.add("dve", lambda e, ssq=ssq, j=j: e.tensor_reduce(ssq, T1[j % 2][:, 0:128], AX.X, ALU.add),
                                  reads=[f"T1{j % 2}"], writes=[ssk])
                            rs, rsk = stat()
                            rstd_from(rs, rsk, ssq, ssk, 1.0 / 128, RMS_EPS)
                            STT("dve", AOK[:, j, 0:128], oo, rs, SUBG, ALU.mult, ALU.mult,
                                [f"OO{j % 2}", rsk, "SUBG"], [f"AOK{j}"])
                    last = (kind == "A" and hi == 1) or (kind == "B" and hi == 3)
                    if last:
                        for j in range(nqc):
                            tb = 7
                            ntr = 1 if kind == "A" else 2
                            for t in range(ntr):
                                TR(PSB[tb][:, t * 128:(t + 1) * 128], AOK[:, j, t * 128:(t + 1) * 128], identb[:],
                                   [f"AOK{j}", "identb"], [f"ps{tb}"])
                            m0 = idx if kind == "A" else 4 + idx * 2
                            tc = qc0 + j
                            CP("act", AOT[:, m0:m0 + ntr, tc * 128:(tc + 1) * 128],
                               PSB[tb][:, 0:ntr * 128].rearrange("p (s t) -> p s t", s=ntr), [f"ps{tb}"], [f"AOT{tc}"])
        barrier()
        reload_X(NCH)
        outproj_ln(0, NCH)

    if stage == 2:
        for c in range(NCH):
            DMA("sp", dbg_d[c * 128:(c + 1) * 128, :], X[:, c, :], [f"X{c}"], ["dbgd"], "dbg")
        S.add("sp", None, reads=list(S.lw.keys()) + ["dbgd"], writes=())
        S.emit(nc, es)
        es.close()
        return nc
    router_d = dram("moe_router", [2, D, NE])
    NEX = 1 if stage == 31 else NE
    wg_d = dram("moe_w_gate", [2, NEX, D, FF])
    wu_d = dram("moe_w_up", [2, NEX, D, FF])
    wd_d = dram("moe_w_down", [2, NEX, FF, D])
    iota_d = dram("iota288", [128, 288])
    utri_d = dram("utri", [128, 128])
    aotb = AOT[:].rearrange("p k t -> p (k t)")
    wob = WO[:].rearrange("p k c -> p (k c)")
    hmb = hT[:].rearrange("p k t -> p (k t)").rearrange("p (c f) -> p c f", c=NCH)
    NRING = 5
    RING = [aotb[:, i * 2048:(i + 1) * 2048] for i in range(NRING)]
    XGT = aotb[:, 10240:12544].rearrange("p (k s) -> p k s", k=8)
    HIDT = aotb[:, 12544:17152].rearrange("p (j s) -> p j s", j=16)
    SELR = [aotb[:, 17152 + i * 288:17152 + (i + 1) * 288] for i in range(2)]
    SELGT = [aotb[:, 17728 + i * 384:17728 + (i + 1) * 384].rearrange("p (s t) -> p s t", s=3) for i in range(1)]
    SELR += [wob[:, 5280 + i * 288:5280 + (i + 1) * 288] for i in range(3)]
    SELGT += [wob[:, 6144 + i * 384:6144 + (i + 1) * 384].rearrange("p (s t) -> p s t", s=3) for i in range(2)]
    NSEL = len(SELR)
    NSGT = len(SELGT)
    YG = wob[:, 0:3072].rearrange("p (s f) -> p s f", s=3)
    IOTA = wob[:, 3072:3648].bitcast(F32)
    RW = wob[:, 3648:3904].bitcast(F32).rearrange("p (k e) -> p k e", k=8)
    UTRI = wob[:, 3904:4032]
    ONESB = wob[:, 4032:4160]
    MASKB = wob[:, 4160:4448].rearrange("p (c e) -> p c e", c=NCH)
    SGR = [wob[:, 4448:5024].bitcast(F32), wob[:, 6912:7488].bitcast(F32)]
    UTF = wob[:, 5024:5280].bitcast(F32)
    WEX = ropet[:].rearrange("p c f -> p (c f)")
    AFF = hb[0][:].bitcast(F32)[:, 0:288].rearrange("p (c e) -> p c e", c=NCH)
    MASK = hb[1][:].bitcast(F32)[:, 0:288].rearrange("p (c e) -> p c e", c=NCH)
    mo2 = sb("mo2", [128, 2, NCH * NE])
    SLOT = mo2[:, 0, :].rearrange("p (c e) -> p c e", c=NCH)
    GS = mo2[:, 1, :].rearrange("p (c e) -> p c e", c=NCH)
    m8 = sb("m8", [16, 8])

    def moe_layer(l, nch, final_out=False):
        ns = 256 + (32 if nch > NLAT else 0)
        nsc = (ns + 127) // 128
        scsz = [min(128, ns - s * 128) for s in range(nsc)]
        barrier()
        DMA("sp", IOTA, iota_d, (), ["IOTA"], "c3")
        DMA("sp", UTF, utri_d, (), ["UTF"], "c3")
        DMA("sp", RW, router_d[l].rearrange("(k p) e -> p k e", p=128), (), ["RW"], "c3")
        fence(["IOTA", "UTF", "RW"])
        CP("dve", UTRI, UTF, ["UTF"], ["UTRI"])
        MEMSET("dve", ONESB, 1.0, ["ONESB"])
        bcast_mod(0, l, 0, 4, True)
        bcast_mod(1, l, 0, 3)
        if nch > NLAT:
            bcast_mod(2, l, 1, 4, True)
            bcast_mod(3, l, 1, 3)
        for c in range(nch):
            a, b_ = (0, 1) if c < NLAT else (2, 3)
            TT("dve", bc[4], X[:, c, :], bc[a], ALU.mult, [f"X{c}", f"bc{a}"], ["bc4"])
            TT("dve", bc[4], bc[4], bc[b_], ALU.add, ["bc4", f"bc{b_}"], ["bc4"])
            CP("act", hmb[:, c, :], bc[4], ["bc4"], [f"hmb{c}"])
            TS("dve", X[:, c, :], X[:, c, :], ALPHA, None, ALU.mult, None, [f"X{c}"], [f"X{c}"])
            for k in range(8):
                pb = k // 4
                TR(PS[pb][:, (k % 4) * 128:(k % 4 + 1) * 128], bc[4][:, k * 128:(k + 1) * 128], identf[:],
                   ["bc4", "identf"], [f"ps{pb}"])
            hmT = bc[5].rearrange("p (k t) -> p k t", k=8)
            CP("act", hmT[:, 0:4, :], PS[0][:, :].rearrange("p (k t) -> p k t", k=4), ["ps0"], ["bc5"])
            CP("dve", hmT[:, 4:8, :], PS[1][:, :].rearrange("p (k t) -> p k t", k=4), ["ps1"], ["bc5"])
            for k in range(8):
                MM(PS[2][:, 0:NE], hmT[:, k, :], RW[:, k, :], k == 0, k == 7, ["bc5", "RW"], ["ps2"])
            mx, mxk = stat()
            S.add("dve", lambda e, mx=mx: e.tensor_reduce(mx, PS[2][:, 0:NE], AX.X, ALU.max), reads=["ps2"], writes=[mxk])
            nmx, nmk = stat()
            TS("dve", nmx, mx, -1.0, None, ALU.mult, None, [mxk], [nmk])
            ACT(AFF[:, c, :], PS[2][:, 0:NE], AF.Exp, ["ps2", nmk], [f"AFF{c}"], bias=nmx)
            sm, smk = stat()
            S.add("dve", lambda e, sm=sm, c=c: e.tensor_reduce(sm, AFF[:, c, :], AX.X, ALU.add), reads=[f"AFF{c}"], writes=[smk])
            rc, rck = stat()
            S.add("dve", lambda e, rc=rc, sm=sm: e.reciprocal(rc, sm), reads=[smk], writes=[rck])
            TS("dve", AFF[:, c, :], AFF[:, c, :], rc, None, ALU.mult, None, [f"AFF{c}", rck], [f"AFF{c}"])
            TR(PS[3][0:NE, 0:128], AFF[:, c, :], identf[:], [f"AFF{c}", "identf"], ["ps3"])
            CP("dve", WEX[0:NE, c * 128:(c + 1) * 128], PS[3][0:NE, 0:128], ["ps3"], ["WEX"])
        sets = [(0, SEQ, 256)] + ([(SEQ, NT, 32)] if nch > NLAT else [])
        for (t0, t1, cap) in sets:
            wv = WEX[0:NE, t0:t1]
            for r in range(cap // 8):
                S.add("dve", lambda e, wv=wv: e.max(m8[:], wv), reads=["WEX"], writes=["m8"])
                S.add("dve", lambda e, wv=wv: e.match_replace(wv, m8[:], wv, -1.0), reads=["WEX", "m8"], writes=["WEX"])
        TS("dve", WEX[0:NE, 0:nch * 128], WEX[0:NE, 0:nch * 128], 0.0, None, ALU.is_lt, None, ["WEX"], ["WEX"])
        for c in range(nch):
            TR(PS[4][:, c * NE:(c + 1) * NE], WEX[0:NE, c * 128:(c + 1) * 128], identf[0:NE, 0:NE], ["WEX", "identf"], ["ps4"])
        mflat = mo2[:, 0, 0:nch * NE]
        CP("dve", MASK[:, 0:nch, :], PS[4][:, 0:nch * NE].rearrange("p (c e) -> p c e", c=nch), ["ps4"], ["MASK"])
        CP("dve", MASKB[:, 0:nch, :], MASK[:, 0:nch, :], ["MASK"], ["MASKB"])
        TT("dve", GS[:, 0:nch, :], AFF[:, 0:nch, :], MASK[:, 0:nch, :], ALU.mult, [f"AFF{c}" for c in range(nch)] + ["MASK"], ["GS"])
        for c in range(nch):
            c0 = 0 if c < NLAT else NLAT
            prev = list(range(c0, c))
            for i, cp in enumerate(prev):
                MM(PS[5][:, c * NE:(c + 1) * NE], ONESB, MASKB[:, cp, :], i == 0, False, ["ONESB", "MASKB"], ["ps5"])
            MM(PS[5][:, c * NE:(c + 1) * NE], UTRI, MASKB[:, c, :], len(prev) == 0, True, ["UTRI", "MASKB"], ["ps5"])
        TS("dve", SLOT[:, 0:NLAT, :], PS[5][:, 0:NLAT * NE].rearrange("p (c e) -> p c e", c=NLAT), 1.0, None, ALU.add, None, ["ps5"], ["SLOT"])
        if nch > NLAT:
            TS("dve", SLOT[:, NLAT:nch, :], PS[5][:, NLAT * NE:nch * NE].rearrange("p (c e) -> p c e", c=nch - NLAT), 257.0, None,
               ALU.add, None, ["ps5"], ["SLOT"])
        TT("dve", SLOT[:, 0:nch, :], SLOT[:, 0:nch, :], MASK[:, 0:nch, :], ALU.mult, ["SLOT", "MASK"], ["SLOT"])
        TS("dve", SLOT[:, 0:nch, :], SLOT[:, 0:nch, :], -1.0, None, ALU.add, None, ["SLOT"], ["SLOT"])
        bcast_mod(0, l, 0, 5)
        if nch > NLAT:
            bcast_mod(1, l, 1, 5)
        bcast_row(2, ln_g_d[l, 1:2, :])
        bcast_row(3, ln_b_d[l, 1:2, :])
        units = []
        for e_ in range(NEX):
            for fu in range(8):
                units.append(("g", e_, fu))
                units.append(("u", e_, fu))
            for du in range(8):
                units.append(("d", e_, du))
        issued = [0]

        def issue_until(n):
            while issued[0] < min(n, len(units)):
                kind, e_, i = units[issued[0]]
                slot = issued[0] % NRING
                if kind == "d":
                    src = wd_d[l, e_, i * 256:(i + 1) * 256, :].rearrange("(j p) c -> p j c", p=128)
                    dst = RING[slot].rearrange("p (j c) -> p j c", j=2)
                else:
                    wsrc = wg_d if kind == "g" else wu_d
                    src = wsrc[l, e_, :, i * 256:(i + 1) * 256].rearrange("(k p) c -> p k c", p=128)
                    dst = RING[slot].rearrange("p (k c) -> p k c", k=8)
                DMA("pool", dst, src, (), [f"wr{slot}"], f"wr{slot}")
                issued[0] += 1

        ui = [0]

        def next_unit():
            i = ui[0]
            ui[0] += 1
            return i % NRING

        def refill():
            issue_until(ui[0] + NRING)

        issue_until(NRING)
        seln = [0]
        sgn = [0]
        for e_ in range(NEX):
            for half in range(2):
                for c in range(nch):
                    si = seln[0] % NSEL
                    seln[0] += 1
                    TS("dve", SELR[si][:, 0:ns], IOTA[:, 0:ns], SLOT[:, c, e_:e_ + 1], None, ALU.is_equal, None,
                       ["IOTA", "SLOT"], [f"SEL{si}"])
                    for f4 in range(4):
                        f = half * 4 + f4
                        MM(PS[f4][:, 0:ns], hmb[:, c, f * 128:(f + 1) * 128], SELR[si][:, 0:ns], c == 0, c == nch - 1,
                           [f"hmb{c}", f"SEL{si}"], [f"ps{f4}"])
                for f4 in range(4):
                    f = half * 4 + f4
                    CP("act" if f4 % 2 == 0 else "dve", XGT[:, f, 0:ns], PS[f4][:, 0:ns], [f"ps{f4}"], ["XGT"])
            for fu in range(8):
                sg_ = next_unit()
                su_ = next_unit()
                wgv = RING[sg_].rearrange("p (k c) -> p k c", k=8)
                wuv = RING[su_].rearrange("p (k c) -> p k c", k=8)
                for j2 in range(2):
                    jg = fu * 2 + j2
                    bg, bu = (4, 5) if jg % 2 == 0 else (6, 7)
                    for k in range(8):
                        MM(PS[bg][:, 0:ns], wgv[:, k, j2 * 128:(j2 + 1) * 128], XGT[:, k, 0:ns], k == 0, k == 7,
                           [f"wr{sg_}", "XGT"], [f"ps{bg}"])
                    for k in range(8):
                        MM(PS[bu][:, 0:ns], wuv[:, k, j2 * 128:(j2 + 1) * 128], XGT[:, k, 0:ns], k == 0, k == 7,
                           [f"wr{su_}", "XGT"], [f"ps{bu}"])
                    SG = SGR[jg % 2]
                    ACT(SG[:, 0:ns], PS[bg][:, 0:ns], AF.Silu, [f"ps{bg}"], [f"SG{jg % 2}"])
                    TT("dve", HIDT[:, jg, 0:ns], PS[bu][:, 0:ns], SG[:, 0:ns], ALU.mult, [f"ps{bu}", f"SG{jg % 2}"], [f"HID{jg}"])
                refill()
            for du in range(8):
                sd_ = next_unit()
                wdv = RING[sd_].rearrange("p (j c) -> p j c", j=2)
                for sc in range(nsc):
                    sz = scsz[sc]
                    for hf in range(2):
                        pb = sc * 2 + hf
                        for jj in range(2):
                            jg = du * 2 + jj
                            MM(PS[pb][0:sz, :], HIDT[:, jg, sc * 128:sc * 128 + sz], wdv[:, jj, hf * 512:(hf + 1) * 512],
                               du == 0 and jj == 0, du == 7 and jj == 1, [f"HID{jg}", f"wr{sd_}"], [f"ps{pb}"])
                refill()
            for sc in range(nsc):
                sz = scsz[sc]
                gt = 0 if sc < 2 else 1
                for hf in range(2):
                    pb = sc * 2 + hf
                    TT("dve", YG[0:sz, sc, hf * 512:(hf + 1) * 512], PS[pb][0:sz, :], bc[gt][0:sz, hf * 512:(hf + 1) * 512], ALU.mult,
                       [f"ps{pb}", f"bc{gt}"], ["YG"])
            if stage == 31:
                d1 = dram("d_slot", [128, 2 * NCH * NE], kind="ExternalOutput")
                DMA("sp", d1, mo2[:].rearrange("p a f -> p (a f)"), ["SLOT", "GS"], ["dbgd"], "dbg")
                d2 = dram("d_xgt", [128, 8 * 288], BF16, kind="ExternalOutput")
                DMA("sp", d2, XGT.rearrange("p k s -> p (k s)"), ["XGT"], ["dbgd"], "dbg")
                d3 = dram("d_yg", [128, 3 * 1024], BF16, kind="ExternalOutput")
                DMA("sp", d3, YG.rearrange("p s f -> p (s f)"), ["YG"], ["dbgd"], "dbg")
                d4 = dram("d_hid", [128, 16 * 288], BF16, kind="ExternalOutput")
                DMA("sp", d4, HIDT.rearrange("p j s -> p (j s)"), [f"HID{j}" for j in range(16)], ["dbgd"], "dbg")
                S.add("sp", None, reads=list(S.lw.keys()) + ["dbgd"], writes=())
                S.emit(nc, es)
                es.close()
                return "STOP"
            def sc_stage_a(c):
                si = seln[0] % NSEL
                seln[0] += 1
                TS("dve", SELR[si][:, 0:ns], IOTA[:, 0:ns], SLOT[:, c, e_:e_ + 1], GS[:, c, e_:e_ + 1], ALU.is_equal, ALU.mult,
                   ["IOTA", "SLOT", "GS"], [f"SEL{si}"])
                tb = 6 + (c % 2)
                for sc in range(nsc):
                    sz = scsz[sc]
                    TR(PSB[tb][0:sz, sc * 128:(sc + 1) * 128], SELR[si][:, sc * 128:sc * 128 + sz], identb[:],
                       [f"SEL{si}", "identb"], [f"ps{tb}"])
                gi_ = sgn[0] % NSGT
                sgn[0] += 1
                sgt = SELGT[gi_]
                CP("act", sgt[:, 0:2, :], PSB[tb][:, 0:256].rearrange("p (s t) -> p s t", s=2), [f"ps{tb}"], [f"SELGT{gi_}"])
                if nsc > 2:
                    CP("act", sgt[0:scsz[2], 2, :], PSB[tb][0:scsz[2], 256:384], [f"ps{tb}"], [f"SELGT{gi_}"])
                return gi_

            def sc_stage_b(c, gi_):
                sgt = SELGT[gi_]
                for hf in range(2):
                    pb = 2 * (c % 2) + hf
                    for sc in range(nsc):
                        sz = scsz[sc]
                        MM(PS[pb][:, :], sgt[0:sz, sc, :], YG[0:sz, sc, hf * 512:(hf + 1) * 512], sc == 0, sc == nsc - 1,
                           [f"SELGT{gi_}", "YG"], [f"ps{pb}"])
                    TT("dve", X[:, c, hf * 512:(hf + 1) * 512], PS[pb][:, :], X[:, c, hf * 512:(hf + 1) * 512], ALU.add,
                       [f"ps{pb}", f"X{c}"], [f"X{c}"])

            prev = None
            for c in range(nch):
                g_ = sc_stage_a(c)
                if prev is not None:
                    sc_stage_b(*prev)
                prev = (c, g_)
            sc_stage_b(*prev)
        for c in range(nch):
            layer_norm_chunk(c, 2, 3)
            if final_out:
                DMA("sp", out_d[c * 128:(c + 1) * 128, :], X[:, c, :], [f"X{c}"], ["outd"], "outw")
        barrier()

    if moe_layer(0, NCH) == "STOP":
        return nc

    if stage == 3:
        for c in range(NCH):
            DMA("sp", dbg_d[c * 128:(c + 1) * 128, :], X[:, c, :], [f"X{c}"], ["dbgd"], "dbg")
        S.add("sp", None, reads=list(S.lw.keys()) + ["dbgd"], writes=())
        S.emit(nc, es)
        es.close()
        return nc

    w_in1_d = dram("l1_w_in", [D, 2048])
    w_out1_d = dram("l1_w_out", [D, D])
    poolw_d = dram("l1_pool_w", [4, 128, 128])
    psc_d = dram("psc", [128, 4])
    band_d = dram("band", [20, 128, 128])
    l1tab_d = dram("l1tab", [NPAT, 8, 128, 128])

    def layer1_mixer():
        l = 1
        barrier()
        DMA("pool", WO[:], w_out1_d.rearrange("(k p) c -> p k c", p=128), (), ["WO"], "wo")
        modulate_T(1, 1, 0, NCH)
        spill_X(NLAT)
        barrier()
        arena_off[0] = 0
        W1R = [xview(2048, None, BF16, "p (k c) -> p k c", k=8) for _ in range(2)]
        QT1 = xview(4 * SEQ // 2, None, BF16, "p (g t) -> p g t", g=4)
        KT1 = xview(4 * NT // 2, None, BF16, "p (g t) -> p g t", g=4)
        VXU = xview(NCH * 8 * 65 // 2, None, BF16)
        VX1 = VXU.rearrange("p (c h f) -> p c h f", c=NCH, h=8)
        U1 = VXU[:, 0:NLAT * 512].rearrange("p (c f) -> p c f", c=NLAT)
        PT1 = [xview(256, None, BF16) for _ in range(3)]
        rp = ropet[:].rearrange("p c f -> p (c f)")
        TBI = rp[:, 0:1280].bitcast(BF16).rearrange("p (a h k) -> p a h k", a=5, h=4)
        TBB = rp[:, 1280:2304].bitcast(BF16).rearrange("p (a h k) -> p a h k", a=4, h=4)
        BAND = hb[0][:].rearrange("p (m t) -> p m t", m=8)
        PW = hb[1][:, 0:512].rearrange("p (g e) -> p g e", g=4)
        AOK1 = hb[1][:, 512:768]
        POOLT = hb[1][:, 768:1024].bitcast(F32) if False else None
        psc = sb("pscs", [128, 4])
        qflat = QT1.rearrange("p g t -> p (g t)")
        bandb = qflat[:, 0:2560].rearrange("p (m t) -> p m t", m=20)
        poolt = [qflat[:, 2560 + i * 512:2560 + (i + 1) * 512] for i in range(2)]
        DMA("sp", psc[:], psc_d, (), ["psc"], "c4")
        DMA("pool", bandb, band_d.rearrange("m a b -> a m b"), (), ["bandb"], "c4p")
        DMA("pool", PW, poolw_d.rearrange("g c e -> c g e"), (), ["PW"], "c4p")
        fence(["psc", "bandb", "PW"])

        def load_w1(sec, slot):
            DMA("pool", W1R[slot][:], w_in1_d[:, sec * 512:(sec + 1) * 512].rearrange("(k p) c -> p k c", p=128),
                (), [f"W1R{slot}"], f"w1r{slot}")

        load_w1(3, 0)
        load_w1(0, 1)
        for c in range(NLAT):
            pb = 4 + (c % 2)
            for k in range(8):
                MM(PS[pb][:, :], hT[:, k, c * 128:(c + 1) * 128], W1R[0][:, k, :], k == 0, k == 7, [f"hT{c}", "W1R0"], [f"ps{pb}"])
            CP("act" if c % 2 == 0 else "dve", U1[:, c, :], PS[pb][:, :], [f"ps{pb}"], [f"U1{c}"])
        pn = [0]
        for g in range(4):
            for cb in range(4):
                pb = 6 + (pn[0] % 2)
                for j in range(4):
                    c = cb * 4 + j
                    srcs = []
                    if c > 0:
                        srcs.append((c - 1, g * 5 + 0))
                    srcs.append((c, g * 5 + (3 if c == 0 else 4 if c == NLAT - 1 else 1)))
                    if c < NLAT - 1:
                        srcs.append((c + 1, g * 5 + 2))
                    for i, (cs, m) in enumerate(srcs):
                        MM(PS[pb][:, j * 128:(j + 1) * 128], U1[:, cs, g * 128:(g + 1) * 128], bandb[:, m, :], i == 0, i == len(srcs) - 1,
                           [f"U1{cs}", "bandb"], [f"ps{pb}"])
                pt_ = poolt[pn[0] % 2]
                CP("act", pt_, PS[pb][:, :], [f"ps{pb}"], [f"poolt{pn[0] % 2}"])
                pb2 = 4 + (pn[0] % 2)
                MM(PS[pb2][:, :], PW[:, g, :], pt_, True, True, ["PW", f"poolt{pn[0] % 2}"], [f"ps{pb2}"])
                TS("dve", AOT[:, 4 + g, cb * 512:(cb + 1) * 512], PS[pb2][:, :], psc[:, g:g + 1], None, ALU.mult, None,
                   [f"ps{pb2}", "psc"], [f"AOT{cb * 4 + j}" for j in range(4)])
                pn[0] += 1
        barrier()
        MEMSET("dve", VX1[:, :, :, 64:65], 1.0, [f"VX{c}" for c in range(NCH)])
        for g in range(4):
            for tb_ in range(4):
                pb = 4 + ((g * 4 + tb_) % 2)
                for k in range(8):
                    MM(PS[pb][:, :], W1R[1][:, k, g * 128:(g + 1) * 128], hT[:, k, tb_ * 512:(tb_ + 1) * 512], k == 0, k == 7,
                       ["W1R1"] + [f"hT{tb_ * 4 + j}" for j in range(4)], [f"ps{pb}"])
                ACT(QT1[:, g, tb_ * 512:(tb_ + 1) * 512], PS[pb][:, :], AF.Identity, [f"ps{pb}"], [f"QT{tb_ * 4 + j}" for j in range(4)], scale=0.125)
        load_w1(1, 0)
        load_w1(2, 1)
        blocks_k = [(i * 512, 512) for i in range(4)] + [(2048, 256)]
        for g in range(4):
            for bi, (t0, tn) in enumerate(blocks_k):
                pb = 4 + ((g * 5 + bi) % 2)
                chs = list(range(t0 // 128, (t0 + tn) // 128))
                for k in range(8):
                    MM(PS[pb][:, 0:tn], W1R[0][:, k, g * 128:(g + 1) * 128], hT[:, k, t0:t0 + tn], k == 0, k == 7,
                       ["W1R0"] + [f"hT{c}" for c in chs], [f"ps{pb}"])
                CP("act" if bi % 2 == 0 else "dve", KT1[:, g, t0:t0 + tn], PS[pb][:, 0:tn], [f"ps{pb}"], [f"KT{c}" for c in chs])
        for c in range(NCH):
            pb = 6 + (c % 2)
            for k in range(8):
                MM(PS[pb][:, :], hT[:, k, c * 128:(c + 1) * 128], W1R[1][:, k, :], k == 0, k == 7, [f"hT{c}", "W1R1"], [f"ps{pb}"])
            CP("act" if c % 2 == 0 else "dve", VX1[:, c, :, 0:64], PS[pb][:, :].rearrange("p (h f) -> p h f", h=8), [f"ps{pb}"], [f"VX{c}"])
        ptn = [0]
        for hh in range(2):
            for a in range(5):
                DMA("pool", TBI[:, a, :, :], l1tab_d[INT_PATS[a], hh * 4:(hh + 1) * 4].rearrange("h q k -> q h k"), (), ["TBI"], "tbi")
            for c in range(NLAT):
                loc = L1_LOCAL[c]
                interior = (2 <= c <= 13)
                if not interior:
                    for a, (kc, pat) in enumerate(loc):
                        DMA("pool", TBB[:, a, :, :], l1tab_d[pat, hh * 4:(hh + 1) * 4].rearrange("h q k -> q h k"), (), ["TBB"], "tbb")
                kcs = [(kc, a) for a, (kc, pat) in enumerate(loc)] + [(16, None), (17, None)]
                def score1(kn):
                    kc, a = kcs[kn]
                    bpair = (4, 5) if ptn[0] % 2 == 0 else (6, 7)
                    pi = ptn[0] % 3
                    ptn[0] += 1
                    for par in range(2):
                        bk = bpair[par]
                        for i2, h4 in enumerate((par, par + 2)):
                            h = hh * 4 + h4
                            g = h // 2
                            pbase = (h % 2) * 64
                            MM(PS[bk][:, i2 * 128:(i2 + 1) * 128], KT1[pbase:pbase + 64, g, kc * 128:(kc + 1) * 128],
                               QT1[pbase:pbase + 64, g, c * 128:(c + 1) * 128], True, a is None, [f"KT{kc}", f"QT{c}"], [f"ps{bk}"])
                            if a is not None:
                                tb_ap = TBI[:, a, h4, :] if interior else TBB[:, a, h4, :]
                                MM(PS[bk][:, i2 * 128:(i2 + 1) * 128], tb_ap, identb[:], False, True,
                                   ["TBI" if interior else "TBB", "identb"], [f"ps{bk}"])
                    return bpair, pi

                pend = score1(0)
                for kn, (kc, a) in enumerate(kcs):
                    nxt = score1(kn + 1) if kn + 1 < len(kcs) else None
                    bpair, pi = pend
                    for par in range(2):
                        bk = bpair[par]
                        ACT(PT1[pi][:, par * 256:(par + 1) * 256], PS[bk][:, 0:256], AF.Exp, [f"ps{bk}"], [f"PT{pi}_{par}"])
                    for h4 in range(4):
                        h = hh * 4 + h4
                        par, i2 = h4 % 2, h4 // 2
                        o_ = par * 256 + i2 * 128
                        MM(PS[h4][:, 0:65], PT1[pi][:, o_:o_ + 128], VX1[:, kc, h, :], kn == 0, kn == len(kcs) - 1,
                           [f"PT{pi}_{par}", f"VX{kc}"], [f"ps{h4}"])
                    pend = nxt
                for h4 in range(4):
                    rz, rk = stat()
                    S.add("dve", lambda e, rz=rz, h4=h4: e.reciprocal(rz, PS[h4][:, 64:65]), reads=[f"ps{h4}"], writes=[rk])
                    TS("dve", AOK1[:, h4 * 64:(h4 + 1) * 64], PS[h4][:, 0:64], rz, None, ALU.mult, None, [f"ps{h4}", rk], ["AOK1"])
                tb = 6 + (c % 2)
                for t in range(2):
                    TR(PSB[tb][:, t * 128:(t + 1) * 128], AOK1[:, t * 128:(t + 1) * 128], identb[:], ["AOK1", "identb"], [f"ps{tb}"])
                CP("act", AOT[:, 2 * hh:2 * hh + 2, c * 128:(c + 1) * 128], PSB[tb][:, 0:256].rearrange("p (s t) -> p s t", s=2),
                   [f"ps{tb}"], [f"AOT{c}"])
        barrier()
        reload_X(NLAT)
        outproj_ln(1, NLAT)

    layer1_mixer()
    if stage == 4:
        for c in range(NLAT):
            DMA("sp", dbg_d[c * 128:(c + 1) * 128, :], X[:, c, :], [f"X{c}"], ["dbgd"], "dbg")
        S.add("sp", None, reads=list(S.lw.keys()) + ["dbgd"], writes=())
        S.emit(nc, es)
        es.close()
        return nc
    moe_layer(1, NLAT, final_out=True)
    S.add("sp", None, reads=list(S.lw.keys()) + ["outd"], writes=())
    S.emit(nc, es)
    es.close()
    return nc


def _rope_table():
    t = np.arange(SEQ)
    inv = (10000.0 ** (-np.arange(16, dtype=np.float32) / 16)).astype(np.float32)
    ar = (t // 64).astype(np.float32)[:, None] * inv
    ac = (t % 64).astype(np.float32)[:, None] * inv
    cr, sr, cc_, sc_ = np.cos(ar), np.sin(ar), np.cos(ac), np.sin(ac)
    cosf = np.concatenate([cr, cr, cc_, cc_], axis=1)
    sins = np.concatenate([-sr, sr, -sc_, sc_], axis=1)
    tab = np.concatenate([cosf, sins], axis=1).astype(np.float32)
    ctxr = np.tile(np.array([1.0] * 64 + [0.0] * 64, np.float32), (CTX, 1))
    return np.concatenate([tab, ctxr], axis=0)


def make_in_maps(inp):
    f = lambda a: np.ascontiguousarray(np.asarray(a, dtype=np.float32))
    shared = {
        "ada_w": f(inp["ada_w"]), "ada_b": f(inp["ada_b"]), "ln_g": f(inp["ln_g"]), "ln_b": f(inp["ln_b"]),
        "l0_w_in": f(inp["l0_w_in"]), "l0_w_out": f(inp["l0_w_out"]),
        "lamv": f(np.stack([inp["l0_lam_q1"], inp["l0_lam_k1"], inp["l0_lam_q2"], inp["l0_lam_k2"]])),
        "l0_subln_g": f(inp["l0_subln_g"]),
        "qkg": f(np.concatenate([np.tile(np.asarray(inp["l0_qnorm_g"]), 4), np.asarray(inp["l0_knorm_g"])])),
        "rope": _rope_table(), "ident": np.eye(128, dtype=np.float32),
        "moe_router": f(inp["moe_router"]), "moe_w_gate": f(inp["moe_w_gate"]), "moe_w_up": f(inp["moe_w_up"]),
        "moe_w_down": f(inp["moe_w_down"]),
        "l1_w_in": f(inp["l1_w_in"]), "l1_w_out": f(inp["l1_w_out"]), "l1_pool_w": f(inp["l1_pool_w"]),
        "psc": f(np.asarray(inp["l1_pool_scale"]).reshape(4, 128).T), "band": _band_mats(),
        "l1tab": _l1_table(inp["l1_rpb"]),
        "iota288": np.tile(np.arange(288, dtype=np.float32), (128, 1)),
        "utri": np.triu(np.ones((128, 128), np.float32), 1),
    }
    maps = []
    x = np.asarray(inp["x"]); ctx = np.asarray(inp["ctx"]); c = np.asarray(inp["c"]); cctx = np.asarray(inp["c_ctx"])
    for b in range(8):
        m = dict(shared)
        m["x"] = f(np.concatenate([x[b], ctx[b]], axis=0))
        cc = np.stack([c[b].reshape(8, 128).T, cctx.reshape(8, 128).T], axis=-1)
        m["cc"] = f(cc)
        maps.append(m)
    return maps


def kernel(**inputs):
    nc = build()
    maps = make_in_maps(inputs)
    res = run_bass_kernel_spmd(nc, maps, core_ids=list(range(8)))
    return np.stack([r["out"] for r in res.results], axis=0)
```

```python
import math
import numpy as np
from contextlib import ExitStack
import concourse.bass as bass
import concourse.mybir as mybir
from concourse.bass_utils import run_bass_kernel_spmd

F32 = mybir.dt.float32
BF16 = mybir.dt.bfloat16
AF = mybir.ActivationFunctionType
ALU = mybir.AluOpType
AX = mybir.AxisListType

D = 1024
SEQ = 2048
CTX = 256
NT = SEQ + CTX
NCH = NT // 128
NLAT = SEQ // 128
NE = 16
FF = 2048
ALPHA = 4.0 ** 0.25
LN_EPS = 1e-5
RMS_EPS = 1e-6
LAM_INIT0 = 0.8 - 0.6 * math.exp(0.0)
NEG = -30000.0


class Sched:
    def __init__(self):
        self.ops = []
        self.lw = {}
        self.rd = {}

    def add(self, eng, fn, reads=(), writes=(), dma=None):
        i = len(self.ops)
        stream = ("dma", dma) if dma is not None else eng
        pk = [k for k in reads if k.startswith("ps") and k[2:].isdigit()]
        if pk:
            writes = list(writes) + [k for k in pk if k not in writes]
        raw = set()
        war = set()
        for k in reads:
            w = self.lw.get(k)
            if w is not None:
                raw.add(w)
        for k in writes:
            w = self.lw.get(k)
            if w is not None:
                raw.add(w)
            for r in self.rd.get(k, {}).values():
                war.add(r)
        if fn is not None:
            for k in writes:
                self.lw[k] = i
                self.rd[k] = {}
            for k in reads:
                self.rd.setdefault(k, {})[stream] = i
        self.ops.append(dict(eng=eng, fn=fn, raw=raw, war=war, dma=dma, stream=stream, cons=False, ms=0))
        return i

    def emit(self, nc, es):
        ops = self.ops
        for o in ops:
            for d in o["raw"] | o["war"]:
                if d == len(ops):
                    continue
                p = ops[d]
                same = (p["stream"] == o["stream"])
                if same and (o["stream"] == "pe"):
                    continue
                if same and d in o["war"] and d not in o["raw"]:
                    continue
                p["cons"] = True
        sems = {}
        cnt = {}

        def get_sem(stream):
            if stream not in sems:
                nm = "s_" + (stream if isinstance(stream, str) else "d_" + str(stream[1]))
                sems[stream] = es.enter_context(nc.semaphore(nm))
                cnt[stream] = 0
            return sems[stream]

        for o in ops:
            st = o["stream"]
            get_sem(st)
            if o["dma"] is not None:
                cnt[st] += 16
                o["ms"] = cnt[st]
            elif o["cons"]:
                cnt[st] += 1
                o["ms"] = cnt[st]
        self.nsem = len(sems)
        block = es.enter_context(nc.Block())
        engs = ["pe", "act", "dve", "pool", "sp"]
        per = {e: [o for o in ops if o["eng"] == e] for e in engs}

        def run(e, eh):
            known = {}
            for o in per[e]:
                need = {}
                for d in o["raw"] | o["war"]:
                    p = ops[d]
                    same = (p["stream"] == o["stream"])
                    if same and o["stream"] == "pe":
                        continue
                    if same and d in o["war"] and d not in o["raw"]:
                        continue
                    if p["ms"] <= 0:
                        continue
                    s = p["stream"]
                    if need.get(s, 0) < p["ms"]:
                        need[s] = p["ms"]
                for s, v in need.items():
                    if known.get(s, 0) < v:
                        eh.wait_ge(sems[s], v)
                        known[s] = v
                if o["fn"] is None:
                    continue
                ins = o["fn"](eh)
                if o["dma"] is not None:
                    ins.then_inc(sems[o["stream"]], 16)
                elif o["cons"]:
                    ins.then_inc(sems[o["stream"]], 1)

        @block.tensor
        def _(eh):
            run("pe", eh)

        @block.scalar
        def _(eh):
            run("act", eh)

        @block.vector
        def _(eh):
            run("dve", eh)

        @block.gpsimd
        def _(eh):
            run("pool", eh)

        @block.sync
        def _(eh):
            run("sp", eh)


def _l1_patterns():
    W, rows, kh, kw = 64, 32, 8, 16
    cols = np.arange(W)
    cs = np.clip(cols - kw // 2, 0, W - kw)
    colok = (cols[None, :] >= cs[:, None]) & (cols[None, :] < cs[:, None] + kw)
    dcol = cols[None, :] - cols[:, None] + 15
    pats, sigs, local = [], {}, []
    for c in range(16):
        lst = []
        for kc in range(16):
            valid = np.zeros((128, 128), bool)
            dr = np.zeros((128, 128), np.int64)
            dc = np.zeros((128, 128), np.int64)
            for ql in range(2):
                r = 2 * c + ql
                rs = min(max(r - kh // 2, 0), rows - kh)
                for kl in range(2):
                    rk = 2 * kc + kl
                    rowok = rs <= rk < rs + kh
                    qs = slice(ql * 64, ql * 64 + 64)
                    ks = slice(kl * 64, kl * 64 + 64)
                    valid[qs, ks] = colok & rowok
                    dr[qs, ks] = rk - r + 7
                    dc[qs, ks] = dcol
            if not valid.any():
                continue
            dr = np.where(valid, dr, 0)
            dc = np.where(valid, dc, 0)
            sig = (valid.tobytes(), dr.tobytes())
            if sig not in sigs:
                sigs[sig] = len(pats)
                pats.append((valid, dr, dc))
            lst.append((kc, sigs[sig]))
        local.append(lst)
    return pats, local


_L1_PATS, L1_LOCAL = _l1_patterns()
NPAT = len(_L1_PATS)
INT_PATS = [p for (_, p) in L1_LOCAL[2]]
for _c in range(2, 14):
    assert [p for (_, p) in L1_LOCAL[_c]] == INT_PATS and len(INT_PATS) == 5
for _c in (0, 1, 14, 15):
    assert len(L1_LOCAL[_c]) <= 4


def _l1_table(rpb):
    rpb = np.asarray(rpb, np.float32)
    tab = np.full((NPAT, 8, 128, 128), NEG, np.float32)
    for i, (valid, dr, dc) in enumerate(_L1_PATS):
        g = rpb[:, dr, dc]
        tab[i] = np.where(valid[None], g, np.float32(NEG))
    return tab


def _band_mats():
    out = np.zeros((20, 128, 128), np.float32)
    tp = np.arange(128)[:, None]
    t = np.arange(128)[None, :]
    for g, w in enumerate((2, 4, 8, 16)):
        half = w // 2
        cnt = float(2 * half)
        out[g * 5 + 0] = (tp >= t + 128 - half) / cnt
        out[g * 5 + 1] = ((tp >= t - half) & (tp < t + half)) / cnt - (tp == t)
        out[g * 5 + 2] = (tp < t + half - 128) / cnt
        lo = np.maximum(t - half, 0)
        hi = t + half
        out[g * 5 + 3] = ((tp >= lo) & (tp < hi)) / (hi - lo).astype(np.float32) - (tp == t)
        lo = t - half
        hi = np.minimum(t + half, 128)
        out[g * 5 + 4] = ((tp >= lo) & (tp < hi)) / (hi - lo).astype(np.float32) - (tp == t)
    return out


GSTOP = 0


def build(stage=99, dbg_cols=1024):
    nc = bass.Bass("TRN2", target_bir_lowering=False)
    S = Sched()
    es = ExitStack()

    def dram(name, shape, dt=F32, kind="ExternalInput"):
        return nc.dram_tensor(name, list(shape), dt, kind=kind).ap()

    x_d = dram("x", [NT, D])
    cc_d = dram("cc", [128, 8, 2])
    ada_w_d = dram("ada_w", [2, D, 6 * D])
    ada_b_d = dram("ada_b", [2, 6 * D])
    ln_g_d = dram("ln_g", [2, 2, D])
    ln_b_d = dram("ln_b", [2, 2, D])
    w_in0_d = dram("l0_w_in", [D, 2304])
    w_out0_d = dram("l0_w_out", [D, D])
    lam_d = dram("lamv", [4, 64])
    subg_d = dram("l0_subln_g", [128])
    qkg_d = dram("qkg", [5 * 64])
    rope_d = dram("rope", [NT, 128])
    ident_d = dram("ident", [128, 128])
    out_d = dram("out", [SEQ, D], kind="ExternalOutput")
    dbg_d = dram("dbg", [128 if stage == 1 else NT, dbg_cols], kind="ExternalOutput") if stage < 99 else None
    modd = dram("modd", [2, 2, 6 * D], kind="Internal")

    def sb(name, shape, dt=F32):
        return es.enter_context(nc.sbuf_tensor(name, list(shape), dt))

    Xraw = sb("X", [128, NCH * D])
    X = Xraw[:].rearrange("p (c f) -> p c f", c=NCH)
    hT = sb("hT", [128, 8, NT], BF16)
    AOT = sb("AOT", [128, 8, NT], BF16)
    WO = sb("WO", [128, 8, D], BF16)
    identb = sb("identb", [128, 128], BF16)
    identf = sb("identf", [128, 128])
    ropet = sb("ropet", [128, NCH, 128])
    big = sb("big", [128, 6 * 1024])
    bc = [big[:, i * 1024:(i + 1) * 1024] for i in range(6)]
    st = sb("st", [128, 64])
    PS = [es.enter_context(nc.psum_tensor(f"ps{i}", [128, 512], F32)) for i in range(8)]
    PSB = [p[:].bitcast(BF16) for p in PS]
    stn = [0]

    def stat():
        i = stn[0] % 64
        stn[0] += 1
        return st[:, i:i + 1], f"st{i}"

    arena_off = [0]

    def xview(nelem_f32, shape, dt=F32, pattern=None, **kw):
        a = arena_off[0]
        arena_off[0] += nelem_f32
        assert arena_off[0] <= NCH * D
        v = Xraw[:, a:a + nelem_f32]
        if dt != F32:
            v = v.bitcast(dt)
        if pattern is not None:
            v = v.rearrange(pattern, **kw)
        return v

    dkn = [0]

    def dk(prefix="d"):
        dkn[0] += 1
        return f"{prefix}{dkn[0]}"

    def DMA(q, out, in_, reads, writes, key):
        return S.add(q, lambda e: e.dma_start(out=out, in_=in_), reads=reads, writes=writes, dma=key)

    def MM(out, lhsT, rhs, start, stop, reads, writes):
        return S.add("pe", lambda e: e.matmul(out, lhsT, rhs, start=start, stop=stop), reads=reads, writes=writes)

    def TR(out, in_, ident, reads, writes):
        return S.add("pe", lambda e: e.transpose(out, in_, ident), reads=reads, writes=writes)

    def ACT(out, in_, func, reads, writes, bias=None, scale=None, accum_out=None):
        kw = {}
        if bias is not None:
            kw["bias"] = bias
        if scale is not None:
            kw["scale"] = scale
        if accum_out is not None:
            kw["accum_out"] = accum_out
        return S.add("act", lambda e: e.activation(out, in_, func, **kw), reads=reads, writes=writes)

    def TT(eng, out, in0, in1, op, reads, writes):
        return S.add(eng, lambda e: e.tensor_tensor(out, in0, in1, op), reads=reads, writes=writes)

    def TS(eng, out, in0, s1, s2, op0, op1, reads, writes):
        if op1 is None:
            return S.add(eng, lambda e: e.tensor_scalar(out, in0, s1, None, op0), reads=reads, writes=writes)
        return S.add(eng, lambda e: e.tensor_scalar(out, in0, s1, s2, op0, op1), reads=reads, writes=writes)

    def STT(eng, out, in0, scalar, in1, op0, op1, reads, writes):
        return S.add(eng, lambda e: e.scalar_tensor_tensor(out, in0, scalar, in1, op0, op1), reads=reads, writes=writes)

    def CP(eng, out, in_, reads, writes):
        if eng == "act":
            return S.add(eng, lambda e: e.copy(out, in_), reads=reads, writes=writes)
        return S.add(eng, lambda e: e.tensor_copy(out, in_), reads=reads, writes=writes)

    def MEMSET(eng, ap, val, writes):
        return S.add(eng, lambda e: e.memset(ap, val), reads=(), writes=writes)

    def fence(keys, engs=("pe", "act", "dve", "pool", "sp")):
        for e in engs:
            S.add(e, None, reads=keys, writes=())

    def barrier():
        allk = list(S.lw.keys())
        for e in ("pe", "act", "dve", "pool", "sp"):
            S.add(e, None, reads=allk, writes=allk)

    nc_lp = es.enter_context(nc.allow_low_precision("bf16 matmul operands, fp32 accumulation"))
    es.enter_context(nc.allow_non_contiguous_dma("small strided constant loads"))

    for c in range(NCH):
        DMA("sp", X[:, c, :], x_d[c * 128:(c + 1) * 128, :], (), [f"X{c}"], "ldx")
    ccs = sb("ccs", [128, 8, 2])
    DMA("sp", ccs[:], cc_d, (), ["ccs"], "const")
    DMA("sp", identf[:], ident_d, (), ["identf"], "const")
    DMA("sp", ropet[:], rope_d.rearrange("(c p) f -> p c f", p=128), (), ["ropet"], "const")
    fence(["ccs", "identf", "ropet"])
    CP("dve", identb[:], identf[:], ["identf"], ["identb"])

    scs = sb("scs", [128, 8, 2])
    S.add("act", lambda e: e.activation(scs[:], ccs[:], AF.Silu), reads=["ccs"], writes=["scs"])
    adab = sb("adab", [2, 512])
    modrow = sb("modrow", [2, 512])
    NST = 3
    aotf = AOT[:].rearrange("p k t -> p (k t)").bitcast(F32)
    hTf = hT[:].rearrange("p k t -> p (k t)").bitcast(F32)
    stg = [aotf[:, 0:4096], aotf[:, 4096:8192], hTf[:, 0:4096]]
    gi = 0
    for l in range(2):
        for j in range(12):
            s = gi % NST
            st3 = stg[s].rearrange("p (k c) -> p k c", k=8)
            DMA("sp", st3, ada_w_d[l, :, j * 512:(j + 1) * 512].rearrange("(k p) c -> p k c", p=128),
                (), [f"stg{s}"], f"adaw{s}")
            for r in range(2):
                DMA("sp", adab[r:r + 1, :], ada_b_d[l:l + 1, j * 512:(j + 1) * 512], (), ["adab"], "adab")
            pb = gi % 2
            for k in range(8):
                MM(PS[pb][0:2, :], scs[:, k, :], st3[:, k, :], k == 0, k == 7,
                   ["scs", f"stg{s}"], [f"ps{pb}"])
            TT("dve", modrow[:], PS[pb][0:2, :], adab[:], ALU.add, [f"ps{pb}", "adab"], ["modrow"])
            DMA("sp", modd[l, :, j * 512:(j + 1) * 512], modrow[:], ["modrow"], ["modd"], "modw")
            gi += 1
    barrier()

    def dbg_out(ap, cols):
        tmpd = sb("tmpd", [128, cols])
        CP("dve", tmpd[:], ap, list(S.lw.keys()), ["tmpd"])
        DMA("sp", dbg_d[:, 0:cols], tmpd[:], ["tmpd"], ["dbgd"], "dbg")

    if stage == 1:
        t1 = sb("t1", [128, 192])
        DMA("sp", t1[:], modd.rearrange("l r (a b) -> (l r a) b", b=192), ["modd"], ["t1"], "t1")
        dbg_out(t1[:], 192)
        S.add("sp", None, reads=list(S.lw.keys()), writes=())
        S.emit(nc, es)
        es.close()
        return nc

    xscr = dram("xscr", [NT, D], kind="Internal")

    def bcast_mod(i, l, r, idx, plus1=False):
        src = modd[l, r:r + 1, idx * 1024:(idx + 1) * 1024].partition_broadcast(128)
        DMA("sp", bc[i].rearrange("p (o f) -> p o f", o=1), src, ["modd"], [f"bc{i}"], f"bcl{i}")
        if plus1:
            TS("dve", bc[i], bc[i], 1.0, None, ALU.add, None, [f"bc{i}"], [f"bc{i}"])

    def bcast_row(i, row_ap):
        DMA("sp", bc[i].rearrange("p (o f) -> p o f", o=1), row_ap.partition_broadcast(128), (), [f"bc{i}"], f"bcl{i}")

    hb = [sb(f"hb{i}", [128, D], BF16) for i in range(2)]

    def modulate_T(l, isc, ish, nch):
        bcast_mod(0, l, 0, isc, True)
        bcast_mod(1, l, 0, ish)
        if nch > NLAT:
            bcast_mod(2, l, 1, isc, True)
            bcast_mod(3, l, 1, ish)
        for c in range(nch):
            a, b_ = (0, 1) if c < NLAT else (2, 3)
            t = 4 + (c % 2)
            TT("dve", bc[t], X[:, c, :], bc[a], ALU.mult, [f"X{c}", f"bc{a}"], [f"bc{t}"])
            TT("dve", hb[c % 2][:], bc[t], bc[b_], ALU.add, [f"bc{t}", f"bc{b_}"], [f"hb{c % 2}"])
            pb = 4 + (c % 2)
            for k in range(8):
                TR(PSB[pb][:, k * 128:(k + 1) * 128], hb[c % 2][:, k * 128:(k + 1) * 128], identb[:],
                   [f"hb{c % 2}", "identb"], [f"ps{pb}"])
            CP("act", hT[:, :, c * 128:(c + 1) * 128], PSB[pb].rearrange("p (k t) -> p k t", k=8),
               [f"ps{pb}"], [f"hT{c}"])

    def spill_X(nch):
        for c in range(nch):
            DMA("sp", xscr[c * 128:(c + 1) * 128, :], X[:, c, :], [f"X{c}"], ["xscr"], "xsp")

    def reload_X(nch):
        for c in range(nch):
            DMA("sp", X[:, c, :], xscr[c * 128:(c + 1) * 128, :], ["xscr"], [f"X{c}"], "xrl")

    def rstd_from(out_ap, okey, in_ap, ikey, scale, eps):
        tmp, tk = stat()
        ACT(tmp, in_ap, AF.Ln, [ikey], [tk], bias=epst[eps], scale=scale)
        ACT(out_ap, tmp, AF.Exp, [tk], [okey], scale=-0.5)

    epsv = sb("epsv", [128, 2])
    MEMSET("dve", epsv[:, 0:1], LN_EPS, ["epsv"])
    MEMSET("dve", epsv[:, 1:2], RMS_EPS, ["epsv"])
    fence(["epsv"], ("act",))
    epst = {LN_EPS: epsv[:, 0:1], RMS_EPS: epsv[:, 1:2]}

    def layer_norm_chunk(c, ig, ib):
        stt = sb(f"lnst{c}", [128, 2, 6]) if False else lnstat
        S.add("dve", lambda e: e.bn_stats(stt[:, 0, :], X[:, c, 0:512]), reads=[f"X{c}"], writes=["lnst0"])
        S.add("dve", lambda e: e.bn_stats(stt[:, 1, :], X[:, c, 512:1024]), reads=[f"X{c}"], writes=["lnst1"])
        mv, mk = lnmv, "lnmv"
        S.add("dve", lambda e: e.bn_aggr(mv[:], stt[:]), reads=["lnst0", "lnst1"], writes=[mk])
        rs, rk = stat()
        rstd_from(rs, rk, mv[:, 1:2], mk, 1.0, LN_EPS)
        nm, nk = stat()
        TS("dve", nm, mv[:, 0:1], rs, -1.0, ALU.mult, ALU.mult, [mk, rk], [nk])
        t = 4 + (c % 2)
        ACT(bc[t], X[:, c, :], AF.Identity, [f"X{c}", rk, nk], [f"bc{t}"], bias=nm, scale=rs)
        TT("dve", bc[t], bc[t], bc[ig], ALU.mult, [f"bc{t}", f"bc{ig}"], [f"bc{t}"])
        TT("dve", X[:, c, :], bc[t], bc[ib], ALU.add, [f"bc{t}", f"bc{ib}"], [f"X{c}"])

    lnstat = sb("lnstat", [128, 2, 6])
    lnmv = sb("lnmv", [128, 2])


    def outproj_ln(l, nch):
        bcast_mod(0, l, 0, 2)
        if nch > NLAT:
            bcast_mod(1, l, 1, 2)
        bcast_row(2, ln_g_d[l, 0:1, :])
        bcast_row(3, ln_b_d[l, 0:1, :])
        for c in range(nch):
            gtile = 0 if c < NLAT else 1
            t = 4 + (c % 2)
            for hf in range(2):
                pb = 2 * (c % 2) + hf
                for m in range(8):
                    MM(PS[pb][:, :], AOT[:, m, c * 128:(c + 1) * 128], WO[:, m, hf * 512:(hf + 1) * 512], m == 0, m == 7,
                       [f"AOT{c}", "WO"], [f"ps{pb}"])
                TT("dve", bc[t][:, hf * 512:(hf + 1) * 512], PS[pb][:, :], bc[gtile][:, hf * 512:(hf + 1) * 512], ALU.mult,
                   [f"ps{pb}", f"bc{gtile}"], [f"bc{t}"])
            STT("dve", X[:, c, :], X[:, c, :], ALPHA, bc[t], ALU.mult, ALU.add, [f"X{c}", f"bc{t}"], [f"X{c}"])
            layer_norm_chunk(c, 2, 3)
        barrier()

    if True:
        l = 0
        DMA("pool", WO[:], w_out0_d.rearrange("(k p) c -> p k c", p=128), (), ["WO"], "wo")
        modulate_T(0, 1, 0, NCH)
        spill_X(NCH)
        barrier()
        if stage == 15:
            dbgb = dram("dbgb", [128, 8 * NT], BF16, kind="ExternalOutput")
            DMA("sp", dbgb, hT[:].rearrange("p k t -> p (k t)"), [f"hT{c}" for c in range(NCH)], ["dbgd"], "dbg")
            S.add("sp", None, reads=list(S.lw.keys()) + ["dbgd"], writes=())
            S.emit(nc, es)
            es.close()
            return nc
        arena_off[0] = 0
        WG = [xview(1536, [128, 8, 384], BF16, "p (k c) -> p k c", k=8) for _ in range(2)]
        QKT = xview(3 * NT // 2, None, BF16, "p (s t) -> p s t", s=3)
        VX = xview(NCH * 130 // 2, None, BF16, "p (c f) -> p c f", c=NCH)
        QKR = [xview(192, None, BF16) for _ in range(2)]
        T1 = [xview(384, None) for _ in range(2)]
        T2 = [xview(384, None) for _ in range(2)]
        SQ = xview(384, None)
        PT = [xview(256, None, BF16) for _ in range(4)]
        O1 = xview(512, None, F32, "p (j f) -> p j f", j=4)
        OO = [xview(128, None) for _ in range(2)]
        AOK = xview(512, None, BF16, "p (j f) -> p j f", j=4)
        SUBG = xview(128, None)
        G5 = xview(320, None)
        LAMT = xview(256, None, F32, "p (a f) -> p a f", a=4)
        lamt = sb("lamt", [128, 4])
        RSS = sb("RSS", [128, 24])
        DMA("sp", SUBG.rearrange("p (o f) -> p o f", o=1), subg_d.rearrange("(o f) -> o f", o=1).partition_broadcast(128),
            (), ["SUBG"], "c2")
        DMA("sp", G5.rearrange("p (o f) -> p o f", o=1), qkg_d.rearrange("(o f) -> o f", o=1).partition_broadcast(128),
            (), ["G5"], "c2")
        for a in range(4):
            DMA("sp", LAMT[:, a:a + 1, :], lam_d[a:a + 1, :].partition_broadcast(128), (), ["LAMT"], "c2")
        fence(["SUBG", "G5", "LAMT"])
        TS("dve", SUBG, SUBG, 1.0 - LAM_INIT0, None, ALU.mult, None, ["SUBG"], ["SUBG"])
        TT("dve", LAMT[:, 0, :], LAMT[:, 0, :], LAMT[:, 1, :], ALU.mult, ["LAMT"], ["LAMT"])
        TT("dve", LAMT[:, 2, :], LAMT[:, 2, :], LAMT[:, 3, :], ALU.mult, ["LAMT"], ["LAMT"])
        S.add("dve", lambda e: e.tensor_reduce(lamt[:, 0:1], LAMT[:, 0, :], AX.X, ALU.add), reads=["LAMT"], writes=["lamt"])
        S.add("dve", lambda e: e.tensor_reduce(lamt[:, 1:2], LAMT[:, 2, :], AX.X, ALU.add), reads=["LAMT"], writes=["lamt"])
        ACT(lamt[:, 0:2], lamt[:, 0:2], AF.Exp, ["lamt"], ["lamt"])
        TT("dve", lamt[:, 2:3], lamt[:, 1:2], lamt[:, 0:1], ALU.subtract, ["lamt"], ["lamt"])
        TS("dve", lamt[:, 3:4], lamt[:, 2:3], -LAM_INIT0, None, ALU.add, None, ["lamt"], ["lamt"])
        nlam = lamt[:, 3:4]

        groups = [("A", h) for h in range(4)] + [("B", n) for n in range(2)]

        def load_wg(gi):
            kind, idx = groups[gi]
            w = WG[gi % 2]
            if kind == "A":
                cols = [(idx * 128, 128), (512 + idx * 128, 128), (1024 + idx * 128, 128)]
            else:
                cols = [(1536 + idx * 256, 256), (2048 + idx * 64, 64), (2176 + idx * 64, 64)]
            o = 0
            for (c0, n) in cols:
                DMA("pool", w[:, :, o:o + n], w_in0_d[:, c0:c0 + n].rearrange("(k p) c -> p k c", p=128),
                    (), [f"WG{gi % 2}"], f"wg{gi % 2}")
                o += n

        load_wg(0)
        ptn = [0]
        for gi, (kind, idx) in enumerate(groups):
            if gi + 1 < len(groups):
                load_wg(gi + 1)
            w = WG[gi % 2]
            nv = 4 if kind == "A" else 5
            nr = nv * 64
            vcol = 256 if kind == "A" else 320
            vw = 128 if kind == "A" else 64
            MEMSET("dve", VX[:, :, vw:vw + 1], 1.0, [f"VX{c}" for c in range(NCH)])
            for c in range(NCH):
                pb = 6 + (c % 2)
                for k in range(8):
                    MM(PS[pb][:, 0:384], hT[:, k, c * 128:(c + 1) * 128], w[:, k, :], k == 0, k == 7,
                       [f"hT{c}", f"WG{gi % 2}"], [f"ps{pb}"])
                CP("act", VX[:, c, 0:vw], PS[pb][:, vcol:vcol + vw], [f"ps{pb}"], [f"VX{c}"])
                src = PS[pb][:, 0:nr]
                skey = f"ps{pb}"
                i2 = c % 2
                if kind == "B":
                    ACT(SQ[:, 0:nr], src, AF.Square, [skey], ["SQ"])
                    S.add("dve", lambda e, c=c, nr=nr: e.tensor_reduce(RSS[:, 0:5], SQ[:, 0:nr].rearrange("p (v f) -> p v f", v=5), AX.X, ALU.add),
                          reads=["SQ"], writes=["RSS"])
                    ACT(RSS[:, 8:13], RSS[:, 0:5], AF.Ln, ["RSS"], ["RSS2"], bias=epst[RMS_EPS], scale=1.0 / 64)
                    ACT(RSS[:, 16:21], RSS[:, 8:13], AF.Exp, ["RSS2"], ["RSS3"], scale=-0.5)
                    TT("dve", SQ[:, 0:nr].rearrange("p (v f) -> p v f", v=5), src.rearrange("p (v f) -> p v f", v=5),
                       RSS[:, 16:21].unsqueeze(2).to_broadcast([128, 5, 64]), ALU.mult, [skey, "RSS3"], ["SQ"])
                    TT("dve", SQ[:, 0:nr], SQ[:, 0:nr], G5, ALU.mult, ["SQ", "G5"], ["SQ"])
                    src = SQ[:, 0:nr]
                    skey = "SQ"
                cosb = ropet[:, c, 0:64].unsqueeze(1).to_broadcast([128, nv, 64])
                TT("dve", T1[i2][:, 0:nr].rearrange("p (v f) -> p v f", v=nv), src.rearrange("p (v f) -> p v f", v=nv),
                   cosb, ALU.mult, [skey, "ropet"], [f"T1{i2}"])
                s5 = src.rearrange("p (v a j f) -> p v a j f", v=nv, a=2, j=2)
                t5 = T2[i2][:, 0:nr].rearrange("p (v a j f) -> p v a j f", v=nv, a=2, j=2)
                sn5 = ropet[:, c, 64:128].rearrange("p (a j f) -> p a j f", a=2, j=2)
                for j in range(2):
                    for a in range(2):
                        sinb = sn5[:, a, j, :].unsqueeze(1).to_broadcast([128, nv, 16])
                        TT("dve", t5[:, :, a, j, :], s5[:, :, a, 1 - j, :], sinb, ALU.mult,
                           [skey, "ropet"], [f"T2{i2}"])
                TT("dve", QKR[i2][:, 0:nr], T1[i2][:, 0:nr], T2[i2][:, 0:nr], ALU.add, [f"T1{i2}", f"T2{i2}"], [f"QKR{i2}"])
                if kind == "B":
                    TT("dve", QKR[i2][:, 320:384], T1[i2][:, 256:320], T2[i2][:, 256:320], ALU.add,
                       [f"T1{i2}", f"T2{i2}"], [f"QKR{i2}"])
                ntr = 2 if kind == "A" else 3
                tb = 4 + (c % 2)
                for t in range(ntr):
                    TR(PSB[tb][:, t * 128:(t + 1) * 128], QKR[i2][:, t * 128:(t + 1) * 128], identb[:],
                       [f"QKR{i2}", "identb"], [f"ps{tb}"])
                CP("act", QKT[:, 0:ntr, c * 128:(c + 1) * 128], PSB[tb][:, 0:ntr * 128].rearrange("p (s t) -> p s t", s=ntr),
                   [f"ps{tb}"], [f"QKT{c}"])
            if stage == 16 and gi == GSTOP:
                dbgb = dram("dbgb", [128, 3 * NT], BF16, kind="ExternalOutput")
                DMA("sp", dbgb, QKT.rearrange("p s t -> p (s t)"), [f"QKT{c}" for c in range(NCH)], ["dbgd"], "dbg")
                dbgv = dram("dbgv", [128, NCH * 130], BF16, kind="ExternalOutput")
                DMA("sp", dbgv, VX.rearrange("p c f -> p (c f)"), [f"VX{c}" for c in range(NCH)], ["dbgd"], "dbg")
                S.add("sp", None, reads=list(S.lw.keys()) + ["dbgd"], writes=())
                S.emit(nc, es)
                es.close()
                return nc
            blocks = [(qb * 4, 4, list(range(NCH))) for qb in range(4)] + [(16, 2, [16, 17])]
            if kind == "A":
                heads = [(0, 0, 1, s * 64) for s in range(2)]
            else:
                heads = [(j4 // 2, 2, (j4 % 2) * 64) for j4 in range(4)]
            for (qc0, nqc, kcs) in blocks:
                ncols = nqc * 128
                q0 = qc0 * 128
                qkeys = [f"QKT{qc0 + j}" for j in range(nqc)]
                for hi, hd in enumerate(heads):
                    if kind == "A":
                        qs, ks, pbase = 0, 1, hi * 64
                    else:
                        qs, ks, pbase = hd
                    def score(kn):
                        kc = kcs[kn]
                        sp_ = 4 + (ptn[0] % 2)
                        pi = ptn[0] % 4
                        ptn[0] += 1
                        MM(PS[sp_][:, 0:ncols], QKT[pbase:pbase + 64, ks, kc * 128:(kc + 1) * 128],
                           QKT[pbase:pbase + 64, qs, q0:q0 + ncols], True, True,
                           [f"QKT{kc}"] + qkeys, [f"ps{sp_}"])
                        return sp_, pi

                    pend = score(0)
                    for kn, kc in enumerate(kcs):
                        nxt = score(kn + 1) if kn + 1 < len(kcs) else None
                        sp_, pi = pend
                        ACT(PT[pi][:, 0:ncols], PS[sp_][:, 0:ncols], AF.Exp, [f"ps{sp_}"], [f"PT{pi}"], scale=0.125)
                        for j in range(nqc):
                            MM(PS[j][:, 0:vw + 1], PT[pi][:, j * 128:(j + 1) * 128], VX[:, kc, 0:vw + 1],
                               kn == 0, kn == len(kcs) - 1, [f"PT{pi}", f"VX{kc}"], [f"ps{j}"])
                        pend = nxt
                    for j in range(nqc):
                        rz, rk = stat()
                        S.add("dve", lambda e, rz=rz, j=j, vw=vw: e.reciprocal(rz, PS[j][:, vw:vw + 1]), reads=[f"ps{j}"], writes=[rk])
                        if kind == "B":
                            TS("dve", AOK[:, j, hi * 64:(hi + 1) * 64], PS[j][:, 0:64], rz, None, ALU.mult, None,
                               [f"ps{j}", rk], [f"AOK{j}"])
                        elif hi == 0:
                            TS("dve", O1[:, j, :], PS[j][:, 0:128], rz, None, ALU.mult, None, [f"ps{j}", rk], [f"O1{j}"])
                        else:
                            r2, r2k = stat()
                            TT("dve", r2, rz, nlam, ALU.mult, [rk, "lamt"], [r2k])
                            oo = OO[j % 2]
                            STT("dve", oo, PS[j][:, 0:128], r2, O1[:, j, :], ALU.mult, ALU.add,
                                [f"ps{j}", r2k, f"O1{j}"], [f"OO{j % 2}"])
                            ssq, ssk = stat()
                            TT("dve", T1[j % 2][:, 0:128], oo, oo, ALU.mult, [f"OO{j % 2}"], [f"T1{j % 2}"])
                            S.add("dve", lambda e, ssq=ssq, j=j: e.tensor_reduce(ssq, T1[j % 2][:, 0:128], AX.X, ALU.add),
                                  reads=[f"T1{j % 2}"], writes=[ssk])
                            rs, rsk = stat()
                            rstd_from(rs, rsk, ssq, ssk, 1.0 / 128, RMS_EPS)
                            STT("dve", AOK[:, j, 0:128], oo, rs, SUBG, ALU.mult, ALU.mult,
                                [f"OO{j % 2}", rsk, "SUBG"], [f"AOK{j}"])
                    last = (kind == "A" and hi == 1) or (kind == "B" and hi == 3)
                    if last:
                        for j in range(nqc):
                            tb = 6 + (j % 2)
                            ntr = 1 if kind == "A" else 2
                            for t in range(ntr):
                                TR(PSB[tb][:, t * 128:(t + 1) * 128], AOK[:, j, t * 128:(t + 1) * 128], identb[:],
                                   [f"AOK{j}", "identb"], [f"ps{tb}"])
                            m0 = idx if kind == "A" else 4 + idx * 2
                            tc = qc0 + j
                            CP("act", AOT[:, m0:m0 + ntr, tc * 128:(tc + 1) * 128],
                               PSB[tb][:, 0:ntr * 128].rearrange("p (s t) -> p s t", s=ntr), [f"ps{tb}"], [f"AOT{tc}"])
        barrier()
        reload_X(NCH)
        outproj_ln(0, NCH)

    if stage == 2:
        for c in range(NCH):
            DMA("sp", dbg_d[c * 128:(c + 1) * 128, :], X[:, c, :], [f"X{c}"], ["dbgd"], "dbg")
        S.add("sp", None, reads=list(S.lw.keys()) + ["dbgd"], writes=())
        S.emit(nc, es)
        es.close()
        return nc
    router_d = dram("moe_router", [2, D, NE])
    NEX = 1 if stage == 31 else NE
    wg_d = dram("moe_w_gate", [2, NEX, D, FF])
    wu_d = dram("moe_w_up", [2, NEX, D, FF])
    wd_d = dram("moe_w_down", [2, NEX, FF, D])
    iota_d = dram("iota288", [128, 288])
    utri_d = dram("utri", [128, 128])
    aotb = AOT[:].rearrange("p k t -> p (k t)")
    wob = WO[:].rearrange("p k c -> p (k c)")
    hmb = hT[:].rearrange("p k t -> p (k t)").rearrange("p (c f) -> p c f", c=NCH)
    NRING = 5
    RING = [aotb[:, i * 2048:(i + 1) * 2048] for i in range(NRING)]
    XGT = aotb[:, 10240:12544].rearrange("p (k s) -> p k s", k=8)
    HIDT = aotb[:, 12544:17152].rearrange("p (j s) -> p j s", j=16)
    SELR = [aotb[:, 17152 + i * 288:17152 + (i + 1) * 288] for i in range(2)]
    SELGT = [aotb[:, 17728 + i * 384:17728 + (i + 1) * 384].rearrange("p (s t) -> p s t", s=3) for i in range(1)]
    SELR += [wob[:, 5280 + i * 288:5280 + (i + 1) * 288] for i in range(3)]
    SELGT += [wob[:, 6144 + i * 384:6144 + (i + 1) * 384].rearrange("p (s t) -> p s t", s=3) for i in range(2)]
    NSEL = len(SELR)
    NSGT = len(SELGT)
    YG = wob[:, 0:3072].rearrange("p (s f) -> p s f", s=3)
    IOTA = wob[:, 3072:3648].bitcast(F32)
    RW = wob[:, 3648:3904].bitcast(F32).rearrange("p (k e) -> p k e", k=8)
    UTRI = wob[:, 3904:4032]
    ONESB = wob[:, 4032:4160]
    MASKB = wob[:, 4160:4448].rearrange("p (c e) -> p c e", c=NCH)
    SGR = [wob[:, 4448:5024].bitcast(F32), wob[:, 6912:7488].bitcast(F32)]
    UTF = wob[:, 5024:5280].bitcast(F32)
    WEX = ropet[:].rearrange("p c f -> p (c f)")
    AFF = hb[0][:].bitcast(F32)[:, 0:288].rearrange("p (c e) -> p c e", c=NCH)
    MASK = hb[1][:].bitcast(F32)[:, 0:288].rearrange("p (c e) -> p c e", c=NCH)
    mo2 = sb("mo2", [128, 2, NCH * NE])
    SLOT = mo2[:, 0, :].rearrange("p (c e) -> p c e", c=NCH)
    GS = mo2[:, 1, :].rearrange("p (c e) -> p c e", c=NCH)
    m8 = sb("m8", [16, 8])

    def moe_layer(l, nch, final_out=False):
        ns = 256 + (32 if nch > NLAT else 0)
        nsc = (ns + 127) // 128
        scsz = [min(128, ns - s * 128) for s in range(nsc)]
        barrier()
        DMA("sp", IOTA, iota_d, (), ["IOTA"], "c3")
        DMA("sp", UTF, utri_d, (), ["UTF"], "c3")
        DMA("sp", RW, router_d[l].rearrange("(k p) e -> p k e", p=128), (), ["RW"], "c3")
        fence(["IOTA", "UTF", "RW"])
        CP("dve", UTRI, UTF, ["UTF"], ["UTRI"])
        MEMSET("dve", ONESB, 1.0, ["ONESB"])
        bcast_mod(0, l, 0, 4, True)
        bcast_mod(1, l, 0, 3)
        if nch > NLAT:
            bcast_mod(2, l, 1, 4, True)
            bcast_mod(3, l, 1, 3)
        for c in range(nch):
            a, b_ = (0, 1) if c < NLAT else (2, 3)
            TT("dve", bc[4], X[:, c, :], bc[a], ALU.mult, [f"X{c}", f"bc{a}"], ["bc4"])
            TT("dve", bc[4], bc[4], bc[b_], ALU.add, ["bc4", f"bc{b_}"], ["bc4"])
            CP("act", hmb[:, c, :], bc[4], ["bc4"], [f"hmb{c}"])
            TS("dve", X[:, c, :], X[:, c, :], ALPHA, None, ALU.mult, None, [f"X{c}"], [f"X{c}"])
            for k in range(8):
                pb = k // 4
                TR(PS[pb][:, (k % 4) * 128:(k % 4 + 1) * 128], bc[4][:, k * 128:(k + 1) * 128], identf[:],
                   ["bc4", "identf"], [f"ps{pb}"])
            hmT = bc[5].rearrange("p (k t) -> p k t", k=8)
            CP("act", hmT[:, 0:4, :], PS[0][:, :].rearrange("p (k t) -> p k t", k=4), ["ps0"], ["bc5"])
            CP("dve", hmT[:, 4:8, :], PS[1][:, :].rearrange("p (k t) -> p k t", k=4), ["ps1"], ["bc5"])
            for k in range(8):
                MM(PS[2][:, 0:NE], hmT[:, k, :], RW[:, k, :], k == 0, k == 7, ["bc5", "RW"], ["ps2"])
            mx, mxk = stat()
            S.add("dve", lambda e, mx=mx: e.tensor_reduce(mx, PS[2][:, 0:NE], AX.X, ALU.max), reads=["ps2"], writes=[mxk])
            nmx, nmk = stat()
            TS("dve", nmx, mx, -1.0, None, ALU.mult, None, [mxk], [nmk])
            ACT(AFF[:, c, :], PS[2][:, 0:NE], AF.Exp, ["ps2", nmk], [f"AFF{c}"], bias=nmx)
            sm, smk = stat()
            S.add("dve", lambda e, sm=sm, c=c: e.tensor_reduce(sm, AFF[:, c, :], AX.X, ALU.add), reads=[f"AFF{c}"], writes=[smk])
            rc, rck = stat()
            S.add("dve", lambda e, rc=rc, sm=sm: e.reciprocal(rc, sm), reads=[smk], writes=[rck])
            TS("dve", AFF[:, c, :], AFF[:, c, :], rc, None, ALU.mult, None, [f"AFF{c}", rck], [f"AFF{c}"])
            TR(PS[3][0:NE, 0:128], AFF[:, c, :], identf[:], [f"AFF{c}", "identf"], ["ps3"])
            CP("dve", WEX[0:NE, c * 128:(c + 1) * 128], PS[3][0:NE, 0:128], ["ps3"], ["WEX"])
        sets = [(0, SEQ, 256)] + ([(SEQ, NT, 32)] if nch > NLAT else [])
        for (t0, t1, cap) in sets:
            wv = WEX[0:NE, t0:t1]
            for r in range(cap // 8):
                S.add("dve", lambda e, wv=wv: e.max(m8[:], wv), reads=["WEX"], writes=["m8"])
                S.add("dve", lambda e, wv=wv: e.match_replace(wv, m8[:], wv, -1.0), reads=["WEX", "m8"], writes=["WEX"])
        TS("dve", WEX[0:NE, 0:nch * 128], WEX[0:NE, 0:nch * 128], 0.0, None, ALU.is_lt, None, ["WEX"], ["WEX"])
        for c in range(nch):
            TR(PS[4][:, c * NE:(c + 1) * NE], WEX[0:NE, c * 128:(c + 1) * 128], identf[0:NE, 0:NE], ["WEX", "identf"], ["ps4"])
        mflat = mo2[:, 0, 0:nch * NE]
        CP("dve", MASK[:, 0:nch, :], PS[4][:, 0:nch * NE].rearrange("p (c e) -> p c e", c=nch), ["ps4"], ["MASK"])
        CP("dve", MASKB[:, 0:nch, :], MASK[:, 0:nch, :], ["MASK"], ["MASKB"])
        TT("dve", GS[:, 0:nch, :], AFF[:, 0:nch, :], MASK[:, 0:nch, :], ALU.mult, [f"AFF{c}" for c in range(nch)] + ["MASK"], ["GS"])
        for c in range(nch):
            c0 = 0 if c < NLAT else NLAT
            prev = list(range(c0, c))
            for i, cp in enumerate(prev):
                MM(PS[5][:, c * NE:(c + 1) * NE], ONESB, MASKB[:, cp, :], i == 0, False, ["ONESB", "MASKB"], ["ps5"])
            MM(PS[5][:, c * NE:(c + 1) * NE], UTRI, MASKB[:, c, :], len(prev) == 0, True, ["UTRI", "MASKB"], ["ps5"])
        TS("dve", SLOT[:, 0:NLAT, :], PS[5][:, 0:NLAT * NE].rearrange("p (c e) -> p c e", c=NLAT), 1.0, None, ALU.add, None, ["ps5"], ["SLOT"])
        if nch > NLAT:
            TS("dve", SLOT[:, NLAT:nch, :], PS[5][:, NLAT * NE:nch * NE].rearrange("p (c e) -> p c e", c=nch - NLAT), 257.0, None,
               ALU.add, None, ["ps5"], ["SLOT"])
        TT("dve", SLOT[:, 0:nch, :], SLOT[:, 0:nch, :], MASK[:, 0:nch, :], ALU.mult, ["SLOT", "MASK"], ["SLOT"])
        TS("dve", SLOT[:, 0:nch, :], SLOT[:, 0:nch, :], -1.0, None, ALU.add, None, ["SLOT"], ["SLOT"])
        bcast_mod(0, l, 0, 5)
        if nch > NLAT:
            bcast_mod(1, l, 1, 5)
        bcast_row(2, ln_g_d[l, 1:2, :])
        bcast_row(3, ln_b_d[l, 1:2, :])
        units = []
        for e_ in range(NEX):
            for fu in range(8):
                units.append(("g", e_, fu))
                units.append(("u", e_, fu))
            for du in range(8):
                units.append(("d", e_, du))
        issued = [0]

        def issue_until(n):
            while issued[0] < min(n, len(units)):
                kind, e_, i = units[issued[0]]
                slot = issued[0] % NRING
                if kind == "d":
                    src = wd_d[l, e_, i * 256:(i + 1) * 256, :].rearrange("(j p) c -> p j c", p=128)
                    dst = RING[slot].rearrange("p (j c) -> p j c", j=2)
                else:
                    wsrc = wg_d if kind == "g" else wu_d
                    src = wsrc[l, e_, :, i * 256:(i + 1) * 256].rearrange("(k p) c -> p k c", p=128)
                    dst = RING[slot].rearrange("p (k c) -> p k c", k=8)
                DMA("pool", dst, src, (), [f"wr{slot}"], f"wr{slot}")
                issued[0] += 1

        ui = [0]

        def next_unit():
            i = ui[0]
            ui[0] += 1
            return i % NRING

        def refill():
            issue_until(ui[0] + NRING)

        issue_until(NRING)
        seln = [0]
        sgn = [0]
        for e_ in range(NEX):
            for half in range(2):
                for c in range(nch):
                    si = seln[0] % NSEL
                    seln[0] += 1
                    s0, s1 = (0, 256) if c < NLAT else (256, ns)
                    cfirst, clast = (0, NLAT - 1) if c < NLAT else (NLAT, nch - 1)
                    TS("dve", SELR[si][:, s0:s1], IOTA[:, s0:s1], SLOT[:, c, e_:e_ + 1], None, ALU.is_equal, None,
                       ["IOTA", "SLOT"], [f"SEL{si}"])
                    for f4 in range(4):
                        f = half * 4 + f4
                        MM(PS[f4][:, s0:s1], hmb[:, c, f * 128:(f + 1) * 128], SELR[si][:, s0:s1], c == cfirst, c == clast,
                           [f"hmb{c}", f"SEL{si}"], [f"ps{f4}"])
                for f4 in range(4):
                    f = half * 4 + f4
                    CP("act" if f4 % 2 == 0 else "dve", XGT[:, f, 0:ns], PS[f4][:, 0:ns], [f"ps{f4}"], ["XGT"])
            for fu in range(8):
                sg_ = next_unit()
                su_ = next_unit()
                wgv = RING[sg_].rearrange("p (k c) -> p k c", k=8)
                wuv = RING[su_].rearrange("p (k c) -> p k c", k=8)
                for j2 in range(2):
                    jg = fu * 2 + j2
                    bg, bu = (4, 5) if jg % 2 == 0 else (6, 7)
                    for k in range(8):
                        MM(PS[bg][:, 0:ns], wgv[:, k, j2 * 128:(j2 + 1) * 128], XGT[:, k, 0:ns], k == 0, k == 7,
                           [f"wr{sg_}", "XGT"], [f"ps{bg}"])
                    for k in range(8):
                        MM(PS[bu][:, 0:ns], wuv[:, k, j2 * 128:(j2 + 1) * 128], XGT[:, k, 0:ns], k == 0, k == 7,
                           [f"wr{su_}", "XGT"], [f"ps{bu}"])
                    SG = SGR[jg % 2]
                    ACT(SG[:, 0:ns], PS[bg][:, 0:ns], AF.Silu, [f"ps{bg}"], [f"SG{jg % 2}"])
                    TT("dve", HIDT[:, jg, 0:ns], PS[bu][:, 0:ns], SG[:, 0:ns], ALU.mult, [f"ps{bu}", f"SG{jg % 2}"], [f"HID{jg}"])
                refill()
            for du in range(8):
                sd_ = next_unit()
                wdv = RING[sd_].rearrange("p (j c) -> p j c", j=2)
                for sc in range(nsc):
                    sz = scsz[sc]
                    for hf in range(2):
                        pb = sc * 2 + hf
                        for jj in range(2):
                            jg = du * 2 + jj
                            MM(PS[pb][0:sz, :], HIDT[:, jg, sc * 128:sc * 128 + sz], wdv[:, jj, hf * 512:(hf + 1) * 512],
                               du == 0 and jj == 0, du == 7 and jj == 1, [f"HID{jg}", f"wr{sd_}"], [f"ps{pb}"])
                refill()
            for sc in range(nsc):
                sz = scsz[sc]
                gt = 0 if sc < 2 else 1
                for hf in range(2):
                    pb = sc * 2 + hf
                    TT("dve", YG[0:sz, sc, hf * 512:(hf + 1) * 512], PS[pb][0:sz, :], bc[gt][0:sz, hf * 512:(hf + 1) * 512], ALU.mult,
                       [f"ps{pb}", f"bc{gt}"], ["YG"])
            if stage == 31:
                d1 = dram("d_slot", [128, 2 * NCH * NE], kind="ExternalOutput")
                DMA("sp", d1, mo2[:].rearrange("p a f -> p (a f)"), ["SLOT", "GS"], ["dbgd"], "dbg")
                d2 = dram("d_xgt", [128, 8 * 288], BF16, kind="ExternalOutput")
                DMA("sp", d2, XGT.rearrange("p k s -> p (k s)"), ["XGT"], ["dbgd"], "dbg")
                d3 = dram("d_yg", [128, 3 * 1024], BF16, kind="ExternalOutput")
                DMA("sp", d3, YG.rearrange("p s f -> p (s f)"), ["YG"], ["dbgd"], "dbg")
                d4 = dram("d_hid", [128, 16 * 288], BF16, kind="ExternalOutput")
                DMA("sp", d4, HIDT.rearrange("p j s -> p (j s)"), [f"HID{j}" for j in range(16)], ["dbgd"], "dbg")
                S.add("sp", None, reads=list(S.lw.keys()) + ["dbgd"], writes=())
                S.emit(nc, es)
                es.close()
                return "STOP"
            def sc_stage_a(c):
                si = seln[0] % NSEL
                seln[0] += 1
                s0, s1 = (0, 256) if c < NLAT else (256, ns)
                scs_ = [0, 1] if c < NLAT else [2]
                TS("dve", SELR[si][:, s0:s1], IOTA[:, s0:s1], SLOT[:, c, e_:e_ + 1], GS[:, c, e_:e_ + 1], ALU.is_equal, ALU.mult,
                   ["IOTA", "SLOT", "GS"], [f"SEL{si}"])
                tb = 6 + (c % 2)
                for sc in scs_:
                    sz = scsz[sc]
                    TR(PSB[tb][0:sz, sc * 128:(sc + 1) * 128], SELR[si][:, sc * 128:sc * 128 + sz], identb[:],
                       [f"SEL{si}", "identb"], [f"ps{tb}"])
                gi_ = sgn[0] % NSGT
                sgn[0] += 1
                sgt = SELGT[gi_]
                if c < NLAT:
                    CP("act", sgt[:, 0:2, :], PSB[tb][:, 0:256].rearrange("p (s t) -> p s t", s=2), [f"ps{tb}"], [f"SELGT{gi_}"])
                else:
                    CP("act", sgt[0:scsz[2], 2, :], PSB[tb][0:scsz[2], 256:384], [f"ps{tb}"], [f"SELGT{gi_}"])
                return gi_

            def sc_stage_b(c, gi_):
                sgt = SELGT[gi_]
                scs_ = [0, 1] if c < NLAT else [2]
                for hf in range(2):
                    pb = 2 * (c % 2) + hf
                    for sc in scs_:
                        sz = scsz[sc]
                        MM(PS[pb][:, :], sgt[0:sz, sc, :], YG[0:sz, sc, hf * 512:(hf + 1) * 512], sc == scs_[0], sc == scs_[-1],
                           [f"SELGT{gi_}", "YG"], [f"ps{pb}"])
                    TT("dve", X[:, c, hf * 512:(hf + 1) * 512], PS[pb][:, :], X[:, c, hf * 512:(hf + 1) * 512], ALU.add,
                       [f"ps{pb}", f"X{c}"], [f"X{c}"])

            prev = None
            for c in range(nch):
                g_ = sc_stage_a(c)
                if prev is not None:
                    sc_stage_b(*prev)
                prev = (c, g_)
            sc_stage_b(*prev)
        for c in range(nch):
            layer_norm_chunk(c, 2, 3)
            if final_out:
                DMA("sp", out_d[c * 128:(c + 1) * 128, :], X[:, c, :], [f"X{c}"], ["outd"], "outw")
        barrier()

    if moe_layer(0, NCH) == "STOP":
        return nc

    if stage == 3:
        for c in range(NCH):
            DMA("sp", dbg_d[c * 128:(c + 1) * 128, :], X[:, c, :], [f"X{c}"], ["dbgd"], "dbg")
        S.add("sp", None, reads=list(S.lw.keys()) + ["dbgd"], writes=())
        S.emit(nc, es)
        es.close()
        return nc

    w_in1_d = dram("l1_w_in", [D, 2048])
    w_out1_d = dram("l1_w_out", [D, D])
    poolw_d = dram("l1_pool_w", [4, 128, 128])
    psc_d = dram("psc", [128, 4])
    band_d = dram("band", [20, 128, 128])
    l1tab_d = dram("l1tab", [NPAT, 8, 128, 128])

    def layer1_mixer():
        l = 1
        barrier()
        DMA("pool", WO[:], w_out1_d.rearrange("(k p) c -> p k c", p=128), (), ["WO"], "wo")
        modulate_T(1, 1, 0, NCH)
        spill_X(NLAT)
        barrier()
        arena_off[0] = 0
        W1R = [xview(2048, None, BF16, "p (k c) -> p k c", k=8) for _ in range(2)]
        QT1 = xview(4 * SEQ // 2, None, BF16, "p (g t) -> p g t", g=4)
        KT1 = xview(4 * NT // 2, None, BF16, "p (g t) -> p g t", g=4)
        VXU = xview(NCH * 8 * 65 // 2, None, BF16)
        VX1 = VXU.rearrange("p (c h f) -> p c h f", c=NCH, h=8)
        U1 = VXU[:, 0:NLAT * 512].rearrange("p (c f) -> p c f", c=NLAT)
        PT1 = [xview(256, None, BF16) for _ in range(3)]
        rp = ropet[:].rearrange("p c f -> p (c f)")
        TBI = rp[:, 0:1280].bitcast(BF16).rearrange("p (a h k) -> p a h k", a=5, h=4)
        TBB = rp[:, 1280:2304].bitcast(BF16).rearrange("p (a h k) -> p a h k", a=4, h=4)
        BAND = hb[0][:].rearrange("p (m t) -> p m t", m=8)
        PW = hb[1][:, 0:512].rearrange("p (g e) -> p g e", g=4)
        AOK1 = hb[1][:, 512:768]
        POOLT = hb[1][:, 768:1024].bitcast(F32) if False else None
        psc = sb("pscs", [128, 4])
        qflat = QT1.rearrange("p g t -> p (g t)")
        bandb = qflat[:, 0:2560].rearrange("p (m t) -> p m t", m=20)
        poolt = [qflat[:, 2560 + i * 512:2560 + (i + 1) * 512] for i in range(2)]
        DMA("sp", psc[:], psc_d, (), ["psc"], "c4")
        DMA("pool", bandb, band_d.rearrange("m a b -> a m b"), (), ["bandb"], "c4p")
        DMA("pool", PW, poolw_d.rearrange("g c e -> c g e"), (), ["PW"], "c4p")
        fence(["psc", "bandb", "PW"])

        def load_w1(sec, slot):
            DMA("pool", W1R[slot][:], w_in1_d[:, sec * 512:(sec + 1) * 512].rearrange("(k p) c -> p k c", p=128),
                (), [f"W1R{slot}"], f"w1r{slot}")

        load_w1(3, 0)
        load_w1(0, 1)
        for c in range(NLAT):
            pb = 4 + (c % 2)
            for k in range(8):
                MM(PS[pb][:, :], hT[:, k, c * 128:(c + 1) * 128], W1R[0][:, k, :], k == 0, k == 7, [f"hT{c}", "W1R0"], [f"ps{pb}"])
            CP("act" if c % 2 == 0 else "dve", U1[:, c, :], PS[pb][:, :], [f"ps{pb}"], [f"U1{c}"])
        pn = [0]
        for g in range(4):
            for cb in range(4):
                pb = 6 + (pn[0] % 2)
                for j in range(4):
                    c = cb * 4 + j
                    srcs = []
                    if c > 0:
                        srcs.append((c - 1, g * 5 + 0))
                    srcs.append((c, g * 5 + (3 if c == 0 else 4 if c == NLAT - 1 else 1)))
                    if c < NLAT - 1:
                        srcs.append((c + 1, g * 5 + 2))
                    for i, (cs, m) in enumerate(srcs):
                        MM(PS[pb][:, j * 128:(j + 1) * 128], U1[:, cs, g * 128:(g + 1) * 128], bandb[:, m, :], i == 0, i == len(srcs) - 1,
                           [f"U1{cs}", "bandb"], [f"ps{pb}"])
                pt_ = poolt[pn[0] % 2]
                CP("act", pt_, PS[pb][:, :], [f"ps{pb}"], [f"poolt{pn[0] % 2}"])
                pb2 = 4 + (pn[0] % 2)
                MM(PS[pb2][:, :], PW[:, g, :], pt_, True, True, ["PW", f"poolt{pn[0] % 2}"], [f"ps{pb2}"])
                TS("dve", AOT[:, 4 + g, cb * 512:(cb + 1) * 512], PS[pb2][:, :], psc[:, g:g + 1], None, ALU.mult, None,
                   [f"ps{pb2}", "psc"], [f"AOT{cb * 4 + j}" for j in range(4)])
                pn[0] += 1
        barrier()
        MEMSET("dve", VX1[:, :, :, 64:65], 1.0, [f"VX{c}" for c in range(NCH)])
        for g in range(4):
            for tb_ in range(4):
                pb = 4 + ((g * 4 + tb_) % 2)
                for k in range(8):
                    MM(PS[pb][:, :], W1R[1][:, k, g * 128:(g + 1) * 128], hT[:, k, tb_ * 512:(tb_ + 1) * 512], k == 0, k == 7,
                       ["W1R1"] + [f"hT{tb_ * 4 + j}" for j in range(4)], [f"ps{pb}"])
                ACT(QT1[:, g, tb_ * 512:(tb_ + 1) * 512], PS[pb][:, :], AF.Identity, [f"ps{pb}"], [f"QT{tb_ * 4 + j}" for j in range(4)], scale=0.125)
        load_w1(1, 0)
        load_w1(2, 1)
        blocks_k = [(i * 512, 512) for i in range(4)] + [(2048, 256)]
        for g in range(4):
            for bi, (t0, tn) in enumerate(blocks_k):
                pb = 4 + ((g * 5 + bi) % 2)
                chs = list(range(t0 // 128, (t0 + tn) // 128))
                for k in range(8):
                    MM(PS[pb][:, 0:tn], W1R[0][:, k, g * 128:(g + 1) * 128], hT[:, k, t0:t0 + tn], k == 0, k == 7,
                       ["W1R0"] + [f"hT{c}" for c in chs], [f"ps{pb}"])
                CP("act" if bi % 2 == 0 else "dve", KT1[:, g, t0:t0 + tn], PS[pb][:, 0:tn], [f"ps{pb}"], [f"KT{c}" for c in chs])
        for c in range(NCH):
            pb = 6 + (c % 2)
            for k in range(8):
                MM(PS[pb][:, :], hT[:, k, c * 128:(c + 1) * 128], W1R[1][:, k, :], k == 0, k == 7, [f"hT{c}", "W1R1"], [f"ps{pb}"])
            CP("act" if c % 2 == 0 else "dve", VX1[:, c, :, 0:64], PS[pb][:, :].rearrange("p (h f) -> p h f", h=8), [f"ps{pb}"], [f"VX{c}"])
        ptn = [0]
        for hh in range(2):
            for a in range(5):
                DMA("pool", TBI[:, a, :, :], l1tab_d[INT_PATS[a], hh * 4:(hh + 1) * 4].rearrange("h q k -> q h k"), (), ["TBI"], "tbi")
            for c in range(NLAT):
                loc = L1_LOCAL[c]
                interior = (2 <= c <= 13)
                if not interior:
                    for a, (kc, pat) in enumerate(loc):
                        DMA("pool", TBB[:, a, :, :], l1tab_d[pat, hh * 4:(hh + 1) * 4].rearrange("h q k -> q h k"), (), ["TBB"], "tbb")
                kcs = [(kc, a) for a, (kc, pat) in enumerate(loc)] + [(16, None), (17, None)]
                def score1(kn):
                    kc, a = kcs[kn]
                    bpair = (4, 5) if ptn[0] % 2 == 0 else (6, 7)
                    pi = ptn[0] % 3
                    ptn[0] += 1
                    for par in range(2):
                        bk = bpair[par]
                        for i2, h4 in enumerate((par, par + 2)):
                            h = hh * 4 + h4
                            g = h // 2
                            pbase = (h % 2) * 64
                            MM(PS[bk][:, i2 * 128:(i2 + 1) * 128], KT1[pbase:pbase + 64, g, kc * 128:(kc + 1) * 128],
                               QT1[pbase:pbase + 64, g, c * 128:(c + 1) * 128], True, a is None, [f"KT{kc}", f"QT{c}"], [f"ps{bk}"])
                            if a is not None:
                                tb_ap = TBI[:, a, h4, :] if interior else TBB[:, a, h4, :]
                                MM(PS[bk][:, i2 * 128:(i2 + 1) * 128], tb_ap, identb[:], False, True,
                                   ["TBI" if interior else "TBB", "identb"], [f"ps{bk}"])
                    return bpair, pi

                pend = score1(0)
                for kn, (kc, a) in enumerate(kcs):
                    nxt = score1(kn + 1) if kn + 1 < len(kcs) else None
                    bpair, pi = pend
                    for par in range(2):
                        bk = bpair[par]
                        ACT(PT1[pi][:, par * 256:(par + 1) * 256], PS[bk][:, 0:256], AF.Exp, [f"ps{bk}"], [f"PT{pi}_{par}"])
                    for h4 in range(4):
                        h = hh * 4 + h4
                        par, i2 = h4 % 2, h4 // 2
                        o_ = par * 256 + i2 * 128
                        MM(PS[h4][:, 0:65], PT1[pi][:, o_:o_ + 128], VX1[:, kc, h, :], kn == 0, kn == len(kcs) - 1,
                           [f"PT{pi}_{par}", f"VX{kc}"], [f"ps{h4}"])
                    pend = nxt
                for h4 in range(4):
                    rz, rk = stat()
                    S.add("dve", lambda e, rz=rz, h4=h4: e.reciprocal(rz, PS[h4][:, 64:65]), reads=[f"ps{h4}"], writes=[rk])
                    TS("dve", AOK1[:, h4 * 64:(h4 + 1) * 64], PS[h4][:, 0:64], rz, None, ALU.mult, None, [f"ps{h4}", rk], ["AOK1"])
                tb = 6 + (c % 2)
                for t in range(2):
                    TR(PSB[tb][:, t * 128:(t + 1) * 128], AOK1[:, t * 128:(t + 1) * 128], identb[:], ["AOK1", "identb"], [f"ps{tb}"])
                CP("act", AOT[:, 2 * hh:2 * hh + 2, c * 128:(c + 1) * 128], PSB[tb][:, 0:256].rearrange("p (s t) -> p s t", s=2),
                   [f"ps{tb}"], [f"AOT{c}"])
        barrier()
        reload_X(NLAT)
        outproj_ln(1, NLAT)

    layer1_mixer()
    if stage == 4:
        for c in range(NLAT):
            DMA("sp", dbg_d[c * 128:(c + 1) * 128, :], X[:, c, :], [f"X{c}"], ["dbgd"], "dbg")
        S.add("sp", None, reads=list(S.lw.keys()) + ["dbgd"], writes=())
        S.emit(nc, es)
        es.close()
        return nc
    moe_layer(1, NLAT, final_out=True)
    S.add("sp", None, reads=list(S.lw.keys()) + ["outd"], writes=())
    S.emit(nc, es)
    es.close()
    return nc


def _rope_table():
    t = np.arange(SEQ)
    inv = (10000.0 ** (-np.arange(16, dtype=np.float32) / 16)).astype(np.float32)
    ar = (t // 64).astype(np.float32)[:, None] * inv
    ac = (t % 64).astype(np.float32)[:, None] * inv
    cr, sr, cc_, sc_ = np.cos(ar), np.sin(ar), np.cos(ac), np.sin(ac)
    cosf = np.concatenate([cr, cr, cc_, cc_], axis=1)
    sins = np.concatenate([-sr, sr, -sc_, sc_], axis=1)
    tab = np.concatenate([cosf, sins], axis=1).astype(np.float32)
    ctxr = np.tile(np.array([1.0] * 64 + [0.0] * 64, np.float32), (CTX, 1))
    return np.concatenate([tab, ctxr], axis=0)


def make_in_maps(inp):
    f = lambda a: np.ascontiguousarray(np.asarray(a, dtype=np.float32))
    shared = {
        "ada_w": f(inp["ada_w"]), "ada_b": f(inp["ada_b"]), "ln_g": f(inp["ln_g"]), "ln_b": f(inp["ln_b"]),
        "l0_w_in": f(inp["l0_w_in"]), "l0_w_out": f(inp["l0_w_out"]),
        "lamv": f(np.stack([inp["l0_lam_q1"], inp["l0_lam_k1"], inp["l0_lam_q2"], inp["l0_lam_k2"]])),
        "l0_subln_g": f(inp["l0_subln_g"]),
        "qkg": f(np.concatenate([np.tile(np.asarray(inp["l0_qnorm_g"]), 4), np.asarray(inp["l0_knorm_g"])])),
        "rope": _rope_table(), "ident": np.eye(128, dtype=np.float32),
        "moe_router": f(inp["moe_router"]), "moe_w_gate": f(inp["moe_w_gate"]), "moe_w_up": f(inp["moe_w_up"]),
        "moe_w_down": f(inp["moe_w_down"]),
        "l1_w_in": f(inp["l1_w_in"]), "l1_w_out": f(inp["l1_w_out"]), "l1_pool_w": f(inp["l1_pool_w"]),
        "psc": f(np.asarray(inp["l1_pool_scale"]).reshape(4, 128).T), "band": _band_mats(),
        "l1tab": _l1_table(inp["l1_rpb"]),
        "iota288": np.tile(np.arange(288, dtype=np.float32), (128, 1)),
        "utri": np.triu(np.ones((128, 128), np.float32), 1),
    }
    maps = []
    x = np.asarray(inp["x"]); ctx = np.asarray(inp["ctx"]); c = np.asarray(inp["c"]); cctx = np.asarray(inp["c_ctx"])
    for b in range(8):
        m = dict(shared)
        m["x"] = f(np.concatenate([x[b], ctx[b]], axis=0))
        cc = np.stack([c[b].reshape(8, 128).T, cctx.reshape(8, 128).T], axis=-1)
        m["cc"] = f(cc)
        maps.append(m)
    return maps


def kernel(**inputs):
    nc = build()
    maps = make_in_maps(inputs)
    res = run_bass_kernel_spmd(nc, maps, core_ids=list(range(8)))
    return np.stack([r["out"] for r in res.results], axis=0)
```

```python
import math
import numpy as np
from contextlib import ExitStack
import concourse.bass as bass
import concourse.mybir as mybir
from concourse.bass_utils import run_bass_kernel_spmd

F32 = mybir.dt.float32
BF16 = mybir.dt.bfloat16
AF = mybir.ActivationFunctionType
ALU = mybir.AluOpType
AX = mybir.AxisListType

D = 1024
SEQ = 2048
CTX = 256
NT = SEQ + CTX
NCH = NT // 128
NLAT = SEQ // 128
NE = 16
FF = 2048
ALPHA = 4.0 ** 0.25
LN_EPS = 1e-5
RMS_EPS = 1e-6
LAM_INIT0 = 0.8 - 0.6 * math.exp(0.0)
NEG = -30000.0


class Sched:
    def __init__(self):
        self.ops = []
        self.lw = {}
        self.rd = {}

    def add(self, eng, fn, reads=(), writes=(), dma=None):
        i = len(self.ops)
        stream = ("dma", dma) if dma is not None else eng
        pk = [k for k in reads if k.startswith("ps") and k[2:].isdigit()]
        if pk:
            writes = list(writes) + [k for k in pk if k not in writes]
        raw = set()
        war = set()
        for k in reads:
            w = self.lw.get(k)
            if w is not None:
                raw.add(w)
        for k in writes:
            w = self.lw.get(k)
            if w is not None:
                raw.add(w)
            for r in self.rd.get(k, {}).values():
                war.add(r)
        if fn is not None:
            for k in writes:
                self.lw[k] = i
                self.rd[k] = {}
            for k in reads:
                self.rd.setdefault(k, {})[stream] = i
        self.ops.append(dict(eng=eng, fn=fn, raw=raw, war=war, dma=dma, stream=stream, cons=False, ms=0))
        return i

    def emit(self, nc, es):
        ops = self.ops
        for o in ops:
            for d in o["raw"] | o["war"]:
                if d == len(ops):
                    continue
                p = ops[d]
                same = (p["stream"] == o["stream"])
                if same and (o["stream"] == "pe"):
                    continue
                if same and d in o["war"] and d not in o["raw"]:
                    continue
                p["cons"] = True
        sems = {}
        cnt = {}

        def get_sem(stream):
            if stream not in sems:
                nm = "s_" + (stream if isinstance(stream, str) else "d_" + str(stream[1]))
                sems[stream] = es.enter_context(nc.semaphore(nm))
                cnt[stream] = 0
            return sems[stream]

        for o in ops:
            st = o["stream"]
            get_sem(st)
            if o["dma"] is not None:
                cnt[st] += 16
                o["ms"] = cnt[st]
            elif o["cons"]:
                cnt[st] += 1
                o["ms"] = cnt[st]
        self.nsem = len(sems)
        block = es.enter_context(nc.Block())
        engs = ["pe", "act", "dve", "pool", "sp"]
        per = {e: [o for o in ops if o["eng"] == e] for e in engs}

        def run(e, eh):
            known = {}
            for o in per[e]:
                need = {}
                for d in o["raw"] | o["war"]:
                    p = ops[d]
                    same = (p["stream"] == o["stream"])
                    if same and o["stream"] == "pe":
                        continue
                    if same and d in o["war"] and d not in o["raw"]:
                        continue
                    if p["ms"] <= 0:
                        continue
                    s = p["stream"]
                    if need.get(s, 0) < p["ms"]:
                        need[s] = p["ms"]
                for s, v in need.items():
                    if known.get(s, 0) < v:
                        eh.wait_ge(sems[s], v)
                        known[s] = v
                if o["fn"] is None:
                    continue
                ins = o["fn"](eh)
                if o["dma"] is not None:
                    ins.then_inc(sems[o["stream"]], 16)
                elif o["cons"]:
                    ins.then_inc(sems[o["stream"]], 1)

        @block.tensor
        def _(eh):
            run("pe", eh)

        @block.scalar
        def _(eh):
            run("act", eh)

        @block.vector
        def _(eh):
            run("dve", eh)

        @block.gpsimd
        def _(eh):
            run("pool", eh)

        @block.sync
        def _(eh):
            run("sp", eh)


def _l1_patterns():
    W, rows, kh, kw = 64, 32, 8, 16
    cols = np.arange(W)
    cs = np.clip(cols - kw // 2, 0, W - kw)
    colok = (cols[None, :] >= cs[:, None]) & (cols[None, :] < cs[:, None] + kw)
    dcol = cols[None, :] - cols[:, None] + 15
    pats, sigs, local = [], {}, []
    for c in range(16):
        lst = []
        for kc in range(16):
            valid = np.zeros((128, 128), bool)
            dr = np.zeros((128, 128), np.int64)
            dc = np.zeros((128, 128), np.int64)
            for ql in range(2):
                r = 2 * c + ql
                rs = min(max(r - kh // 2, 0), rows - kh)
                for kl in range(2):
                    rk = 2 * kc + kl
                    rowok = rs <= rk < rs + kh
                    qs = slice(ql * 64, ql * 64 + 64)
                    ks = slice(kl * 64, kl * 64 + 64)
                    valid[qs, ks] = colok & rowok
                    dr[qs, ks] = rk - r + 7
                    dc[qs, ks] = dcol
            if not valid.any():
                continue
            dr = np.where(valid, dr, 0)
            dc = np.where(valid, dc, 0)
            sig = (valid.tobytes(), dr.tobytes())
            if sig not in sigs:
                sigs[sig] = len(pats)
                pats.append((valid, dr, dc))
            lst.append((kc, sigs[sig]))
        local.append(lst)
    return pats, local


_L1_PATS, L1_LOCAL = _l1_patterns()
NPAT = len(_L1_PATS)
INT_PATS = [p for (_, p) in L1_LOCAL[2]]
for _c in range(2, 14):
    assert [p for (_, p) in L1_LOCAL[_c]] == INT_PATS and len(INT_PATS) == 5
for _c in (0, 1, 14, 15):
    assert len(L1_LOCAL[_c]) <= 4


def _l1_table(rpb):
    rpb = np.asarray(rpb, np.float32)
    tab = np.full((NPAT, 8, 128, 128), NEG, np.float32)
    for i, (valid, dr, dc) in enumerate(_L1_PATS):
        g = rpb[:, dr, dc]
        tab[i] = np.where(valid[None], g, np.float32(NEG))
    return tab


def _band_mats():
    out = np.zeros((20, 128, 128), np.float32)
    tp = np.arange(128)[:, None]
    t = np.arange(128)[None, :]
    for g, w in enumerate((2, 4, 8, 16)):
        half = w // 2
        cnt = float(2 * half)
        out[g * 5 + 0] = (tp >= t + 128 - half) / cnt
        out[g * 5 + 1] = ((tp >= t - half) & (tp < t + half)) / cnt - (tp == t)
        out[g * 5 + 2] = (tp < t + half - 128) / cnt
        lo = np.maximum(t - half, 0)
        hi = t + half
        out[g * 5 + 3] = ((tp >= lo) & (tp < hi)) / (hi - lo).astype(np.float32) - (tp == t)
        lo = t - half
        hi = np.minimum(t + half, 128)
        out[g * 5 + 4] = ((tp >= lo) & (tp < hi)) / (hi - lo).astype(np.float32) - (tp == t)
    return out


GSTOP = 0


def build(stage=99, dbg_cols=1024):
    nc = bass.Bass("TRN2", target_bir_lowering=False)
    S = Sched()
    es = ExitStack()

    def dram(name, shape, dt=F32, kind="ExternalInput"):
        return nc.dram_tensor(name, list(shape), dt, kind=kind).ap()

    x_d = dram("x", [NT, D])
    cc_d = dram("cc", [128, 8, 2])
    ada_w_d = dram("ada_w", [2, D, 6 * D])
    ada_b_d = dram("ada_b", [2, 6 * D])
    ln_g_d = dram("ln_g", [2, 2, D])
    ln_b_d = dram("ln_b", [2, 2, D])
    w_in0_d = dram("l0_w_in", [D, 2304])
    w_out0_d = dram("l0_w_out", [D, D])
    lam_d = dram("lamv", [4, 64])
    subg_d = dram("l0_subln_g", [128])
    qkg_d = dram("qkg", [5 * 64])
    rope_d = dram("rope", [NT, 128])
    ident_d = dram("ident", [128, 128])
    out_d = dram("out", [SEQ, D], kind="ExternalOutput")
    dbg_d = dram("dbg", [128 if stage == 1 else NT, dbg_cols], kind="ExternalOutput") if stage < 99 else None
    modd = dram("modd", [2, 2, 6 * D], kind="Internal")

    def sb(name, shape, dt=F32):
        return es.enter_context(nc.sbuf_tensor(name, list(shape), dt))

    Xraw = sb("X", [128, NCH * D])
    X = Xraw[:].rearrange("p (c f) -> p c f", c=NCH)
    hT = sb("hT", [128, 8, NT], BF16)
    AOT = sb("AOT", [128, 8, NT], BF16)
    WO = sb("WO", [128, 8, D], BF16)
    identb = sb("identb", [128, 128], BF16)
    identf = sb("identf", [128, 128])
    ropet = sb("ropet", [128, NCH, 128])
    big = sb("big", [128, 6 * 1024])
    bc = [big[:, i * 1024:(i + 1) * 1024] for i in range(6)]
    st = sb("st", [128, 64])
    PS = [es.enter_context(nc.psum_tensor(f"ps{i}", [128, 512], F32)) for i in range(8)]
    PSB = [p[:].bitcast(BF16) for p in PS]
    stn = [0]

    def stat():
        i = stn[0] % 64
        stn[0] += 1
        return st[:, i:i + 1], f"st{i}"

    arena_off = [0]

    def xview(nelem_f32, shape, dt=F32, pattern=None, **kw):
        a = arena_off[0]
        arena_off[0] += nelem_f32
        assert arena_off[0] <= NCH * D
        v = Xraw[:, a:a + nelem_f32]
        if dt != F32:
            v = v.bitcast(dt)
        if pattern is not None:
            v = v.rearrange(pattern, **kw)
        return v

    dkn = [0]

    def dk(prefix="d"):
        dkn[0] += 1
        return f"{prefix}{dkn[0]}"

    def DMA(q, out, in_, reads, writes, key):
        return S.add(q, lambda e: e.dma_start(out=out, in_=in_), reads=reads, writes=writes, dma=key)

    def MM(out, lhsT, rhs, start, stop, reads, writes):
        return S.add("pe", lambda e: e.matmul(out, lhsT, rhs, start=start, stop=stop), reads=reads, writes=writes)

    def TR(out, in_, ident, reads, writes):
        return S.add("pe", lambda e: e.transpose(out, in_, ident), reads=reads, writes=writes)

    def ACT(out, in_, func, reads, writes, bias=None, scale=None, accum_out=None):
        kw = {}
        if bias is not None:
            kw["bias"] = bias
        if scale is not None:
            kw["scale"] = scale
        if accum_out is not None:
            kw["accum_out"] = accum_out
        return S.add("act", lambda e: e.activation(out, in_, func, **kw), reads=reads, writes=writes)

    def TT(eng, out, in0, in1, op, reads, writes):
        return S.add(eng, lambda e: e.tensor_tensor(out, in0, in1, op), reads=reads, writes=writes)

    def TS(eng, out, in0, s1, s2, op0, op1, reads, writes):
        if op1 is None:
            return S.add(eng, lambda e: e.tensor_scalar(out, in0, s1, None, op0), reads=reads, writes=writes)
        return S.add(eng, lambda e: e.tensor_scalar(out, in0, s1, s2, op0, op1), reads=reads, writes=writes)

    def STT(eng, out, in0, scalar, in1, op0, op1, reads, writes):
        return S.add(eng, lambda e: e.scalar_tensor_tensor(out, in0, scalar, in1, op0, op1), reads=reads, writes=writes)

    def CP(eng, out, in_, reads, writes):
        if eng == "act":
            return S.add(eng, lambda e: e.copy(out, in_), reads=reads, writes=writes)
        return S.add(eng, lambda e: e.tensor_copy(out, in_), reads=reads, writes=writes)

    def MEMSET(eng, ap, val, writes):
        return S.add(eng, lambda e: e.memset(ap, val), reads=(), writes=writes)

    def fence(keys, engs=("pe", "act", "dve", "pool", "sp")):
        for e in engs:
            S.add(e, None, reads=keys, writes=())

    def barrier():
        allk = list(S.lw.keys())
        for e in ("pe", "act", "dve", "pool", "sp"):
            S.add(e, None, reads=allk, writes=allk)

    nc_lp = es.enter_context(nc.allow_low_precision("bf16 matmul operands, fp32 accumulation"))
    es.enter_context(nc.allow_non_contiguous_dma("small strided constant loads"))

    for c in range(NCH):
        DMA("sp", X[:, c, :], x_d[c * 128:(c + 1) * 128, :], (), [f"X{c}"], "ldx")
    ccs = sb("ccs", [128, 8, 2])
    DMA("sp", ccs[:], cc_d, (), ["ccs"], "const")
    DMA("sp", identf[:], ident_d, (), ["identf"], "const")
    DMA("sp", ropet[:], rope_d.rearrange("(c p) f -> p c f", p=128), (), ["ropet"], "const")
    fence(["ccs", "identf", "ropet"])
    CP("dve", identb[:], identf[:], ["identf"], ["identb"])

    scs = sb("scs", [128, 8, 2])
    S.add("act", lambda e: e.activation(scs[:], ccs[:], AF.Silu), reads=["ccs"], writes=["scs"])
    adab = sb("adab", [2, 512])
    modrow = sb("modrow", [2, 512])
    NST = 3
    aotf = AOT[:].rearrange("p k t -> p (k t)").bitcast(F32)
    hTf = hT[:].rearrange("p k t -> p (k t)").bitcast(F32)
    stg = [aotf[:, 0:4096], aotf[:, 4096:8192], hTf[:, 0:4096]]
    gi = 0
    for l in range(2):
        for j in range(12):
            s = gi % NST
            st3 = stg[s].rearrange("p (k c) -> p k c", k=8)
            DMA("pool", st3, ada_w_d[l, :, j * 512:(j + 1) * 512].rearrange("(k p) c -> p k c", p=128),
                (), [f"stg{s}"], f"adaw{s}")
            for r in range(2):
                DMA("sp", adab[r:r + 1, :], ada_b_d[l:l + 1, j * 512:(j + 1) * 512], (), ["adab"], "adab")
            pb = gi % 2
            for k in range(8):
                MM(PS[pb][0:2, :], scs[:, k, :], st3[:, k, :], k == 0, k == 7,
                   ["scs", f"stg{s}"], [f"ps{pb}"])
            TT("dve", modrow[:], PS[pb][0:2, :], adab[:], ALU.add, [f"ps{pb}", "adab"], ["modrow"])
            DMA("sp", modd[l, :, j * 512:(j + 1) * 512], modrow[:], ["modrow"], ["modd"], "modw")
            gi += 1
    barrier()

    def dbg_out(ap, cols):
        tmpd = sb("tmpd", [128, cols])
        CP("dve", tmpd[:], ap, list(S.lw.keys()), ["tmpd"])
        DMA("sp", dbg_d[:, 0:cols], tmpd[:], ["tmpd"], ["dbgd"], "dbg")

    if stage == 1:
        t1 = sb("t1", [128, 192])
        DMA("sp", t1[:], modd.rearrange("l r (a b) -> (l r a) b", b=192), ["modd"], ["t1"], "t1")
        dbg_out(t1[:], 192)
        S.add("sp", None, reads=list(S.lw.keys()), writes=())
        S.emit(nc, es)
        es.close()
        return nc

    xscr = dram("xscr", [NT, D], kind="Internal")

    def bcast_mod(i, l, r, idx, plus1=False):
        src = modd[l, r:r + 1, idx * 1024:(idx + 1) * 1024].partition_broadcast(128)
        DMA("sp", bc[i].rearrange("p (o f) -> p o f", o=1), src, ["modd"], [f"bc{i}"], f"bcl{i}")
        if plus1:
            TS("dve", bc[i], bc[i], 1.0, None, ALU.add, None, [f"bc{i}"], [f"bc{i}"])

    def bcast_row(i, row_ap):
        DMA("sp", bc[i].rearrange("p (o f) -> p o f", o=1), row_ap.partition_broadcast(128), (), [f"bc{i}"], f"bcl{i}")

    hb = [sb(f"hb{i}", [128, D], BF16) for i in range(2)]

    def modulate_T(l, isc, ish, nch):
        bcast_mod(0, l, 0, isc, True)
        bcast_mod(1, l, 0, ish)
        if nch > NLAT:
            bcast_mod(2, l, 1, isc, True)
            bcast_mod(3, l, 1, ish)
        for c in range(nch):
            a, b_ = (0, 1) if c < NLAT else (2, 3)
            t = 4 + (c % 2)
            TT("dve", bc[t], X[:, c, :], bc[a], ALU.mult, [f"X{c}", f"bc{a}"], [f"bc{t}"])
            TT("dve", hb[c % 2][:], bc[t], bc[b_], ALU.add, [f"bc{t}", f"bc{b_}"], [f"hb{c % 2}"])
            pb = 4 + (c % 2)
            for k in range(8):
                TR(PSB[pb][:, k * 128:(k + 1) * 128], hb[c % 2][:, k * 128:(k + 1) * 128], identb[:],
                   [f"hb{c % 2}", "identb"], [f"ps{pb}"])
            CP("act", hT[:, :, c * 128:(c + 1) * 128], PSB[pb].rearrange("p (k t) -> p k t", k=8),
               [f"ps{pb}"], [f"hT{c}"])

    def spill_X(nch):
        for c in range(nch):
            DMA("sp", xscr[c * 128:(c + 1) * 128, :], X[:, c, :], [f"X{c}"], ["xscr"], "xsp")

    def reload_X(nch):
        for c in range(nch):
            DMA("sp", X[:, c, :], xscr[c * 128:(c + 1) * 128, :], ["xscr"], [f"X{c}"], "xrl")

    def rstd_from(out_ap, okey, in_ap, ikey, scale, eps):
        tmp, tk = stat()
        ACT(tmp, in_ap, AF.Ln, [ikey], [tk], bias=epst[eps], scale=scale)
        ACT(out_ap, tmp, AF.Exp, [tk], [okey], scale=-0.5)

    epsv = sb("epsv", [128, 2])
    MEMSET("dve", epsv[:, 0:1], LN_EPS, ["epsv"])
    MEMSET("dve", epsv[:, 1:2], RMS_EPS, ["epsv"])
    fence(["epsv"], ("act",))
    epst = {LN_EPS: epsv[:, 0:1], RMS_EPS: epsv[:, 1:2]}

    def layer_norm_chunk(c, ig, ib):
        stt = sb(f"lnst{c}", [128, 2, 6]) if False else lnstat
        S.add("dve", lambda e: e.bn_stats(stt[:, 0, :], X[:, c, 0:512]), reads=[f"X{c}"], writes=["lnst0"])
        S.add("dve", lambda e: e.bn_stats(stt[:, 1, :], X[:, c, 512:1024]), reads=[f"X{c}"], writes=["lnst1"])
        mv, mk = lnmv, "lnmv"
        S.add("dve", lambda e: e.bn_aggr(mv[:], stt[:]), reads=["lnst0", "lnst1"], writes=[mk])
        rs, rk = stat()
        rstd_from(rs, rk, mv[:, 1:2], mk, 1.0, LN_EPS)
        nm, nk = stat()
        TS("dve", nm, mv[:, 0:1], rs, -1.0, ALU.mult, ALU.mult, [mk, rk], [nk])
        t = 4 + (c % 2)
        ACT(bc[t], X[:, c, :], AF.Identity, [f"X{c}", rk, nk], [f"bc{t}"], bias=nm, scale=rs)
        TT("dve", bc[t], bc[t], bc[ig], ALU.mult, [f"bc{t}", f"bc{ig}"], [f"bc{t}"])
        TT("dve", X[:, c, :], bc[t], bc[ib], ALU.add, [f"bc{t}", f"bc{ib}"], [f"X{c}"])

    lnstat = sb("lnstat", [128, 2, 6])
    lnmv = sb("lnmv", [128, 2])


    def outproj_ln(l, nch):
        bcast_mod(0, l, 0, 2)
        if nch > NLAT:
            bcast_mod(1, l, 1, 2)
        bcast_row(2, ln_g_d[l, 0:1, :])
        bcast_row(3, ln_b_d[l, 0:1, :])
        for c in range(nch):
            gtile = 0 if c < NLAT else 1
            t = 4 + (c % 2)
            for hf in range(2):
                pb = 2 * (c % 2) + hf
                for m in range(8):
                    MM(PS[pb][:, :], AOT[:, m, c * 128:(c + 1) * 128], WO[:, m, hf * 512:(hf + 1) * 512], m == 0, m == 7,
                       [f"AOT{c}", "WO"], [f"ps{pb}"])
                TT("dve", bc[t][:, hf * 512:(hf + 1) * 512], PS[pb][:, :], bc[gtile][:, hf * 512:(hf + 1) * 512], ALU.mult,
                   [f"ps{pb}", f"bc{gtile}"], [f"bc{t}"])
            STT("dve", X[:, c, :], X[:, c, :], ALPHA, bc[t], ALU.mult, ALU.add, [f"X{c}", f"bc{t}"], [f"X{c}"])
            layer_norm_chunk(c, 2, 3)
        barrier()

    if True:
        l = 0
        DMA("pool", WO[:], w_out0_d.rearrange("(k p) c -> p k c", p=128), (), ["WO"], "wo")
        modulate_T(0, 1, 0, NCH)
        spill_X(NCH)
        barrier()
        if stage == 15:
            dbgb = dram("dbgb", [128, 8 * NT], BF16, kind="ExternalOutput")
            DMA("sp", dbgb, hT[:].rearrange("p k t -> p (k t)"), [f"hT{c}" for c in range(NCH)], ["dbgd"], "dbg")
            S.add("sp", None, reads=list(S.lw.keys()) + ["dbgd"], writes=())
            S.emit(nc, es)
            es.close()
            return nc
        arena_off[0] = 0
        WG = [xview(1536, [128, 8, 384], BF16, "p (k c) -> p k c", k=8) for _ in range(2)]
        QKT = xview(3 * NT // 2, None, BF16, "p (s t) -> p s t", s=3)
        VX = xview(NCH * 130 // 2, None, BF16, "p (c f) -> p c f", c=NCH)
        QKR = [xview(192, None, BF16) for _ in range(2)]
        T1 = [xview(384, None) for _ in range(2)]
        T2 = [xview(384, None) for _ in range(2)]
        SQ = xview(384, None)
        PT = [xview(256, None, BF16) for _ in range(4)]
        O1 = xview(512, None, F32, "p (j f) -> p j f", j=4)
        OO = [xview(128, None) for _ in range(2)]
        AOK = xview(512, None, BF16, "p (j f) -> p j f", j=4)
        SUBG = xview(128, None)
        G5 = xview(320, None)
        LAMT = xview(256, None, F32, "p (a f) -> p a f", a=4)
        lamt = sb("lamt", [128, 4])
        RSS = sb("RSS", [128, 24])
        DMA("sp", SUBG.rearrange("p (o f) -> p o f", o=1), subg_d.rearrange("(o f) -> o f", o=1).partition_broadcast(128),
            (), ["SUBG"], "c2")
        DMA("sp", G5.rearrange("p (o f) -> p o f", o=1), qkg_d.rearrange("(o f) -> o f", o=1).partition_broadcast(128),
            (), ["G5"], "c2")
        for a in range(4):
            DMA("sp", LAMT[:, a:a + 1, :], lam_d[a:a + 1, :].partition_broadcast(128), (), ["LAMT"], "c2")
        fence(["SUBG", "G5", "LAMT"])
        TS("dve", SUBG, SUBG, 1.0 - LAM_INIT0, None, ALU.mult, None, ["SUBG"], ["SUBG"])
        TT("dve", LAMT[:, 0, :], LAMT[:, 0, :], LAMT[:, 1, :], ALU.mult, ["LAMT"], ["LAMT"])
        TT("dve", LAMT[:, 2, :], LAMT[:, 2, :], LAMT[:, 3, :], ALU.mult, ["LAMT"], ["LAMT"])
        S.add("dve", lambda e: e.tensor_reduce(lamt[:, 0:1], LAMT[:, 0, :], AX.X, ALU.add), reads=["LAMT"], writes=["lamt"])
        S.add("dve", lambda e: e.tensor_reduce(lamt[:, 1:2], LAMT[:, 2, :], AX.X, ALU.add), reads=["LAMT"], writes=["lamt"])
        ACT(lamt[:, 0:2], lamt[:, 0:2], AF.Exp, ["lamt"], ["lamt"])
        TT("dve", lamt[:, 2:3], lamt[:, 1:2], lamt[:, 0:1], ALU.subtract, ["lamt"], ["lamt"])
        TS("dve", lamt[:, 3:4], lamt[:, 2:3], -LAM_INIT0, None, ALU.add, None, ["lamt"], ["lamt"])
        nlam = lamt[:, 3:4]

        groups = [("A", h) for h in range(4)] + [("B", n) for n in range(2)]

        def load_wg(gi):
            kind, idx = groups[gi]
            w = WG[gi % 2]
            if kind == "A":
                cols = [(idx * 128, 128), (512 + idx * 128, 128), (1024 + idx * 128, 128)]
            else:
                cols = [(1536 + idx * 256, 256), (2048 + idx * 64, 64), (2176 + idx * 64, 64)]
            o = 0
            for (c0, n) in cols:
                DMA("pool", w[:, :, o:o + n], w_in0_d[:, c0:c0 + n].rearrange("(k p) c -> p k c", p=128),
                    (), [f"WG{gi % 2}"], f"wg{gi % 2}")
                o += n

        load_wg(0)
        ptn = [0]
        for gi, (kind, idx) in enumerate(groups):
            if gi + 1 < len(groups):
                load_wg(gi + 1)
            w = WG[gi % 2]
            nv = 4 if kind == "A" else 5
            nr = nv * 64
            vcol = 256 if kind == "A" else 320
            vw = 128 if kind == "A" else 64
            MEMSET("dve", VX[:, :, vw:vw + 1], 1.0, [f"VX{c}" for c in range(NCH)])
            for c in range(NCH):
                pb = 6 + (c % 2)
                for k in range(8):
                    MM(PS[pb][:, 0:384], hT[:, k, c * 128:(c + 1) * 128], w[:, k, :], k == 0, k == 7,
                       [f"hT{c}", f"WG{gi % 2}"], [f"ps{pb}"])
                CP("act", VX[:, c, 0:vw], PS[pb][:, vcol:vcol + vw], [f"ps{pb}"], [f"VX{c}"])
                src = PS[pb][:, 0:nr]
                skey = f"ps{pb}"
                i2 = c % 2
                if kind == "B":
                    ACT(SQ[:, 0:nr], src, AF.Square, [skey], ["SQ"])
                    S.add("dve", lambda e, c=c, nr=nr: e.tensor_reduce(RSS[:, 0:5], SQ[:, 0:nr].rearrange("p (v f) -> p v f", v=5), AX.X, ALU.add),
                          reads=["SQ"], writes=["RSS"])
                    ACT(RSS[:, 8:13], RSS[:, 0:5], AF.Ln, ["RSS"], ["RSS2"], bias=epst[RMS_EPS], scale=1.0 / 64)
                    ACT(RSS[:, 16:21], RSS[:, 8:13], AF.Exp, ["RSS2"], ["RSS3"], scale=-0.5)
                    TT("dve", SQ[:, 0:nr].rearrange("p (v f) -> p v f", v=5), src.rearrange("p (v f) -> p v f", v=5),
                       RSS[:, 16:21].unsqueeze(2).to_broadcast([128, 5, 64]), ALU.mult, [skey, "RSS3"], ["SQ"])
                    TT("dve", SQ[:, 0:nr], SQ[:, 0:nr], G5, ALU.mult, ["SQ", "G5"], ["SQ"])
                    src = SQ[:, 0:nr]
                    skey = "SQ"
                cosb = ropet[:, c, 0:64].unsqueeze(1).to_broadcast([128, nv, 64])
                TT("dve", T1[i2][:, 0:nr].rearrange("p (v f) -> p v f", v=nv), src.rearrange("p (v f) -> p v f", v=nv),
                   cosb, ALU.mult, [skey, "ropet"], [f"T1{i2}"])
                s5 = src.rearrange("p (v a j f) -> p v a j f", v=nv, a=2, j=2)
                t5 = T2[i2][:, 0:nr].rearrange("p (v a j f) -> p v a j f", v=nv, a=2, j=2)
                sn5 = ropet[:, c, 64:128].rearrange("p (a j f) -> p a j f", a=2, j=2)
                for j in range(2):
                    for a in range(2):
                        sinb = sn5[:, a, j, :].unsqueeze(1).to_broadcast([128, nv, 16])
                        TT("dve", t5[:, :, a, j, :], s5[:, :, a, 1 - j, :], sinb, ALU.mult,
                           [skey, "ropet"], [f"T2{i2}"])
                TT("dve", QKR[i2][:, 0:nr], T1[i2][:, 0:nr], T2[i2][:, 0:nr], ALU.add, [f"T1{i2}", f"T2{i2}"], [f"QKR{i2}"])
                if kind == "B":
                    TT("dve", QKR[i2][:, 320:384], T1[i2][:, 256:320], T2[i2][:, 256:320], ALU.add,
                       [f"T1{i2}", f"T2{i2}"], [f"QKR{i2}"])
                ntr = 2 if kind == "A" else 3
                tb = 4 + (c % 2)
                for t in range(ntr):
                    TR(PSB[tb][:, t * 128:(t + 1) * 128], QKR[i2][:, t * 128:(t + 1) * 128], identb[:],
                       [f"QKR{i2}", "identb"], [f"ps{tb}"])
                CP("act", QKT[:, 0:ntr, c * 128:(c + 1) * 128], PSB[tb][:, 0:ntr * 128].rearrange("p (s t) -> p s t", s=ntr),
                   [f"ps{tb}"], [f"QKT{c}"])
            if stage == 16 and gi == GSTOP:
                dbgb = dram("dbgb", [128, 3 * NT], BF16, kind="ExternalOutput")
                DMA("sp", dbgb, QKT.rearrange("p s t -> p (s t)"), [f"QKT{c}" for c in range(NCH)], ["dbgd"], "dbg")
                dbgv = dram("dbgv", [128, NCH * 130], BF16, kind="ExternalOutput")
                DMA("sp", dbgv, VX.rearrange("p c f -> p (c f)"), [f"VX{c}" for c in range(NCH)], ["dbgd"], "dbg")
                S.add("sp", None, reads=list(S.lw.keys()) + ["dbgd"], writes=())
                S.emit(nc, es)
                es.close()
                return nc
            blocks = [(qb * 4, 4, list(range(NCH))) for qb in range(4)] + [(16, 2, [16, 17])]
            if kind == "A":
                heads = [(0, 0, 1, s * 64) for s in range(2)]
            else:
                heads = [(j4 // 2, 2, (j4 % 2) * 64) for j4 in range(4)]
            for (qc0, nqc, kcs) in blocks:
                ncols = nqc * 128
                q0 = qc0 * 128
                qkeys = [f"QKT{qc0 + j}" for j in range(nqc)]
                for hi, hd in enumerate(heads):
                    if kind == "A":
                        qs, ks, pbase = 0, 1, hi * 64
                    else:
                        qs, ks, pbase = hd
                    def score(kn):
                        kc = kcs[kn]
                        sp_ = 4 + (ptn[0] % 2)
                        pi = ptn[0] % 4
                        ptn[0] += 1
                        MM(PS[sp_][:, 0:ncols], QKT[pbase:pbase + 64, ks, kc * 128:(kc + 1) * 128],
                           QKT[pbase:pbase + 64, qs, q0:q0 + ncols], True, True,
                           [f"QKT{kc}"] + qkeys, [f"ps{sp_}"])
                        return sp_, pi

                    pend = score(0)
                    for kn, kc in enumerate(kcs):
                        nxt = score(kn + 1) if kn + 1 < len(kcs) else None
                        sp_, pi = pend
                        ACT(PT[pi][:, 0:ncols], PS[sp_][:, 0:ncols], AF.Exp, [f"ps{sp_}"], [f"PT{pi}"], scale=0.125)
                        for j in range(nqc):
                            MM(PS[j][:, 0:vw + 1], PT[pi][:, j * 128:(j + 1) * 128], VX[:, kc, 0:vw + 1],
                               kn == 0, kn == len(kcs) - 1, [f"PT{pi}", f"VX{kc}"], [f"ps{j}"])
                        pend = nxt
                    for j in range(nqc):
                        rz, rk = stat()
                        S.add("dve", lambda e, rz=rz, j=j, vw=vw: e.reciprocal(rz, PS[j][:, vw:vw + 1]), reads=[f"ps{j}"], writes=[rk])
                        if kind == "B":
                            TS("dve", AOK[:, j, hi * 64:(hi + 1) * 64], PS[j][:, 0:64], rz, None, ALU.mult, None,
                               [f"ps{j}", rk], [f"AOK{j}"])
                        elif hi == 0:
                            TS("dve", O1[:, j, :], PS[j][:, 0:128], rz, None, ALU.mult, None, [f"ps{j}", rk], [f"O1{j}"])
                        else:
                            r2, r2k = stat()
                            TT("dve", r2, rz, nlam, ALU.mult, [rk, "lamt"], [r2k])
                            oo = OO[j % 2]
                            STT("dve", oo, PS[j][:, 0:128], r2, O1[:, j, :], ALU.mult, ALU.add,
                                [f"ps{j}", r2k, f"O1{j}"], [f"OO{j % 2}"])
                            ssq, ssk = stat()
                            TT("dve", T1[j % 2][:, 0:128], oo, oo, ALU.mult, [f"OO{j % 2}"], [f"T1{j % 2}"])
                            S.add("dve", lambda e, ssq=ssq, j=j: e.tensor_reduce(ssq, T1[j % 2][:, 0:128], AX.X, ALU.add),
                                  reads=[f"T1{j % 2}"], writes=[ssk])
                            rs, rsk = stat()
                            rstd_from(rs, rsk, ssq, ssk, 1.0 / 128, RMS_EPS)
                            STT("dve", AOK[:, j, 0:128], oo, rs, SUBG, ALU.mult, ALU.mult,
                                [f"OO{j % 2}", rsk, "SUBG"], [f"AOK{j}"])
                    last = (kind == "A" and hi == 1) or (kind == "B" and hi == 3)
                    if last:
                        for j in range(nqc):
                            tb = 6 + (j % 2)
                            ntr = 1 if kind == "A" else 2
                            for t in range(ntr):
                                TR(PSB[tb][:, t * 128:(t + 1) * 128], AOK[:, j, t * 128:(t + 1) * 128], identb[:],
                                   [f"AOK{j}", "identb"], [f"ps{tb}"])
                            m0 = idx if kind == "A" else 4 + idx * 2
                            tc = qc0 + j
                            CP("act", AOT[:, m0:m0 + ntr, tc * 128:(tc + 1) * 128],
                               PSB[tb][:, 0:ntr * 128].rearrange("p (s t) -> p s t", s=ntr), [f"ps{tb}"], [f"AOT{tc}"])
        barrier()
        reload_X(NCH)
        outproj_ln(0, NCH)

    if stage == 2:
        for c in range(NCH):
            DMA("sp", dbg_d[c * 128:(c + 1) * 128, :], X[:, c, :], [f"X{c}"], ["dbgd"], "dbg")
        S.add("sp", None, reads=list(S.lw.keys()) + ["dbgd"], writes=())
        S.emit(nc, es)
        es.close()
        return nc
    router_d = dram("moe_router", [2, D, NE])
    NEX = 1 if stage == 31 else NE
    wg_d = dram("moe_w_gate", [2, NEX, D, FF])
    wu_d = dram("moe_w_up", [2, NEX, D, FF])
    wd_d = dram("moe_w_down", [2, NEX, FF, D])
    iota_d = dram("iota288", [128, 288])
    utri_d = dram("utri", [128, 128])
    aotb = AOT[:].rearrange("p k t -> p (k t)")
    wob = WO[:].rearrange("p k c -> p (k c)")
    hmb = hT[:].rearrange("p k t -> p (k t)").rearrange("p (c f) -> p c f", c=NCH)
    RING = [aotb[:, i * 2048:(i + 1) * 2048] for i in range(5)]
    RING += [bc[i].bitcast(BF16) for i in (2, 3, 4, 5)]
    _rpb = ropet[:].rearrange("p c f -> p (c f)").bitcast(BF16)
    RING += [_rpb[:, i * 2048:(i + 1) * 2048] for i in range(2)]
    NRING = len(RING)
    XGT = aotb[:, 10240:12544].rearrange("p (k s) -> p k s", k=8)
    HIDT = aotb[:, 12544:17152].rearrange("p (j s) -> p j s", j=16)
    SELR = [aotb[:, 17152 + i * 288:17152 + (i + 1) * 288] for i in range(2)]
    SELGT = [aotb[:, 17728 + i * 384:17728 + (i + 1) * 384].rearrange("p (s t) -> p s t", s=3) for i in range(1)]
    SELR += [wob[:, 5280 + i * 288:5280 + (i + 1) * 288] for i in range(3)]
    SELGT += [wob[:, 6144 + i * 384:6144 + (i + 1) * 384].rearrange("p (s t) -> p s t", s=3) for i in range(2)]
    NSEL = len(SELR)
    NSGT = len(SELGT)
    YG = wob[:, 0:3072].rearrange("p (s f) -> p s f", s=3)
    IOTA = wob[:, 3072:3648].bitcast(F32)
    RW = wob[:, 3648:3904].bitcast(F32).rearrange("p (k e) -> p k e", k=8)
    UTRI = wob[:, 3904:4032]
    ONESB = wob[:, 4032:4160]
    MASKB = wob[:, 4160:4448].rearrange("p (c e) -> p c e", c=NCH)
    SGR = [wob[:, 4448:5024].bitcast(F32), wob[:, 6912:7488].bitcast(F32)]
    UTF = wob[:, 5024:5280].bitcast(F32)
    WEX = ropet[:].rearrange("p c f -> p (c f)")
    AFF = hb[0][:].bitcast(F32)[:, 0:288].rearrange("p (c e) -> p c e", c=NCH)
    MASK = hb[1][:].bitcast(F32)[:, 0:288].rearrange("p (c e) -> p c e", c=NCH)
    mo2 = sb("mo2", [128, 2, NCH * NE])
    SLOT = mo2[:, 0, :].rearrange("p (c e) -> p c e", c=NCH)
    GS = mo2[:, 1, :].rearrange("p (c e) -> p c e", c=NCH)
    m8 = sb("m8", [16, 8])

    def moe_layer(l, nch, final_out=False):
        ns = 256 + (32 if nch > NLAT else 0)
        nsc = (ns + 127) // 128
        scsz = [min(128, ns - s * 128) for s in range(nsc)]
        barrier()
        DMA("sp", IOTA, iota_d, (), ["IOTA"], "c3")
        DMA("sp", UTF, utri_d, (), ["UTF"], "c3")
        DMA("sp", RW, router_d[l].rearrange("(k p) e -> p k e", p=128), (), ["RW"], "c3")
        fence(["IOTA", "UTF", "RW"])
        CP("dve", UTRI, UTF, ["UTF"], ["UTRI"])
        MEMSET("dve", ONESB, 1.0, ["ONESB"])
        bcast_mod(0, l, 0, 4, True)
        bcast_mod(1, l, 0, 3)
        if nch > NLAT:
            bcast_mod(2, l, 1, 4, True)
            bcast_mod(3, l, 1, 3)
        for c in range(nch):
            a, b_ = (0, 1) if c < NLAT else (2, 3)
            TT("dve", bc[4], X[:, c, :], bc[a], ALU.mult, [f"X{c}", f"bc{a}"], ["bc4"])
            TT("dve", bc[4], bc[4], bc[b_], ALU.add, ["bc4", f"bc{b_}"], ["bc4"])
            CP("act", hmb[:, c, :], bc[4], ["bc4"], [f"hmb{c}"])
            TS("dve", X[:, c, :], X[:, c, :], ALPHA, None, ALU.mult, None, [f"X{c}"], [f"X{c}"])
            for k in range(8):
                pb = k // 4
                TR(PS[pb][:, (k % 4) * 128:(k % 4 + 1) * 128], bc[4][:, k * 128:(k + 1) * 128], identf[:],
                   ["bc4", "identf"], [f"ps{pb}"])
            hmT = bc[5].rearrange("p (k t) -> p k t", k=8)
            CP("act", hmT[:, 0:4, :], PS[0][:, :].rearrange("p (k t) -> p k t", k=4), ["ps0"], ["bc5"])
            CP("dve", hmT[:, 4:8, :], PS[1][:, :].rearrange("p (k t) -> p k t", k=4), ["ps1"], ["bc5"])
            for k in range(8):
                MM(PS[2][:, 0:NE], hmT[:, k, :], RW[:, k, :], k == 0, k == 7, ["bc5", "RW"], ["ps2"])
            mx, mxk = stat()
            S.add("dve", lambda e, mx=mx: e.tensor_reduce(mx, PS[2][:, 0:NE], AX.X, ALU.max), reads=["ps2"], writes=[mxk])
            nmx, nmk = stat()
            TS("dve", nmx, mx, -1.0, None, ALU.mult, None, [mxk], [nmk])
            ACT(AFF[:, c, :], PS[2][:, 0:NE], AF.Exp, ["ps2", nmk], [f"AFF{c}"], bias=nmx)
            sm, smk = stat()
            S.add("dve", lambda e, sm=sm, c=c: e.tensor_reduce(sm, AFF[:, c, :], AX.X, ALU.add), reads=[f"AFF{c}"], writes=[smk])
            rc, rck = stat()
            S.add("dve", lambda e, rc=rc, sm=sm: e.reciprocal(rc, sm), reads=[smk], writes=[rck])
            TS("dve", AFF[:, c, :], AFF[:, c, :], rc, None, ALU.mult, None, [f"AFF{c}", rck], [f"AFF{c}"])
            TR(PS[3][0:NE, 0:128], AFF[:, c, :], identf[:], [f"AFF{c}", "identf"], ["ps3"])
            CP("dve", WEX[0:NE, c * 128:(c + 1) * 128], PS[3][0:NE, 0:128], ["ps3"], ["WEX"])
        sets = [(0, SEQ, 256)] + ([(SEQ, NT, 32)] if nch > NLAT else [])
        for (t0, t1, cap) in sets:
            wv = WEX[0:NE, t0:t1]
            for r in range(cap // 8):
                S.add("dve", lambda e, wv=wv: e.max(m8[:], wv), reads=["WEX"], writes=["m8"])
                S.add("dve", lambda e, wv=wv: e.match_replace(wv, m8[:], wv, -1.0), reads=["WEX", "m8"], writes=["WEX"])
        TS("dve", WEX[0:NE, 0:nch * 128], WEX[0:NE, 0:nch * 128], 0.0, None, ALU.is_lt, None, ["WEX"], ["WEX"])
        for c in range(nch):
            TR(PS[4][:, c * NE:(c + 1) * NE], WEX[0:NE, c * 128:(c + 1) * 128], identf[0:NE, 0:NE], ["WEX", "identf"], ["ps4"])
        mflat = mo2[:, 0, 0:nch * NE]
        CP("dve", MASK[:, 0:nch, :], PS[4][:, 0:nch * NE].rearrange("p (c e) -> p c e", c=nch), ["ps4"], ["MASK"])
        CP("dve", MASKB[:, 0:nch, :], MASK[:, 0:nch, :], ["MASK"], ["MASKB"])
        TT("dve", GS[:, 0:nch, :], AFF[:, 0:nch, :], MASK[:, 0:nch, :], ALU.mult, [f"AFF{c}" for c in range(nch)] + ["MASK"], ["GS"])
        for c in range(nch):
            c0 = 0 if c < NLAT else NLAT
            prev = list(range(c0, c))
            for i, cp in enumerate(prev):
                MM(PS[5][:, c * NE:(c + 1) * NE], ONESB, MASKB[:, cp, :], i == 0, False, ["ONESB", "MASKB"], ["ps5"])
            MM(PS[5][:, c * NE:(c + 1) * NE], UTRI, MASKB[:, c, :], len(prev) == 0, True, ["UTRI", "MASKB"], ["ps5"])
        TS("dve", SLOT[:, 0:NLAT, :], PS[5][:, 0:NLAT * NE].rearrange("p (c e) -> p c e", c=NLAT), 1.0, None, ALU.add, None, ["ps5"], ["SLOT"])
        if nch > NLAT:
            TS("dve", SLOT[:, NLAT:nch, :], PS[5][:, NLAT * NE:nch * NE].rearrange("p (c e) -> p c e", c=nch - NLAT), 257.0, None,
               ALU.add, None, ["ps5"], ["SLOT"])
        TT("dve", SLOT[:, 0:nch, :], SLOT[:, 0:nch, :], MASK[:, 0:nch, :], ALU.mult, ["SLOT", "MASK"], ["SLOT"])
        TS("dve", SLOT[:, 0:nch, :], SLOT[:, 0:nch, :], -1.0, None, ALU.add, None, ["SLOT"], ["SLOT"])
        bcast_mod(0, l, 0, 5)
        if nch > NLAT:
            bcast_mod(1, l, 1, 5)
        barrier()
        units = []
        for e_ in range(NEX):
            for fu in range(8):
                units.append(("g", e_, fu))
                units.append(("u", e_, fu))
            for du in range(8):
                units.append(("d", e_, du))
        issued = [0]

        def issue_until(n):
            while issued[0] < min(n, len(units)):
                kind, e_, i = units[issued[0]]
                slot = issued[0] % NRING
                if kind == "d":
                    src = wd_d[l, e_, i * 256:(i + 1) * 256, :].rearrange("(j p) c -> p j c", p=128)
                    dst = RING[slot].rearrange("p (j c) -> p j c", j=2)
                else:
                    wsrc = wg_d if kind == "g" else wu_d
                    src = wsrc[l, e_, :, i * 256:(i + 1) * 256].rearrange("(k p) c -> p k c", p=128)
                    dst = RING[slot].rearrange("p (k c) -> p k c", k=8)
                DMA("pool", dst, src, (), [f"wr{slot}"], f"wr{slot}")
                issued[0] += 1

        ui = [0]

        def next_unit():
            i = ui[0]
            ui[0] += 1
            return i % NRING

        def refill():
            issue_until(ui[0] + NRING)

        issue_until(NRING)
        seln = [0]
        sgn = [0]
        for e_ in range(NEX):
            for half in range(2):
                for c in range(nch):
                    si = seln[0] % NSEL
                    seln[0] += 1
                    s0, s1 = (0, 256) if c < NLAT else (256, ns)
                    cfirst, clast = (0, NLAT - 1) if c < NLAT else (NLAT, nch - 1)
                    TS("dve", SELR[si][:, s0:s1], IOTA[:, s0:s1], SLOT[:, c, e_:e_ + 1], None, ALU.is_equal, None,
                       ["IOTA", "SLOT"], [f"SEL{si}"])
                    for f4 in range(4):
                        f = half * 4 + f4
                        MM(PS[f4][:, s0:s1], hmb[:, c, f * 128:(f + 1) * 128], SELR[si][:, s0:s1], c == cfirst, c == clast,
                           [f"hmb{c}", f"SEL{si}"], [f"ps{f4}"])
                for f4 in range(4):
                    f = half * 4 + f4
                    CP("act" if f4 % 2 == 0 else "dve", XGT[:, f, 0:ns], PS[f4][:, 0:ns], [f"ps{f4}"], ["XGT"])
            for fu in range(8):
                sg_ = next_unit()
                su_ = next_unit()
                wgv = RING[sg_].rearrange("p (k c) -> p k c", k=8)
                wuv = RING[su_].rearrange("p (k c) -> p k c", k=8)
                for j2 in range(2):
                    jg = fu * 2 + j2
                    bg, bu = (4, 5) if jg % 2 == 0 else (6, 7)
                    for k in range(8):
                        MM(PS[bg][:, 0:ns], wgv[:, k, j2 * 128:(j2 + 1) * 128], XGT[:, k, 0:ns], k == 0, k == 7,
                           [f"wr{sg_}", "XGT"], [f"ps{bg}"])
                    for k in range(8):
                        MM(PS[bu][:, 0:ns], wuv[:, k, j2 * 128:(j2 + 1) * 128], XGT[:, k, 0:ns], k == 0, k == 7,
                           [f"wr{su_}", "XGT"], [f"ps{bu}"])
                    SG = SGR[jg % 2]
                    ACT(SG[:, 0:ns], PS[bg][:, 0:ns], AF.Silu, [f"ps{bg}"], [f"SG{jg % 2}"])
                    TT("dve", HIDT[:, jg, 0:ns], PS[bu][:, 0:ns], SG[:, 0:ns], ALU.mult, [f"ps{bu}", f"SG{jg % 2}"], [f"HID{jg}"])
                refill()
            for du in range(8):
                sd_ = next_unit()
                wdv = RING[sd_].rearrange("p (j c) -> p j c", j=2)
                for sc in range(nsc):
                    sz = scsz[sc]
                    for hf in range(2):
                        pb = sc * 2 + hf
                        for jj in range(2):
                            jg = du * 2 + jj
                            MM(PS[pb][0:sz, :], HIDT[:, jg, sc * 128:sc * 128 + sz], wdv[:, jj, hf * 512:(hf + 1) * 512],
                               du == 0 and jj == 0, du == 7 and jj == 1, [f"HID{jg}", f"wr{sd_}"], [f"ps{pb}"])
                refill()
            for sc in range(nsc):
                sz = scsz[sc]
                gt = 0 if sc < 2 else 1
                for hf in range(2):
                    pb = sc * 2 + hf
                    TT("dve", YG[0:sz, sc, hf * 512:(hf + 1) * 512], PS[pb][0:sz, :], bc[gt][0:sz, hf * 512:(hf + 1) * 512], ALU.mult,
                       [f"ps{pb}", f"bc{gt}"], ["YG"])
            if stage == 31:
                d1 = dram("d_slot", [128, 2 * NCH * NE], kind="ExternalOutput")
                DMA("sp", d1, mo2[:].rearrange("p a f -> p (a f)"), ["SLOT", "GS"], ["dbgd"], "dbg")
                d2 = dram("d_xgt", [128, 8 * 288], BF16, kind="ExternalOutput")
                DMA("sp", d2, XGT.rearrange("p k s -> p (k s)"), ["XGT"], ["dbgd"], "dbg")
                d3 = dram("d_yg", [128, 3 * 1024], BF16, kind="ExternalOutput")
                DMA("sp", d3, YG.rearrange("p s f -> p (s f)"), ["YG"], ["dbgd"], "dbg")
                d4 = dram("d_hid", [128, 16 * 288], BF16, kind="ExternalOutput")
                DMA("sp", d4, HIDT.rearrange("p j s -> p (j s)"), [f"HID{j}" for j in range(16)], ["dbgd"], "dbg")
                S.add("sp", None, reads=list(S.lw.keys()) + ["dbgd"], writes=())
                S.emit(nc, es)
                es.close()
                return "STOP"
            def sc_stage_a(c):
                si = seln[0] % NSEL
                seln[0] += 1
                s0, s1 = (0, 256) if c < NLAT else (256, ns)
                scs_ = [0, 1] if c < NLAT else [2]
                TS("dve", SELR[si][:, s0:s1], IOTA[:, s0:s1], SLOT[:, c, e_:e_ + 1], GS[:, c, e_:e_ + 1], ALU.is_equal, ALU.mult,
                   ["IOTA", "SLOT", "GS"], [f"SEL{si}"])
                tb = 6 + (c % 2)
                for sc in scs_:
                    sz = scsz[sc]
                    TR(PSB[tb][0:sz, sc * 128:(sc + 1) * 128], SELR[si][:, sc * 128:sc * 128 + sz], identb[:],
                       [f"SEL{si}", "identb"], [f"ps{tb}"])
                gi_ = sgn[0] % NSGT
                sgn[0] += 1
                sgt = SELGT[gi_]
                if c < NLAT:
                    CP("act", sgt[:, 0:2, :], PSB[tb][:, 0:256].rearrange("p (s t) -> p s t", s=2), [f"ps{tb}"], [f"SELGT{gi_}"])
                else:
                    CP("act", sgt[0:scsz[2], 2, :], PSB[tb][0:scsz[2], 256:384], [f"ps{tb}"], [f"SELGT{gi_}"])
                return gi_

            def sc_stage_b(c, gi_):
                sgt = SELGT[gi_]
                scs_ = [0, 1] if c < NLAT else [2]
                for hf in range(2):
                    pb = 2 * (c % 2) + hf
                    for sc in scs_:
                        sz = scsz[sc]
                        MM(PS[pb][:, :], sgt[0:sz, sc, :], YG[0:sz, sc, hf * 512:(hf + 1) * 512], sc == scs_[0], sc == scs_[-1],
                           [f"SELGT{gi_}", "YG"], [f"ps{pb}"])
                    TT("dve", X[:, c, hf * 512:(hf + 1) * 512], PS[pb][:, :], X[:, c, hf * 512:(hf + 1) * 512], ALU.add,
                       [f"ps{pb}", f"X{c}"], [f"X{c}"])

            prev = None
            for c in range(nch):
                g_ = sc_stage_a(c)
                if prev is not None:
                    sc_stage_b(*prev)
                prev = (c, g_)
            sc_stage_b(*prev)
        barrier()
        bcast_row(2, ln_g_d[l, 1:2, :])
        bcast_row(3, ln_b_d[l, 1:2, :])
        for c in range(nch):
            layer_norm_chunk(c, 2, 3)
            if final_out:
                DMA("sp", out_d[c * 128:(c + 1) * 128, :], X[:, c, :], [f"X{c}"], ["outd"], "outw")
        barrier()

    if moe_layer(0, NCH) == "STOP":
        return nc

    if stage == 3:
        for c in range(NCH):
            DMA("sp", dbg_d[c * 128:(c + 1) * 128, :], X[:, c, :], [f"X{c}"], ["dbgd"], "dbg")
        S.add("sp", None, reads=list(S.lw.keys()) + ["dbgd"], writes=())
        S.emit(nc, es)
        es.close()
        return nc

    w_in1_d = dram("l1_w_in", [D, 2048])
    w_out1_d = dram("l1_w_out", [D, D])
    poolw_d = dram("l1_pool_w", [4, 128, 128])
    psc_d = dram("psc", [128, 4])
    band_d = dram("band", [20, 128, 128])
    l1tab_d = dram("l1tab", [NPAT, 8, 128, 128])

    def layer1_mixer():
        l = 1
        barrier()
        DMA("pool", WO[:], w_out1_d.rearrange("(k p) c -> p k c", p=128), (), ["WO"], "wo")
        modulate_T(1, 1, 0, NCH)
        spill_X(NLAT)
        barrier()
        arena_off[0] = 0
        W1R = [xview(2048, None, BF16, "p (k c) -> p k c", k=8) for _ in range(2)]
        QT1 = xview(4 * SEQ // 2, None, BF16, "p (g t) -> p g t", g=4)
        KT1 = xview(4 * NT // 2, None, BF16, "p (g t) -> p g t", g=4)
        VXU = xview(NCH * 8 * 65 // 2, None, BF16)
        VX1 = VXU.rearrange("p (c h f) -> p c h f", c=NCH, h=8)
        U1 = VXU[:, 0:NLAT * 512].rearrange("p (c f) -> p c f", c=NLAT)
        PT1 = [xview(256, None, BF16) for _ in range(3)]
        rp = ropet[:].rearrange("p c f -> p (c f)")
        TBI = rp[:, 0:1280].bitcast(BF16).rearrange("p (a h k) -> p a h k", a=5, h=4)
        TBB = rp[:, 1280:2304].bitcast(BF16).rearrange("p (a h k) -> p a h k", a=4, h=4)
        BAND = hb[0][:].rearrange("p (m t) -> p m t", m=8)
        PW = hb[1][:, 0:512].rearrange("p (g e) -> p g e", g=4)
        AOK1 = hb[1][:, 512:768]
        POOLT = hb[1][:, 768:1024].bitcast(F32) if False else None
        psc = sb("pscs", [128, 4])
        qflat = QT1.rearrange("p g t -> p (g t)")
        bandb = qflat[:, 0:2560].rearrange("p (m t) -> p m t", m=20)
        poolt = [qflat[:, 2560 + i * 512:2560 + (i + 1) * 512] for i in range(2)]
        DMA("sp", psc[:], psc_d, (), ["psc"], "c4")
        DMA("pool", bandb, band_d.rearrange("m a b -> a m b"), (), ["bandb"], "c4p")
        DMA("pool", PW, poolw_d.rearrange("g c e -> c g e"), (), ["PW"], "c4p")
        fence(["psc", "bandb", "PW"])

        def load_w1(sec, slot):
            DMA("pool", W1R[slot][:], w_in1_d[:, sec * 512:(sec + 1) * 512].rearrange("(k p) c -> p k c", p=128),
                (), [f"W1R{slot}"], f"w1r{slot}")

        load_w1(3, 0)
        load_w1(0, 1)
        for c in range(NLAT):
            pb = 4 + (c % 2)
            for k in range(8):
                MM(PS[pb][:, :], hT[:, k, c * 128:(c + 1) * 128], W1R[0][:, k, :], k == 0, k == 7, [f"hT{c}", "W1R0"], [f"ps{pb}"])
            CP("act" if c % 2 == 0 else "dve", U1[:, c, :], PS[pb][:, :], [f"ps{pb}"], [f"U1{c}"])
        pn = [0]
        for g in range(4):
            for cb in range(4):
                pb = 6 + (pn[0] % 2)
                for j in range(4):
                    c = cb * 4 + j
                    srcs = []
                    if c > 0:
                        srcs.append((c - 1, g * 5 + 0))
                    srcs.append((c, g * 5 + (3 if c == 0 else 4 if c == NLAT - 1 else 1)))
                    if c < NLAT - 1:
                        srcs.append((c + 1, g * 5 + 2))
                    for i, (cs, m) in enumerate(srcs):
                        MM(PS[pb][:, j * 128:(j + 1) * 128], U1[:, cs, g * 128:(g + 1) * 128], bandb[:, m, :], i == 0, i == len(srcs) - 1,
                           [f"U1{cs}", "bandb"], [f"ps{pb}"])
                pt_ = poolt[pn[0] % 2]
                CP("act", pt_, PS[pb][:, :], [f"ps{pb}"], [f"poolt{pn[0] % 2}"])
                pb2 = 4 + (pn[0] % 2)
                MM(PS[pb2][:, :], PW[:, g, :], pt_, True, True, ["PW", f"poolt{pn[0] % 2}"], [f"ps{pb2}"])
                TS("dve", AOT[:, 4 + g, cb * 512:(cb + 1) * 512], PS[pb2][:, :], psc[:, g:g + 1], None, ALU.mult, None,
                   [f"ps{pb2}", "psc"], [f"AOT{cb * 4 + j}" for j in range(4)])
                pn[0] += 1
        barrier()
        MEMSET("dve", VX1[:, :, :, 64:65], 1.0, [f"VX{c}" for c in range(NCH)])
        for g in range(4):
            for tb_ in range(4):
                pb = 4 + ((g * 4 + tb_) % 2)
                for k in range(8):
                    MM(PS[pb][:, :], W1R[1][:, k, g * 128:(g + 1) * 128], hT[:, k, tb_ * 512:(tb_ + 1) * 512], k == 0, k == 7,
                       ["W1R1"] + [f"hT{tb_ * 4 + j}" for j in range(4)], [f"ps{pb}"])
                ACT(QT1[:, g, tb_ * 512:(tb_ + 1) * 512], PS[pb][:, :], AF.Identity, [f"ps{pb}"], [f"QT{tb_ * 4 + j}" for j in range(4)], scale=0.125)
        load_w1(1, 0)
        load_w1(2, 1)
        blocks_k = [(i * 512, 512) for i in range(4)] + [(2048, 256)]
        for g in range(4):
            for bi, (t0, tn) in enumerate(blocks_k):
                pb = 4 + ((g * 5 + bi) % 2)
                chs = list(range(t0 // 128, (t0 + tn) // 128))
                for k in range(8):
                    MM(PS[pb][:, 0:tn], W1R[0][:, k, g * 128:(g + 1) * 128], hT[:, k, t0:t0 + tn], k == 0, k == 7,
                       ["W1R0"] + [f"hT{c}" for c in chs], [f"ps{pb}"])
                CP("act" if bi % 2 == 0 else "dve", KT1[:, g, t0:t0 + tn], PS[pb][:, 0:tn], [f"ps{pb}"], [f"KT{c}" for c in chs])
        for c in range(NCH):
            pb = 6 + (c % 2)
            for k in range(8):
                MM(PS[pb][:, :], hT[:, k, c * 128:(c + 1) * 128], W1R[1][:, k, :], k == 0, k == 7, [f"hT{c}", "W1R1"], [f"ps{pb}"])
            CP("act" if c % 2 == 0 else "dve", VX1[:, c, :, 0:64], PS[pb][:, :].rearrange("p (h f) -> p h f", h=8), [f"ps{pb}"], [f"VX{c}"])
        ptn = [0]
        for hh in range(2):
            for a in range(5):
                DMA("pool", TBI[:, a, :, :], l1tab_d[INT_PATS[a], hh * 4:(hh + 1) * 4].rearrange("h q k -> q h k"), (), ["TBI"], "tbi")
            for c in range(NLAT):
                loc = L1_LOCAL[c]
                interior = (2 <= c <= 13)
                if not interior:
                    for a, (kc, pat) in enumerate(loc):
                        DMA("pool", TBB[:, a, :, :], l1tab_d[pat, hh * 4:(hh + 1) * 4].rearrange("h q k -> q h k"), (), ["TBB"], "tbb")
                kcs = [(kc, a) for a, (kc, pat) in enumerate(loc)] + [(16, None), (17, None)]
                def score1(kn):
                    kc, a = kcs[kn]
                    bpair = (4, 5) if ptn[0] % 2 == 0 else (6, 7)
                    pi = ptn[0] % 3
                    ptn[0] += 1
                    for par in range(2):
                        bk = bpair[par]
                        for i2, h4 in enumerate((par, par + 2)):
                            h = hh * 4 + h4
                            g = h // 2
                            pbase = (h % 2) * 64
                            MM(PS[bk][:, i2 * 128:(i2 + 1) * 128], KT1[pbase:pbase + 64, g, kc * 128:(kc + 1) * 128],
                               QT1[pbase:pbase + 64, g, c * 128:(c + 1) * 128], True, a is None, [f"KT{kc}", f"QT{c}"], [f"ps{bk}"])
                            if a is not None:
                                tb_ap = TBI[:, a, h4, :] if interior else TBB[:, a, h4, :]
                                MM(PS[bk][:, i2 * 128:(i2 + 1) * 128], tb_ap, identb[:], False, True,
                                   ["TBI" if interior else "TBB", "identb"], [f"ps{bk}"])
                    return bpair, pi

                pend = score1(0)
                for kn, (kc, a) in enumerate(kcs):
                    nxt = score1(kn + 1) if kn + 1 < len(kcs) else None
                    bpair, pi = pend
                    for par in range(2):
                        bk = bpair[par]
                        ACT(PT1[pi][:, par * 256:(par + 1) * 256], PS[bk][:, 0:256], AF.Exp, [f"ps{bk}"], [f"PT{pi}_{par}"])
                    for h4 in range(4):
                        h = hh * 4 + h4
                        par, i2 = h4 % 2, h4 // 2
                        o_ = par * 256 + i2 * 128
                        MM(PS[h4][:, 0:65], PT1[pi][:, o_:o_ + 128], VX1[:, kc, h, :], kn == 0, kn == len(kcs) - 1,
                           [f"PT{pi}_{par}", f"VX{kc}"], [f"ps{h4}"])
                    pend = nxt
                for h4 in range(4):
                    rz, rk = stat()
                    S.add("dve", lambda e, rz=rz, h4=h4: e.reciprocal(rz, PS[h4][:, 64:65]), reads=[f"ps{h4}"], writes=[rk])
                    TS("dve", AOK1[:, h4 * 64:(h4 + 1) * 64], PS[h4][:, 0:64], rz, None, ALU.mult, None, [f"ps{h4}", rk], ["AOK1"])
                tb = 6 + (c % 2)
                for t in range(2):
                    TR(PSB[tb][:, t * 128:(t + 1) * 128], AOK1[:, t * 128:(t + 1) * 128], identb[:], ["AOK1", "identb"], [f"ps{tb}"])
                CP("act", AOT[:, 2 * hh:2 * hh + 2, c * 128:(c + 1) * 128], PSB[tb][:, 0:256].rearrange("p (s t) -> p s t", s=2),
                   [f"ps{tb}"], [f"AOT{c}"])
        barrier()
        reload_X(NLAT)
        outproj_ln(1, NLAT)

    layer1_mixer()
    if stage == 4:
        for c in range(NLAT):
            DMA("sp", dbg_d[c * 128:(c + 1) * 128, :], X[:, c, :], [f"X{c}"], ["dbgd"], "dbg")
        S.add("sp", None, reads=list(S.lw.keys()) + ["dbgd"], writes=())
        S.emit(nc, es)
        es.close()
        return nc
    moe_layer(1, NLAT, final_out=True)
    S.add("sp", None, reads=list(S.lw.keys()) + ["outd"], writes=())
    S.emit(nc, es)
    es.close()
    return nc


def _rope_table():
    t = np.arange(SEQ)
    inv = (10000.0 ** (-np.arange(16, dtype=np.float32) / 16)).astype(np.float32)
    ar = (t // 64).astype(np.float32)[:, None] * inv
    ac = (t % 64).astype(np.float32)[:, None] * inv
    cr, sr, cc_, sc_ = np.cos(ar), np.sin(ar), np.cos(ac), np.sin(ac)
    cosf = np.concatenate([cr, cr, cc_, cc_], axis=1)
    sins = np.concatenate([-sr, sr, -sc_, sc_], axis=1)
    tab = np.concatenate([cosf, sins], axis=1).astype(np.float32)
    ctxr = np.tile(np.array([1.0] * 64 + [0.0] * 64, np.float32), (CTX, 1))
    return np.concatenate([tab, ctxr], axis=0)


def make_in_maps(inp):
    f = lambda a: np.ascontiguousarray(np.asarray(a, dtype=np.float32))
    shared = {
        "ada_w": f(inp["ada_w"]), "ada_b": f(inp["ada_b"]), "ln_g": f(inp["ln_g"]), "ln_b": f(inp["ln_b"]),
        "l0_w_in": f(inp["l0_w_in"]), "l0_w_out": f(inp["l0_w_out"]),
        "lamv": f(np.stack([inp["l0_lam_q1"], inp["l0_lam_k1"], inp["l0_lam_q2"], inp["l0_lam_k2"]])),
        "l0_subln_g": f(inp["l0_subln_g"]),
        "qkg": f(np.concatenate([np.tile(np.asarray(inp["l0_qnorm_g"]), 4), np.asarray(inp["l0_knorm_g"])])),
        "rope": _rope_table(), "ident": np.eye(128, dtype=np.float32),
        "moe_router": f(inp["moe_router"]), "moe_w_gate": f(inp["moe_w_gate"]), "moe_w_up": f(inp["moe_w_up"]),
        "moe_w_down": f(inp["moe_w_down"]),
        "l1_w_in": f(inp["l1_w_in"]), "l1_w_out": f(inp["l1_w_out"]), "l1_pool_w": f(inp["l1_pool_w"]),
        "psc": f(np.asarray(inp["l1_pool_scale"]).reshape(4, 128).T), "band": _band_mats(),
        "l1tab": _l1_table(inp["l1_rpb"]),
        "iota288": np.tile(np.arange(288, dtype=np.float32), (128, 1)),
        "utri": np.triu(np.ones((128, 128), np.float32), 1),
    }
    maps = []
    x = np.asarray(inp["x"]); ctx = np.asarray(inp["ctx"]); c = np.asarray(inp["c"]); cctx = np.asarray(inp["c_ctx"])
    for b in range(8):
        m = dict(shared)
        m["x"] = f(np.concatenate([x[b], ctx[b]], axis=0))
        cc = np.stack([c[b].reshape(8, 128).T, cctx.reshape(8, 128).T], axis=-1)
        m["cc"] = f(cc)
        maps.append(m)
    return maps


def kernel(**inputs):
    nc = build()
    maps = make_in_maps(inputs)
    res = run_bass_kernel_spmd(nc, maps, core_ids=list(range(8)))
    return np.stack([r["out"] for r in res.results], axis=0)
```

```python
import math
import numpy as np
from contextlib import ExitStack
import concourse.bass as bass
import concourse.mybir as mybir
from concourse.bass_utils import run_bass_kernel_spmd

F32 = mybir.dt.float32
BF16 = mybir.dt.bfloat16
AF = mybir.ActivationFunctionType
ALU = mybir.AluOpType
AX = mybir.AxisListType

D = 1024
SEQ = 2048
CTX = 256
NT = SEQ + CTX
NCH = NT // 128
NLAT = SEQ // 128
NE = 16
FF = 2048
ALPHA = 4.0 ** 0.25
LN_EPS = 1e-5
RMS_EPS = 1e-6
LAM_INIT0 = 0.8 - 0.6 * math.exp(0.0)
NEG = -30000.0


class Sched:
    def __init__(self):
        self.ops = []
        self.lw = {}
        self.rd = {}

    def add(self, eng, fn, reads=(), writes=(), dma=None):
        i = len(self.ops)
        stream = ("dma", dma) if dma is not None else eng
        pk = [k for k in reads if k.startswith("ps") and k[2:].isdigit()]
        if pk:
            writes = list(writes) + [k for k in pk if k not in writes]
        raw = set()
        war = set()
        for k in reads:
            w = self.lw.get(k)
            if w is not None:
                raw.add(w)
        for k in writes:
            w = self.lw.get(k)
            if w is not None:
                raw.add(w)
            for r in self.rd.get(k, {}).values():
                war.add(r)
        if fn is not None:
            for k in writes:
                self.lw[k] = i
                self.rd[k] = {}
            for k in reads:
                self.rd.setdefault(k, {})[stream] = i
        self.ops.append(dict(eng=eng, fn=fn, raw=raw, war=war, dma=dma, stream=stream, cons=False, ms=0))
        return i

    def emit(self, nc, es):
        ops = self.ops
        for o in ops:
            for d in o["raw"] | o["war"]:
                if d == len(ops):
                    continue
                p = ops[d]
                same = (p["stream"] == o["stream"])
                if same and (o["stream"] == "pe"):
                    continue
                if same and d in o["war"] and d not in o["raw"]:
                    continue
                p["cons"] = True
        sems = {}
        cnt = {}

        def get_sem(stream):
            if stream not in sems:
                nm = "s_" + (stream if isinstance(stream, str) else "d_" + str(stream[1]))
                sems[stream] = es.enter_context(nc.semaphore(nm))
                cnt[stream] = 0
            return sems[stream]

        for o in ops:
            st = o["stream"]
            get_sem(st)
            if o["dma"] is not None:
                cnt[st] += 16
                o["ms"] = cnt[st]
            elif o["cons"]:
                cnt[st] += 1
                o["ms"] = cnt[st]
        self.nsem = len(sems)
        block = es.enter_context(nc.Block())
        engs = ["pe", "act", "dve", "pool", "sp"]
        per = {e: [o for o in ops if o["eng"] == e] for e in engs}

        def run(e, eh):
            known = {}
            for o in per[e]:
                need = {}
                for d in o["raw"] | o["war"]:
                    p = ops[d]
                    same = (p["stream"] == o["stream"])
                    if same and o["stream"] == "pe":
                        continue
                    if same and d in o["war"] and d not in o["raw"]:
                        continue
                    if p["ms"] <= 0:
                        continue
                    s = p["stream"]
                    if need.get(s, 0) < p["ms"]:
                        need[s] = p["ms"]
                for s, v in need.items():
                    if known.get(s, 0) < v:
                        eh.wait_ge(sems[s], v)
                        known[s] = v
                if o["fn"] is None:
                    continue
                ins = o["fn"](eh)
                if o["dma"] is not None:
                    ins.then_inc(sems[o["stream"]], 16)
                elif o["cons"]:
                    ins.then_inc(sems[o["stream"]], 1)

        @block.tensor
        def _(eh):
            run("pe", eh)

        @block.scalar
        def _(eh):
            run("act", eh)

        @block.vector
        def _(eh):
            run("dve", eh)

        @block.gpsimd
        def _(eh):
            run("pool", eh)

        @block.sync
        def _(eh):
            run("sp", eh)


def _l1_patterns():
    W, rows, kh, kw = 64, 32, 8, 16
    cols = np.arange(W)
    cs = np.clip(cols - kw // 2, 0, W - kw)
    colok = (cols[None, :] >= cs[:, None]) & (cols[None, :] < cs[:, None] + kw)
    dcol = cols[None, :] - cols[:, None] + 15
    pats, sigs, local = [], {}, []
    for c in range(16):
        lst = []
        for kc in range(16):
            valid = np.zeros((128, 128), bool)
            dr = np.zeros((128, 128), np.int64)
            dc = np.zeros((128, 128), np.int64)
            for ql in range(2):
                r = 2 * c + ql
                rs = min(max(r - kh // 2, 0), rows - kh)
                for kl in range(2):
                    rk = 2 * kc + kl
                    rowok = rs <= rk < rs + kh
                    qs = slice(ql * 64, ql * 64 + 64)
                    ks = slice(kl * 64, kl * 64 + 64)
                    valid[qs, ks] = colok & rowok
                    dr[qs, ks] = rk - r + 7
                    dc[qs, ks] = dcol
            if not valid.any():
                continue
            dr = np.where(valid, dr, 0)
            dc = np.where(valid, dc, 0)
            sig = (valid.tobytes(), dr.tobytes())
            if sig not in sigs:
                sigs[sig] = len(pats)
                pats.append((valid, dr, dc))
            lst.append((kc, sigs[sig]))
        local.append(lst)
    return pats, local


_L1_PATS, L1_LOCAL = _l1_patterns()
NPAT = len(_L1_PATS)
INT_PATS = [p for (_, p) in L1_LOCAL[2]]
for _c in range(2, 14):
    assert [p for (_, p) in L1_LOCAL[_c]] == INT_PATS and len(INT_PATS) == 5
for _c in (0, 1, 14, 15):
    assert len(L1_LOCAL[_c]) <= 4


def _l1_table(rpb):
    rpb = np.asarray(rpb, np.float32)
    tab = np.full((NPAT, 8, 128, 128), NEG, np.float32)
    for i, (valid, dr, dc) in enumerate(_L1_PATS):
        g = rpb[:, dr, dc]
        tab[i] = np.where(valid[None], g, np.float32(NEG))
    return tab


def _band_mats():
    out = np.zeros((20, 128, 128), np.float32)
    tp = np.arange(128)[:, None]
    t = np.arange(128)[None, :]
    for g, w in enumerate((2, 4, 8, 16)):
        half = w // 2
        cnt = float(2 * half)
        out[g * 5 + 0] = (tp >= t + 128 - half) / cnt
        out[g * 5 + 1] = ((tp >= t - half) & (tp < t + half)) / cnt - (tp == t)
        out[g * 5 + 2] = (tp < t + half - 128) / cnt
        lo = np.maximum(t - half, 0)
        hi = t + half
        out[g * 5 + 3] = ((tp >= lo) & (tp < hi)) / (hi - lo).astype(np.float32) - (tp == t)
        lo = t - half
        hi = np.minimum(t + half, 128)
        out[g * 5 + 4] = ((tp >= lo) & (tp < hi)) / (hi - lo).astype(np.float32) - (tp == t)
    return out


GSTOP = 0


def build(stage=99, dbg_cols=1024):
    nc = bass.Bass("TRN2", target_bir_lowering=False)
    S = Sched()
    es = ExitStack()

    def dram(name, shape, dt=F32, kind="ExternalInput"):
        return nc.dram_tensor(name, list(shape), dt, kind=kind).ap()

    x_d = dram("x", [NT, D])
    cc_d = dram("cc", [128, 8, 2])
    ada_w_d = dram("ada_w", [2, D, 6 * D])
    ada_b_d = dram("ada_b", [2, 6 * D])
    ln_g_d = dram("ln_g", [2, 2, D])
    ln_b_d = dram("ln_b", [2, 2, D])
    w_in0_d = dram("l0_w_in", [D, 2304])
    w_out0_d = dram("l0_w_out", [D, D])
    lam_d = dram("lamv", [4, 64])
    subg_d = dram("l0_subln_g", [128])
    qkg_d = dram("qkg", [5 * 64])
    rope_d = dram("rope", [NT, 128])
    ident_d = dram("ident", [128, 128])
    out_d = dram("out", [SEQ, D], kind="ExternalOutput")
    dbg_d = dram("dbg", [128 if stage == 1 else NT, dbg_cols], kind="ExternalOutput") if stage < 99 else None
    modd = dram("modd", [2, 2, 6 * D], kind="Internal")

    def sb(name, shape, dt=F32):
        return es.enter_context(nc.sbuf_tensor(name, list(shape), dt))

    Xraw = sb("X", [128, NCH * D])
    X = Xraw[:].rearrange("p (c f) -> p c f", c=NCH)
    hT = sb("hT", [128, 8, NT], BF16)
    AOT = sb("AOT", [128, 8, NT], BF16)
    WO = sb("WO", [128, 8, D], BF16)
    identb = sb("identb", [128, 128], BF16)
    identf = sb("identf", [128, 128])
    ropet = sb("ropet", [128, NCH, 128])
    big = sb("big", [128, 6 * 1024])
    bc = [big[:, i * 1024:(i + 1) * 1024] for i in range(6)]
    st = sb("st", [128, 64])
    PS = [es.enter_context(nc.psum_tensor(f"ps{i}", [128, 512], F32)) for i in range(8)]
    PSB = [p[:].bitcast(BF16) for p in PS]
    stn = [0]

    def stat():
        i = stn[0] % 64
        stn[0] += 1
        return st[:, i:i + 1], f"st{i}"

    arena_off = [0]

    def xview(nelem_f32, shape, dt=F32, pattern=None, **kw):
        a = arena_off[0]
        arena_off[0] += nelem_f32
        assert arena_off[0] <= NCH * D
        v = Xraw[:, a:a + nelem_f32]
        if dt != F32:
            v = v.bitcast(dt)
        if pattern is not None:
            v = v.rearrange(pattern, **kw)
        return v

    dkn = [0]

    def dk(prefix="d"):
        dkn[0] += 1
        return f"{prefix}{dkn[0]}"

    def DMA(q, out, in_, reads, writes, key):
        return S.add(q, lambda e: e.dma_start(out=out, in_=in_), reads=reads, writes=writes, dma=key)

    def MM(out, lhsT, rhs, start, stop, reads, writes):
        return S.add("pe", lambda e: e.matmul(out, lhsT, rhs, start=start, stop=stop), reads=reads, writes=writes)

    def TR(out, in_, ident, reads, writes):
        return S.add("pe", lambda e: e.transpose(out, in_, ident), reads=reads, writes=writes)

    def ACT(out, in_, func, reads, writes, bias=None, scale=None, accum_out=None):
        kw = {}
        if bias is not None:
            kw["bias"] = bias
        if scale is not None:
            kw["scale"] = scale
        if accum_out is not None:
            kw["accum_out"] = accum_out
        return S.add("act", lambda e: e.activation(out, in_, func, **kw), reads=reads, writes=writes)

    def TT(eng, out, in0, in1, op, reads, writes):
        return S.add(eng, lambda e: e.tensor_tensor(out, in0, in1, op), reads=reads, writes=writes)

    def TS(eng, out, in0, s1, s2, op0, op1, reads, writes):
        if op1 is None:
            return S.add(eng, lambda e: e.tensor_scalar(out, in0, s1, None, op0), reads=reads, writes=writes)
        return S.add(eng, lambda e: e.tensor_scalar(out, in0, s1, s2, op0, op1), reads=reads, writes=writes)

    def STT(eng, out, in0, scalar, in1, op0, op1, reads, writes):
        return S.add(eng, lambda e: e.scalar_tensor_tensor(out, in0, scalar, in1, op0, op1), reads=reads, writes=writes)

    def CP(eng, out, in_, reads, writes):
        if eng == "act":
            return S.add(eng, lambda e: e.copy(out, in_), reads=reads, writes=writes)
        return S.add(eng, lambda e: e.tensor_copy(out, in_), reads=reads, writes=writes)

    def MEMSET(eng, ap, val, writes):
        return S.add(eng, lambda e: e.memset(ap, val), reads=(), writes=writes)

    def fence(keys, engs=("pe", "act", "dve", "pool", "sp")):
        for e in engs:
            S.add(e, None, reads=keys, writes=())

    def barrier():
        allk = list(S.lw.keys())
        for e in ("pe", "act", "dve", "pool", "sp"):
            S.add(e, None, reads=allk, writes=allk)

    nc_lp = es.enter_context(nc.allow_low_precision("bf16 matmul operands, fp32 accumulation"))
    es.enter_context(nc.allow_non_contiguous_dma("small strided constant loads"))

    for c in range(NCH):
        DMA("sp", X[:, c, :], x_d[c * 128:(c + 1) * 128, :], (), [f"X{c}"], "ldx")
    ccs = sb("ccs", [128, 8, 2])
    DMA("sp", ccs[:], cc_d, (), ["ccs"], "const")
    DMA("sp", identf[:], ident_d, (), ["identf"], "const")
    DMA("sp", ropet[:], rope_d.rearrange("(c p) f -> p c f", p=128), (), ["ropet"], "const")
    fence(["ccs", "identf", "ropet"])
    CP("dve", identb[:], identf[:], ["identf"], ["identb"])

    scs = sb("scs", [128, 8, 2])
    S.add("act", lambda e: e.activation(scs[:], ccs[:], AF.Silu), reads=["ccs"], writes=["scs"])
    adab = sb("adab", [2, 512])
    modrow = sb("modrow", [2, 512])
    NST = 3
    aotf = AOT[:].rearrange("p k t -> p (k t)").bitcast(F32)
    hTf = hT[:].rearrange("p k t -> p (k t)").bitcast(F32)
    stg = [aotf[:, 0:4096], aotf[:, 4096:8192], hTf[:, 0:4096]]
    gi = 0
    for l in range(2):
        for j in range(12):
            s = gi % NST
            st3 = stg[s].rearrange("p (k c) -> p k c", k=8)
            DMA("pool", st3, ada_w_d[l, :, j * 512:(j + 1) * 512].rearrange("(k p) c -> p k c", p=128),
                (), [f"stg{s}"], f"adaw{s}")
            for r in range(2):
                DMA("sp", adab[r:r + 1, :], ada_b_d[l:l + 1, j * 512:(j + 1) * 512], (), ["adab"], "adab")
            pb = gi % 2
            for k in range(8):
                MM(PS[pb][0:2, :], scs[:, k, :], st3[:, k, :], k == 0, k == 7,
                   ["scs", f"stg{s}"], [f"ps{pb}"])
            TT("dve", modrow[:], PS[pb][0:2, :], adab[:], ALU.add, [f"ps{pb}", "adab"], ["modrow"])
            DMA("sp", modd[l, :, j * 512:(j + 1) * 512], modrow[:], ["modrow"], ["modd"], "modw")
            gi += 1
    barrier()

    def dbg_out(ap, cols):
        tmpd = sb("tmpd", [128, cols])
        CP("dve", tmpd[:], ap, list(S.lw.keys()), ["tmpd"])
        DMA("sp", dbg_d[:, 0:cols], tmpd[:], ["tmpd"], ["dbgd"], "dbg")

    if stage == 1:
        t1 = sb("t1", [128, 192])
        DMA("sp", t1[:], modd.rearrange("l r (a b) -> (l r a) b", b=192), ["modd"], ["t1"], "t1")
        dbg_out(t1[:], 192)
        S.add("sp", None, reads=list(S.lw.keys()), writes=())
        S.emit(nc, es)
        es.close()
        return nc

    xscr = dram("xscr", [NT, D], kind="Internal")

    def bcast_mod(i, l, r, idx, plus1=False):
        src = modd[l, r:r + 1, idx * 1024:(idx + 1) * 1024].partition_broadcast(128)
        DMA("sp", bc[i].rearrange("p (o f) -> p o f", o=1), src, ["modd"], [f"bc{i}"], f"bcl{i}")
        if plus1:
            TS("dve", bc[i], bc[i], 1.0, None, ALU.add, None, [f"bc{i}"], [f"bc{i}"])

    def bcast_row(i, row_ap):
        DMA("sp", bc[i].rearrange("p (o f) -> p o f", o=1), row_ap.partition_broadcast(128), (), [f"bc{i}"], f"bcl{i}")

    hb = [sb(f"hb{i}", [128, D], BF16) for i in range(2)]

    def modulate_T(l, isc, ish, nch):
        bcast_mod(0, l, 0, isc, True)
        bcast_mod(1, l, 0, ish)
        if nch > NLAT:
            bcast_mod(2, l, 1, isc, True)
            bcast_mod(3, l, 1, ish)
        for c in range(nch):
            a, b_ = (0, 1) if c < NLAT else (2, 3)
            t = 4 + (c % 2)
            TT("dve", bc[t], X[:, c, :], bc[a], ALU.mult, [f"X{c}", f"bc{a}"], [f"bc{t}"])
            TT("dve", hb[c % 2][:], bc[t], bc[b_], ALU.add, [f"bc{t}", f"bc{b_}"], [f"hb{c % 2}"])
            pb = 4 + (c % 2)
            for k in range(8):
                TR(PSB[pb][:, k * 128:(k + 1) * 128], hb[c % 2][:, k * 128:(k + 1) * 128], identb[:],
                   [f"hb{c % 2}", "identb"], [f"ps{pb}"])
            CP("act", hT[:, :, c * 128:(c + 1) * 128], PSB[pb].rearrange("p (k t) -> p k t", k=8),
               [f"ps{pb}"], [f"hT{c}"])

    def spill_X(nch):
        for c in range(nch):
            DMA("sp", xscr[c * 128:(c + 1) * 128, :], X[:, c, :], [f"X{c}"], ["xscr"], "xsp")

    def reload_X(nch):
        for c in range(nch):
            DMA("sp", X[:, c, :], xscr[c * 128:(c + 1) * 128, :], ["xscr"], [f"X{c}"], "xrl")

    def rstd_from(out_ap, okey, in_ap, ikey, scale, eps):
        tmp, tk = stat()
        ACT(tmp, in_ap, AF.Ln, [ikey], [tk], bias=epst[eps], scale=scale)
        ACT(out_ap, tmp, AF.Exp, [tk], [okey], scale=-0.5)

    epsv = sb("epsv", [128, 2])
    MEMSET("dve", epsv[:, 0:1], LN_EPS, ["epsv"])
    MEMSET("dve", epsv[:, 1:2], RMS_EPS, ["epsv"])
    fence(["epsv"], ("act",))
    epst = {LN_EPS: epsv[:, 0:1], RMS_EPS: epsv[:, 1:2]}

    def layer_norm_chunk(c, ig, ib):
        stt = sb(f"lnst{c}", [128, 2, 6]) if False else lnstat
        S.add("dve", lambda e: e.bn_stats(stt[:, 0, :], X[:, c, 0:512]), reads=[f"X{c}"], writes=["lnst0"])
        S.add("dve", lambda e: e.bn_stats(stt[:, 1, :], X[:, c, 512:1024]), reads=[f"X{c}"], writes=["lnst1"])
        mv, mk = lnmv, "lnmv"
        S.add("dve", lambda e: e.bn_aggr(mv[:], stt[:]), reads=["lnst0", "lnst1"], writes=[mk])
        rs, rk = stat()
        rstd_from(rs, rk, mv[:, 1:2], mk, 1.0, LN_EPS)
        nm, nk = stat()
        TS("dve", nm, mv[:, 0:1], rs, -1.0, ALU.mult, ALU.mult, [mk, rk], [nk])
        t = 4 + (c % 2)
        ACT(bc[t], X[:, c, :], AF.Identity, [f"X{c}", rk, nk], [f"bc{t}"], bias=nm, scale=rs)
        TT("dve", bc[t], bc[t], bc[ig], ALU.mult, [f"bc{t}", f"bc{ig}"], [f"bc{t}"])
        TT("dve", X[:, c, :], bc[t], bc[ib], ALU.add, [f"bc{t}", f"bc{ib}"], [f"X{c}"])

    lnstat = sb("lnstat", [128, 2, 6])
    lnmv = sb("lnmv", [128, 2])


    def outproj_ln(l, nch):
        bcast_mod(0, l, 0, 2)
        if nch > NLAT:
            bcast_mod(1, l, 1, 2)
        bcast_row(2, ln_g_d[l, 0:1, :])
        bcast_row(3, ln_b_d[l, 0:1, :])
        for c in range(nch):
            gtile = 0 if c < NLAT else 1
            t = 4 + (c % 2)
            for hf in range(2):
                pb = 2 * (c % 2) + hf
                for m in range(8):
                    MM(PS[pb][:, :], AOT[:, m, c * 128:(c + 1) * 128], WO[:, m, hf * 512:(hf + 1) * 512], m == 0, m == 7,
                       [f"AOT{c}", "WO"], [f"ps{pb}"])
                TT("dve", bc[t][:, hf * 512:(hf + 1) * 512], PS[pb][:, :], bc[gtile][:, hf * 512:(hf + 1) * 512], ALU.mult,
                   [f"ps{pb}", f"bc{gtile}"], [f"bc{t}"])
            STT("dve", X[:, c, :], X[:, c, :], ALPHA, bc[t], ALU.mult, ALU.add, [f"X{c}", f"bc{t}"], [f"X{c}"])
            layer_norm_chunk(c, 2, 3)
        barrier()

    if True:
        l = 0
        DMA("pool", WO[:], w_out0_d.rearrange("(k p) c -> p k c", p=128), (), ["WO"], "wo")
        modulate_T(0, 1, 0, NCH)
        spill_X(NCH)
        barrier()
        if stage == 15:
            dbgb = dram("dbgb", [128, 8 * NT], BF16, kind="ExternalOutput")
            DMA("sp", dbgb, hT[:].rearrange("p k t -> p (k t)"), [f"hT{c}" for c in range(NCH)], ["dbgd"], "dbg")
            S.add("sp", None, reads=list(S.lw.keys()) + ["dbgd"], writes=())
            S.emit(nc, es)
            es.close()
            return nc
        arena_off[0] = 0
        WG = [xview(1536, [128, 8, 384], BF16, "p (k c) -> p k c", k=8) for _ in range(2)]
        QKT = xview(3 * NT // 2, None, BF16, "p (s t) -> p s t", s=3)
        VX = xview(NCH * 130 // 2, None, BF16, "p (c f) -> p c f", c=NCH)
        QKR = [xview(192, None, BF16) for _ in range(2)]
        T1 = [xview(384, None) for _ in range(2)]
        T2 = [xview(384, None) for _ in range(2)]
        SQ = xview(384, None)
        PT = [xview(256, None, BF16) for _ in range(4)]
        O1 = xview(512, None, F32, "p (j f) -> p j f", j=4)
        OO = [xview(128, None) for _ in range(2)]
        AOK = xview(512, None, BF16, "p (j f) -> p j f", j=4)
        SUBG = xview(128, None)
        G5 = xview(320, None)
        LAMT = xview(256, None, F32, "p (a f) -> p a f", a=4)
        lamt = sb("lamt", [128, 4])
        RSS = sb("RSS", [128, 24])
        DMA("sp", SUBG.rearrange("p (o f) -> p o f", o=1), subg_d.rearrange("(o f) -> o f", o=1).partition_broadcast(128),
            (), ["SUBG"], "c2")
        DMA("sp", G5.rearrange("p (o f) -> p o f", o=1), qkg_d.rearrange("(o f) -> o f", o=1).partition_broadcast(128),
            (), ["G5"], "c2")
        for a in range(4):
            DMA("sp", LAMT[:, a:a + 1, :], lam_d[a:a + 1, :].partition_broadcast(128), (), ["LAMT"], "c2")
        fence(["SUBG", "G5", "LAMT"])
        TS("dve", SUBG, SUBG, 1.0 - LAM_INIT0, None, ALU.mult, None, ["SUBG"], ["SUBG"])
        TT("dve", LAMT[:, 0, :], LAMT[:, 0, :], LAMT[:, 1, :], ALU.mult, ["LAMT"], ["LAMT"])
        TT("dve", LAMT[:, 2, :], LAMT[:, 2, :], LAMT[:, 3, :], ALU.mult, ["LAMT"], ["LAMT"])
        S.add("dve", lambda e: e.tensor_reduce(lamt[:, 0:1], LAMT[:, 0, :], AX.X, ALU.add), reads=["LAMT"], writes=["lamt"])
        S.add("dve", lambda e: e.tensor_reduce(lamt[:, 1:2], LAMT[:, 2, :], AX.X, ALU.add), reads=["LAMT"], writes=["lamt"])
        ACT(lamt[:, 0:2], lamt[:, 0:2], AF.Exp, ["lamt"], ["lamt"])
        TT("dve", lamt[:, 2:3], lamt[:, 1:2], lamt[:, 0:1], ALU.subtract, ["lamt"], ["lamt"])
        TS("dve", lamt[:, 3:4], lamt[:, 2:3], -LAM_INIT0, None, ALU.add, None, ["lamt"], ["lamt"])
        nlam = lamt[:, 3:4]

        groups = [("A", h) for h in range(4)] + [("B", n) for n in range(2)]

        def load_wg(gi):
            kind, idx = groups[gi]
            w = WG[gi % 2]
            if kind == "A":
                cols = [(idx * 128, 128), (512 + idx * 128, 128), (1024 + idx * 128, 128)]
            else:
                cols = [(1536 + idx * 256, 256), (2048 + idx * 64, 64), (2176 + idx * 64, 64)]
            o = 0
            for (c0, n) in cols:
                DMA("pool", w[:, :, o:o + n], w_in0_d[:, c0:c0 + n].rearrange("(k p) c -> p k c", p=128),
                    (), [f"WG{gi % 2}"], f"wg{gi % 2}")
                o += n

        load_wg(0)
        ptn = [0]
        for gi, (kind, idx) in enumerate(groups):
            if gi + 1 < len(groups):
                load_wg(gi + 1)
            w = WG[gi % 2]
            nv = 4 if kind == "A" else 5
            nr = nv * 64
            vcol = 256 if kind == "A" else 320
            vw = 128 if kind == "A" else 64
            MEMSET("dve", VX[:, :, vw:vw + 1], 1.0, [f"VX{c}" for c in range(NCH)])
            for c in range(NCH):
                pb = 6 + (c % 2)
                for k in range(8):
                    MM(PS[pb][:, 0:384], hT[:, k, c * 128:(c + 1) * 128], w[:, k, :], k == 0, k == 7,
                       [f"hT{c}", f"WG{gi % 2}"], [f"ps{pb}"])
                CP("act", VX[:, c, 0:vw], PS[pb][:, vcol:vcol + vw], [f"ps{pb}"], [f"VX{c}"])
                src = PS[pb][:, 0:nr]
                skey = f"ps{pb}"
                i2 = c % 2
                if kind == "B":
                    ACT(SQ[:, 0:nr], src, AF.Square, [skey], ["SQ"])
                    S.add("dve", lambda e, c=c, nr=nr: e.tensor_reduce(RSS[:, 0:5], SQ[:, 0:nr].rearrange("p (v f) -> p v f", v=5), AX.X, ALU.add),
                          reads=["SQ"], writes=["RSS"])
                    ACT(RSS[:, 8:13], RSS[:, 0:5], AF.Ln, ["RSS"], ["RSS2"], bias=epst[RMS_EPS], scale=1.0 / 64)
                    ACT(RSS[:, 16:21], RSS[:, 8:13], AF.Exp, ["RSS2"], ["RSS3"], scale=-0.5)
                    TT("dve", SQ[:, 0:nr].rearrange("p (v f) -> p v f", v=5), src.rearrange("p (v f) -> p v f", v=5),
                       RSS[:, 16:21].unsqueeze(2).to_broadcast([128, 5, 64]), ALU.mult, [skey, "RSS3"], ["SQ"])
                    TT("dve", SQ[:, 0:nr], SQ[:, 0:nr], G5, ALU.mult, ["SQ", "G5"], ["SQ"])
                    src = SQ[:, 0:nr]
                    skey = "SQ"
                cosb = ropet[:, c, 0:64].unsqueeze(1).to_broadcast([128, nv, 64])
                TT("dve", T1[i2][:, 0:nr].rearrange("p (v f) -> p v f", v=nv), src.rearrange("p (v f) -> p v f", v=nv),
                   cosb, ALU.mult, [skey, "ropet"], [f"T1{i2}"])
                s5 = src.rearrange("p (v a j f) -> p v a j f", v=nv, a=2, j=2)
                t5 = T2[i2][:, 0:nr].rearrange("p (v a j f) -> p v a j f", v=nv, a=2, j=2)
                sn5 = ropet[:, c, 64:128].rearrange("p (a j f) -> p a j f", a=2, j=2)
                for j in range(2):
                    for a in range(2):
                        sinb = sn5[:, a, j, :].unsqueeze(1).to_broadcast([128, nv, 16])
                        TT("dve", t5[:, :, a, j, :], s5[:, :, a, 1 - j, :], sinb, ALU.mult,
                           [skey, "ropet"], [f"T2{i2}"])
                TT("dve", QKR[i2][:, 0:nr], T1[i2][:, 0:nr], T2[i2][:, 0:nr], ALU.add, [f"T1{i2}", f"T2{i2}"], [f"QKR{i2}"])
                if kind == "B":
                    TT("dve", QKR[i2][:, 320:384], T1[i2][:, 256:320], T2[i2][:, 256:320], ALU.add,
                       [f"T1{i2}", f"T2{i2}"], [f"QKR{i2}"])
                ntr = 2 if kind == "A" else 3
                tb = 4 + (c % 2)
                for t in range(ntr):
                    TR(PSB[tb][:, t * 128:(t + 1) * 128], QKR[i2][:, t * 128:(t + 1) * 128], identb[:],
                       [f"QKR{i2}", "identb"], [f"ps{tb}"])
                CP("act", QKT[:, 0:ntr, c * 128:(c + 1) * 128], PSB[tb][:, 0:ntr * 128].rearrange("p (s t) -> p s t", s=ntr),
                   [f"ps{tb}"], [f"QKT{c}"])
            if stage == 16 and gi == GSTOP:
                dbgb = dram("dbgb", [128, 3 * NT], BF16, kind="ExternalOutput")
                DMA("sp", dbgb, QKT.rearrange("p s t -> p (s t)"), [f"QKT{c}" for c in range(NCH)], ["dbgd"], "dbg")
                dbgv = dram("dbgv", [128, NCH * 130], BF16, kind="ExternalOutput")
                DMA("sp", dbgv, VX.rearrange("p c f -> p (c f)"), [f"VX{c}" for c in range(NCH)], ["dbgd"], "dbg")
                S.add("sp", None, reads=list(S.lw.keys()) + ["dbgd"], writes=())
                S.emit(nc, es)
                es.close()
                return nc
            blocks = [(qb * 4, 4, list(range(NCH))) for qb in range(4)] + [(16, 2, [16, 17])]
            if kind == "A":
                heads = [(0, 0, 1, s * 64) for s in range(2)]
            else:
                heads = [(j4 // 2, 2, (j4 % 2) * 64) for j4 in range(4)]
            for (qc0, nqc, kcs) in blocks:
                ncols = nqc * 128
                q0 = qc0 * 128
                qkeys = [f"QKT{qc0 + j}" for j in range(nqc)]
                for hi, hd in enumerate(heads):
                    if kind == "A":
                        qs, ks, pbase = 0, 1, hi * 64
                    else:
                        qs, ks, pbase = hd
                    def score(kn):
                        kc = kcs[kn]
                        sp_ = 4 + (ptn[0] % 2)
                        pi = ptn[0] % 4
                        ptn[0] += 1
                        MM(PS[sp_][:, 0:ncols], QKT[pbase:pbase + 64, ks, kc * 128:(kc + 1) * 128],
                           QKT[pbase:pbase + 64, qs, q0:q0 + ncols], True, True,
                           [f"QKT{kc}"] + qkeys, [f"ps{sp_}"])
                        return sp_, pi

                    pend = score(0)
                    for kn, kc in enumerate(kcs):
                        nxt = score(kn + 1) if kn + 1 < len(kcs) else None
                        sp_, pi = pend
                        ACT(PT[pi][:, 0:ncols], PS[sp_][:, 0:ncols], AF.Exp, [f"ps{sp_}"], [f"PT{pi}"], scale=0.125)
                        for j in range(nqc):
                            MM(PS[j][:, 0:vw + 1], PT[pi][:, j * 128:(j + 1) * 128], VX[:, kc, 0:vw + 1],
                               kn == 0, kn == len(kcs) - 1, [f"PT{pi}", f"VX{kc}"], [f"ps{j}"])
                        pend = nxt
                    for j in range(nqc):
                        rz, rk = stat()
                        S.add("dve", lambda e, rz=rz, j=j, vw=vw: e.reciprocal(rz, PS[j][:, vw:vw + 1]), reads=[f"ps{j}"], writes=[rk])
                        if kind == "B":
                            TS("dve", AOK[:, j, hi * 64:(hi + 1) * 64], PS[j][:, 0:64], rz, None, ALU.mult, None,
                               [f"ps{j}", rk], [f"AOK{j}"])
                        elif hi == 0:
                            TS("dve", O1[:, j, :], PS[j][:, 0:128], rz, None, ALU.mult, None, [f"ps{j}", rk], [f"O1{j}"])
                        else:
                            r2, r2k = stat()
                            TT("dve", r2, rz, nlam, ALU.mult, [rk, "lamt"], [r2k])
                            oo = OO[j % 2]
                            STT("dve", oo, PS[j][:, 0:128], r2, O1[:, j, :], ALU.mult, ALU.add,
                                [f"ps{j}", r2k, f"O1{j}"], [f"OO{j % 2}"])
                            ssq, ssk = stat()
                            TT("dve", T1[j % 2][:, 0:128], oo, oo, ALU.mult, [f"OO{j % 2}"], [f"T1{j % 2}"])
                            S.add("dve", lambda e, ssq=ssq, j=j: e.tensor_reduce(ssq, T1[j % 2][:, 0:128], AX.X, ALU.add),
                                  reads=[f"T1{j % 2}"], writes=[ssk])
                            rs, rsk = stat()
                            rstd_from(rs, rsk, ssq, ssk, 1.0 / 128, RMS_EPS)
                            STT("dve", AOK[:, j, 0:128], oo, rs, SUBG, ALU.mult, ALU.mult,
                                [f"OO{j % 2}", rsk, "SUBG"], [f"AOK{j}"])
                    last = (kind == "A" and hi == 1) or (kind == "B" and hi == 3)
                    if last:
                        for j in range(nqc):
                            tb = 6 + (j % 2)
                            ntr = 1 if kind == "A" else 2
                            for t in range(ntr):
                                TR(PSB[tb][:, t * 128:(t + 1) * 128], AOK[:, j, t * 128:(t + 1) * 128], identb[:],
                                   [f"AOK{j}", "identb"], [f"ps{tb}"])
                            m0 = idx if kind == "A" else 4 + idx * 2
                            tc = qc0 + j
                            CP("act", AOT[:, m0:m0 + ntr, tc * 128:(tc + 1) * 128],
                               PSB[tb][:, 0:ntr * 128].rearrange("p (s t) -> p s t", s=ntr), [f"ps{tb}"], [f"AOT{tc}"])
        barrier()
        reload_X(NCH)
        outproj_ln(0, NCH)

    if stage == 2:
        for c in range(NCH):
            DMA("sp", dbg_d[c * 128:(c + 1) * 128, :], X[:, c, :], [f"X{c}"], ["dbgd"], "dbg")
        S.add("sp", None, reads=list(S.lw.keys()) + ["dbgd"], writes=())
        S.emit(nc, es)
        es.close()
        return nc
    router_d = dram("moe_router", [2, D, NE])
    NEX = 1 if stage == 31 else NE
    wg_d = dram("moe_w_gate", [2, NEX, D, FF])
    wu_d = dram("moe_w_up", [2, NEX, D, FF])
    wd_d = dram("moe_w_down", [2, NEX, FF, D])
    iota_d = dram("iota288", [128, 288])
    utri_d = dram("utri", [128, 128])
    aotb = AOT[:].rearrange("p k t -> p (k t)")
    wob = WO[:].rearrange("p k c -> p (k c)")
    hmb = hT[:].rearrange("p k t -> p (k t)").rearrange("p (c f) -> p c f", c=NCH)
    RING = [aotb[:, i * 2048:(i + 1) * 2048] for i in range(5)]
    RING += [bc[i].bitcast(BF16) for i in (2, 3, 4, 5)]
    _rpb = ropet[:].rearrange("p c f -> p (c f)").bitcast(BF16)
    RING += [_rpb[:, i * 2048:(i + 1) * 2048] for i in range(2)]
    NRING = len(RING)
    XGT = aotb[:, 10240:12544].rearrange("p (k s) -> p k s", k=8)
    HIDT = aotb[:, 12544:17152].rearrange("p (j s) -> p j s", j=16)
    SELR = [aotb[:, 17152 + i * 288:17152 + (i + 1) * 288] for i in range(2)]
    SELGT = [aotb[:, 17728 + i * 384:17728 + (i + 1) * 384].rearrange("p (s t) -> p s t", s=3) for i in range(1)]
    SELR += [wob[:, 5280 + i * 288:5280 + (i + 1) * 288] for i in range(3)]
    SELGT += [wob[:, 6144 + i * 384:6144 + (i + 1) * 384].rearrange("p (s t) -> p s t", s=3) for i in range(2)]
    NSEL = len(SELR)
    NSGT = len(SELGT)
    YG = wob[:, 0:3072].rearrange("p (s f) -> p s f", s=3)
    IOTA = wob[:, 3072:3648].bitcast(F32)
    RW = wob[:, 3648:3904].bitcast(F32).rearrange("p (k e) -> p k e", k=8)
    UTRI = wob[:, 3904:4032]
    ONESB = wob[:, 4032:4160]
    MASKB = wob[:, 4160:4448].rearrange("p (c e) -> p c e", c=NCH)
    SGR = [wob[:, 4448:5024].bitcast(F32), wob[:, 6912:7488].bitcast(F32)]
    UTF = wob[:, 5024:5280].bitcast(F32)
    WEX = ropet[:].rearrange("p c f -> p (c f)")
    AFF = hb[0][:].bitcast(F32)[:, 0:288].rearrange("p (c e) -> p c e", c=NCH)
    MASK = hb[1][:].bitcast(F32)[:, 0:288].rearrange("p (c e) -> p c e", c=NCH)
    mo2 = sb("mo2", [128, 2, NCH * NE])
    SLOT = mo2[:, 0, :].rearrange("p (c e) -> p c e", c=NCH)
    GS = mo2[:, 1, :].rearrange("p (c e) -> p c e", c=NCH)
    m8 = sb("m8", [16, 8])

    def moe_layer(l, nch, final_out=False):
        ns = 256 + (32 if nch > NLAT else 0)
        nsc = (ns + 127) // 128
        scsz = [min(128, ns - s * 128) for s in range(nsc)]
        barrier()
        DMA("sp", IOTA, iota_d, (), ["IOTA"], "c3")
        DMA("sp", UTF, utri_d, (), ["UTF"], "c3")
        DMA("sp", RW, router_d[l].rearrange("(k p) e -> p k e", p=128), (), ["RW"], "c3")
        fence(["IOTA", "UTF", "RW"])
        CP("dve", UTRI, UTF, ["UTF"], ["UTRI"])
        MEMSET("dve", ONESB, 1.0, ["ONESB"])
        bcast_mod(0, l, 0, 4, True)
        bcast_mod(1, l, 0, 3)
        if nch > NLAT:
            bcast_mod(2, l, 1, 4, True)
            bcast_mod(3, l, 1, 3)
        for c in range(nch):
            a, b_ = (0, 1) if c < NLAT else (2, 3)
            TT("dve", bc[4], X[:, c, :], bc[a], ALU.mult, [f"X{c}", f"bc{a}"], ["bc4"])
            TT("dve", bc[4], bc[4], bc[b_], ALU.add, ["bc4", f"bc{b_}"], ["bc4"])
            CP("act", hmb[:, c, :], bc[4], ["bc4"], [f"hmb{c}"])
            TS("dve", X[:, c, :], X[:, c, :], ALPHA, None, ALU.mult, None, [f"X{c}"], [f"X{c}"])
            for k in range(8):
                pb = k // 4
                TR(PS[pb][:, (k % 4) * 128:(k % 4 + 1) * 128], bc[4][:, k * 128:(k + 1) * 128], identf[:],
                   ["bc4", "identf"], [f"ps{pb}"])
            hmT = bc[5].rearrange("p (k t) -> p k t", k=8)
            CP("act", hmT[:, 0:4, :], PS[0][:, :].rearrange("p (k t) -> p k t", k=4), ["ps0"], ["bc5"])
            CP("dve", hmT[:, 4:8, :], PS[1][:, :].rearrange("p (k t) -> p k t", k=4), ["ps1"], ["bc5"])
            for k in range(8):
                MM(PS[2][:, 0:NE], hmT[:, k, :], RW[:, k, :], k == 0, k == 7, ["bc5", "RW"], ["ps2"])
            mx, mxk = stat()
            S.add("dve", lambda e, mx=mx: e.tensor_reduce(mx, PS[2][:, 0:NE], AX.X, ALU.max), reads=["ps2"], writes=[mxk])
            nmx, nmk = stat()
            TS("dve", nmx, mx, -1.0, None, ALU.mult, None, [mxk], [nmk])
            ACT(AFF[:, c, :], PS[2][:, 0:NE], AF.Exp, ["ps2", nmk], [f"AFF{c}"], bias=nmx)
            sm, smk = stat()
            S.add("dve", lambda e, sm=sm, c=c: e.tensor_reduce(sm, AFF[:, c, :], AX.X, ALU.add), reads=[f"AFF{c}"], writes=[smk])
            rc, rck = stat()
            S.add("dve", lambda e, rc=rc, sm=sm: e.reciprocal(rc, sm), reads=[smk], writes=[rck])
            TS("dve", AFF[:, c, :], AFF[:, c, :], rc, None, ALU.mult, None, [f"AFF{c}", rck], [f"AFF{c}"])
            TR(PS[3][0:NE, 0:128], AFF[:, c, :], identf[:], [f"AFF{c}", "identf"], ["ps3"])
            CP("dve", WEX[0:NE, c * 128:(c + 1) * 128], PS[3][0:NE, 0:128], ["ps3"], ["WEX"])
        sets = [(0, SEQ, 256)] + ([(SEQ, NT, 32)] if nch > NLAT else [])
        for (t0, t1, cap) in sets:
            wv = WEX[0:NE, t0:t1]
            for r in range(cap // 8):
                S.add("dve", lambda e, wv=wv: e.max(m8[:], wv), reads=["WEX"], writes=["m8"])
                S.add("dve", lambda e, wv=wv: e.match_replace(wv, m8[:], wv, -1.0), reads=["WEX", "m8"], writes=["WEX"])
        TS("dve", WEX[0:NE, 0:nch * 128], WEX[0:NE, 0:nch * 128], 0.0, None, ALU.is_lt, None, ["WEX"], ["WEX"])
        for c in range(nch):
            TR(PS[4][:, c * NE:(c + 1) * NE], WEX[0:NE, c * 128:(c + 1) * 128], identf[0:NE, 0:NE], ["WEX", "identf"], ["ps4"])
        mflat = mo2[:, 0, 0:nch * NE]
        CP("dve", MASK[:, 0:nch, :], PS[4][:, 0:nch * NE].rearrange("p (c e) -> p c e", c=nch), ["ps4"], ["MASK"])
        CP("dve", MASKB[:, 0:nch, :], MASK[:, 0:nch, :], ["MASK"], ["MASKB"])
        TT("dve", GS[:, 0:nch, :], AFF[:, 0:nch, :], MASK[:, 0:nch, :], ALU.mult, [f"AFF{c}" for c in range(nch)] + ["MASK"], ["GS"])
        for c in range(nch):
            c0 = 0 if c < NLAT else NLAT
            prev = list(range(c0, c))
            for i, cp in enumerate(prev):
                MM(PS[5][:, c * NE:(c + 1) * NE], ONESB, MASKB[:, cp, :], i == 0, False, ["ONESB", "MASKB"], ["ps5"])
            MM(PS[5][:, c * NE:(c + 1) * NE], UTRI, MASKB[:, c, :], len(prev) == 0, True, ["UTRI", "MASKB"], ["ps5"])
        TS("dve", SLOT[:, 0:NLAT, :], PS[5][:, 0:NLAT * NE].rearrange("p (c e) -> p c e", c=NLAT), 1.0, None, ALU.add, None, ["ps5"], ["SLOT"])
        if nch > NLAT:
            TS("dve", SLOT[:, NLAT:nch, :], PS[5][:, NLAT * NE:nch * NE].rearrange("p (c e) -> p c e", c=nch - NLAT), 257.0, None,
               ALU.add, None, ["ps5"], ["SLOT"])
        TT("dve", SLOT[:, 0:nch, :], SLOT[:, 0:nch, :], MASK[:, 0:nch, :], ALU.mult, ["SLOT", "MASK"], ["SLOT"])
        TS("dve", SLOT[:, 0:nch, :], SLOT[:, 0:nch, :], -1.0, None, ALU.add, None, ["SLOT"], ["SLOT"])
        bcast_mod(0, l, 0, 5)
        if nch > NLAT:
            bcast_mod(1, l, 1, 5)
        barrier()
        units = []
        for e_ in range(NEX):
            for fu in range(8):
                units.append(("g", e_, fu))
                units.append(("u", e_, fu))
            for du in range(8):
                units.append(("d", e_, du))
        issued = [0]

        def issue_until(n):
            while issued[0] < min(n, len(units)):
                kind, e_, i = units[issued[0]]
                slot = issued[0] % NRING
                if kind == "d":
                    src = wd_d[l, e_, i * 256:(i + 1) * 256, :].rearrange("(j p) c -> p j c", p=128)
                    dst = RING[slot].rearrange("p (j c) -> p j c", j=2)
                else:
                    wsrc = wg_d if kind == "g" else wu_d
                    src = wsrc[l, e_, :, i * 256:(i + 1) * 256].rearrange("(k p) c -> p k c", p=128)
                    dst = RING[slot].rearrange("p (k c) -> p k c", k=8)
                DMA("pool", dst, src, (), [f"wr{slot}"], f"wr{slot}")
                issued[0] += 1

        ui = [0]

        def next_unit():
            i = ui[0]
            ui[0] += 1
            return i % NRING

        def refill():
            issue_until(ui[0] + NRING)

        issue_until(NRING)
        seln = [0]
        sgn = [0]
        for e_ in range(NEX):
            for half in range(2):
                for c in range(nch):
                    si = seln[0] % NSEL
                    seln[0] += 1
                    s0, s1 = (0, 256) if c < NLAT else (256, ns)
                    cfirst, clast = (0, NLAT - 1) if c < NLAT else (NLAT, nch - 1)
                    TS("dve", SELR[si][:, s0:s1], IOTA[:, s0:s1], SLOT[:, c, e_:e_ + 1], None, ALU.is_equal, None,
                       ["IOTA", "SLOT"], [f"SEL{si}"])
                    for f4 in range(4):
                        f = half * 4 + f4
                        MM(PS[f4][:, s0:s1], hmb[:, c, f * 128:(f + 1) * 128], SELR[si][:, s0:s1], c == cfirst, c == clast,
                           [f"hmb{c}", f"SEL{si}"], [f"ps{f4}"])
                for f4 in range(4):
                    f = half * 4 + f4
                    CP("act" if f4 % 2 == 0 else "dve", XGT[:, f, 0:ns], PS[f4][:, 0:ns], [f"ps{f4}"], ["XGT"])
            for fu in range(8):
                sg_ = next_unit()
                su_ = next_unit()
                wgv = RING[sg_].rearrange("p (k c) -> p k c", k=8)
                wuv = RING[su_].rearrange("p (k c) -> p k c", k=8)
                for j2 in range(2):
                    jg = fu * 2 + j2
                    bg, bu = (4, 5) if jg % 2 == 0 else (6, 7)
                    for k in range(8):
                        MM(PS[bg][:, 0:ns], wgv[:, k, j2 * 128:(j2 + 1) * 128], XGT[:, k, 0:ns], k == 0, k == 7,
                           [f"wr{sg_}", "XGT"], [f"ps{bg}"])
                    for k in range(8):
                        MM(PS[bu][:, 0:ns], wuv[:, k, j2 * 128:(j2 + 1) * 128], XGT[:, k, 0:ns], k == 0, k == 7,
                           [f"wr{su_}", "XGT"], [f"ps{bu}"])
                    SG = SGR[jg % 2]
                    ACT(SG[:, 0:ns], PS[bg][:, 0:ns], AF.Silu, [f"ps{bg}"], [f"SG{jg % 2}"])
                    TT("dve", HIDT[:, jg, 0:ns], PS[bu][:, 0:ns], SG[:, 0:ns], ALU.mult, [f"ps{bu}", f"SG{jg % 2}"], [f"HID{jg}"])
                refill()
            for du in range(8):
                sd_ = next_unit()
                wdv = RING[sd_].rearrange("p (j c) -> p j c", j=2)
                for sc in range(nsc):
                    sz = scsz[sc]
                    for hf in range(2):
                        pb = sc * 2 + hf
                        for jj in range(2):
                            jg = du * 2 + jj
                            MM(PS[pb][0:sz, :], HIDT[:, jg, sc * 128:sc * 128 + sz], wdv[:, jj, hf * 512:(hf + 1) * 512],
                               du == 0 and jj == 0, du == 7 and jj == 1, [f"HID{jg}", f"wr{sd_}"], [f"ps{pb}"])
                refill()
            for sc in range(nsc):
                sz = scsz[sc]
                gt = 0 if sc < 2 else 1
                for hf in range(2):
                    pb = sc * 2 + hf
                    TT("dve", YG[0:sz, sc, hf * 512:(hf + 1) * 512], PS[pb][0:sz, :], bc[gt][0:sz, hf * 512:(hf + 1) * 512], ALU.mult,
                       [f"ps{pb}", f"bc{gt}"], ["YG"])
            if stage == 31:
                d1 = dram("d_slot", [128, 2 * NCH * NE], kind="ExternalOutput")
                DMA("sp", d1, mo2[:].rearrange("p a f -> p (a f)"), ["SLOT", "GS"], ["dbgd"], "dbg")
                d2 = dram("d_xgt", [128, 8 * 288], BF16, kind="ExternalOutput")
                DMA("sp", d2, XGT.rearrange("p k s -> p (k s)"), ["XGT"], ["dbgd"], "dbg")
                d3 = dram("d_yg", [128, 3 * 1024], BF16, kind="ExternalOutput")
                DMA("sp", d3, YG.rearrange("p s f -> p (s f)"), ["YG"], ["dbgd"], "dbg")
                d4 = dram("d_hid", [128, 16 * 288], BF16, kind="ExternalOutput")
                DMA("sp", d4, HIDT.rearrange("p j s -> p (j s)"), [f"HID{j}" for j in range(16)], ["dbgd"], "dbg")
                S.add("sp", None, reads=list(S.lw.keys()) + ["dbgd"], writes=())
                S.emit(nc, es)
                es.close()
                return "STOP"
            def sc_stage_a(c):
                si = seln[0] % NSEL
                seln[0] += 1
                s0, s1 = (0, 256) if c < NLAT else (256, ns)
                scs_ = [0, 1] if c < NLAT else [2]
                TS("dve", SELR[si][:, s0:s1], IOTA[:, s0:s1], SLOT[:, c, e_:e_ + 1], GS[:, c, e_:e_ + 1], ALU.is_equal, ALU.mult,
                   ["IOTA", "SLOT", "GS"], [f"SEL{si}"])
                tb = 6 + (c % 2)
                for sc in scs_:
                    sz = scsz[sc]
                    TR(PSB[tb][0:sz, sc * 128:(sc + 1) * 128], SELR[si][:, sc * 128:sc * 128 + sz], identb[:],
                       [f"SEL{si}", "identb"], [f"ps{tb}"])
                gi_ = sgn[0] % NSGT
                sgn[0] += 1
                sgt = SELGT[gi_]
                if c < NLAT:
                    CP("act", sgt[:, 0:2, :], PSB[tb][:, 0:256].rearrange("p (s t) -> p s t", s=2), [f"ps{tb}"], [f"SELGT{gi_}"])
                else:
                    CP("act", sgt[0:scsz[2], 2, :], PSB[tb][0:scsz[2], 256:384], [f"ps{tb}"], [f"SELGT{gi_}"])
                return gi_

            def sc_stage_b(c, gi_):
                sgt = SELGT[gi_]
                scs_ = [0, 1] if c < NLAT else [2]
                for hf in range(2):
                    pb = 2 * (c % 2) + hf
                    for sc in scs_:
                        sz = scsz[sc]
                        MM(PS[pb][:, :], sgt[0:sz, sc, :], YG[0:sz, sc, hf * 512:(hf + 1) * 512], sc == scs_[0], sc == scs_[-1],
                           [f"SELGT{gi_}", "YG"], [f"ps{pb}"])
                    TT("dve", X[:, c, hf * 512:(hf + 1) * 512], PS[pb][:, :], X[:, c, hf * 512:(hf + 1) * 512], ALU.add,
                       [f"ps{pb}", f"X{c}"], [f"X{c}"])

            SLOOK = 2
            pend_sc = []
            for c in range(nch):
                pend_sc.append((c, sc_stage_a(c)))
                if len(pend_sc) > SLOOK:
                    sc_stage_b(*pend_sc.pop(0))
            while pend_sc:
                sc_stage_b(*pend_sc.pop(0))
        barrier()
        bcast_row(2, ln_g_d[l, 1:2, :])
        bcast_row(3, ln_b_d[l, 1:2, :])
        for c in range(nch):
            layer_norm_chunk(c, 2, 3)
            if final_out:
                DMA("sp", out_d[c * 128:(c + 1) * 128, :], X[:, c, :], [f"X{c}"], ["outd"], "outw")
        barrier()

    if moe_layer(0, NCH) == "STOP":
        return nc

    if stage == 3:
        for c in range(NCH):
            DMA("sp", dbg_d[c * 128:(c + 1) * 128, :], X[:, c, :], [f"X{c}"], ["dbgd"], "dbg")
        S.add("sp", None, reads=list(S.lw.keys()) + ["dbgd"], writes=())
        S.emit(nc, es)
        es.close()
        return nc

    w_in1_d = dram("l1_w_in", [D, 2048])
    w_out1_d = dram("l1_w_out", [D, D])
    poolw_d = dram("l1_pool_w", [4, 128, 128])
    psc_d = dram("psc", [128, 4])
    band_d = dram("band", [20, 128, 128])
    l1tab_d = dram("l1tab", [NPAT, 8, 128, 128])

    def layer1_mixer():
        l = 1
        barrier()
        DMA("pool", WO[:], w_out1_d.rearrange("(k p) c -> p k c", p=128), (), ["WO"], "wo")
        modulate_T(1, 1, 0, NCH)
        spill_X(NLAT)
        barrier()
        arena_off[0] = 0
        W1R = [xview(2048, None, BF16, "p (k c) -> p k c", k=8) for _ in range(2)]
        QT1 = xview(4 * SEQ // 2, None, BF16, "p (g t) -> p g t", g=4)
        KT1 = xview(4 * NT // 2, None, BF16, "p (g t) -> p g t", g=4)
        VXU = xview(NCH * 8 * 65 // 2, None, BF16)
        VX1 = VXU.rearrange("p (c h f) -> p c h f", c=NCH, h=8)
        U1 = VXU[:, 0:NLAT * 512].rearrange("p (c f) -> p c f", c=NLAT)
        PT1 = [xview(256, None, BF16) for _ in range(3)]
        rp = ropet[:].rearrange("p c f -> p (c f)")
        TBI = rp[:, 0:1280].bitcast(BF16).rearrange("p (a h k) -> p a h k", a=5, h=4)
        TBB = rp[:, 1280:2304].bitcast(BF16).rearrange("p (a h k) -> p a h k", a=4, h=4)
        BAND = hb[0][:].rearrange("p (m t) -> p m t", m=8)
        PW = hb[1][:, 0:512].rearrange("p (g e) -> p g e", g=4)
        AOK1 = hb[1][:, 512:768]
        POOLT = hb[1][:, 768:1024].bitcast(F32) if False else None
        psc = sb("pscs", [128, 4])
        qflat = QT1.rearrange("p g t -> p (g t)")
        bandb = qflat[:, 0:2560].rearrange("p (m t) -> p m t", m=20)
        poolt = [qflat[:, 2560 + i * 512:2560 + (i + 1) * 512] for i in range(2)]
        DMA("sp", psc[:], psc_d, (), ["psc"], "c4")
        DMA("pool", bandb, band_d.rearrange("m a b -> a m b"), (), ["bandb"], "c4p")
        DMA("pool", PW, poolw_d.rearrange("g c e -> c g e"), (), ["PW"], "c4p")
        fence(["psc", "bandb", "PW"])

        def load_w1(sec, slot):
            DMA("pool", W1R[slot][:], w_in1_d[:, sec * 512:(sec + 1) * 512].rearrange("(k p) c -> p k c", p=128),
                (), [f"W1R{slot}"], f"w1r{slot}")

        load_w1(3, 0)
        load_w1(0, 1)
        for c in range(NLAT):
            pb = 4 + (c % 2)
            for k in range(8):
                MM(PS[pb][:, :], hT[:, k, c * 128:(c + 1) * 128], W1R[0][:, k, :], k == 0, k == 7, [f"hT{c}", "W1R0"], [f"ps{pb}"])
            CP("act" if c % 2 == 0 else "dve", U1[:, c, :], PS[pb][:, :], [f"ps{pb}"], [f"U1{c}"])
        pn = [0]
        for g in range(4):
            for cb in range(4):
                pb = 6 + (pn[0] % 2)
                for j in range(4):
                    c = cb * 4 + j
                    srcs = []
                    if c > 0:
                        srcs.append((c - 1, g * 5 + 0))
                    srcs.append((c, g * 5 + (3 if c == 0 else 4 if c == NLAT - 1 else 1)))
                    if c < NLAT - 1:
                        srcs.append((c + 1, g * 5 + 2))
                    for i, (cs, m) in enumerate(srcs):
                        MM(PS[pb][:, j * 128:(j + 1) * 128], U1[:, cs, g * 128:(g + 1) * 128], bandb[:, m, :], i == 0, i == len(srcs) - 1,
                           [f"U1{cs}", "bandb"], [f"ps{pb}"])
                pt_ = poolt[pn[0] % 2]
                CP("act", pt_, PS[pb][:, :], [f"ps{pb}"], [f"poolt{pn[0] % 2}"])
                pb2 = 4 + (pn[0] % 2)
                MM(PS[pb2][:, :], PW[:, g, :], pt_, True, True, ["PW", f"poolt{pn[0] % 2}"], [f"ps{pb2}"])
                TS("dve", AOT[:, 4 + g, cb * 512:(cb + 1) * 512], PS[pb2][:, :], psc[:, g:g + 1], None, ALU.mult, None,
                   [f"ps{pb2}", "psc"], [f"AOT{cb * 4 + j}" for j in range(4)])
                pn[0] += 1
        barrier()
        MEMSET("dve", VX1[:, :, :, 64:65], 1.0, [f"VX{c}" for c in range(NCH)])
        for g in range(4):
            for tb_ in range(4):
                pb = 4 + ((g * 4 + tb_) % 2)
                for k in range(8):
                    MM(PS[pb][:, :], W1R[1][:, k, g * 128:(g + 1) * 128], hT[:, k, tb_ * 512:(tb_ + 1) * 512], k == 0, k == 7,
                       ["W1R1"] + [f"hT{tb_ * 4 + j}" for j in range(4)], [f"ps{pb}"])
                ACT(QT1[:, g, tb_ * 512:(tb_ + 1) * 512], PS[pb][:, :], AF.Identity, [f"ps{pb}"], [f"QT{tb_ * 4 + j}" for j in range(4)], scale=0.125)
        load_w1(1, 0)
        load_w1(2, 1)
        blocks_k = [(i * 512, 512) for i in range(4)] + [(2048, 256)]
        for g in range(4):
            for bi, (t0, tn) in enumerate(blocks_k):
                pb = 4 + ((g * 5 + bi) % 2)
                chs = list(range(t0 // 128, (t0 + tn) // 128))
                for k in range(8):
                    MM(PS[pb][:, 0:tn], W1R[0][:, k, g * 128:(g + 1) * 128], hT[:, k, t0:t0 + tn], k == 0, k == 7,
                       ["W1R0"] + [f"hT{c}" for c in chs], [f"ps{pb}"])
                CP("act" if bi % 2 == 0 else "dve", KT1[:, g, t0:t0 + tn], PS[pb][:, 0:tn], [f"ps{pb}"], [f"KT{c}" for c in chs])
        for c in range(NCH):
            pb = 6 + (c % 2)
            for k in range(8):
                MM(PS[pb][:, :], hT[:, k, c * 128:(c + 1) * 128], W1R[1][:, k, :], k == 0, k == 7, [f"hT{c}", "W1R1"], [f"ps{pb}"])
            CP("act" if c % 2 == 0 else "dve", VX1[:, c, :, 0:64], PS[pb][:, :].rearrange("p (h f) -> p h f", h=8), [f"ps{pb}"], [f"VX{c}"])
        ptn = [0]
        for hh in range(2):
            for a in range(5):
                DMA("pool", TBI[:, a, :, :], l1tab_d[INT_PATS[a], hh * 4:(hh + 1) * 4].rearrange("h q k -> q h k"), (), ["TBI"], "tbi")
            for c in range(NLAT):
                loc = L1_LOCAL[c]
                interior = (2 <= c <= 13)
                if not interior:
                    for a, (kc, pat) in enumerate(loc):
                        DMA("pool", TBB[:, a, :, :], l1tab_d[pat, hh * 4:(hh + 1) * 4].rearrange("h q k -> q h k"), (), ["TBB"], "tbb")
                kcs = [(kc, a) for a, (kc, pat) in enumerate(loc)] + [(16, None), (17, None)]
                def score1(kn):
                    kc, a = kcs[kn]
                    bpair = (4, 5) if ptn[0] % 2 == 0 else (6, 7)
                    pi = ptn[0] % 3
                    ptn[0] += 1
                    for par in range(2):
                        bk = bpair[par]
                        for i2, h4 in enumerate((par, par + 2)):
                            h = hh * 4 + h4
                            g = h // 2
                            pbase = (h % 2) * 64
                            MM(PS[bk][:, i2 * 128:(i2 + 1) * 128], KT1[pbase:pbase + 64, g, kc * 128:(kc + 1) * 128],
                               QT1[pbase:pbase + 64, g, c * 128:(c + 1) * 128], True, a is None, [f"KT{kc}", f"QT{c}"], [f"ps{bk}"])
                            if a is not None:
                                tb_ap = TBI[:, a, h4, :] if interior else TBB[:, a, h4, :]
                                MM(PS[bk][:, i2 * 128:(i2 + 1) * 128], tb_ap, identb[:], False, True,
                                   ["TBI" if interior else "TBB", "identb"], [f"ps{bk}"])
                    return bpair, pi

                pend = score1(0)
                for kn, (kc, a) in enumerate(kcs):
                    nxt = score1(kn + 1) if kn + 1 < len(kcs) else None
                    bpair, pi = pend
                    for par in range(2):
                        bk = bpair[par]
                        ACT(PT1[pi][:, par * 256:(par + 1) * 256], PS[bk][:, 0:256], AF.Exp, [f"ps{bk}"], [f"PT{pi}_{par}"])
                    for h4 in range(4):
                        h = hh * 4 + h4
                        par, i2 = h4 % 2, h4 // 2
                        o_ = par * 256 + i2 * 128
                        MM(PS[h4][:, 0:65], PT1[pi][:, o_:o_ + 128], VX1[:, kc, h, :], kn == 0, kn == len(kcs) - 1,
                           [f"PT{pi}_{par}", f"VX{kc}"], [f"ps{h4}"])
                    pend = nxt
                for h4 in range(4):
                    rz, rk = stat()
                    S.add("dve", lambda e, rz=rz, h4=h4: e.reciprocal(rz, PS[h4][:, 64:65]), reads=[f"ps{h4}"], writes=[rk])
                    TS("dve", AOK1[:, h4 * 64:(h4 + 1) * 64], PS[h4][:, 0:64], rz, None, ALU.mult, None, [f"ps{h4}", rk], ["AOK1"])
                tb = 6 + (c % 2)
                for t in range(2):
                    TR(PSB[tb][:, t * 128:(t + 1) * 128], AOK1[:, t * 128:(t + 1) * 128], identb[:], ["AOK1", "identb"], [f"ps{tb}"])
                CP("act", AOT[:, 2 * hh:2 * hh + 2, c * 128:(c + 1) * 128], PSB[tb][:, 0:256].rearrange("p (s t) -> p s t", s=2),
                   [f"ps{tb}"], [f"AOT{c}"])
        barrier()
        reload_X(NLAT)
        outproj_ln(1, NLAT)

    layer1_mixer()
    if stage == 4:
        for c in range(NLAT):
            DMA("sp", dbg_d[c * 128:(c + 1) * 128, :], X[:, c, :], [f"X{c}"], ["dbgd"], "dbg")
        S.add("sp", None, reads=list(S.lw.keys()) + ["dbgd"], writes=())
        S.emit(nc, es)
        es.close()
        return nc
    moe_layer(1, NLAT, final_out=True)
    S.add("sp", None, reads=list(S.lw.keys()) + ["outd"], writes=())
    S.emit(nc, es)
    es.close()
    return nc


def _rope_table():
    t = np.arange(SEQ)
    inv = (10000.0 ** (-np.arange(16, dtype=np.float32) / 16)).astype(np.float32)
    ar = (t // 64).astype(np.float32)[:, None] * inv
    ac = (t % 64).astype(np.float32)[:, None] * inv
    cr, sr, cc_, sc_ = np.cos(ar), np.sin(ar), np.cos(ac), np.sin(ac)
    cosf = np.concatenate([cr, cr, cc_, cc_], axis=1)
    sins = np.concatenate([-sr, sr, -sc_, sc_], axis=1)
    tab = np.concatenate([cosf, sins], axis=1).astype(np.float32)
    ctxr = np.tile(np.array([1.0] * 64 + [0.0] * 64, np.float32), (CTX, 1))
    return np.concatenate([tab, ctxr], axis=0)


def make_in_maps(inp):
    f = lambda a: np.ascontiguousarray(np.asarray(a, dtype=np.float32))
    shared = {
        "ada_w": f(inp["ada_w"]), "ada_b": f(inp["ada_b"]), "ln_g": f(inp["ln_g"]), "ln_b": f(inp["ln_b"]),
        "l0_w_in": f(inp["l0_w_in"]), "l0_w_out": f(inp["l0_w_out"]),
        "lamv": f(np.stack([inp["l0_lam_q1"], inp["l0_lam_k1"], inp["l0_lam_q2"], inp["l0_lam_k2"]])),
        "l0_subln_g": f(inp["l0_subln_g"]),
        "qkg": f(np.concatenate([np.tile(np.asarray(inp["l0_qnorm_g"]), 4), np.asarray(inp["l0_knorm_g"])])),
        "rope": _rope_table(), "ident": np.eye(128, dtype=np.float32),
        "moe_router": f(inp["moe_router"]), "moe_w_gate": f(inp["moe_w_gate"]), "moe_w_up": f(inp["moe_w_up"]),
        "moe_w_down": f(inp["moe_w_down"]),
        "l1_w_in": f(inp["l1_w_in"]), "l1_w_out": f(inp["l1_w_out"]), "l1_pool_w": f(inp["l1_pool_w"]),
        "psc": f(np.asarray(inp["l1_pool_scale"]).reshape(4, 128).T), "band": _band_mats(),
        "l1tab": _l1_table(inp["l1_rpb"]),
        "iota288": np.tile(np.arange(288, dtype=np.float32), (128, 1)),
        "utri": np.triu(np.ones((128, 128), np.float32), 1),
    }
    maps = []
    x = np.asarray(inp["x"]); ctx = np.asarray(inp["ctx"]); c = np.asarray(inp["c"]); cctx = np.asarray(inp["c_ctx"])
    for b in range(8):
        m = dict(shared)
        m["x"] = f(np.concatenate([x[b], ctx[b]], axis=0))
        cc = np.stack([c[b].reshape(8, 128).T, cctx.reshape(8, 128).T], axis=-1)
        m["cc"] = f(cc)
        maps.append(m)
    return maps


def kernel(**inputs):
    nc = build()
    maps = make_in_maps(inputs)
    res = run_bass_kernel_spmd(nc, maps, core_ids=list(range(8)))
    return np.stack([r["out"] for r in res.results], axis=0)
```

```python
import math
import numpy as np
from contextlib import ExitStack
import concourse.bass as bass
import concourse.mybir as mybir
from concourse.bass_utils import run_bass_kernel_spmd

F32 = mybir.dt.float32
BF16 = mybir.dt.bfloat16
AF = mybir.ActivationFunctionType
ALU = mybir.AluOpType
AX = mybir.AxisListType

D = 1024
SEQ = 2048
CTX = 256
NT = SEQ + CTX
NCH = NT // 128
NLAT = SEQ // 128
NE = 16
FF = 2048
ALPHA = 4.0 ** 0.25
LN_EPS = 1e-5
RMS_EPS = 1e-6
LAM_INIT0 = 0.8 - 0.6 * math.exp(0.0)
NEG = -30000.0


class Sched:
    def __init__(self):
        self.ops = []
        self.lw = {}
        self.rd = {}

    def add(self, eng, fn, reads=(), writes=(), dma=None):
        i = len(self.ops)
        stream = ("dma", dma) if dma is not None else eng
        pk = [k for k in reads if k.startswith("ps") and k[2:].isdigit()]
        if pk:
            writes = list(writes) + [k for k in pk if k not in writes]
        raw = set()
        war = set()
        for k in reads:
            w = self.lw.get(k)
            if w is not None:
                raw.add(w)
        for k in writes:
            w = self.lw.get(k)
            if w is not None:
                raw.add(w)
            for r in self.rd.get(k, {}).values():
                war.add(r)
        if fn is not None:
            for k in writes:
                self.lw[k] = i
                self.rd[k] = {}
            for k in reads:
                self.rd.setdefault(k, {})[stream] = i
        self.ops.append(dict(eng=eng, fn=fn, raw=raw, war=war, dma=dma, stream=stream, cons=False, ms=0))
        return i

    def emit(self, nc, es):
        ops = self.ops
        for o in ops:
            for d in o["raw"] | o["war"]:
                if d == len(ops):
                    continue
                p = ops[d]
                same = (p["stream"] == o["stream"])
                if same and (o["stream"] == "pe"):
                    continue
                if same and d in o["war"] and d not in o["raw"]:
                    continue
                p["cons"] = True
        sems = {}
        cnt = {}

        def get_sem(stream):
            if stream not in sems:
                nm = "s_" + (stream if isinstance(stream, str) else "d_" + str(stream[1]))
                sems[stream] = es.enter_context(nc.semaphore(nm))
                cnt[stream] = 0
            return sems[stream]

        for o in ops:
            st = o["stream"]
            get_sem(st)
            if o["dma"] is not None:
                cnt[st] += 16
                o["ms"] = cnt[st]
            elif o["cons"]:
                cnt[st] += 1
                o["ms"] = cnt[st]
        self.nsem = len(sems)
        block = es.enter_context(nc.Block())
        engs = ["pe", "act", "dve", "pool", "sp"]
        per = {e: [o for o in ops if o["eng"] == e] for e in engs}

        def run(e, eh):
            known = {}
            for o in per[e]:
                need = {}
                for d in o["raw"] | o["war"]:
                    p = ops[d]
                    same = (p["stream"] == o["stream"])
                    if same and o["stream"] == "pe":
                        continue
                    if same and d in o["war"] and d not in o["raw"]:
                        continue
                    if p["ms"] <= 0:
                        continue
                    s = p["stream"]
                    if need.get(s, 0) < p["ms"]:
                        need[s] = p["ms"]
                for s, v in need.items():
                    if known.get(s, 0) < v:
                        eh.wait_ge(sems[s], v)
                        known[s] = v
                if o["fn"] is None:
                    continue
                ins = o["fn"](eh)
                if o["dma"] is not None:
                    ins.then_inc(sems[o["stream"]], 16)
                elif o["cons"]:
                    ins.then_inc(sems[o["stream"]], 1)

        @block.tensor
        def _(eh):
            run("pe", eh)

        @block.scalar
        def _(eh):
            run("act", eh)

        @block.vector
        def _(eh):
            run("dve", eh)

        @block.gpsimd
        def _(eh):
            run("pool", eh)

        @block.sync
        def _(eh):
            run("sp", eh)


def _l1_patterns():
    W, rows, kh, kw = 64, 32, 8, 16
    cols = np.arange(W)
    cs = np.clip(cols - kw // 2, 0, W - kw)
    colok = (cols[None, :] >= cs[:, None]) & (cols[None, :] < cs[:, None] + kw)
    dcol = cols[None, :] - cols[:, None] + 15
    pats, sigs, local = [], {}, []
    for c in range(16):
        lst = []
        for kc in range(16):
            valid = np.zeros((128, 128), bool)
            dr = np.zeros((128, 128), np.int64)
            dc = np.zeros((128, 128), np.int64)
            for ql in range(2):
                r = 2 * c + ql
                rs = min(max(r - kh // 2, 0), rows - kh)
                for kl in range(2):
                    rk = 2 * kc + kl
                    rowok = rs <= rk < rs + kh
                    qs = slice(ql * 64, ql * 64 + 64)
                    ks = slice(kl * 64, kl * 64 + 64)
                    valid[qs, ks] = colok & rowok
                    dr[qs, ks] = rk - r + 7
                    dc[qs, ks] = dcol
            if not valid.any():
                continue
            dr = np.where(valid, dr, 0)
            dc = np.where(valid, dc, 0)
            sig = (valid.tobytes(), dr.tobytes())
            if sig not in sigs:
                sigs[sig] = len(pats)
                pats.append((valid, dr, dc))
            lst.append((kc, sigs[sig]))
        local.append(lst)
    return pats, local


_L1_PATS, L1_LOCAL = _l1_patterns()
NPAT = len(_L1_PATS)
INT_PATS = [p for (_, p) in L1_LOCAL[2]]
for _c in range(2, 14):
    assert [p for (_, p) in L1_LOCAL[_c]] == INT_PATS and len(INT_PATS) == 5
for _c in (0, 1, 14, 15):
    assert len(L1_LOCAL[_c]) <= 4


def _l1_table(rpb):
    rpb = np.asarray(rpb, np.float32)
    tab = np.full((NPAT, 8, 128, 128), NEG, np.float32)
    for i, (valid, dr, dc) in enumerate(_L1_PATS):
        g = rpb[:, dr, dc]
        tab[i] = np.where(valid[None], g, np.float32(NEG))
    return tab


def _band_mats():
    out = np.zeros((20, 128, 128), np.float32)
    tp = np.arange(128)[:, None]
    t = np.arange(128)[None, :]
    for g, w in enumerate((2, 4, 8, 16)):
        half = w // 2
        cnt = float(2 * half)
        out[g * 5 + 0] = (tp >= t + 128 - half) / cnt
        out[g * 5 + 1] = ((tp >= t - half) & (tp < t + half)) / cnt - (tp == t)
        out[g * 5 + 2] = (tp < t + half - 128) / cnt
        lo = np.maximum(t - half, 0)
        hi = t + half
        out[g * 5 + 3] = ((tp >= lo) & (tp < hi)) / (hi - lo).astype(np.float32) - (tp == t)
        lo = t - half
        hi = np.minimum(t + half, 128)
        out[g * 5 + 4] = ((tp >= lo) & (tp < hi)) / (hi - lo).astype(np.float32) - (tp == t)
    return out


GSTOP = 0


def build(stage=99, dbg_cols=1024):
    nc = bass.Bass("TRN2", target_bir_lowering=False)
    S = Sched()
    es = ExitStack()

    def dram(name, shape, dt=F32, kind="ExternalInput"):
        return nc.dram_tensor(name, list(shape), dt, kind=kind).ap()

    x_d = dram("x", [NT, D])
    cc_d = dram("cc", [128, 8, 2])
    ada_w_d = dram("ada_w", [2, D, 6 * D])
    ada_b_d = dram("ada_b", [2, 6 * D])
    ln_g_d = dram("ln_g", [2, 2, D])
    ln_b_d = dram("ln_b", [2, 2, D])
    w_in0_d = dram("l0_w_in", [D, 2304])
    w_out0_d = dram("l0_w_out", [D, D])
    lam_d = dram("lamv", [4, 64])
    subg_d = dram("l0_subln_g", [128])
    qkg_d = dram("qkg", [5 * 64])
    rope_d = dram("rope", [NT, 128])
    ident_d = dram("ident", [128, 128])
    out_d = dram("out", [SEQ, D], kind="ExternalOutput")
    dbg_d = dram("dbg", [128 if stage == 1 else NT, dbg_cols], kind="ExternalOutput") if stage < 99 else None
    modd = dram("modd", [2, 2, 6 * D], kind="Internal")

    def sb(name, shape, dt=F32):
        return es.enter_context(nc.sbuf_tensor(name, list(shape), dt))

    Xraw = sb("X", [128, NCH * D])
    X = Xraw[:].rearrange("p (c f) -> p c f", c=NCH)
    hT = sb("hT", [128, 8, NT], BF16)
    AOT = sb("AOT", [128, 8, NT], BF16)
    WO = sb("WO", [128, 8, D], BF16)
    identb = sb("identb", [128, 128], BF16)
    identf = sb("identf", [128, 128])
    ropet = sb("ropet", [128, NCH, 128])
    big = sb("big", [128, 6 * 1024])
    bc = [big[:, i * 1024:(i + 1) * 1024] for i in range(6)]
    st = sb("st", [128, 64])
    PS = [es.enter_context(nc.psum_tensor(f"ps{i}", [128, 512], F32)) for i in range(8)]
    PSB = [p[:].bitcast(BF16) for p in PS]
    stn = [0]

    def stat():
        i = stn[0] % 64
        stn[0] += 1
        return st[:, i:i + 1], f"st{i}"

    arena_off = [0]

    def xview(nelem_f32, shape, dt=F32, pattern=None, **kw):
        a = arena_off[0]
        arena_off[0] += nelem_f32
        assert arena_off[0] <= NCH * D
        v = Xraw[:, a:a + nelem_f32]
        if dt != F32:
            v = v.bitcast(dt)
        if pattern is not None:
            v = v.rearrange(pattern, **kw)
        return v

    dkn = [0]

    def dk(prefix="d"):
        dkn[0] += 1
        return f"{prefix}{dkn[0]}"

    def DMA(q, out, in_, reads, writes, key):
        return S.add(q, lambda e: e.dma_start(out=out, in_=in_), reads=reads, writes=writes, dma=key)

    def MM(out, lhsT, rhs, start, stop, reads, writes):
        return S.add("pe", lambda e: e.matmul(out, lhsT, rhs, start=start, stop=stop), reads=reads, writes=writes)

    def TR(out, in_, ident, reads, writes):
        return S.add("pe", lambda e: e.transpose(out, in_, ident), reads=reads, writes=writes)

    def ACT(out, in_, func, reads, writes, bias=None, scale=None, accum_out=None):
        kw = {}
        if bias is not None:
            kw["bias"] = bias
        if scale is not None:
            kw["scale"] = scale
        if accum_out is not None:
            kw["accum_out"] = accum_out
        return S.add("act", lambda e: e.activation(out, in_, func, **kw), reads=reads, writes=writes)

    def TT(eng, out, in0, in1, op, reads, writes):
        return S.add(eng, lambda e: e.tensor_tensor(out, in0, in1, op), reads=reads, writes=writes)

    def TS(eng, out, in0, s1, s2, op0, op1, reads, writes):
        if op1 is None:
            return S.add(eng, lambda e: e.tensor_scalar(out, in0, s1, None, op0), reads=reads, writes=writes)
        return S.add(eng, lambda e: e.tensor_scalar(out, in0, s1, s2, op0, op1), reads=reads, writes=writes)

    def STT(eng, out, in0, scalar, in1, op0, op1, reads, writes):
        return S.add(eng, lambda e: e.scalar_tensor_tensor(out, in0, scalar, in1, op0, op1), reads=reads, writes=writes)

    def CP(eng, out, in_, reads, writes):
        if eng == "act":
            return S.add(eng, lambda e: e.copy(out, in_), reads=reads, writes=writes)
        return S.add(eng, lambda e: e.tensor_copy(out, in_), reads=reads, writes=writes)

    def MEMSET(eng, ap, val, writes):
        return S.add(eng, lambda e: e.memset(ap, val), reads=(), writes=writes)

    def fence(keys, engs=("pe", "act", "dve", "pool", "sp")):
        for e in engs:
            S.add(e, None, reads=keys, writes=())

    def barrier():
        allk = list(S.lw.keys())
        for e in ("pe", "act", "dve", "pool", "sp"):
            S.add(e, None, reads=allk, writes=allk)

    nc_lp = es.enter_context(nc.allow_low_precision("bf16 matmul operands, fp32 accumulation"))
    es.enter_context(nc.allow_non_contiguous_dma("small strided constant loads"))

    for c in range(NCH):
        DMA("sp", X[:, c, :], x_d[c * 128:(c + 1) * 128, :], (), [f"X{c}"], "ldx")
    ccs = sb("ccs", [128, 8, 2])
    DMA("sp", ccs[:], cc_d, (), ["ccs"], "const")
    DMA("sp", identf[:], ident_d, (), ["identf"], "const")
    DMA("sp", ropet[:], rope_d.rearrange("(c p) f -> p c f", p=128), (), ["ropet"], "const")
    fence(["ccs", "identf", "ropet"])
    CP("dve", identb[:], identf[:], ["identf"], ["identb"])

    scs = sb("scs", [128, 8, 2])
    S.add("act", lambda e: e.activation(scs[:], ccs[:], AF.Silu), reads=["ccs"], writes=["scs"])
    adab = sb("adab", [2, 512])
    modrow = sb("modrow", [2, 512])
    NST = 3
    aotf = AOT[:].rearrange("p k t -> p (k t)").bitcast(F32)
    hTf = hT[:].rearrange("p k t -> p (k t)").bitcast(F32)
    stg = [aotf[:, 0:4096], aotf[:, 4096:8192], hTf[:, 0:4096]]
    gi = 0
    for l in range(2):
        for j in range(12):
            s = gi % NST
            st3 = stg[s].rearrange("p (k c) -> p k c", k=8)
            DMA("pool", st3, ada_w_d[l, :, j * 512:(j + 1) * 512].rearrange("(k p) c -> p k c", p=128),
                (), [f"stg{s}"], f"adaw{s}")
            for r in range(2):
                DMA("sp", adab[r:r + 1, :], ada_b_d[l:l + 1, j * 512:(j + 1) * 512], (), ["adab"], "adab")
            pb = gi % 2
            for k in range(8):
                MM(PS[pb][0:2, :], scs[:, k, :], st3[:, k, :], k == 0, k == 7,
                   ["scs", f"stg{s}"], [f"ps{pb}"])
            TT("dve", modrow[:], PS[pb][0:2, :], adab[:], ALU.add, [f"ps{pb}", "adab"], ["modrow"])
            DMA("sp", modd[l, :, j * 512:(j + 1) * 512], modrow[:], ["modrow"], ["modd"], "modw")
            gi += 1
    barrier()

    def dbg_out(ap, cols):
        tmpd = sb("tmpd", [128, cols])
        CP("dve", tmpd[:], ap, list(S.lw.keys()), ["tmpd"])
        DMA("sp", dbg_d[:, 0:cols], tmpd[:], ["tmpd"], ["dbgd"], "dbg")

    if stage == 1:
        t1 = sb("t1", [128, 192])
        DMA("sp", t1[:], modd.rearrange("l r (a b) -> (l r a) b", b=192), ["modd"], ["t1"], "t1")
        dbg_out(t1[:], 192)
        S.add("sp", None, reads=list(S.lw.keys()), writes=())
        S.emit(nc, es)
        es.close()
        return nc

    xscr = dram("xscr", [NT, D], kind="Internal")

    def bcast_mod(i, l, r, idx, plus1=False):
        src = modd[l, r:r + 1, idx * 1024:(idx + 1) * 1024].partition_broadcast(128)
        DMA("sp", bc[i].rearrange("p (o f) -> p o f", o=1), src, ["modd"], [f"bc{i}"], f"bcl{i}")
        if plus1:
            TS("dve", bc[i], bc[i], 1.0, None, ALU.add, None, [f"bc{i}"], [f"bc{i}"])

    def bcast_row(i, row_ap):
        DMA("sp", bc[i].rearrange("p (o f) -> p o f", o=1), row_ap.partition_broadcast(128), (), [f"bc{i}"], f"bcl{i}")

    hb = [sb(f"hb{i}", [128, D], BF16) for i in range(2)]

    def modulate_T(l, isc, ish, nch):
        bcast_mod(0, l, 0, isc, True)
        bcast_mod(1, l, 0, ish)
        if nch > NLAT:
            bcast_mod(2, l, 1, isc, True)
            bcast_mod(3, l, 1, ish)
        for c in range(nch):
            a, b_ = (0, 1) if c < NLAT else (2, 3)
            t = 4 + (c % 2)
            TT("dve", bc[t], X[:, c, :], bc[a], ALU.mult, [f"X{c}", f"bc{a}"], [f"bc{t}"])
            TT("dve", hb[c % 2][:], bc[t], bc[b_], ALU.add, [f"bc{t}", f"bc{b_}"], [f"hb{c % 2}"])
            pb = 4 + (c % 2)
            for k in range(8):
                TR(PSB[pb][:, k * 128:(k + 1) * 128], hb[c % 2][:, k * 128:(k + 1) * 128], identb[:],
                   [f"hb{c % 2}", "identb"], [f"ps{pb}"])
            CP("act", hT[:, :, c * 128:(c + 1) * 128], PSB[pb].rearrange("p (k t) -> p k t", k=8),
               [f"ps{pb}"], [f"hT{c}"])

    def spill_X(nch):
        for c in range(nch):
            DMA("sp", xscr[c * 128:(c + 1) * 128, :], X[:, c, :], [f"X{c}"], ["xscr"], "xsp")

    def reload_X(nch):
        for c in range(nch):
            DMA("sp", X[:, c, :], xscr[c * 128:(c + 1) * 128, :], ["xscr"], [f"X{c}"], "xrl")

    def rstd_from(out_ap, okey, in_ap, ikey, scale, eps):
        tmp, tk = stat()
        ACT(tmp, in_ap, AF.Ln, [ikey], [tk], bias=epst[eps], scale=scale)
        ACT(out_ap, tmp, AF.Exp, [tk], [okey], scale=-0.5)

    epsv = sb("epsv", [128, 2])
    MEMSET("dve", epsv[:, 0:1], LN_EPS, ["epsv"])
    MEMSET("dve", epsv[:, 1:2], RMS_EPS, ["epsv"])
    fence(["epsv"], ("act",))
    epst = {LN_EPS: epsv[:, 0:1], RMS_EPS: epsv[:, 1:2]}

    def layer_norm_chunk(c, ig, ib):
        stt = sb(f"lnst{c}", [128, 2, 6]) if False else lnstat
        S.add("dve", lambda e: e.bn_stats(stt[:, 0, :], X[:, c, 0:512]), reads=[f"X{c}"], writes=["lnst0"])
        S.add("dve", lambda e: e.bn_stats(stt[:, 1, :], X[:, c, 512:1024]), reads=[f"X{c}"], writes=["lnst1"])
        mv, mk = lnmv, "lnmv"
        S.add("dve", lambda e: e.bn_aggr(mv[:], stt[:]), reads=["lnst0", "lnst1"], writes=[mk])
        rs, rk = stat()
        rstd_from(rs, rk, mv[:, 1:2], mk, 1.0, LN_EPS)
        nm, nk = stat()
        TS("dve", nm, mv[:, 0:1], rs, -1.0, ALU.mult, ALU.mult, [mk, rk], [nk])
        t = 4 + (c % 2)
        ACT(bc[t], X[:, c, :], AF.Identity, [f"X{c}", rk, nk], [f"bc{t}"], bias=nm, scale=rs)
        TT("dve", bc[t], bc[t], bc[ig], ALU.mult, [f"bc{t}", f"bc{ig}"], [f"bc{t}"])
        TT("dve", X[:, c, :], bc[t], bc[ib], ALU.add, [f"bc{t}", f"bc{ib}"], [f"X{c}"])

    lnstat = sb("lnstat", [128, 2, 6])
    lnmv = sb("lnmv", [128, 2])


    def outproj_ln(l, nch):
        bcast_mod(0, l, 0, 2)
        if nch > NLAT:
            bcast_mod(1, l, 1, 2)
        bcast_row(2, ln_g_d[l, 0:1, :])
        bcast_row(3, ln_b_d[l, 0:1, :])
        for c in range(nch):
            gtile = 0 if c < NLAT else 1
            t = 4 + (c % 2)
            for hf in range(2):
                pb = 2 * (c % 2) + hf
                for m in range(8):
                    MM(PS[pb][:, :], AOT[:, m, c * 128:(c + 1) * 128], WO[:, m, hf * 512:(hf + 1) * 512], m == 0, m == 7,
                       [f"AOT{c}", "WO"], [f"ps{pb}"])
                TT("dve", bc[t][:, hf * 512:(hf + 1) * 512], PS[pb][:, :], bc[gtile][:, hf * 512:(hf + 1) * 512], ALU.mult,
                   [f"ps{pb}", f"bc{gtile}"], [f"bc{t}"])
            STT("dve", X[:, c, :], X[:, c, :], ALPHA, bc[t], ALU.mult, ALU.add, [f"X{c}", f"bc{t}"], [f"X{c}"])
            layer_norm_chunk(c, 2, 3)
        barrier()

    if True:
        l = 0
        DMA("pool", WO[:], w_out0_d.rearrange("(k p) c -> p k c", p=128), (), ["WO"], "wo")
        modulate_T(0, 1, 0, NCH)
        spill_X(NCH)
        barrier()
        if stage == 15:
            dbgb = dram("dbgb", [128, 8 * NT], BF16, kind="ExternalOutput")
            DMA("sp", dbgb, hT[:].rearrange("p k t -> p (k t)"), [f"hT{c}" for c in range(NCH)], ["dbgd"], "dbg")
            S.add("sp", None, reads=list(S.lw.keys()) + ["dbgd"], writes=())
            S.emit(nc, es)
            es.close()
            return nc
        arena_off[0] = 0
        WG = [xview(1536, [128, 8, 384], BF16, "p (k c) -> p k c", k=8) for _ in range(2)]
        QKT = xview(3 * NT // 2, None, BF16, "p (s t) -> p s t", s=3)
        VX = xview(NCH * 130 // 2, None, BF16, "p (c f) -> p c f", c=NCH)
        QKR = [xview(192, None, BF16) for _ in range(2)]
        T1 = [xview(384, None) for _ in range(2)]
        T2 = [xview(384, None) for _ in range(2)]
        SQ = xview(384, None)
        PT = [xview(256, None, BF16) for _ in range(4)]
        O1 = xview(512, None, F32, "p (j f) -> p j f", j=4)
        OO = [xview(128, None) for _ in range(2)]
        AOK = xview(512, None, BF16, "p (j f) -> p j f", j=4)
        SUBG = xview(128, None)
        G5 = xview(320, None)
        LAMT = xview(256, None, F32, "p (a f) -> p a f", a=4)
        lamt = sb("lamt", [128, 4])
        QZ = xview(4 * NT // 2, None, BF16, "p (s t) -> p s t", s=4)
        MEMSET("dve", QZ, 0.0, [f"QKT{c}" for c in range(NCH)])
        RSS = sb("RSS", [128, 24])
        DMA("sp", SUBG.rearrange("p (o f) -> p o f", o=1), subg_d.rearrange("(o f) -> o f", o=1).partition_broadcast(128),
            (), ["SUBG"], "c2")
        DMA("sp", G5.rearrange("p (o f) -> p o f", o=1), qkg_d.rearrange("(o f) -> o f", o=1).partition_broadcast(128),
            (), ["G5"], "c2")
        for a in range(4):
            DMA("sp", LAMT[:, a:a + 1, :], lam_d[a:a + 1, :].partition_broadcast(128), (), ["LAMT"], "c2")
        fence(["SUBG", "G5", "LAMT"])
        TS("dve", SUBG, SUBG, 1.0 - LAM_INIT0, None, ALU.mult, None, ["SUBG"], ["SUBG"])
        TT("dve", LAMT[:, 0, :], LAMT[:, 0, :], LAMT[:, 1, :], ALU.mult, ["LAMT"], ["LAMT"])
        TT("dve", LAMT[:, 2, :], LAMT[:, 2, :], LAMT[:, 3, :], ALU.mult, ["LAMT"], ["LAMT"])
        S.add("dve", lambda e: e.tensor_reduce(lamt[:, 0:1], LAMT[:, 0, :], AX.X, ALU.add), reads=["LAMT"], writes=["lamt"])
        S.add("dve", lambda e: e.tensor_reduce(lamt[:, 1:2], LAMT[:, 2, :], AX.X, ALU.add), reads=["LAMT"], writes=["lamt"])
        ACT(lamt[:, 0:2], lamt[:, 0:2], AF.Exp, ["lamt"], ["lamt"])
        TT("dve", lamt[:, 2:3], lamt[:, 1:2], lamt[:, 0:1], ALU.subtract, ["lamt"], ["lamt"])
        TS("dve", lamt[:, 3:4], lamt[:, 2:3], -LAM_INIT0, None, ALU.add, None, ["lamt"], ["lamt"])
        nlam = lamt[:, 3:4]

        groups = [("A", h) for h in range(4)] + [("B", n) for n in range(2)]

        def load_wg(gi):
            kind, idx = groups[gi]
            w = WG[gi % 2]
            if kind == "A":
                cols = [(idx * 128, 128), (512 + idx * 128, 128), (1024 + idx * 128, 128)]
            else:
                cols = [(1536 + idx * 256, 256), (2048 + idx * 64, 64), (2176 + idx * 64, 64)]
            o = 0
            for (c0, n) in cols:
                DMA("pool", w[:, :, o:o + n], w_in0_d[:, c0:c0 + n].rearrange("(k p) c -> p k c", p=128),
                    (), [f"WG{gi % 2}"], f"wg{gi % 2}")
                o += n

        load_wg(0)
        ptn = [0]
        for gi, (kind, idx) in enumerate(groups):
            if gi + 1 < len(groups):
                load_wg(gi + 1)
            w = WG[gi % 2]
            nv = 4 if kind == "A" else 5
            nr = nv * 64
            vcol = 256 if kind == "A" else 320
            vw = 128 if kind == "A" else 64
            MEMSET("dve", VX[:, :, vw:vw + 1], 1.0, [f"VX{c}" for c in range(NCH)])
            for c in range(NCH):
                pb = 6 + (c % 2)
                for k in range(8):
                    MM(PS[pb][:, 0:384], hT[:, k, c * 128:(c + 1) * 128], w[:, k, :], k == 0, k == 7,
                       [f"hT{c}", f"WG{gi % 2}"], [f"ps{pb}"])
                CP("act", VX[:, c, 0:vw], PS[pb][:, vcol:vcol + vw], [f"ps{pb}"], [f"VX{c}"])
                src = PS[pb][:, 0:nr]
                skey = f"ps{pb}"
                i2 = c % 2
                if kind == "B":
                    ACT(SQ[:, 0:nr], src, AF.Square, [skey], ["SQ"])
                    S.add("dve", lambda e, c=c, nr=nr: e.tensor_reduce(RSS[:, 0:5], SQ[:, 0:nr].rearrange("p (v f) -> p v f", v=5), AX.X, ALU.add),
                          reads=["SQ"], writes=["RSS"])
                    ACT(RSS[:, 8:13], RSS[:, 0:5], AF.Ln, ["RSS"], ["RSS2"], bias=epst[RMS_EPS], scale=1.0 / 64)
                    ACT(RSS[:, 16:21], RSS[:, 8:13], AF.Exp, ["RSS2"], ["RSS3"], scale=-0.5)
                    TT("dve", SQ[:, 0:nr].rearrange("p (v f) -> p v f", v=5), src.rearrange("p (v f) -> p v f", v=5),
                       RSS[:, 16:21].unsqueeze(2).to_broadcast([128, 5, 64]), ALU.mult, [skey, "RSS3"], ["SQ"])
                    TT("dve", SQ[:, 0:nr], SQ[:, 0:nr], G5, ALU.mult, ["SQ", "G5"], ["SQ"])
                    src = SQ[:, 0:nr]
                    skey = "SQ"
                cosb = ropet[:, c, 0:64].unsqueeze(1).to_broadcast([128, nv, 64])
                TT("dve", T1[i2][:, 0:nr].rearrange("p (v f) -> p v f", v=nv), src.rearrange("p (v f) -> p v f", v=nv),
                   cosb, ALU.mult, [skey, "ropet"], [f"T1{i2}"])
                s5 = src.rearrange("p (v a j f) -> p v a j f", v=nv, a=2, j=2)
                t5 = T2[i2][:, 0:nr].rearrange("p (v a j f) -> p v a j f", v=nv, a=2, j=2)
                sn5 = ropet[:, c, 64:128].rearrange("p (a j f) -> p a j f", a=2, j=2)
                for j in range(2):
                    for a in range(2):
                        sinb = sn5[:, a, j, :].unsqueeze(1).to_broadcast([128, nv, 16])
                        TT("dve", t5[:, :, a, j, :], s5[:, :, a, 1 - j, :], sinb, ALU.mult,
                           [skey, "ropet"], [f"T2{i2}"])
                TT("dve", QKR[i2][:, 0:nr], T1[i2][:, 0:nr], T2[i2][:, 0:nr], ALU.add, [f"T1{i2}", f"T2{i2}"], [f"QKR{i2}"])
                if kind == "B":
                    TT("dve", QKR[i2][:, 320:384], T1[i2][:, 256:320], T2[i2][:, 256:320], ALU.add,
                       [f"T1{i2}", f"T2{i2}"], [f"QKR{i2}"])
                ntr = 2 if kind == "A" else 3
                tb = 4 + (c % 2)
                for t in range(ntr):
                    TR(PSB[tb][:, t * 128:(t + 1) * 128], QKR[i2][:, t * 128:(t + 1) * 128], identb[:],
                       [f"QKR{i2}", "identb"], [f"ps{tb}"])
                CP("act", QKT[:, 0:ntr, c * 128:(c + 1) * 128], PSB[tb][:, 0:ntr * 128].rearrange("p (s t) -> p s t", s=ntr),
                   [f"ps{tb}"], [f"QKT{c}"])
                nqz = 2 if kind == "A" else 4
                for z in range(nqz):
                    src_slot = 0 if kind == "A" else z // 2
                    r0 = (z % 2) * 64
                    CP("act" if z % 2 == 0 else "dve", QZ[r0:r0 + 64, z, c * 128:(c + 1) * 128],
                       PSB[tb][r0:r0 + 64, src_slot * 128:(src_slot + 1) * 128], [f"ps{tb}"], [f"QKT{c}"])
            if stage == 16 and gi == GSTOP:
                dbgb = dram("dbgb", [128, 3 * NT], BF16, kind="ExternalOutput")
                DMA("sp", dbgb, QKT.rearrange("p s t -> p (s t)"), [f"QKT{c}" for c in range(NCH)], ["dbgd"], "dbg")
                dbgv = dram("dbgv", [128, NCH * 130], BF16, kind="ExternalOutput")
                DMA("sp", dbgv, VX.rearrange("p c f -> p (c f)"), [f"VX{c}" for c in range(NCH)], ["dbgd"], "dbg")
                S.add("sp", None, reads=list(S.lw.keys()) + ["dbgd"], writes=())
                S.emit(nc, es)
                es.close()
                return nc
            blocks = [(qb * 4, 4, list(range(NCH))) for qb in range(4)] + [(16, 2, [16, 17])]
            if kind == "A":
                heads = [(0, 0, 1, s * 64) for s in range(2)]
            else:
                heads = [(j4 // 2, 2, (j4 % 2) * 64) for j4 in range(4)]
            for (qc0, nqc, kcs) in blocks:
                ncols = nqc * 128
                q0 = qc0 * 128
                qkeys = [f"QKT{qc0 + j}" for j in range(nqc)]
                for hi, hd in enumerate(heads):
                    if kind == "A":
                        qs, ks, pbase = 0, 1, hi * 64
                    else:
                        qs, ks, pbase = hd
                    def score(kn):
                        kc = kcs[kn]
                        sp_ = 4 + (ptn[0] % 2)
                        pi = ptn[0] % 4
                        ptn[0] += 1
                        zslot = hi if kind == "A" else hi
                        MM(PS[sp_][:, 0:ncols], QKT[:, ks, kc * 128:(kc + 1) * 128],
                           QZ[:, zslot, q0:q0 + ncols], True, True,
                           [f"QKT{kc}"] + qkeys, [f"ps{sp_}"])
                        return sp_, pi

                    pend = score(0)
                    for kn, kc in enumerate(kcs):
                        nxt = score(kn + 1) if kn + 1 < len(kcs) else None
                        sp_, pi = pend
                        ACT(PT[pi][:, 0:ncols], PS[sp_][:, 0:ncols], AF.Exp, [f"ps{sp_}"], [f"PT{pi}"], scale=0.125)
                        for j in range(nqc):
                            MM(PS[j][:, 0:vw + 1], PT[pi][:, j * 128:(j + 1) * 128], VX[:, kc, 0:vw + 1],
                               kn == 0, kn == len(kcs) - 1, [f"PT{pi}", f"VX{kc}"], [f"ps{j}"])
                        pend = nxt
                    for j in range(nqc):
                        rz, rk = stat()
                        S.add("dve", lambda e, rz=rz, j=j, vw=vw: e.reciprocal(rz, PS[j][:, vw:vw + 1]), reads=[f"ps{j}"], writes=[rk])
                        if kind == "B":
                            TS("dve", AOK[:, j, hi * 64:(hi + 1) * 64], PS[j][:, 0:64], rz, None, ALU.mult, None,
                               [f"ps{j}", rk], [f"AOK{j}"])
                        elif hi == 0:
                            TS("dve", O1[:, j, :], PS[j][:, 0:128], rz, None, ALU.mult, None, [f"ps{j}", rk], [f"O1{j}"])
                        else:
                            r2, r2k = stat()
                            TT("dve", r2, rz, nlam, ALU.mult, [rk, "lamt"], [r2k])
                            oo = OO[j % 2]
                            STT("dve", oo, PS[j][:, 0:128], r2, O1[:, j, :], ALU.mult, ALU.add,
                                [f"ps{j}", r2k, f"O1{j}"], [f"OO{j % 2}"])
                            ssq, ssk = stat()
                            TT("dve", T1[j % 2][:, 0:128], oo, oo, ALU.mult, [f"OO{j % 2}"], [f"T1{j % 2}"])
                            S.add("dve", lambda e, ssq=ssq, j=j: e.tensor_reduce(ssq, T1[j % 2][:, 0:128], AX.X, ALU.add),
                                  reads=[f"T1{j % 2}"], writes=[ssk])
                            rs, rsk = stat()
                            rstd_from(rs, rsk, ssq, ssk, 1.0 / 128, RMS_EPS)
                            STT("dve", AOK[:, j, 0:128], oo, rs, SUBG, ALU.mult, ALU.mult,
                                [f"OO{j % 2}", rsk, "SUBG"], [f"AOK{j}"])
                    last = (kind == "A" and hi == 1) or (kind == "B" and hi == 3)
                    if last:
                        for j in range(nqc):
                            tb = 6 + (j % 2)
                            ntr = 1 if kind == "A" else 2
                            for t in range(ntr):
                                TR(PSB[tb][:, t * 128:(t + 1) * 128], AOK[:, j, t * 128:(t + 1) * 128], identb[:],
                                   [f"AOK{j}", "identb"], [f"ps{tb}"])
                            m0 = idx if kind == "A" else 4 + idx * 2
                            tc = qc0 + j
                            CP("act", AOT[:, m0:m0 + ntr, tc * 128:(tc + 1) * 128],
                               PSB[tb][:, 0:ntr * 128].rearrange("p (s t) -> p s t", s=ntr), [f"ps{tb}"], [f"AOT{tc}"])
        barrier()
        reload_X(NCH)
        outproj_ln(0, NCH)

    if stage == 2:
        for c in range(NCH):
            DMA("sp", dbg_d[c * 128:(c + 1) * 128, :], X[:, c, :], [f"X{c}"], ["dbgd"], "dbg")
        S.add("sp", None, reads=list(S.lw.keys()) + ["dbgd"], writes=())
        S.emit(nc, es)
        es.close()
        return nc
    router_d = dram("moe_router", [2, D, NE])
    NEX = 1 if stage == 31 else NE
    wg_d = dram("moe_w_gate", [2, NEX, D, FF])
    wu_d = dram("moe_w_up", [2, NEX, D, FF])
    wd_d = dram("moe_w_down", [2, NEX, FF, D])
    iota_d = dram("iota288", [128, 288])
    utri_d = dram("utri", [128, 128])
    aotb = AOT[:].rearrange("p k t -> p (k t)")
    wob = WO[:].rearrange("p k c -> p (k c)")
    hmb = hT[:].rearrange("p k t -> p (k t)").rearrange("p (c f) -> p c f", c=NCH)
    RING = [aotb[:, i * 2048:(i + 1) * 2048] for i in range(5)]
    RING += [bc[i].bitcast(BF16) for i in (2, 3, 4, 5)]
    _rpb = ropet[:].rearrange("p c f -> p (c f)").bitcast(BF16)
    RING += [_rpb[:, i * 2048:(i + 1) * 2048] for i in range(2)]
    NRING = len(RING)
    XGT = aotb[:, 10240:12544].rearrange("p (k s) -> p k s", k=8)
    HIDT = aotb[:, 12544:17152].rearrange("p (j s) -> p j s", j=16)
    SELR = [aotb[:, 17152 + i * 288:17152 + (i + 1) * 288] for i in range(2)]
    SELGT = [aotb[:, 17728 + i * 384:17728 + (i + 1) * 384].rearrange("p (s t) -> p s t", s=3) for i in range(1)]
    SELR += [wob[:, 5280 + i * 288:5280 + (i + 1) * 288] for i in range(3)]
    SELGT += [wob[:, 6144 + i * 384:6144 + (i + 1) * 384].rearrange("p (s t) -> p s t", s=3) for i in range(2)]
    NSEL = len(SELR)
    NSGT = len(SELGT)
    YG = wob[:, 0:3072].rearrange("p (s f) -> p s f", s=3)
    IOTA = wob[:, 3072:3648].bitcast(F32)
    RW = wob[:, 3648:3904].bitcast(F32).rearrange("p (k e) -> p k e", k=8)
    UTRI = wob[:, 3904:4032]
    ONESB = wob[:, 4032:4160]
    MASKB = wob[:, 4160:4448].rearrange("p (c e) -> p c e", c=NCH)
    SGR = [wob[:, 4448:5024].bitcast(F32), wob[:, 6912:7488].bitcast(F32)]
    UTF = wob[:, 5024:5280].bitcast(F32)
    WEX = ropet[:].rearrange("p c f -> p (c f)")
    AFF = hb[0][:].bitcast(F32)[:, 0:288].rearrange("p (c e) -> p c e", c=NCH)
    MASK = hb[1][:].bitcast(F32)[:, 0:288].rearrange("p (c e) -> p c e", c=NCH)
    mo2 = sb("mo2", [128, 2, NCH * NE])
    SLOT = mo2[:, 0, :].rearrange("p (c e) -> p c e", c=NCH)
    GS = mo2[:, 1, :].rearrange("p (c e) -> p c e", c=NCH)
    m8 = sb("m8", [16, 8])

    def moe_layer(l, nch, final_out=False):
        ns = 256 + (32 if nch > NLAT else 0)
        nsc = (ns + 127) // 128
        scsz = [min(128, ns - s * 128) for s in range(nsc)]
        barrier()
        DMA("sp", IOTA, iota_d, (), ["IOTA"], "c3")
        DMA("sp", UTF, utri_d, (), ["UTF"], "c3")
        DMA("sp", RW, router_d[l].rearrange("(k p) e -> p k e", p=128), (), ["RW"], "c3")
        fence(["IOTA", "UTF", "RW"])
        CP("dve", UTRI, UTF, ["UTF"], ["UTRI"])
        MEMSET("dve", ONESB, 1.0, ["ONESB"])
        bcast_mod(0, l, 0, 4, True)
        bcast_mod(1, l, 0, 3)
        if nch > NLAT:
            bcast_mod(2, l, 1, 4, True)
            bcast_mod(3, l, 1, 3)
        for c in range(nch):
            a, b_ = (0, 1) if c < NLAT else (2, 3)
            TT("dve", bc[4], X[:, c, :], bc[a], ALU.mult, [f"X{c}", f"bc{a}"], ["bc4"])
            TT("dve", bc[4], bc[4], bc[b_], ALU.add, ["bc4", f"bc{b_}"], ["bc4"])
            CP("act", hmb[:, c, :], bc[4], ["bc4"], [f"hmb{c}"])
            TS("dve", X[:, c, :], X[:, c, :], ALPHA, None, ALU.mult, None, [f"X{c}"], [f"X{c}"])
            for k in range(8):
                pb = k // 4
                TR(PS[pb][:, (k % 4) * 128:(k % 4 + 1) * 128], bc[4][:, k * 128:(k + 1) * 128], identf[:],
                   ["bc4", "identf"], [f"ps{pb}"])
            hmT = bc[5].rearrange("p (k t) -> p k t", k=8)
            CP("act", hmT[:, 0:4, :], PS[0][:, :].rearrange("p (k t) -> p k t", k=4), ["ps0"], ["bc5"])
            CP("dve", hmT[:, 4:8, :], PS[1][:, :].rearrange("p (k t) -> p k t", k=4), ["ps1"], ["bc5"])
            for k in range(8):
                MM(PS[2][:, 0:NE], hmT[:, k, :], RW[:, k, :], k == 0, k == 7, ["bc5", "RW"], ["ps2"])
            mx, mxk = stat()
            S.add("dve", lambda e, mx=mx: e.tensor_reduce(mx, PS[2][:, 0:NE], AX.X, ALU.max), reads=["ps2"], writes=[mxk])
            nmx, nmk = stat()
            TS("dve", nmx, mx, -1.0, None, ALU.mult, None, [mxk], [nmk])
            ACT(AFF[:, c, :], PS[2][:, 0:NE], AF.Exp, ["ps2", nmk], [f"AFF{c}"], bias=nmx)
            sm, smk = stat()
            S.add("dve", lambda e, sm=sm, c=c: e.tensor_reduce(sm, AFF[:, c, :], AX.X, ALU.add), reads=[f"AFF{c}"], writes=[smk])
            rc, rck = stat()
            S.add("dve", lambda e, rc=rc, sm=sm: e.reciprocal(rc, sm), reads=[smk], writes=[rck])
            TS("dve", AFF[:, c, :], AFF[:, c, :], rc, None, ALU.mult, None, [f"AFF{c}", rck], [f"AFF{c}"])
            TR(PS[3][0:NE, 0:128], AFF[:, c, :], identf[:], [f"AFF{c}", "identf"], ["ps3"])
            CP("dve", WEX[0:NE, c * 128:(c + 1) * 128], PS[3][0:NE, 0:128], ["ps3"], ["WEX"])
        sets = [(0, SEQ, 256)] + ([(SEQ, NT, 32)] if nch > NLAT else [])
        for (t0, t1, cap) in sets:
            wv = WEX[0:NE, t0:t1]
            for r in range(cap // 8):
                S.add("dve", lambda e, wv=wv: e.max(m8[:], wv), reads=["WEX"], writes=["m8"])
                S.add("dve", lambda e, wv=wv: e.match_replace(wv, m8[:], wv, -1.0), reads=["WEX", "m8"], writes=["WEX"])
        TS("dve", WEX[0:NE, 0:nch * 128], WEX[0:NE, 0:nch * 128], 0.0, None, ALU.is_lt, None, ["WEX"], ["WEX"])
        for c in range(nch):
            TR(PS[4][:, c * NE:(c + 1) * NE], WEX[0:NE, c * 128:(c + 1) * 128], identf[0:NE, 0:NE], ["WEX", "identf"], ["ps4"])
        mflat = mo2[:, 0, 0:nch * NE]
        CP("dve", MASK[:, 0:nch, :], PS[4][:, 0:nch * NE].rearrange("p (c e) -> p c e", c=nch), ["ps4"], ["MASK"])
        CP("dve", MASKB[:, 0:nch, :], MASK[:, 0:nch, :], ["MASK"], ["MASKB"])
        TT("dve", GS[:, 0:nch, :], AFF[:, 0:nch, :], MASK[:, 0:nch, :], ALU.mult, [f"AFF{c}" for c in range(nch)] + ["MASK"], ["GS"])
        for c in range(nch):
            c0 = 0 if c < NLAT else NLAT
            prev = list(range(c0, c))
            for i, cp in enumerate(prev):
                MM(PS[5][:, c * NE:(c + 1) * NE], ONESB, MASKB[:, cp, :], i == 0, False, ["ONESB", "MASKB"], ["ps5"])
            MM(PS[5][:, c * NE:(c + 1) * NE], UTRI, MASKB[:, c, :], len(prev) == 0, True, ["UTRI", "MASKB"], ["ps5"])
        TS("dve", SLOT[:, 0:NLAT, :], PS[5][:, 0:NLAT * NE].rearrange("p (c e) -> p c e", c=NLAT), 1.0, None, ALU.add, None, ["ps5"], ["SLOT"])
        if nch > NLAT:
            TS("dve", SLOT[:, NLAT:nch, :], PS[5][:, NLAT * NE:nch * NE].rearrange("p (c e) -> p c e", c=nch - NLAT), 257.0, None,
               ALU.add, None, ["ps5"], ["SLOT"])
        TT("dve", SLOT[:, 0:nch, :], SLOT[:, 0:nch, :], MASK[:, 0:nch, :], ALU.mult, ["SLOT", "MASK"], ["SLOT"])
        TS("dve", SLOT[:, 0:nch, :], SLOT[:, 0:nch, :], -1.0, None, ALU.add, None, ["SLOT"], ["SLOT"])
        bcast_mod(0, l, 0, 5)
        if nch > NLAT:
            bcast_mod(1, l, 1, 5)
        barrier()
        units = []
        for e_ in range(NEX):
            for fu in range(8):
                units.append(("g", e_, fu))
                units.append(("u", e_, fu))
            for du in range(8):
                units.append(("d", e_, du))
        issued = [0]

        def issue_until(n):
            while issued[0] < min(n, len(units)):
                kind, e_, i = units[issued[0]]
                slot = issued[0] % NRING
                if kind == "d":
                    src = wd_d[l, e_, i * 256:(i + 1) * 256, :].rearrange("(j p) c -> p j c", p=128)
                    dst = RING[slot].rearrange("p (j c) -> p j c", j=2)
                else:
                    wsrc = wg_d if kind == "g" else wu_d
                    src = wsrc[l, e_, :, i * 256:(i + 1) * 256].rearrange("(k p) c -> p k c", p=128)
                    dst = RING[slot].rearrange("p (k c) -> p k c", k=8)
                DMA("pool", dst, src, (), [f"wr{slot}"], f"wr{slot}")
                issued[0] += 1

        ui = [0]

        def next_unit():
            i = ui[0]
            ui[0] += 1
            return i % NRING

        def refill():
            issue_until(ui[0] + NRING)

        issue_until(NRING)
        seln = [0]
        sgn = [0]
        for e_ in range(NEX):
            for half in range(2):
                for c in range(nch):
                    si = seln[0] % NSEL
                    seln[0] += 1
                    s0, s1 = (0, 256) if c < NLAT else (256, ns)
                    cfirst, clast = (0, NLAT - 1) if c < NLAT else (NLAT, nch - 1)
                    TS("dve", SELR[si][:, s0:s1], IOTA[:, s0:s1], SLOT[:, c, e_:e_ + 1], None, ALU.is_equal, None,
                       ["IOTA", "SLOT"], [f"SEL{si}"])
                    for f4 in range(4):
                        f = half * 4 + f4
                        MM(PS[f4][:, s0:s1], hmb[:, c, f * 128:(f + 1) * 128], SELR[si][:, s0:s1], c == cfirst, c == clast,
                           [f"hmb{c}", f"SEL{si}"], [f"ps{f4}"])
                for f4 in range(4):
                    f = half * 4 + f4
                    CP("act" if f4 % 2 == 0 else "dve", XGT[:, f, 0:ns], PS[f4][:, 0:ns], [f"ps{f4}"], ["XGT"])
            for fu in range(8):
                sg_ = next_unit()
                su_ = next_unit()
                wgv = RING[sg_].rearrange("p (k c) -> p k c", k=8)
                wuv = RING[su_].rearrange("p (k c) -> p k c", k=8)
                for j2 in range(2):
                    jg = fu * 2 + j2
                    bg, bu = (4, 5) if jg % 2 == 0 else (6, 7)
                    for k in range(8):
                        MM(PS[bg][:, 0:ns], wgv[:, k, j2 * 128:(j2 + 1) * 128], XGT[:, k, 0:ns], k == 0, k == 7,
                           [f"wr{sg_}", "XGT"], [f"ps{bg}"])
                    for k in range(8):
                        MM(PS[bu][:, 0:ns], wuv[:, k, j2 * 128:(j2 + 1) * 128], XGT[:, k, 0:ns], k == 0, k == 7,
                           [f"wr{su_}", "XGT"], [f"ps{bu}"])
                    SG = SGR[jg % 2]
                    ACT(SG[:, 0:ns], PS[bg][:, 0:ns], AF.Silu, [f"ps{bg}"], [f"SG{jg % 2}"])
                    TT("dve", HIDT[:, jg, 0:ns], PS[bu][:, 0:ns], SG[:, 0:ns], ALU.mult, [f"ps{bu}", f"SG{jg % 2}"], [f"HID{jg}"])
                refill()
            for du in range(8):
                sd_ = next_unit()
                wdv = RING[sd_].rearrange("p (j c) -> p j c", j=2)
                for sc in range(nsc):
                    sz = scsz[sc]
                    for hf in range(2):
                        pb = sc * 2 + hf
                        for jj in range(2):
                            jg = du * 2 + jj
                            MM(PS[pb][0:sz, :], HIDT[:, jg, sc * 128:sc * 128 + sz], wdv[:, jj, hf * 512:(hf + 1) * 512],
                               du == 0 and jj == 0, du == 7 and jj == 1, [f"HID{jg}", f"wr{sd_}"], [f"ps{pb}"])
                refill()
            for sc in range(nsc):
                sz = scsz[sc]
                gt = 0 if sc < 2 else 1
                for hf in range(2):
                    pb = sc * 2 + hf
                    TT("dve", YG[0:sz, sc, hf * 512:(hf + 1) * 512], PS[pb][0:sz, :], bc[gt][0:sz, hf * 512:(hf + 1) * 512], ALU.mult,
                       [f"ps{pb}", f"bc{gt}"], ["YG"])
            if stage == 31:
                d1 = dram("d_slot", [128, 2 * NCH * NE], kind="ExternalOutput")
                DMA("sp", d1, mo2[:].rearrange("p a f -> p (a f)"), ["SLOT", "GS"], ["dbgd"], "dbg")
                d2 = dram("d_xgt", [128, 8 * 288], BF16, kind="ExternalOutput")
                DMA("sp", d2, XGT.rearrange("p k s -> p (k s)"), ["XGT"], ["dbgd"], "dbg")
                d3 = dram("d_yg", [128, 3 * 1024], BF16, kind="ExternalOutput")
                DMA("sp", d3, YG.rearrange("p s f -> p (s f)"), ["YG"], ["dbgd"], "dbg")
                d4 = dram("d_hid", [128, 16 * 288], BF16, kind="ExternalOutput")
                DMA("sp", d4, HIDT.rearrange("p j s -> p (j s)"), [f"HID{j}" for j in range(16)], ["dbgd"], "dbg")
                S.add("sp", None, reads=list(S.lw.keys()) + ["dbgd"], writes=())
                S.emit(nc, es)
                es.close()
                return "STOP"
            def sc_stage_a(c):
                si = seln[0] % NSEL
                seln[0] += 1
                s0, s1 = (0, 256) if c < NLAT else (256, ns)
                scs_ = [0, 1] if c < NLAT else [2]
                TS("dve", SELR[si][:, s0:s1], IOTA[:, s0:s1], SLOT[:, c, e_:e_ + 1], GS[:, c, e_:e_ + 1], ALU.is_equal, ALU.mult,
                   ["IOTA", "SLOT", "GS"], [f"SEL{si}"])
                tb = 6 + (c % 2)
                for sc in scs_:
                    sz = scsz[sc]
                    TR(PSB[tb][0:sz, sc * 128:(sc + 1) * 128], SELR[si][:, sc * 128:sc * 128 + sz], identb[:],
                       [f"SEL{si}", "identb"], [f"ps{tb}"])
                gi_ = sgn[0] % NSGT
                sgn[0] += 1
                sgt = SELGT[gi_]
                if c < NLAT:
                    CP("act", sgt[:, 0:2, :], PSB[tb][:, 0:256].rearrange("p (s t) -> p s t", s=2), [f"ps{tb}"], [f"SELGT{gi_}"])
                else:
                    CP("act", sgt[0:scsz[2], 2, :], PSB[tb][0:scsz[2], 256:384], [f"ps{tb}"], [f"SELGT{gi_}"])
                return gi_

            def sc_stage_b(c, gi_):
                sgt = SELGT[gi_]
                scs_ = [0, 1] if c < NLAT else [2]
                for hf in range(2):
                    pb = 2 * (c % 2) + hf
                    for sc in scs_:
                        sz = scsz[sc]
                        MM(PS[pb][:, :], sgt[0:sz, sc, :], YG[0:sz, sc, hf * 512:(hf + 1) * 512], sc == scs_[0], sc == scs_[-1],
                           [f"SELGT{gi_}", "YG"], [f"ps{pb}"])
                    TT("dve", X[:, c, hf * 512:(hf + 1) * 512], PS[pb][:, :], X[:, c, hf * 512:(hf + 1) * 512], ALU.add,
                       [f"ps{pb}", f"X{c}"], [f"X{c}"])

            SLOOK = 2
            pend_sc = []
            for c in range(nch):
                pend_sc.append((c, sc_stage_a(c)))
                if len(pend_sc) > SLOOK:
                    sc_stage_b(*pend_sc.pop(0))
            while pend_sc:
                sc_stage_b(*pend_sc.pop(0))
        barrier()
        bcast_row(2, ln_g_d[l, 1:2, :])
        bcast_row(3, ln_b_d[l, 1:2, :])
        for c in range(nch):
            layer_norm_chunk(c, 2, 3)
            if final_out:
                DMA("sp", out_d[c * 128:(c + 1) * 128, :], X[:, c, :], [f"X{c}"], ["outd"], "outw")
        barrier()

    if moe_layer(0, NCH) == "STOP":
        return nc

    if stage == 3:
        for c in range(NCH):
            DMA("sp", dbg_d[c * 128:(c + 1) * 128, :], X[:, c, :], [f"X{c}"], ["dbgd"], "dbg")
        S.add("sp", None, reads=list(S.lw.keys()) + ["dbgd"], writes=())
        S.emit(nc, es)
        es.close()
        return nc

    w_in1_d = dram("l1_w_in", [D, 2048])
    w_out1_d = dram("l1_w_out", [D, D])
    poolw_d = dram("l1_pool_w", [4, 128, 128])
    psc_d = dram("psc", [128, 4])
    band_d = dram("band", [20, 128, 128])
    l1tab_d = dram("l1tab", [NPAT, 8, 128, 128])

    def layer1_mixer():
        l = 1
        barrier()
        DMA("pool", WO[:], w_out1_d.rearrange("(k p) c -> p k c", p=128), (), ["WO"], "wo")
        modulate_T(1, 1, 0, NCH)
        spill_X(NLAT)
        barrier()
        arena_off[0] = 0
        W1R = [xview(2048, None, BF16, "p (k c) -> p k c", k=8) for _ in range(2)]
        QT1 = xview(4 * SEQ // 2, None, BF16, "p (g t) -> p g t", g=4)
        KT1 = xview(4 * NT // 2, None, BF16, "p (g t) -> p g t", g=4)
        VXU = xview(NCH * 8 * 65 // 2, None, BF16)
        VX1 = VXU.rearrange("p (c h f) -> p c h f", c=NCH, h=8)
        U1 = VXU[:, 0:NLAT * 512].rearrange("p (c f) -> p c f", c=NLAT)
        PT1 = [xview(256, None, BF16) for _ in range(3)]
        rp = ropet[:].rearrange("p c f -> p (c f)")
        TBI = rp[:, 0:1280].bitcast(BF16).rearrange("p (a h k) -> p a h k", a=5, h=4)
        TBB = rp[:, 1280:2304].bitcast(BF16).rearrange("p (a h k) -> p a h k", a=4, h=4)
        BAND = hb[0][:].rearrange("p (m t) -> p m t", m=8)
        PW = hb[1][:, 0:512].rearrange("p (g e) -> p g e", g=4)
        AOK1 = hb[1][:, 512:768]
        POOLT = hb[1][:, 768:1024].bitcast(F32) if False else None
        psc = sb("pscs", [128, 4])
        qflat = QT1.rearrange("p g t -> p (g t)")
        bandb = qflat[:, 0:2560].rearrange("p (m t) -> p m t", m=20)
        poolt = [qflat[:, 2560 + i * 512:2560 + (i + 1) * 512] for i in range(2)]
        DMA("sp", psc[:], psc_d, (), ["psc"], "c4")
        DMA("pool", bandb, band_d.rearrange("m a b -> a m b"), (), ["bandb"], "c4p")
        DMA("pool", PW, poolw_d.rearrange("g c e -> c g e"), (), ["PW"], "c4p")
        fence(["psc", "bandb", "PW"])

        def load_w1(sec, slot):
            DMA("pool", W1R[slot][:], w_in1_d[:, sec * 512:(sec + 1) * 512].rearrange("(k p) c -> p k c", p=128),
                (), [f"W1R{slot}"], f"w1r{slot}")

        load_w1(3, 0)
        load_w1(0, 1)
        for c in range(NLAT):
            pb = 4 + (c % 2)
            for k in range(8):
                MM(PS[pb][:, :], hT[:, k, c * 128:(c + 1) * 128], W1R[0][:, k, :], k == 0, k == 7, [f"hT{c}", "W1R0"], [f"ps{pb}"])
            CP("act" if c % 2 == 0 else "dve", U1[:, c, :], PS[pb][:, :], [f"ps{pb}"], [f"U1{c}"])
        pn = [0]
        for g in range(4):
            for cb in range(4):
                pb = 6 + (pn[0] % 2)
                for j in range(4):
                    c = cb * 4 + j
                    srcs = []
                    if c > 0:
                        srcs.append((c - 1, g * 5 + 0))
                    srcs.append((c, g * 5 + (3 if c == 0 else 4 if c == NLAT - 1 else 1)))
                    if c < NLAT - 1:
                        srcs.append((c + 1, g * 5 + 2))
                    for i, (cs, m) in enumerate(srcs):
                        MM(PS[pb][:, j * 128:(j + 1) * 128], U1[:, cs, g * 128:(g + 1) * 128], bandb[:, m, :], i == 0, i == len(srcs) - 1,
                           [f"U1{cs}", "bandb"], [f"ps{pb}"])
                pt_ = poolt[pn[0] % 2]
                CP("act", pt_, PS[pb][:, :], [f"ps{pb}"], [f"poolt{pn[0] % 2}"])
                pb2 = 4 + (pn[0] % 2)
                MM(PS[pb2][:, :], PW[:, g, :], pt_, True, True, ["PW", f"poolt{pn[0] % 2}"], [f"ps{pb2}"])
                TS("dve", AOT[:, 4 + g, cb * 512:(cb + 1) * 512], PS[pb2][:, :], psc[:, g:g + 1], None, ALU.mult, None,
                   [f"ps{pb2}", "psc"], [f"AOT{cb * 4 + j}" for j in range(4)])
                pn[0] += 1
        barrier()
        MEMSET("dve", VX1[:, :, :, 64:65], 1.0, [f"VX{c}" for c in range(NCH)])
        for g in range(4):
            for tb_ in range(4):
                pb = 4 + ((g * 4 + tb_) % 2)
                for k in range(8):
                    MM(PS[pb][:, :], W1R[1][:, k, g * 128:(g + 1) * 128], hT[:, k, tb_ * 512:(tb_ + 1) * 512], k == 0, k == 7,
                       ["W1R1"] + [f"hT{tb_ * 4 + j}" for j in range(4)], [f"ps{pb}"])
                ACT(QT1[:, g, tb_ * 512:(tb_ + 1) * 512], PS[pb][:, :], AF.Identity, [f"ps{pb}"], [f"QT{tb_ * 4 + j}" for j in range(4)], scale=0.125)
        load_w1(1, 0)
        load_w1(2, 1)
        blocks_k = [(i * 512, 512) for i in range(4)] + [(2048, 256)]
        for g in range(4):
            for bi, (t0, tn) in enumerate(blocks_k):
                pb = 4 + ((g * 5 + bi) % 2)
                chs = list(range(t0 // 128, (t0 + tn) // 128))
                for k in range(8):
                    MM(PS[pb][:, 0:tn], W1R[0][:, k, g * 128:(g + 1) * 128], hT[:, k, t0:t0 + tn], k == 0, k == 7,
                       ["W1R0"] + [f"hT{c}" for c in chs], [f"ps{pb}"])
                CP("act" if bi % 2 == 0 else "dve", KT1[:, g, t0:t0 + tn], PS[pb][:, 0:tn], [f"ps{pb}"], [f"KT{c}" for c in chs])
        for c in range(NCH):
            pb = 6 + (c % 2)
            for k in range(8):
                MM(PS[pb][:, :], hT[:, k, c * 128:(c + 1) * 128], W1R[1][:, k, :], k == 0, k == 7, [f"hT{c}", "W1R1"], [f"ps{pb}"])
            CP("act" if c % 2 == 0 else "dve", VX1[:, c, :, 0:64], PS[pb][:, :].rearrange("p (h f) -> p h f", h=8), [f"ps{pb}"], [f"VX{c}"])
        ptn = [0]
        for hh in range(2):
            for a in range(5):
                DMA("pool", TBI[:, a, :, :], l1tab_d[INT_PATS[a], hh * 4:(hh + 1) * 4].rearrange("h q k -> q h k"), (), ["TBI"], "tbi")
            for c in range(NLAT):
                loc = L1_LOCAL[c]
                interior = (2 <= c <= 13)
                if not interior:
                    for a, (kc, pat) in enumerate(loc):
                        DMA("pool", TBB[:, a, :, :], l1tab_d[pat, hh * 4:(hh + 1) * 4].rearrange("h q k -> q h k"), (), ["TBB"], "tbb")
                kcs = [(kc, a) for a, (kc, pat) in enumerate(loc)] + [(16, None), (17, None)]
                def score1(kn):
                    kc, a = kcs[kn]
                    bpair = (4, 5) if ptn[0] % 2 == 0 else (6, 7)
                    pi = ptn[0] % 3
                    ptn[0] += 1
                    for par in range(2):
                        bk = bpair[par]
                        for i2, h4 in enumerate((par, par + 2)):
                            h = hh * 4 + h4
                            g = h // 2
                            pbase = (h % 2) * 64
                            MM(PS[bk][:, i2 * 128:(i2 + 1) * 128], KT1[pbase:pbase + 64, g, kc * 128:(kc + 1) * 128],
                               QT1[pbase:pbase + 64, g, c * 128:(c + 1) * 128], True, a is None, [f"KT{kc}", f"QT{c}"], [f"ps{bk}"])
                            if a is not None:
                                tb_ap = TBI[:, a, h4, :] if interior else TBB[:, a, h4, :]
                                MM(PS[bk][:, i2 * 128:(i2 + 1) * 128], tb_ap, identb[:], False, True,
                                   ["TBI" if interior else "TBB", "identb"], [f"ps{bk}"])
                    return bpair, pi

                pend = score1(0)
                for kn, (kc, a) in enumerate(kcs):
                    nxt = score1(kn + 1) if kn + 1 < len(kcs) else None
                    bpair, pi = pend
                    for par in range(2):
                        bk = bpair[par]
                        ACT(PT1[pi][:, par * 256:(par + 1) * 256], PS[bk][:, 0:256], AF.Exp, [f"ps{bk}"], [f"PT{pi}_{par}"])
                    for h4 in range(4):
                        h = hh * 4 + h4
                        par, i2 = h4 % 2, h4 // 2
                        o_ = par * 256 + i2 * 128
                        MM(PS[h4][:, 0:65], PT1[pi][:, o_:o_ + 128], VX1[:, kc, h, :], kn == 0, kn == len(kcs) - 1,
                           [f"PT{pi}_{par}", f"VX{kc}"], [f"ps{h4}"])
                    pend = nxt
                for h4 in range(4):
                    rz, rk = stat()
                    S.add("dve", lambda e, rz=rz, h4=h4: e.reciprocal(rz, PS[h4][:, 64:65]), reads=[f"ps{h4}"], writes=[rk])
                    TS("dve", AOK1[:, h4 * 64:(h4 + 1) * 64], PS[h4][:, 0:64], rz, None, ALU.mult, None, [f"ps{h4}", rk], ["AOK1"])
                tb = 6 + (c % 2)
                for t in range(2):
                    TR(PSB[tb][:, t * 128:(t + 1) * 128], AOK1[:, t * 128:(t + 1) * 128], identb[:], ["AOK1", "identb"], [f"ps{tb}"])
                CP("act", AOT[:, 2 * hh:2 * hh + 2, c * 128:(c + 1) * 128], PSB[tb][:, 0:256].rearrange("p (s t) -> p s t", s=2),
                   [f"ps{tb}"], [f"AOT{c}"])
        barrier()
        reload_X(NLAT)
        outproj_ln(1, NLAT)

    layer1_mixer()
    if stage == 4:
        for c in range(NLAT):
            DMA("sp", dbg_d[c * 128:(c + 1) * 128, :], X[:, c, :], [f"X{c}"], ["dbgd"], "dbg")
        S.add("sp", None, reads=list(S.lw.keys()) + ["dbgd"], writes=())
        S.emit(nc, es)
        es.close()
        return nc
    moe_layer(1, NLAT, final_out=True)
    S.add("sp", None, reads=list(S.lw.keys()) + ["outd"], writes=())
    S.emit(nc, es)
    es.close()
    return nc


def _rope_table():
    t = np.arange(SEQ)
    inv = (10000.0 ** (-np.arange(16, dtype=np.float32) / 16)).astype(np.float32)
    ar = (t // 64).astype(np.float32)[:, None] * inv
    ac = (t % 64).astype(np.float32)[:, None] * inv
    cr, sr, cc_, sc_ = np.cos(ar), np.sin(ar), np.cos(ac), np.sin(ac)
    cosf = np.concatenate([cr, cr, cc_, cc_], axis=1)
    sins = np.concatenate([-sr, sr, -sc_, sc_], axis=1)
    tab = np.concatenate([cosf, sins], axis=1).astype(np.float32)
    ctxr = np.tile(np.array([1.0] * 64 + [0.0] * 64, np.float32), (CTX, 1))
    return np.concatenate([tab, ctxr], axis=0)


def make_in_maps(inp):
    f = lambda a: np.ascontiguousarray(np.asarray(a, dtype=np.float32))
    shared = {
        "ada_w": f(inp["ada_w"]), "ada_b": f(inp["ada_b"]), "ln_g": f(inp["ln_g"]), "ln_b": f(inp["ln_b"]),
        "l0_w_in": f(inp["l0_w_in"]), "l0_w_out": f(inp["l0_w_out"]),
        "lamv": f(np.stack([inp["l0_lam_q1"], inp["l0_lam_k1"], inp["l0_lam_q2"], inp["l0_lam_k2"]])),
        "l0_subln_g": f(inp["l0_subln_g"]),
        "qkg": f(np.concatenate([np.tile(np.asarray(inp["l0_qnorm_g"]), 4), np.asarray(inp["l0_knorm_g"])])),
        "rope": _rope_table(), "ident": np.eye(128, dtype=np.float32),
        "moe_router": f(inp["moe_router"]), "moe_w_gate": f(inp["moe_w_gate"]), "moe_w_up": f(inp["moe_w_up"]),
        "moe_w_down": f(inp["moe_w_down"]),
        "l1_w_in": f(inp["l1_w_in"]), "l1_w_out": f(inp["l1_w_out"]), "l1_pool_w": f(inp["l1_pool_w"]),
        "psc": f(np.asarray(inp["l1_pool_scale"]).reshape(4, 128).T), "band": _band_mats(),
        "l1tab": _l1_table(inp["l1_rpb"]),
        "iota288": np.tile(np.arange(288, dtype=np.float32), (128, 1)),
        "utri": np.triu(np.ones((128, 128), np.float32), 1),
    }
    maps = []
    x = np.asarray(inp["x"]); ctx = np.asarray(inp["ctx"]); c = np.asarray(inp["c"]); cctx = np.asarray(inp["c_ctx"])
    for b in range(8):
        m = dict(shared)
        m["x"] = f(np.concatenate([x[b], ctx[b]], axis=0))
        cc = np.stack([c[b].reshape(8, 128).T, cctx.reshape(8, 128).T], axis=-1)
        m["cc"] = f(cc)
        maps.append(m)
    return maps


def kernel(**inputs):
    nc = build()
    maps = make_in_maps(inputs)
    res = run_bass_kernel_spmd(nc, maps, core_ids=list(range(8)))
    return np.stack([r["out"] for r in res.results], axis=0)
```
